# Optimizing a Trainium2 kernel written in Bass

```python
import math
import jax, jax.numpy as jnp
from jax import lax
import numpy as np

D_MODEL = 2048
BATCH = 2
SEQ = 4096
DEPTH = 1
DEC_BATCH = 128
DEC_SEQ = 1
PAST_LEN = 16384
PAGE_SIZE = 128

HEAD_DIM = 64
ATT_WIDTH = D_MODEL // 2
ATT_HEADS = ATT_WIDTH // HEAD_DIM
ATT_KV_HEADS = ATT_HEADS // 4
ATT_GROUP = ATT_HEADS // ATT_KV_HEADS
KV_WIDTH = ATT_KV_HEADS * HEAD_DIM
ATT_PROJ = ATT_WIDTH + 2 * KV_WIDTH
WINDOW = 128
ATT_BLOCK = WINDOW
ATT_SCALE = HEAD_DIM ** -0.5
ROPE_THETA = 500000.0
ROT_DIM = HEAD_DIM // 4

RWKV_WIDTH = D_MODEL - ATT_WIDTH
RWKV_HEAD_DIM = 64
RWKV_HEADS = RWKV_WIDTH // RWKV_HEAD_DIM
DECAY_LORA = max(32, int(round(1.8 * RWKV_WIDTH ** 0.5 / 32)) * 32)
AAA_LORA = max(32, int(round(1.8 * RWKV_WIDTH ** 0.5 / 32)) * 32)
GATE_LORA = max(32, int(round(0.6 * RWKV_WIDTH ** 0.8 / 32)) * 32)
RWKV_PROJ = 3 * RWKV_WIDTH + DECAY_LORA + AAA_LORA + GATE_LORA
IN_PROJ = ATT_PROJ + RWKV_PROJ
MIX_WIDTH = ATT_WIDTH + RWKV_WIDTH

N_MEM = 256
XATT_HEADS = 4
XATT_HEAD_DIM = 128
XATT_WIDTH = XATT_HEADS * XATT_HEAD_DIM

N_EXPERT_GROUPS = 8
EXPERTS_PER_GROUP = 8
N_EXPERTS = N_EXPERT_GROUPS * EXPERTS_PER_GROUP
TOP_K = 2
EXPERT_FF = D_MODEL // 4
MOE_BLOCK = 128

RMS_EPS = 1e-6
GN_EPS = 64e-5

kernel_name = 'hymba_swa_rwkv7_hmoe_memxattn_step'


def rmsnorm(x, w):
    xf = x.astype(jnp.float32)
    y = xf * lax.rsqrt(jnp.mean(xf * xf, axis=-1, keepdims=True) + RMS_EPS)
    return (y * w.astype(jnp.float32)).astype(x.dtype)


def rope_partial(x, pos):
    half = ROT_DIM // 2
    inv = ROPE_THETA ** (-jnp.arange(half, dtype=jnp.float32) * 2.0 / ROT_DIM)
    ang = pos.astype(jnp.float32)[:, None] * inv[None, :]
    cos = jnp.cos(ang)[:, None, :]
    sin = jnp.sin(ang)[:, None, :]
    xf = x[..., :ROT_DIM].astype(jnp.float32)
    x1, x2 = xf[..., :half], xf[..., half:]
    rot = jnp.concatenate([x1 * cos - x2 * sin, x2 * cos + x1 * sin], axis=-1).astype(x.dtype)
    return jnp.concatenate([rot, x[..., ROT_DIM:]], axis=-1)


def mixer_in(x, lw):
    p = rmsnorm(x, lw['ln1_w']) @ lw['w_in']
    return p[..., :ATT_PROJ], p[..., ATT_PROJ:]


def mixer_out(x, att_o, rw_o, lw):
    return x + jnp.concatenate([att_o, rw_o.astype(att_o.dtype)], axis=-1) @ lw['w_out']


def attn_qkv(pa, pos, lw):
    B, T, _ = pa.shape
    q = pa[..., :ATT_WIDTH].reshape(B, T, ATT_HEADS, HEAD_DIM)
    k = pa[..., ATT_WIDTH:ATT_WIDTH + KV_WIDTH].reshape(B, T, ATT_KV_HEADS, HEAD_DIM)
    v = pa[..., ATT_WIDTH + KV_WIDTH:].reshape(B, T, ATT_KV_HEADS, HEAD_DIM)
    q = rope_partial(rmsnorm(q, lw['q_norm_w']), pos)
    k = rope_partial(rmsnorm(k, lw['k_norm_w']), pos)
    return q, k, v


def sink_softmax(s, sinks):
    sk = sinks.astype(jnp.float32).reshape(ATT_KV_HEADS, ATT_GROUP)[:, :, None, None]
    m = jnp.maximum(jnp.max(s, axis=-1, keepdims=True), sk)
    e = jnp.exp(s - m)
    return e / (jnp.sum(e, axis=-1, keepdims=True) + jnp.exp(sk - m))


def swa_banded(q, k, v, sinks):
    B, T, _, _ = q.shape
    nb = T // ATT_BLOCK
    qb = q.reshape(B, nb, ATT_BLOCK, ATT_KV_HEADS, ATT_GROUP, HEAD_DIM)
    kb = k.reshape(B, nb, ATT_BLOCK, ATT_KV_HEADS, HEAD_DIM)
    vb = v.reshape(B, nb, ATT_BLOCK, ATT_KV_HEADS, HEAD_DIM)
    pad = ((0, 0), (1, 0), (0, 0), (0, 0), (0, 0))
    kc = jnp.concatenate([jnp.pad(kb, pad)[:, :-1], kb], axis=2)
    vc = jnp.concatenate([jnp.pad(vb, pad)[:, :-1], vb], axis=2)
    s = jnp.einsum('bnqkgd,bnskd->bnkgqs', qb, kc, preferred_element_type=jnp.float32) * ATT_SCALE
    qi = jnp.arange(ATT_BLOCK)[:, None] + ATT_BLOCK
    si = jnp.arange(2 * ATT_BLOCK)[None, :]
    rel = qi - si
    band = (rel >= 0) & (rel <= WINDOW)
    blk = jnp.arange(nb)[:, None, None]
    valid = band[None] & ((blk > 0) | (si[None] >= ATT_BLOCK))
    s = jnp.where(valid[None, :, None, None], s, -jnp.inf)
    p = sink_softmax(s, sinks)
    o = jnp.einsum('bnkgqs,bnskd->bnqkgd', p.astype(vc.dtype), vc)
    return o.reshape(B, T, ATT_WIDTH)


def swa_cached(q, k_new, v_new, buf_k, buf_v, sinks):
    B, T = q.shape[:2]
    nbuf = buf_k.shape[1]
    kc = jnp.concatenate([buf_k.astype(k_new.dtype), k_new], axis=1)
    vc = jnp.concatenate([buf_v.astype(v_new.dtype), v_new], axis=1)
    q_pos = PAST_LEN + jnp.arange(T)
    k_pos = jnp.concatenate([PAST_LEN - nbuf + jnp.arange(nbuf), q_pos])
    rel = q_pos[:, None] - k_pos[None, :]
    valid = (rel >= 0) & (rel <= WINDOW)
    qg = q.reshape(B, T, ATT_KV_HEADS, ATT_GROUP, HEAD_DIM)
    s = jnp.einsum('bqkgd,bskd->bkgqs', qg, kc, preferred_element_type=jnp.float32) * ATT_SCALE
    s = jnp.where(valid, s, -jnp.inf)
    p = sink_softmax(s, sinks)
    o = jnp.einsum('bkgqs,bskd->bqkgd', p.astype(vc.dtype), vc).reshape(B, T, ATT_WIDTH)
    return o, kc[:, -nbuf:], vc[:, -nbuf:]


def rwkv7_mix(pr, prev_row, s0, lw):
    B, T, _ = pr.shape
    H, N, C = RWKV_HEADS, RWKV_HEAD_DIM, RWKV_WIDTH
    f32 = jnp.float32
    prev = jnp.concatenate([prev_row[:, None, :].astype(pr.dtype), pr[:, :-1]], axis=1)
    xm = (pr + (prev - pr) * lw['rw_mu']).astype(f32)
    xr, xk, xv = xm[..., :C], xm[..., C:2 * C], xm[..., 2 * C:3 * C]
    o = 3 * C
    xw = xm[..., o:o + DECAY_LORA]
    o += DECAY_LORA
    xa = xm[..., o:o + AAA_LORA]
    o += AAA_LORA
    xg = xm[..., o:]
    w_log = -jax.nn.softplus(-(lw['rw_w0'].astype(f32) + jnp.tanh(xw) @ lw['rw_w2'].astype(f32))) - 0.5
    decay = jnp.exp(-jnp.exp(w_log))
    a = jax.nn.sigmoid(lw['rw_a0'].astype(f32) + xa @ lw['rw_a2'].astype(f32))
    g = jax.nn.sigmoid(xg) @ lw['rw_g2'].astype(f32)
    r, k, v, decay, a = [t.reshape(B, T, H, N) for t in (xr, xk, xv, decay, a)]
    kk = k * lw['rw_k_k'].astype(f32).reshape(H, N)
    kk = kk / jnp.maximum(jnp.sqrt(jnp.sum(kk * kk, axis=-1, keepdims=True)), 1e-12)
    k = k * (1.0 + (a - 1.0) * lw['rw_k_a'].astype(f32).reshape(H, N))

    def step(S, inp):
        r_t, w_t, k_t, v_t, kk_t, b_t = inp
        sa = jnp.einsum('bhvk,bhk->bhv', S, -kk_t)
        S = S * w_t[:, :, None, :] + sa[..., None] * b_t[:, :, None, :] + v_t[..., None] * k_t[:, :, None, :]
        return S, jnp.einsum('bhvk,bhk->bhv', S, r_t)

    xs = tuple(jnp.swapaxes(t, 0, 1) for t in (r, decay, k, v, kk, kk * a))
    S, y = lax.scan(step, s0.astype(f32), xs)
    y = jnp.swapaxes(y, 0, 1)
    mean = jnp.mean(y, axis=-1, keepdims=True)
    var = jnp.mean((y - mean) ** 2, axis=-1, keepdims=True)
    yn = (y - mean) * lax.rsqrt(var + GN_EPS) * lw['rw_ln_w'].astype(f32).reshape(H, N) + lw['rw_ln_b'].astype(f32).reshape(H, N)
    bonus = jnp.sum(r * k * lw['rw_r_k'].astype(f32).reshape(H, N), axis=-1, keepdims=True) * v
    out = ((yn + bonus).reshape(B, T, C) * g).astype(pr.dtype)
    return out, S.astype(s0.dtype), pr[:, -1]


def mem_kv(mem, lw):
    B, M, _ = mem.shape
    kv = rmsnorm(mem, lw['mem_norm_w']) @ lw['xkv_w']
    k = rmsnorm(kv[..., :XATT_WIDTH].reshape(B, M, XATT_HEADS, XATT_HEAD_DIM), lw['xk_norm_w'])
    v = kv[..., XATT_WIDTH:].reshape(B, M, XATT_HEADS, XATT_HEAD_DIM)
    return k, v


def cross_attend(x, mem_k, mem_v, lw):
    B, T, _ = x.shape
    h = rmsnorm(x, lw['ln2_w'])
    q = rmsnorm((h @ lw['xq_w']).reshape(B, T, XATT_HEADS, XATT_HEAD_DIM), lw['xq_norm_w'])
    s = jnp.einsum('bqhd,bmhd->bhqm', q, mem_k.astype(q.dtype), preferred_element_type=jnp.float32) / math.sqrt(XATT_HEAD_DIM)
    p = jax.nn.softmax(s, axis=-1)
    o = jnp.einsum('bhqm,bmhd->bqhd', p.astype(q.dtype), mem_v.astype(q.dtype)).reshape(B, T, XATT_WIDTH)
    return x + o @ lw['xo_w']


def expert_dispatch(u, e_idx, gates, w_gate, w_up, w_down):
    M, D = u.shape
    A = M * TOP_K
    e_flat = e_idx.reshape(A)
    w_flat = gates.reshape(A)
    tok_flat = jnp.arange(A, dtype=jnp.int32) // TOP_K
    order = jnp.argsort(e_flat)
    e_s, tok_s, w_s = e_flat[order], tok_flat[order], w_flat[order]
    counts = jnp.bincount(e_flat, length=N_EXPERTS)
    pad_counts = (counts + MOE_BLOCK - 1) // MOE_BLOCK * MOE_BLOCK
    starts = jnp.cumsum(counts) - counts
    pad_ends = jnp.cumsum(pad_counts)
    pad_starts = pad_ends - pad_counts
    dest = pad_starts[e_s] + jnp.arange(A, dtype=jnp.int32) - starts[e_s]
    n_blocks = -(-A // MOE_BLOCK) + N_EXPERTS
    P = n_blocks * MOE_BLOCK
    row_tok = jnp.full((P,), M, jnp.int32).at[dest].set(tok_s)
    row_w = jnp.zeros((P,), w_flat.dtype).at[dest].set(w_s)
    block_exp = jnp.minimum(jnp.searchsorted(pad_ends, jnp.arange(n_blocks) * MOE_BLOCK, side='right'), N_EXPERTS - 1)
    u_pad = jnp.concatenate([u, jnp.zeros((1, D), u.dtype)], axis=0)
    xb = u_pad[row_tok].reshape(n_blocks, MOE_BLOCK, D)

    def run_block(args):
        xblk, e = args
        return (jax.nn.silu(xblk @ w_gate[e]) * (xblk @ w_up[e])) @ w_down[e]

    yb = lax.map(run_block, (xb, block_exp)).reshape(P, D)
    out = jnp.zeros((M + 1, D), yb.dtype).at[row_tok].add(yb * row_w[:, None].astype(yb.dtype))
    return out[:M]


def hmoe_block(x, lw):
    B, T, D = x.shape
    M = B * T
    f32 = jnp.float32
    u = rmsnorm(x, lw['ln3_w']).reshape(M, D)
    g_logit = (u @ lw['router_group_w']).astype(f32) + lw['router_group_b'].astype(f32)
    g_idx = jnp.argmax(g_logit, axis=-1).astype(jnp.int32)
    g_gate = jnp.take_along_axis(jax.nn.softmax(g_logit, axis=-1), g_idx[:, None], axis=-1)
    e_logit = ((u @ lw['router_expert_w']).astype(f32) + lw['router_expert_b'].astype(f32)).reshape(M, N_EXPERT_GROUPS, EXPERTS_PER_GROUP)
    e_logit = jnp.take_along_axis(e_logit, g_idx[:, None, None], axis=1)[:, 0]
    top_v, top_i = lax.top_k(e_logit, TOP_K)
    gates = jax.nn.softmax(top_v, axis=-1) * g_gate
    e_idx = g_idx[:, None] * EXPERTS_PER_GROUP + top_i.astype(jnp.int32)
    y = expert_dispatch(u, e_idx, gates, lw['exp_w_gate'], lw['exp_w_up'], lw['exp_w_down'])
    return x + y.reshape(B, T, D).astype(x.dtype)


def setup_inputs(seed: int = 0) -> dict:
    key = jax.random.key(seed)
    ks = iter(list(jax.random.split(key, 64)))
    f32 = jnp.float32
    L = DEPTH
    win_buf = min(WINDOW, PAST_LEN)

    def nrm(shape, scale=1.0):
        return jax.random.normal(next(ks), shape, f32) * scale

    def gain(shape, center=1.0):
        return center + 0.02 * jax.random.normal(next(ks), shape, f32)

    return {
        'x_prompt': nrm((BATCH, SEQ, D_MODEL)),
        'x_sample': nrm((DEC_BATCH, DEC_SEQ, D_MODEL)),
        'cache_win_k': nrm((L, DEC_BATCH, win_buf, ATT_KV_HEADS, HEAD_DIM)),
        'cache_win_v': nrm((L, DEC_BATCH, win_buf, ATT_KV_HEADS, HEAD_DIM)),
        'state_wkv': nrm((L, DEC_BATCH, RWKV_HEADS, RWKV_HEAD_DIM, RWKV_HEAD_DIM), 0.5),
        'state_shift': nrm((L, DEC_BATCH, RWKV_PROJ)),
        'cache_mem_k': nrm((L, DEC_BATCH, N_MEM, XATT_HEADS, XATT_HEAD_DIM)),
        'cache_mem_v': nrm((L, DEC_BATCH, N_MEM, XATT_HEADS, XATT_HEAD_DIM)),
        'mem_prompt': nrm((BATCH, N_MEM, D_MODEL)),
        'ln1_w': gain((L, D_MODEL)),
        'w_in': nrm((L, D_MODEL, IN_PROJ), D_MODEL ** -0.5),
        'q_norm_w': gain((L, HEAD_DIM)),
        'k_norm_w': gain((L, HEAD_DIM)),
        'attn_sinks': nrm((L, ATT_HEADS), 0.5),
        'rw_mu': jax.random.uniform(next(ks), (L, RWKV_PROJ), f32),
        'rw_w0': jax.random.uniform(next(ks), (L, RWKV_WIDTH), f32, -6.0, -1.0),
        'rw_w2': nrm((L, DECAY_LORA, RWKV_WIDTH), 0.1),
        'rw_a0': nrm((L, RWKV_WIDTH), 0.1),
        'rw_a2': nrm((L, AAA_LORA, RWKV_WIDTH), 0.1),
        'rw_g2': nrm((L, GATE_LORA, RWKV_WIDTH), GATE_LORA ** -0.5),
        'rw_k_k': gain((L, RWKV_WIDTH), 0.85),
        'rw_k_a': gain((L, RWKV_WIDTH)),
        'rw_r_k': nrm((L, RWKV_WIDTH), 0.1),
        'rw_ln_w': gain((L, RWKV_WIDTH)),
        'rw_ln_b': nrm((L, RWKV_WIDTH), 0.01),
        'w_out': nrm((L, MIX_WIDTH, D_MODEL), MIX_WIDTH ** -0.5),
        'ln2_w': gain((L, D_MODEL)),
        'mem_norm_w': gain((L, D_MODEL)),
        'xq_w': nrm((L, D_MODEL, XATT_WIDTH), D_MODEL ** -0.5),
        'xkv_w': nrm((L, D_MODEL, 2 * XATT_WIDTH), D_MODEL ** -0.5),
        'xq_norm_w': gain((L, XATT_HEAD_DIM)),
        'xk_norm_w': gain((L, XATT_HEAD_DIM)),
        'xo_w': nrm((L, XATT_WIDTH, D_MODEL), XATT_WIDTH ** -0.5),
        'ln3_w': gain((L, D_MODEL)),
        'router_group_w': nrm((L, D_MODEL, N_EXPERT_GROUPS), D_MODEL ** -0.5),
        'router_group_b': nrm((L, N_EXPERT_GROUPS), 0.01),
        'router_expert_w': nrm((L, D_MODEL, N_EXPERTS), D_MODEL ** -0.5),
        'router_expert_b': nrm((L, N_EXPERTS), 0.01),
        'exp_w_gate': nrm((L, N_EXPERTS, D_MODEL, EXPERT_FF), D_MODEL ** -0.5),
        'exp_w_up': nrm((L, N_EXPERTS, D_MODEL, EXPERT_FF), D_MODEL ** -0.5),
        'exp_w_down': nrm((L, N_EXPERTS, EXPERT_FF, D_MODEL), EXPERT_FF ** -0.5),
    }


def reference(x_prompt, x_sample, cache_win_k, cache_win_v, state_wkv, state_shift, cache_mem_k, cache_mem_v,
              mem_prompt, ln1_w, w_in, q_norm_w, k_norm_w, attn_sinks, rw_mu, rw_w0, rw_w2, rw_a0, rw_a2, rw_g2,
              rw_k_k, rw_k_a, rw_r_k, rw_ln_w, rw_ln_b, w_out, ln2_w, mem_norm_w, xq_w, xkv_w, xq_norm_w,
              xk_norm_w, xo_w, ln3_w, router_group_w, router_group_b, router_expert_w, router_expert_b,
              exp_w_gate, exp_w_up, exp_w_down):
    pos_p = jnp.arange(SEQ, dtype=jnp.int32)
    pos_s = PAST_LEN + jnp.arange(DEC_SEQ, dtype=jnp.int32)
    win_p = min(WINDOW, SEQ)
    hp, hs = x_prompt, x_sample
    p_wk, p_wv, p_wkv, p_sh, p_mk, p_mv = [], [], [], [], [], []
    s_wk, s_wv, s_wkv, s_sh = [], [], [], []
    for l in range(DEPTH):
        lw = {
            'ln1_w': ln1_w[l], 'w_in': w_in[l], 'q_norm_w': q_norm_w[l], 'k_norm_w': k_norm_w[l],
            'attn_sinks': attn_sinks[l], 'rw_mu': rw_mu[l], 'rw_w0': rw_w0[l], 'rw_w2': rw_w2[l],
            'rw_a0': rw_a0[l], 'rw_a2': rw_a2[l], 'rw_g2': rw_g2[l], 'rw_k_k': rw_k_k[l], 'rw_k_a': rw_k_a[l],
            'rw_r_k': rw_r_k[l], 'rw_ln_w': rw_ln_w[l], 'rw_ln_b': rw_ln_b[l], 'w_out': w_out[l],
            'ln2_w': ln2_w[l], 'mem_norm_w': mem_norm_w[l], 'xq_w': xq_w[l], 'xkv_w': xkv_w[l],
            'xq_norm_w': xq_norm_w[l], 'xk_norm_w': xk_norm_w[l], 'xo_w': xo_w[l], 'ln3_w': ln3_w[l],
            'router_group_w': router_group_w[l], 'router_group_b': router_group_b[l],
            'router_expert_w': router_expert_w[l], 'router_expert_b': router_expert_b[l],
            'exp_w_gate': exp_w_gate[l], 'exp_w_up': exp_w_up[l], 'exp_w_down': exp_w_down[l],
        }
        pa, pr = mixer_in(hp, lw)
        q, k, v = attn_qkv(pa, pos_p, lw)
        att = swa_banded(q, k, v, lw['attn_sinks'])
        rw, wkv, shift = rwkv7_mix(pr, jnp.zeros((BATCH, RWKV_PROJ), pr.dtype),
                                   jnp.zeros((BATCH, RWKV_HEADS, RWKV_HEAD_DIM, RWKV_HEAD_DIM), jnp.float32), lw)
        hp = mixer_out(hp, att, rw, lw)
        mk, mv = mem_kv(mem_prompt, lw)
        hp = hmoe_block(cross_attend(hp, mk, mv, lw), lw)
        p_wk.append(k[:, SEQ - win_p:])
        p_wv.append(v[:, SEQ - win_p:])
        p_wkv.append(wkv)
        p_sh.append(shift)
        p_mk.append(mk)
        p_mv.append(mv)
        sa, sr = mixer_in(hs, lw)
        q, k, v = attn_qkv(sa, pos_s, lw)
        att, nk, nv = swa_cached(q, k, v, cache_win_k[l], cache_win_v[l], lw['attn_sinks'])
        rw, wkv, shift = rwkv7_mix(sr, state_shift[l], state_wkv[l], lw)
        hs = mixer_out(hs, att, rw, lw)
        hs = hmoe_block(cross_attend(hs, cache_mem_k[l], cache_mem_v[l], lw), lw)
        s_wk.append(nk)
        s_wv.append(nv)
        s_wkv.append(wkv)
        s_sh.append(shift)
    return (hp, hs, jnp.stack(p_wk), jnp.stack(p_wv), jnp.stack(p_wkv), jnp.stack(p_sh), jnp.stack(p_mk),
            jnp.stack(p_mv), jnp.stack(s_wk), jnp.stack(s_wv), jnp.stack(s_wkv), jnp.stack(s_sh))
```

```python
import numpy as np
import ml_dtypes
import concourse.bass as bass
import concourse.mybir as mybir

F32 = mybir.dt.float32
BF16 = mybir.dt.bfloat16
I32 = mybir.dt.int32
ALU = mybir.AluOpType
AF = mybir.ActivationFunctionType
AX = mybir.AxisListType


class Res:
    def __init__(self, name, h, kind):
        self.name = name
        self.h = h
        self.kind = kind
        self.w = {}
        self.r = {}
        self.prev = {}
        self.dsem = None
        self.dcnt = 0

    def __getitem__(self, idx):
        return self.h[idx]

    def ap(self):
        return self.h.ap() if self.kind in ('dram', 'in', 'out') else self.h[:]


def _merge(dst, src):
    for k, v in src.items():
        if dst.get(k, 0) < v:
            dst[k] = v


class K:
    ENG = ('pe', 'act', 'dve', 'pool', 'sp')

    def __init__(self, nc):
        self.nc = nc
        self.prog = {e: [] for e in self.ENG}
        self.sems = {}
        self.cnt = {}
        for e in self.ENG:
            self.sems[e] = nc.alloc_semaphore('s_' + e)
            self.cnt[e] = 0
        self.waited = {e: {} for e in self.ENG}
        self.out_tickets = {}
        self.n_dsem = 0
        self.nres = 0
        self.dtot = {}
        self.ring_i = 0
        self.NRING = 64

    def sb(self, name, shape, dt=F32):
        self.nres += 1
        name = '%s_%d' % (name, self.nres)
        return Res(name, self.nc.alloc_sbuf_tensor(name, list(shape), dt), 'sb')

    def ps(self, name, shape, dt=F32):
        return Res(name, self.nc.alloc_psum_tensor(name, list(shape), dt), 'ps')

    def dram(self, name, shape, dt=F32):
        return Res(name, self.nc.dram_tensor(name, list(shape), dt), 'dram')

    def inp(self, name, shape, dt=F32):
        return Res(name, self.nc.dram_tensor(name, list(shape), dt, kind='ExternalInput'), 'in')

    def outp(self, name, shape, dt=F32):
        return Res(name, self.nc.dram_tensor(name, list(shape), dt, kind='ExternalOutput'), 'out')

    def _wait(self, e, deps):
        for key, val in deps.items():
            if key == 'pe' and e == 'pe':
                continue
            if self.waited[e].get(key, 0) >= val:
                continue
            self.waited[e][key] = val
            sem = self.sems[key]
            self.prog[e].append(lambda eng, sem=sem, val=val: eng.wait_ge(sem, val))

    def begin_fill(self, *ress):
        for b in ress:
            b.prev = {}
            _merge(b.prev, b.w)
            _merge(b.prev, b.r)
            b.w = {}
            b.r = {}

    def op(self, e, fn, reads=(), writes=(), pwrites=()):
        deps = {}
        for b in reads:
            _merge(deps, b.w)
        for b in writes:
            _merge(deps, b.w)
            _merge(deps, b.r)
        for b in pwrites:
            _merge(deps, b.prev)
        self._wait(e, deps)
        self.cnt[e] += 1
        t = {e: self.cnt[e]}
        sem = self.sems[e]
        self.prog[e].append(lambda eng, fn=fn, sem=sem: fn(eng).then_inc(sem, 1))
        for b in reads:
            _merge(b.r, t)
        for b in writes:
            b.w = dict(t)
            b.r = {}
        for b in pwrites:
            _merge(b.w, t)
        return t

    def dma(self, q, dst, dst_ap, src, src_ap, partial=False, **kw):
        deps = {}
        _merge(deps, src.w)
        if partial:
            _merge(deps, dst.prev)
        else:
            _merge(deps, dst.w)
            _merge(deps, dst.r)
        self._wait(q, deps)
        slot = self.ring_i % self.NRING
        self.ring_i += 1
        key = 'r%d' % slot
        if key not in self.sems:
            self.sems[key] = self.nc.alloc_semaphore(key)
            self.dtot[key] = 0
        self._wait(q, {key: self.dtot[key]})
        self.dtot[key] += 16
        t = {key: self.dtot[key]}
        sem = self.sems[key]
        self.prog[q].append(
            lambda eng, o=dst_ap, i=src_ap, sem=sem, kw=kw: eng.dma_start(out=o, in_=i, **kw).then_inc(sem, 16))
        _merge(src.r, t)
        if partial:
            _merge(dst.w, t)
        else:
            dst.w = dict(t)
            dst.r = {}
        if dst.kind == 'out':
            _merge(self.out_tickets, t)
        return t

    def custom(self, e, fn, inc, semkey_res, reads=(), writes=()):
        deps = {}
        for b in reads:
            _merge(deps, b.w)
        for b in writes:
            _merge(deps, b.w)
            _merge(deps, b.r)
        self._wait(e, deps)
        sres = semkey_res
        if sres.dsem is None:
            key = 'd%d' % self.n_dsem
            self.n_dsem += 1
            self.sems[key] = self.nc.alloc_semaphore(key)
            sres.dsem = key
        sres.dcnt += inc
        t = {sres.dsem: sres.dcnt}
        self.dtot[sres.dsem] = sres.dcnt
        sem = self.sems[sres.dsem]
        self.prog[e].append(lambda eng, fn=fn, sem=sem, inc=inc: fn(eng).then_inc(sem, inc))
        for b in reads:
            _merge(b.r, t)
        for b in writes:
            b.w = dict(t)
            b.r = {}
        return t

    def finish(self):
        self._wait('sp', self.out_tickets)
        allt = {e: self.cnt[e] for e in self.ENG if self.cnt[e] > 0}
        self._wait('sp', allt)
        nc = self.nc
        prog = self.prog
        with nc.Block() as block:
            @block.sync
            def _(eng):
                for f in prog['sp']:
                    f(eng)

            @block.tensor
            def _(eng):
                for f in prog['pe']:
                    f(eng)

            @block.scalar
            def _(eng):
                for f in prog['act']:
                    f(eng)

            @block.vector
            def _(eng):
                for f in prog['dve']:
                    f(eng)

            @block.gpsimd
            def _(eng):
                for f in prog['pool']:
                    f(eng)
        return nc


def _k_i(self, e, meth, reads=(), writes=(), pwrites=(), **kw):
    return self.op(e, lambda eng, meth=meth, kw=kw: getattr(eng, meth)(**kw), reads=reads, writes=writes, pwrites=pwrites)


K.i = _k_i


class ResView:
    def __init__(self, parent, ap, name=None):
        self.p = parent
        self.h = ap
        self.name = name or parent.name + '_v'
        self.kind = parent.kind

    def __getitem__(self, idx):
        return self.h[idx]

    w = property(lambda s: s.p.w, lambda s, v: setattr(s.p, 'w', v))
    r = property(lambda s: s.p.r, lambda s, v: setattr(s.p, 'r', v))
    prev = property(lambda s: s.p.prev, lambda s, v: setattr(s.p, 'prev', v))
    dsem = property(lambda s: s.p.dsem, lambda s, v: setattr(s.p, 'dsem', v))
    dcnt = property(lambda s: s.p.dcnt, lambda s, v: setattr(s.p, 'dcnt', v))


def _k_barrier(self):
    allt = {e: self.cnt[e] for e in self.ENG if self.cnt[e] > 0}
    for key, v in self.dtot.items():
        allt[key] = v
    for e in self.ENG:
        self._wait(e, allt)


K.barrier = _k_barrier


class P:
    pass

C_DEC = 0.6065306597126334
GN_EPS = 64e-5


def rwkv_consts(k, p, inp):
    c = P()
    p.rc = c
    c.mu = k.sb('r_mu', [128, 9])
    c.omm = k.sb('r_omm', [128, 9])
    k.dma('sp', c.mu, c.mu[:], inp['mu_h'], inp['mu_h'].ap().rearrange("(c p) -> p c", p=128), allow_slow_non_contiguous=True)
    k.i('dve', 'tensor_scalar', reads=[c.mu], writes=[c.omm], out=c.omm[:], in0=c.mu[:], scalar1=-1.0, scalar2=1.0, op0=ALU.mult, op1=ALU.add)
    c.pv = k.sb('r_pv', [128, 7, 2])
    k.dma('sp', c.pv, c.pv[:], inp['pv_h'], inp['pv_h'].ap().rearrange("v (g p) -> p v g", p=128), allow_slow_non_contiguous=True)
    c.omka = k.sb('r_omka', [128, 2])
    k.i('dve', 'tensor_scalar', reads=[c.pv], writes=[c.omka], out=c.omka[:], in0=c.pv[:, 3, :], scalar1=-1.0, scalar2=1.0, op0=ALU.mult, op1=ALU.add)
    lf = k.sb('r_lf', [128, 3, 256])
    c.lw = k.sb('r_lw', [128, 3, 256], BF16)
    k.i('pool', 'memset', writes=[lf], ap=lf[:], constant=0.0)
    k.dma('sp', lf, lf[0:64, 0, :], inp['w2_h'], inp['w2_h'].ap())
    k.dma('sp', lf, lf[64:128, 0, :], inp['a2_h'], inp['a2_h'].ap(), partial=True)
    k.dma('sp', lf, lf[:, 1, :], inp['g2_h'], inp['g2_h'].ap()[0:128, :], partial=True)
    k.dma('sp', lf, lf[0:32, 2, :], inp['g2_h'], inp['g2_h'].ap()[128:160, :], partial=True)
    k.i('dve', 'tensor_copy', reads=[lf], writes=[c.lw], out=c.lw[:], in_=lf[:])
    mf = k.sb('r_mf', [128, 12, 128])
    k.dma('sp', mf, mf[:], inp['rmask'], inp['rmask'].ap().rearrange("m p n -> p m n"))
    c.mf = mf
    c.mb = k.sb('r_mb', [128, 12, 128], BF16)
    k.i('dve', 'tensor_copy', reads=[mf], writes=[c.mb], out=c.mb[:], in_=mf[:])
    return c


def rwkv_phase(k, p, inp, T, x_src, x_ap_fn, WH, lnb, front, out_rw, out_state, out_shift, after_st=None):
    c = p.rc
    identb, identf = p.identb, p.identf
    NST = T // 512
    sb = k.sb
    uTall = sb('rk_uT', [128, 16, 512], BF16)
    PR = sb('rk_PR', [128, 9, 513])
    XM = sb('rk_XM', [128, 9, 512])
    k.i('pool', 'memset', writes=[PR], ap=PR[:], constant=0.0)
    f32t = {n: sb('rk_' + n, [128, 512]) for n in ['lw', 'a', 'kk', 'kmod', 'bv', 'cum', 't1', 't2']}
    b16t = {n: sb('rk_' + n, [128, 512], BF16) for n in ['tw', 'sg7', 'sg8']}
    bd = {n: sb('rk_bd_' + n, [128, 2, 8, 128], BF16) for n in ['kkt', 'rt', 'bh', 'kh', 'v']}
    gC = sb('rk_gC', [128, 2, 8])
    GATE = sb('rk_gate', [128, 2, 512])
    BONUS = sb('rk_bonus', [128, 2, 512])
    YV = sb('rk_yv', [128, 2, 512])
    S32 = sb('rk_S32', [128, 2, 128])
    S16 = sb('rk_S16', [128, 2, 128], BF16)
    S0g = sb('rk_S0g', [128, 2, 128])
    k.i('pool', 'memset', writes=[S32], ap=S32[:], constant=0.0)
    k.i('pool', 'memset', writes=[S16], ap=S16[:], constant=0.0)
    NA = [sb('rk_NA%d' % i, [128, 4, 128], BF16) for i in range(2)]
    AB = [sb('rk_AB%d' % i, [128, 4, 128], BF16) for i in range(2)]
    KR = [sb('rk_KR%d' % i, [128, 2, 128], BF16) for i in range(2)]
    TM = [sb('rk_TM%d' % i, [128, 6, 128], BF16) for i in range(2)]
    PQ = [sb('rk_PQ%d' % i, [128, 4, 128], BF16) for i in range(2)]
    TT = [sb('rk_TT%d' % i, [128, 2, 128], BF16) for i in range(2)]
    TTF = [sb('rk_TTF%d' % i, [128, 2, 128], BF16) for i in range(2)]
    U0 = sb('rk_U0', [128, 2, 128], BF16)
    UU = sb('rk_U', [128, 2, 128], BF16)
    rwo = [sb('rk_rwo%d' % i, [128, 512], BF16) for i in range(2)]
    pW = p.pM
    pC = p.pC
    pH = p.pH
    st = P()
    st.q = 0
    st.ev = 0

    def nb():
        s_ = pC[st.q % len(pC)]
        st.q += 1
        return s_

    def mmg(ps, slot_, terms, first_in_bank):
        n = len(terms)
        for i, (L, Lap, Rr, Rap) in enumerate(terms):
            fresh = first_in_bank and i == 0
            k.i('pe', 'matmul', reads=[L, Rr], writes=[ps] if fresh else [], pwrites=[] if fresh else [ps],
                out=ps[:, slot_, :], lhsT=Lap, rhs=Rap, start=(i == 0), stop=(i == n - 1))

    def cp(dst, dst_ap, ps, ps_ap, scale=None):
        st.ev += 1
        if scale is not None:
            k.i('act', 'activation', reads=[ps], writes=[dst], out=dst_ap, in_=ps_ap, func=AF.Identity, scale=scale)
        elif st.ev % 2 == 0:
            k.i('act', 'copy', reads=[ps], writes=[dst], out=dst_ap, in_=ps_ap)
        else:
            k.i('dve', 'tensor_copy', reads=[ps], writes=[dst], out=dst_ap, in_=ps_ap)

    BONES = c.mf[:, 10, :]
    BM = c.mb[:, 11, :].rearrange("p (j s) -> p j s", j=2).unsqueeze(1).to_broadcast([128, 8, 2, 64])
    for stile in range(NST):
        k.begin_fill(uTall)
        for tt in range(4):
            front(x_src, x_ap_fn(stile * 4 + tt), 128, lnb, uTall,
                  lambda half, tt=tt: uTall[:, half * 8:(half + 1) * 8, tt * 128:(tt + 1) * 128])
        if stile > 0:
            k.i('act', 'copy', reads=[PR], writes=[PR], out=PR[:, :, 0:1], in_=PR[:, :, 512:513])
        k.begin_fill(PR)
        for cc in range(9):
            ps = pW[p.mi % len(pW)]
            p.mi += 1
            for dc in range(16):
                k.i('pe', 'matmul', reads=[uTall, WH], writes=[ps] if dc == 0 else [], pwrites=[] if dc == 0 else [ps],
                    out=ps[:, :], lhsT=WH[:, dc, cc * 128:(cc + 1) * 128], rhs=uTall[:, dc, :], start=(dc == 0), stop=(dc == 15))
            if cc % 2 == 0:
                k.i('act', 'copy', reads=[ps], pwrites=[PR], out=PR[:, cc, 1:513], in_=ps[:, :])
            else:
                k.i('dve', 'tensor_copy', reads=[ps], pwrites=[PR], out=PR[:, cc, 1:513], in_=ps[:, :])
        if stile == NST - 1:
            k.dma('sp', out_shift, out_shift.ap().rearrange("(c p) -> p c", p=128), PR, PR[:, :, 512], allow_slow_non_contiguous=True)
        k.begin_fill(XM)
        for cc in range(9):
            eng = 'dve' if cc % 2 == 0 else 'pool'
            t1 = f32t['t1'] if cc % 2 == 0 else f32t['t2']
            k.i(eng, 'tensor_scalar', reads=[PR, c.mu], writes=[t1], out=t1[:], in0=PR[:, cc, 0:512], scalar1=c.mu[:, cc:cc + 1],
                scalar2=None, op0=ALU.mult)
            k.i('dve', 'scalar_tensor_tensor', reads=[PR, c.omm, t1], pwrites=[XM], out=XM[:, cc, :], in0=PR[:, cc, 1:513],
                scalar=c.omm[:, cc:cc + 1], in1=t1[:], op0=ALU.mult, op1=ALU.add)
        tw, sg7, sg8 = b16t['tw'], b16t['sg7'], b16t['sg8']
        k.i('act', 'activation', reads=[XM], writes=[tw], out=tw[0:64, :], in_=XM[0:64, 6, :], func=AF.Tanh)
        k.i('act', 'copy', reads=[XM], pwrites=[tw], out=tw[64:128, :], in_=XM[64:128, 6, :])
        k.i('act', 'activation', reads=[XM], writes=[sg7], out=sg7[:], in_=XM[:, 7, :], func=AF.Sigmoid)
        k.i('act', 'activation', reads=[XM], writes=[sg8], out=sg8[0:32, :], in_=XM[0:32, 8, :], func=AF.Sigmoid)
        k.begin_fill(GATE, BONUS, gC, *bd.values())
        for g in range(2):
            gs = slice(g * 128, (g + 1) * 128)
            lw, a, kk, kmod, bv, cum, t1, t2 = [f32t[n] for n in ['lw', 'a', 'kk', 'kmod', 'bv', 'cum', 't1', 't2']]
            xr, xk, xv = XM[:, 0 + g, :], XM[:, 2 + g, :], XM[:, 4 + g, :]
            pvg = lambda i, g=g: c.pv[:, i, g:g + 1]
            ps = pW[p.mi % len(pW)]; p.mi += 1
            k.i('pe', 'matmul', reads=[c.lw, tw], writes=[ps], out=ps[:, :], lhsT=c.lw[0:64, 0, gs], rhs=tw[0:64, :], start=True, stop=True)
            k.i('act', 'activation', reads=[ps, c.pv], writes=[lw], out=lw[:], in_=ps[:, :], func=AF.Sigmoid, bias=pvg(0))
            k.i('dve', 'tensor_scalar', reads=[lw], writes=[lw], out=lw[:], in0=lw[:], scalar1=-C_DEC, scalar2=None, op0=ALU.mult)
            ps = pW[p.mi % len(pW)]; p.mi += 1
            k.i('pe', 'matmul', reads=[c.lw, tw], writes=[ps], out=ps[:, :], lhsT=c.lw[64:128, 0, gs], rhs=tw[64:128, :], start=True, stop=True)
            k.i('act', 'activation', reads=[ps, c.pv], writes=[a], out=a[:], in_=ps[:, :], func=AF.Sigmoid, bias=pvg(1))
            ps = pW[p.mi % len(pW)]; p.mi += 1
            k.i('pe', 'matmul', reads=[c.lw, sg7], writes=[ps], out=ps[:, :], lhsT=c.lw[:, 1, gs], rhs=sg7[:], start=True, stop=False)
            k.i('pe', 'matmul', reads=[c.lw, sg8], pwrites=[ps], out=ps[:, :], lhsT=c.lw[0:32, 2, gs], rhs=sg8[0:32, :], start=False, stop=True)
            k.i('act', 'copy', reads=[ps], pwrites=[GATE], out=GATE[:, g, :], in_=ps[:, :])
            k.i('dve', 'tensor_scalar', reads=[XM, c.pv], writes=[kk], out=kk[:], in0=xk, scalar1=pvg(2), scalar2=None, op0=ALU.mult)
            k.i('pool', 'tensor_tensor', reads=[kk], writes=[t1], out=t1[:], in0=kk[:], in1=kk[:], op=ALU.mult)
            ps = pW[p.mi % len(pW)]; p.mi += 1
            k.i('pe', 'matmul', reads=[c.mf, t1], writes=[ps], out=ps[:, :], lhsT=BONES, rhs=t1[:], start=True, stop=True)
            k.i('act', 'activation', reads=[ps], writes=[t2], out=t2[:], in_=ps[:, :], func=AF.Sqrt)
            k.i('dve', 'tensor_scalar', reads=[t2], writes=[t2], out=t2[:], in0=t2[:], scalar1=1e-12, scalar2=None, op0=ALU.max)
            k.i('dve', 'reciprocal', reads=[t2], writes=[t2], out=t2[:], in_=t2[:])
            k.i('dve', 'tensor_tensor', reads=[kk, t2], writes=[kk], out=kk[:], in0=kk[:], in1=t2[:], op=ALU.mult)
            k.i('dve', 'tensor_scalar', reads=[a, c.pv, c.omka], writes=[t1], out=t1[:], in0=a[:], scalar1=pvg(3), scalar2=c.omka[:, g:g + 1],
                op0=ALU.mult, op1=ALU.add)
            k.i('pool', 'tensor_tensor', reads=[XM, t1], writes=[kmod], out=kmod[:], in0=xk, in1=t1[:], op=ALU.mult)
            k.i('pool', 'tensor_tensor', reads=[kk, a], writes=[bv], out=bv[:], in0=kk[:], in1=a[:], op=ALU.mult)
            k.i('dve', 'scalar_tensor_tensor', reads=[XM, kmod, c.pv], writes=[t1], out=t1[:], in0=xr, scalar=pvg(4), in1=kmod[:], op0=ALU.mult, op1=ALU.mult)
            ps = pW[p.mi % len(pW)]; p.mi += 1
            k.i('pe', 'matmul', reads=[c.mf, t1], writes=[ps], out=ps[:, :], lhsT=BONES, rhs=t1[:], start=True, stop=True)
            k.i('dve', 'tensor_tensor', reads=[ps, XM], pwrites=[BONUS], out=BONUS[:, g, :], in0=ps[:, :], in1=xv, op=ALU.mult)
            for ch in range(8):
                cs_ = slice(ch * 64, (ch + 1) * 64)
                k.i('dve', 'tensor_tensor_scan', reads=[lw, p.ones64], writes=[] if ch else [cum], pwrites=[cum] if ch else [],
                    out=cum[:, cs_], data0=p.ones64[:, :], data1=lw[:, cs_], initial=0.0, op0=ALU.mult, op1=ALU.add)
            k.i('act', 'activation', reads=[cum], pwrites=[gC], out=gC[:, g, :], in_=cum[:].rearrange("p (c s) -> p c s", s=64)[:, :, 63], func=AF.Exp)
            k.i('act', 'activation', reads=[cum], writes=[t1], out=t1[:], in_=cum[:], func=AF.Exp)
            k.i('act', 'activation', reads=[cum], writes=[t2], out=t2[:], in_=cum[:], func=AF.Exp, scale=-1.0)
            k.i('dve', 'tensor_tensor', reads=[cum, lw], writes=[lw], out=lw[:], in0=cum[:], in1=lw[:], op=ALU.subtract)
            k.i('act', 'activation', reads=[lw], writes=[lw], out=lw[:], in_=lw[:], func=AF.Exp)

            def mk_bd(dstn, a_res, a_ap, b_res, b_ap, g=g, cum=cum):
                k.i('dve', 'tensor_tensor', reads=[a_res, b_res], writes=[cum], out=cum[:], in0=a_ap, in1=b_ap, op=ALU.mult)
                d = bd[dstn]
                k.i('dve', 'tensor_tensor', reads=[cum, c.mb], pwrites=[d],
                    out=d[:, g, :, :].rearrange("p c (j s) -> p c j s", j=2),
                    in0=cum[:].rearrange("p (c s) -> p c s", s=64).unsqueeze(2).to_broadcast([128, 8, 2, 64]), in1=BM, op=ALU.mult)
            mk_bd('kkt', kk, kk[:], lw, lw[:])
            mk_bd('rt', XM, xr, t1, t1[:])
            mk_bd('bh', bv, bv[:], t2, t2[:])
            mk_bd('kh', kmod, kmod[:], t2, t2[:])
            k.i('dve', 'tensor_tensor', reads=[XM, c.mb], pwrites=[bd['v']],
                out=bd['v'][:, g, :, :].rearrange("p c (j s) -> p c j s", j=2),
                in0=xv.rearrange("p (c s) -> p c s", s=64).unsqueeze(2).to_broadcast([128, 8, 2, 64]), in1=BM, op=ALU.mult)
        kkt, rt, bh, kh, vb = [bd[n] for n in ['kkt', 'rt', 'bh', 'kh', 'v']]
        k.begin_fill(YV)
        def dep_pieces(ch, stile=stile):
            par = ch % 2
            na, ab, kr, tm = NA[par], AB[par], KR[par], TM[par]
            cur = TTF[par]
            bank = {}

            def p0():
                for g in range(2):
                    k.i('act', 'activation', reads=[S32, gC], writes=[S0g] if g == 0 else [], pwrites=[] if g == 0 else [S0g],
                        out=S0g[:, g, :], in_=S32[:, g, :], func=AF.Identity, scale=gC[:, g, ch:ch + 1])
                ps = nb()
                for g in range(2):
                    mmg(ps, g, [(kkt, kkt[:, g, ch, :], S16, S16[:, g, :]), (ab, ab[:, g, :], tm, tm[:, g * 3, :])], g == 0)
                cp(U0, U0[:], ps, ps[:, 0:2, :], scale=-1.0)

            def p1():
                ps = nb()
                for g in range(2):
                    mmg(ps, g, [(cur, cur[:, g, :], U0, U0[:, g, :])], g == 0)
                cp(UU, UU[:], ps, ps[:, 0:2, :])

            def p2():
                ps = nb()
                bank['ps'] = ps
                for g in range(2):
                    mmg(ps, g, [(S16, S16[:, g, :], rt, rt[:, g, ch, :]), (UU, UU[:, g, :], ab, ab[:, 2 + g, :]),
                                (tm, tm[:, g * 3, :], kr, kr[:, g, :])], g == 0)
                for g in range(2):
                    mmg(ps, 2 + g, [(tm, tm[:, g * 3 + 1, :], UU, UU[:, g, :]), (tm, tm[:, g * 3 + 2, :], tm, tm[:, g * 3, :])], False)

            def p3():
                ps = bank['ps']
                k.i('act', 'copy', reads=[ps], pwrites=[YV], out=YV[0:64, :, ch * 64:(ch + 1) * 64], in_=ps[0:64, 0:2, 0:64])
                k.i('dve', 'tensor_copy', reads=[ps], pwrites=[YV], out=YV[64:128, :, ch * 64:(ch + 1) * 64], in_=ps[64:128, 0:2, 64:128])
                for g in range(2):
                    k.i('dve', 'scalar_tensor_tensor', reads=[ps, gC, S0g], writes=[S32] if g == 0 else [], pwrites=[] if g == 0 else [S32],
                        out=S32[:, g, :], in0=ps[:, 2 + g, :], scalar=gC[:, g, ch:ch + 1], in1=S0g[:, g, :], op0=ALU.mult, op1=ALU.add)

            def p4():
                k.i('act', 'copy', reads=[S32], writes=[S16], out=S16[:], in_=S32[:])
            return [p0, p1, p2, p3, p4]

        pend = []
        for ch in range(8):
            par = ch % 2
            na, ab, kr, tm = NA[par], AB[par], KR[par], TM[par]
            ps = nb()
            for g in range(2):
                mmg(ps, 2 * g, [(bh, bh[:, g, ch, :], kkt, kkt[:, g, ch, :])], g == 0)
                mmg(ps, 2 * g + 1, [(kkt, kkt[:, g, ch, :], bh, bh[:, g, ch, :])], False)
            k.i('dve', 'tensor_tensor', reads=[ps, c.mb], writes=[na], out=na[:], in0=ps[:, :, :], in1=c.mb[:, 0:4, :], op=ALU.mult)
            ps = nb()
            for g in range(2):
                mmg(ps, g, [(kh, kh[:, g, ch, :], kkt, kkt[:, g, ch, :])], g == 0)
            for g in range(2):
                mmg(ps, 2 + g, [(bh, bh[:, g, ch, :], rt, rt[:, g, ch, :])], False)
            k.i('dve', 'tensor_tensor', reads=[ps, c.mb], writes=[ab], out=ab[:], in0=ps[:, :, :], in1=c.mb[:, 4:8, :], op=ALU.mult)
            ps = nb()
            for g in range(2):
                mmg(ps, g, [(kh, kh[:, g, ch, :], rt, rt[:, g, ch, :])], g == 0)
            k.i('dve', 'tensor_tensor', reads=[ps, c.mb], writes=[kr], out=kr[:], in0=ps[:, 0:2, :], in1=c.mb[:, 8:10, :], op=ALU.mult)
            first = True
            for g in range(2):
                for j_, src in enumerate((vb, bh, kh)):
                    k.i('pe', 'transpose', reads=[src, identb], writes=[pH] if first else [], pwrites=[] if first else [pH],
                        out=pH[:, g * 3 + j_, :], in_=src[:, g, ch, :], identity=identb[:])
                    first = False
            cp(tm, tm[:], pH, pH[:, 0:6, :])
            cur = TT[0]
            k.i('pool', 'tensor_tensor', reads=[identb, na], writes=[cur], out=cur[:], in0=identb[:].unsqueeze(1).to_broadcast([128, 2, 128]),
                in1=na[:, 0:4:2, :], op=ALU.subtract)
            Pc = [(na, na[:, 0, :]), (na, na[:, 2, :])]
            Qc = [(na, na[:, 1, :]), (na, na[:, 3, :])]
            for lev in range(5):
                pq = PQ[lev % 2]
                ps = nb()
                for g in range(2):
                    if lev < 4:
                        mmg(ps, 2 * g, [(Qc[g][0], Qc[g][1], Pc[g][0], Pc[g][1])], g == 0)
                    mmg(ps, 2 * g + 1, [(Pc[g][0], Pc[g][1], Qc[g][0], Qc[g][1])], (g == 0 and lev == 4))
                if lev < 4:
                    cp(pq, pq[:], ps, ps[:, :, :])
                else:
                    cp(pq, pq[:, 1:4:2, :], ps, ps[:, 1:4:2, :])
                Pn = [(pq, pq[:, 0, :]), (pq, pq[:, 2, :])]
                Qn = [(pq, pq[:, 1, :]), (pq, pq[:, 3, :])]
                nxt = TT[(lev + 1) % 2] if lev < 4 else TTF[par]
                ps = nb()
                for g in range(2):
                    mmg(ps, g, [(identb, identb[:], cur, cur[:, g, :]), (Qn[g][0], Qn[g][1], cur, cur[:, g, :])], g == 0)
                cp(nxt, nxt[:], ps, ps[:, 0:2, :])
                cur = nxt
                Pc, Qc = Pn, Qn
                if pend:
                    pend.pop(0)()
            while pend:
                pend.pop(0)()
            pend = dep_pieces(ch)
        while pend:
            pend.pop(0)()
        for g in range(2):
            t1, t2 = f32t['t1'], f32t['t2']
            pvg = lambda i, g=g: c.pv[:, i, g:g + 1]
            ps = pW[p.mi % len(pW)]; p.mi += 1
            k.i('pe', 'matmul', reads=[c.mf, YV], writes=[ps], out=ps[:, :], lhsT=BONES, rhs=YV[:, g, :], start=True, stop=True)
            k.i('dve', 'scalar_tensor_tensor', reads=[ps, YV], writes=[t1], out=t1[:], in0=ps[:, :], scalar=-1.0 / 64, in1=YV[:, g, :], op0=ALU.mult, op1=ALU.add)
            k.i('pool', 'tensor_tensor', reads=[t1], writes=[t2], out=t2[:], in0=t1[:], in1=t1[:], op=ALU.mult)
            ps = pW[p.mi % len(pW)]; p.mi += 1
            k.i('pe', 'matmul', reads=[c.mf, t2], writes=[ps], out=ps[:, :], lhsT=BONES, rhs=t2[:], start=True, stop=True)
            k.i('act', 'activation', reads=[ps], writes=[t2], out=t2[:], in_=ps[:, :], func=AF.Sqrt, scale=1.0 / 64, bias=GN_EPS)
            k.i('dve', 'reciprocal', reads=[t2], writes=[t2], out=t2[:], in_=t2[:])
            k.i('dve', 'tensor_tensor', reads=[t1, t2], writes=[t1], out=t1[:], in0=t1[:], in1=t2[:], op=ALU.mult)
            k.i('dve', 'tensor_scalar', reads=[t1, c.pv], writes=[t1], out=t1[:], in0=t1[:], scalar1=pvg(5), scalar2=pvg(6), op0=ALU.mult, op1=ALU.add)
            k.i('pool', 'tensor_tensor', reads=[t1, BONUS], writes=[t1], out=t1[:], in0=t1[:], in1=BONUS[:, g, :], op=ALU.add)
            ro = rwo[g]
            k.i('dve', 'tensor_tensor', reads=[t1, GATE], writes=[ro], out=ro[:], in0=t1[:], in1=GATE[:, g, :], op=ALU.mult)
            ow = out_rw[stile]
            k.dma('sp', ow, ow.ap()[g * 128:(g + 1) * 128, :], ro, ro[:], partial=True)
        if after_st is not None:
            after_st(stile)
    so = S0g
    ps = nb()
    for g in range(2):
        k.i('pe', 'transpose', reads=[S32, identf], writes=[ps] if g == 0 else [], pwrites=[] if g == 0 else [ps],
            out=ps[:, g, :], in_=S32[:, g, :], identity=identf[:])
    k.i('dve', 'tensor_copy', reads=[ps], writes=[so], out=so[:], in_=ps[:, 0:2, :])
    for g in range(2):
        for j2 in range(2):
            k.dma('sp', out_state, out_state.ap()[g * 2 + j2], so, so[j2 * 64:(j2 + 1) * 64, g, j2 * 64:(j2 + 1) * 64], partial=True)

from concourse.bass_utils import run_bass_kernel_spmd
import math

D = 2048
EPS = 1e-6
ATT_COLS = 1536
RW_COLS = 3360
RWH = 1152
N_CORES = 8
T_SEQ = 4096
NEG = -30000.0


class P:
    pass


def build(stages):
    nc = bass.Bass("TRN2", target_bir_lowering=False)
    k = K(nc)
    p = P()
    p.k = k
    inp = {}

    del _INPUT_NAMES[:]

    def I(name, shape, dt=F32):
        inp[name] = k.inp(name, shape, dt)
        _INPUT_NAMES.append(name)
        return inp[name]

    I('x_tok', [1168, D]); I('x_seq', [T_SEQ, D]); I('mem', [256, D])
    USED_LN = ('ln1', 'memn') + (('ln2',) if 'OX' in stages else ()) + (('ln3',) if 'M' in stages else ())
    for n in USED_LN:
        I(n, [D])
    I('qn', [64]); I('kn', [64]); I('xkn', [128])
    if 'OX' in stages:
        I('xqn', [128])
    I('w_att', [D, ATT_COLS]); I('w_rw', [D, RW_COLS]); I('w_rwh', [D, RWH]); I('w_xkv', [D, 1024])
    I('cs_all', [1168, 16]); I('ident', [128, 128]); I('masks', [2, 128, 256]); I('sinks', [16])
    I('cwk', [16, 128, 256]); I('cwv', [16, 128, 256])
    if 'SR' in stages:
        I('sh_s', [16, RW_COLS]); I('wkv_s', [16, 16, 64, 64]); I('mu_f', [RW_COLS]); I('pvf', [7, 1024])
        I('w2_f', [64, 1024]); I('a2_f', [64, 1024]); I('g2_f', [160, 1024]); I('lnwb_s', [2, 256, 64])
    if 'SA' in stages:
        I('sinks_s', [64, 4])
    if 'SX' in stages:
        I('cmk_s', [16, 256, 512]); I('cmv_s', [16, 256, 512])
    if 'M' in stages:
        I('w_rt', [D, 72]); I('b_rt', [72]); I('e_g', [64, D, 512]); I('e_u', [64, D, 512]); I('e_d', [64, 512, D]); I('iota64', [64])
    if 'OX' in stages:
        I('w_out', [D, D]); I('w_xq', [D, 512]); I('w_xo', [512, D]); I('ohj', [4])
    I('mu_h', [RWH]); I('pv_h', [7, 256]); I('w2_h', [64, 256]); I('a2_h', [64, 256]); I('g2_h', [160, 256]); I('rmask', [12, 128, 128])
    o_y = k.outp('o_y', [1040, D])
    o_wkp = k.outp('o_wkp', [128, 256]); o_wvp = k.outp('o_wvp', [128, 256])
    o_shp = k.outp('o_shp', [RWH]); o_wkvp = k.outp('o_wkvp', [4, 64, 64])
    o_mk = k.outp('o_mk', [256, 512]); o_mv = k.outp('o_mv', [256, 512])
    o_swk = k.outp('o_swk', [16, 128, 256]); o_swv = k.outp('o_swv', [16, 128, 256])
    o_wkvs = k.outp('o_wkvs', [16, 16, 64, 64]); o_shs = k.outp('o_shs', [16, RW_COLS])
    ATT_O = k.dram('ATT_O', [1152, 1024], BF16)
    RWIN = [k.dram('RWIN%d' % i, [256, 512], BF16) for i in range(8)]
    RWG = [k.dram('RWG%d' % i, [1024, 512], BF16) for i in range(8)]
    QS = k.dram('QS', [16, 1536])
    H2 = k.dram('H2d', [1152, D])
    RWS = k.dram('RWS', [16, 1024], BF16)
    SRd = k.dram('SRd', [16, RW_COLS])
    SV = k.dram('SV', [6, 16, 1024])
    YNd = k.dram('YNd', [256, 64])
    QXd = k.dram('QXd', [16, 512])
    OXd = k.dram('OXd', [16, 512])
    H = [k.ps('H%d' % i, [128, 8, 128], BF16) for i in range(3)]
    Fb = [k.ps('F%d' % i, [128, 512]) for i in range(5)]
    p.pM = Fb[0:2]
    p.pC = [ResView(Fb[2 + i], Fb[2 + i].h[:, :].rearrange("p (a b) -> p a b", a=4)) for i in range(3)]
    p.pH = H[2]
    pT = H[0:2]
    p.identf = k.sb('identf', [128, 128]); p.identb = k.sb('identb', [128, 128], BF16)
    identb = p.identb
    k.dma('sp', p.identf, p.identf[:], inp['ident'], inp['ident'].ap())
    k.i('dve', 'tensor_copy', reads=[p.identf], writes=[identb], out=identb[:], in_=p.identf[:])
    p.ones64 = k.sb('ones64', [128, 64])
    k.i('pool', 'memset', writes=[p.ones64], ap=p.ones64[:], constant=1.0)
    k.dma('sp', o_swk, o_swk.ap()[:, 0:127, :], inp['cwk'], inp['cwk'].ap()[:, 1:128, :])
    k.dma('sp', o_swv, o_swv.ap()[:, 0:127, :], inp['cwv'], inp['cwv'].ap()[:, 1:128, :])
    k.begin_fill(o_swk, o_swv)

    fb = P()

    def alloc_front():
        fb.xt = [k.sb('xt%d' % i, [128, D]) for i in range(2)]
        fb.ub = [k.sb('ub%d' % i, [128, D], BF16) for i in range(2)]
        fb.ssb = [k.sb('ss%d' % i, [128, 1]) for i in range(4)]
        fb.lnA = k.sb('lnA', [128, D])
    p.alloc_front = alloc_front
    p.fb = fb
    p.ti = 0
    p.mi = 0

    def load_ln(name):
        k.dma('sp', fb.lnA, fb.lnA[:], inp[name], inp[name].ap().partition_broadcast(128))
        return fb.lnA

    def bcast_load(name, n):
        t = k.sb('bc_' + name, [128, n])
        k.dma('sp', t, t[:], inp[name], inp[name].ap().partition_broadcast(128))
        return t

    def front(src, src_ap, n, lnb, uT, uT_ap_fn, x_keep=None):
        i = p.ti; p.ti += 1
        x = fb.xt[i % 2] if x_keep is None else x_keep
        u = fb.ub[i % 2]; ss = fb.ssb[i % 4]
        k.dma('sp', x, x[0:n, :], src, src_ap)
        k.i('act', 'activation', reads=[x], writes=[u, ss], out=u[0:n, :], in_=x[0:n, :], func=AF.Square, accum_out=ss[0:n, :])
        k.i('act', 'activation', reads=[ss], writes=[ss], out=ss[0:n, :], in_=ss[0:n, :], func=AF.Sqrt, scale=1.0 / D, bias=EPS)
        k.i('dve', 'reciprocal', reads=[ss], writes=[ss], out=ss[0:n, :], in_=ss[0:n, :])
        k.i('dve', 'scalar_tensor_tensor', reads=[x, ss, lnb], writes=[u], out=u[0:n, :], in0=x[0:n, :], scalar=ss[0:n, 0:1], in1=lnb[0:n, :],
            op0=ALU.mult, op1=ALU.mult)
        transpose16(u, n, uT, uT_ap_fn)

    def transpose16(u, n, uT, uT_ap_fn, nchunks=16):
        for half in range((nchunks + 7) // 8):
            pt = pT[half % 2]
            m = min(8, nchunks - half * 8)
            for cc in range(m):
                dc = half * 8 + cc
                k.i('pe', 'transpose', reads=[u, identb], writes=[pt] if cc == 0 else [], pwrites=[] if cc == 0 else [pt],
                    out=pt[:, cc, 0:n], in_=u[0:n, dc * 128:(dc + 1) * 128], identity=identb[0:n, 0:n])
            dst = uT_ap_fn(half)
            if half % 2 == 0:
                k.i('act', 'copy', reads=[pt], pwrites=[uT], out=dst, in_=pt[:, 0:m, 0:n])
            else:
                k.i('dve', 'tensor_copy', reads=[pt], pwrites=[uT], out=dst, in_=pt[:, 0:m, 0:n])

    def linear_tm(uT, uT_ap_fn, n, W, W_ap_fn, ncols, nk=16, ps=None):
        if ps is None:
            ps = p.pM[p.mi % len(p.pM)]
            p.mi += 1
        for dc in range(nk):
            k.i('pe', 'matmul', reads=[uT, W], writes=[ps] if dc == 0 else [], pwrites=[] if dc == 0 else [ps],
                out=ps[0:n, 0:ncols], lhsT=uT_ap_fn(dc), rhs=W_ap_fn(dc), start=(dc == 0), stop=(dc == nk - 1))
        return ps

    def load_w(dst, src_name, rows, c0, c1, step=512, q='pool'):
        k.begin_fill(dst)
        for c in range(c0, c1, step):
            ce = min(c + step, c1)
            k.dma(q, dst, dst[:, :, c - c0:ce - c0], inp[src_name],
                  inp[src_name].ap()[:, c:ce].rearrange("(c p) n -> p c n", p=128), partial=True)

    p.front = front; p.transpose16 = transpose16; p.linear_tm = linear_tm; p.load_w = load_w
    p.inp = inp; p.nc = nc; p.H = H; p.Fb = Fb; p.pT = pT; p.load_ln = load_ln; p.bcast_load = bcast_load
    p.out = dict(o_y=o_y, o_wkp=o_wkp, o_wvp=o_wvp, o_shp=o_shp, o_wkvp=o_wkvp, o_mk=o_mk, o_mv=o_mv, o_swk=o_swk, o_swv=o_swv,
                 o_wkvs=o_wkvs, o_shs=o_shs)
    p.H2t = [ResView(Res('H2t%d' % i, H2.h, 'dram'), H2.h) for i in range(9)]
    p.dr = dict(ATT_O=ATT_O, RWIN=RWIN, RWG=RWG, QS=QS, H2=H2, RWS=RWS, SRd=SRd, SV=SV, YNd=YNd, QXd=QXd, OXd=OXd)

    p.MEMKT = k.sb('MEMKT', [128, 4, 256], BF16)
    p.MEMV = k.sb('MEMV', [128, 2, 512], BF16)
    k.begin_fill(p.MEMKT, p.MEMV)
    mark0 = nc.sbuf_base

    def phase_end():
        k.barrier()
        nc.sbuf_base = mark0

    if 'A' in stages:
        alloc_front()
        phase_A(k, p)
        phase_end()
    if 'MEM' in stages:
        alloc_front()
        phase_MEM(k, p)
        phase_end()
    if 'R' in stages:
        alloc_front()
        ln1b = load_ln('ln1')
        WH = k.sb('WH', [128, 16, RWH], BF16)
        load_w(WH, 'w_rwh', D, 0, RWH, step=384)
        rwkv_consts(k, p, inp)
        def gather_st(st_):
            k.custom('pool', lambda e, st_=st_: e.collective_compute("AllGather", ALU.bypass, replica_groups=[[0, 1, 2, 3], [4, 5, 6, 7]],
                                                                   ins=[RWIN[st_].h.ap().opt()], outs=[RWG[st_].h.ap().opt()]),
                     1, RWG[st_], reads=[RWIN[st_]], writes=[RWG[st_]])
        rwkv_phase(k, p, inp, T_SEQ, inp['x_seq'], lambda t: inp['x_seq'].ap()[t * 128:(t + 1) * 128, :], WH, ln1b, front,
                   RWIN, o_wkvp, o_shp, after_st=gather_st)
        phase_end()
    if 'SR' in stages:
        phase_SR(k, p)
        phase_end()
    if 'SA' in stages:
        phase_SA(k, p)
        phase_end()
    if 'OX' in stages:
        alloc_front()
        phase_OX(k, p)
        phase_end()
    if 'MT' in stages:
        h2in = I('h2_in', [1152, D])
        for t_ in range(9):
            k.dma('sp', p.H2t[t_], H2.ap()[t_ * 128:(t_ + 1) * 128, :], h2in, h2in.ap()[t_ * 128:(t_ + 1) * 128, :])
    if 'SX' in stages:
        phase_SX(k, p)
        phase_end()
    if 'M' in stages:
        phase_M(k, p, mark0)
        phase_end()
    if 'DBG' in stages:
        o_dbg = k.outp('o_dbg', [1152, 1024], BF16)
        k.dma('sp', o_dbg, o_dbg.ap(), ATT_O, ATT_O.ap())
        o_dbg2 = k.outp('o_dbg2', [1024, T_SEQ], BF16)
        for i_ in range(8):
            k.dma('sp', o_dbg2, o_dbg2.ap()[:, i_ * 512:(i_ + 1) * 512], RWG[i_], RWG[i_].ap(), partial=True)
        o_dbg4 = k.outp('o_dbg4', [16, 1024], BF16)
        k.dma('sp', o_dbg4, o_dbg4.ap(), RWS, RWS.ap())
        o_dbg3 = k.outp('o_dbg3', [1152, D])
        k.dma('sp', o_dbg3, o_dbg3.ap(), H2, H2.ap())
    k.finish()
    print('instr counts', {e: len(k.prog[e]) for e in k.prog}, 'dma sems', k.n_dsem, flush=True)
    return nc


def head_norm(k, p, src, src2d, n, nh, hd, nwb, dst, dst2d, scr, cs=None):
    sq, st, xn, r1, r2, r3 = scr
    w = nh * hd
    k.i('act', 'activation', reads=[src], writes=[sq], out=sq[0:n, 0:w], in_=src2d, func=AF.Square)
    k.i('dve', 'tensor_reduce', reads=[sq], writes=[st], out=st[0:n, 0:nh], in_=sq[0:n, 0:w].rearrange("p (a b) -> p a b", a=nh), axis=AX.X, op=ALU.add)
    k.i('act', 'activation', reads=[st], writes=[st], out=st[0:n, 0:nh], in_=st[0:n, 0:nh], func=AF.Sqrt, scale=1.0 / hd, bias=EPS)
    k.i('dve', 'reciprocal', reads=[st], writes=[st], out=st[0:n, 0:nh], in_=st[0:n, 0:nh])
    k.i('dve', 'tensor_tensor', reads=[src, st], writes=[xn], out=xn[0:n, 0:w].rearrange("p (a b) -> p a b", a=nh),
        in0=src2d.rearrange("p (a b) -> p a b", a=nh), in1=st[0:n, 0:nh].unsqueeze(2).to_broadcast([n, nh, hd]), op=ALU.mult)
    d3 = dst2d.rearrange("p (a b) -> p a b", a=nh)
    k.i('dve', 'tensor_tensor', reads=[xn, nwb], writes=[dst], out=d3, in0=xn[0:n, 0:w].rearrange("p (a b) -> p a b", a=nh),
        in1=nwb[0:n, 0:hd].unsqueeze(1).to_broadcast([n, nh, hd]), op=ALU.mult)
    if cs is not None:
        x1 = d3[:, :, 0:8]; x2 = d3[:, :, 8:16]
        cosb = cs[0:n, 0:8].unsqueeze(1).to_broadcast([n, nh, 8])
        sinb = cs[0:n, 8:16].unsqueeze(1).to_broadcast([n, nh, 8])
        a1, a2, a3 = r1[0:n, 0:nh, :], r2[0:n, 0:nh, :], r3[0:n, 0:nh, :]
        k.i('dve', 'tensor_tensor', reads=[dst, cs], writes=[r1], out=a1, in0=x1, in1=cosb, op=ALU.mult)
        k.i('dve', 'tensor_tensor', reads=[dst, cs], writes=[r2], out=a2, in0=x2, in1=sinb, op=ALU.mult)
        k.i('dve', 'tensor_tensor', reads=[dst, cs], writes=[r3], out=a3, in0=x1, in1=sinb, op=ALU.mult)
        k.i('dve', 'tensor_tensor', reads=[r1, r2], writes=[r1], out=a1, in0=a1, in1=a2, op=ALU.subtract)
        k.i('dve', 'tensor_tensor', reads=[dst, cs], writes=[r2], out=a2, in0=x2, in1=cosb, op=ALU.mult)
        k.i('dve', 'tensor_tensor', reads=[r2, r3], writes=[dst], out=x2, in0=a2, in1=a3, op=ALU.add)
        k.i('dve', 'tensor_copy', reads=[r1], writes=[dst], out=x1, in_=a1)


def phase_A(k, p):
    inp, sb = p.inp, k.sb
    H, Fb = p.H, p.Fb
    identb = p.identb
    ln1b = p.load_ln('ln1')
    qnb = p.bcast_load('qn', 64); knb = p.bcast_load('kn', 64); sinkb = p.bcast_load('sinks', 16)
    maskb = sb('maskb', [128, 2, 256])
    k.dma('sp', maskb, maskb[:], inp['masks'], inp['masks'].ap().rearrange("m p n -> p m n"))
    WA = sb('WA', [128, 16, ATT_COLS], BF16)
    p.load_w(WA, 'w_att', D, 0, ATT_COLS)
    WS = sb('WSr', [128, 16, 480], BF16)
    scr = (sb('sq', [128, 1024]), sb('st', [128, 16]), sb('xn', [128, 1024]), sb('r1', [128, 16, 8]), sb('r2', [128, 16, 8]), sb('r3', [128, 16, 8]))
    uTs = [sb('uT%d' % i, [128, 16, 128], BF16) for i in range(2)]
    cst = [sb('cst%d' % i, [128, 16]) for i in range(2)]
    QF = sb('QF', [128, 1024]); QB = sb('QB', [128, 1024], BF16)
    KF = [sb('KF%d' % i, [128, 512]) for i in range(2)]
    kdup = sb('kdup', [128, 4, 2, 64], BF16)
    KT2 = [sb('KT2_%d' % i, [128, 4, 128], BF16) for i in range(2)]
    VB = [sb('VB%d' % i, [128, 256], BF16) for i in range(2)]
    qT = sb('qT', [128, 8, 128], BF16)
    SM = sb('SM', [128, 4, 256]); E = sb('E', [128, 4, 256], BF16); ET = sb('ET', [128, 8, 128], BF16)
    mx = sb('mx', [128, 4]); negm = sb('negm', [128, 4]); rs = sb('rs', [128, 4]); es = sb('es', [128, 4])
    ATTO = [sb('ATTO%d' % i, [128, 1024], BF16) for i in range(2)]
    srs = [sb('srs%d' % i, [16, 480]) for i in range(2)]
    HT = H[2]
    for t in range(10):
        n = 16 if t == 9 else 128
        r0 = t * 128
        slot = t % 2
        uT = uTs[t % 2]
        k.begin_fill(uT)
        cs = cst[t % 2]
        k.dma('sp', cs, cs[0:n, :], inp['cs_all'], inp['cs_all'].ap()[r0:r0 + n, :])
        p.front(inp['x_tok'], inp['x_tok'].ap()[r0:r0 + n, :], n, ln1b, uT, lambda half, uT=uT, n=n: uT[:, half * 8:(half + 1) * 8, 0:n])
        uf = lambda dc, uT=uT, n=n: uT[:, dc, 0:n]
        kf = KF[slot]
        if t >= 1:
            for hq in range(2):
                ps = p.linear_tm(uT, uf, n, WA, lambda dc, hq=hq: WA[:, dc, hq * 512:(hq + 1) * 512], 512)
                head_norm(k, p, ps, ps[0:n, 0:512], n, 8, 64, qnb, QF, QF[0:n, hq * 512:(hq + 1) * 512], scr, cs=cs)
            k.i('act', 'activation', reads=[QF], writes=[QB], out=QB[0:n, :], in_=QF[0:n, :], func=AF.Identity, scale=0.125)
        ps = p.linear_tm(uT, uf, n, WA, lambda dc: WA[:, dc, 1024:1536], 512)
        head_norm(k, p, ps, ps[0:n, 0:256], n, 4, 64, knb, kf, kf[0:n, 0:256], scr, cs=cs)
        k.i('dve', 'tensor_copy', reads=[ps], writes=[], pwrites=[kf], out=kf[0:n, 256:512], in_=ps[0:n, 256:512])
        if t == 8:
            k.dma('sp', p.out['o_wkp'], p.out['o_wkp'].ap(), kf, kf[:, 0:256])
            k.dma('sp', p.out['o_wvp'], p.out['o_wvp'].ap(), kf, kf[:, 256:512])
        if t == 9:
            k.dma('sp', p.out['o_swk'], p.out['o_swk'].ap()[:, 127, :], kf, kf[0:16, 0:256], partial=True)
            k.dma('sp', p.out['o_swv'], p.out['o_swv'].ap()[:, 127, :], kf, kf[0:16, 256:512], partial=True)
            QSd = p.dr['QS']
            k.dma('sp', QSd, QSd.ap()[:, 0:1024], QF, QF[0:16, :])
            k.dma('sp', QSd, QSd.ap()[:, 1024:1536], kf, kf[0:16, :], partial=True)
            for c in range(7):
                k.dma('pool', WS, WS[:], inp['w_rw'], inp['w_rw'].ap()[:, c * 480:(c + 1) * 480].rearrange("(c p) n -> p c n", p=128))
                ps = p.linear_tm(uT, uf, 16, WS, lambda dc: WS[:, dc, :], 480)
                sr = srs[c % 2]
                k.i('act' if c % 2 == 0 else 'dve', 'copy' if c % 2 == 0 else 'tensor_copy', reads=[ps], writes=[sr], out=sr[0:16, :], in_=ps[0:16, 0:480])
                k.dma('sp', p.out['o_shs'], p.out['o_shs'].ap()[:, c * 480:(c + 1) * 480], sr, sr[:], partial=(c > 0))
                k.dma('sp', p.dr['SRd'], p.dr['SRd'].ap()[:, c * 480:(c + 1) * 480], sr, sr[:], partial=True)
            continue
        k.i('dve', 'tensor_copy', reads=[kf], writes=[kdup], out=kdup[:],
            in_=kf[:, 0:256].rearrange("p (a b) -> p a b", a=4).unsqueeze(2).to_broadcast([128, 4, 2, 64]))
        k.i('act', 'copy', reads=[kf], writes=[VB[slot]], out=VB[slot][:], in_=kf[:, 256:512])
        for kh in range(4):
            k.i('pe', 'transpose', reads=[kdup, identb], writes=[HT] if kh == 0 else [], pwrites=[] if kh == 0 else [HT],
                out=HT[:, kh, :], in_=kdup[:, kh, :, :].rearrange("p a b -> p (a b)"), identity=identb[:])
        k.i('act', 'copy', reads=[HT], writes=[KT2[slot]], out=KT2[slot][:], in_=HT[:, 0:4, :])
        if t == 0:
            continue
        for m in range(8):
            k.i('pe', 'transpose', reads=[QB, identb], writes=[HT] if m == 0 else [], pwrites=[] if m == 0 else [HT],
                out=HT[:, m, :], in_=QB[:, m * 128:(m + 1) * 128], identity=identb[:])
        k.i('dve', 'tensor_copy', reads=[HT], writes=[qT], out=qT[:], in_=HT[:, :, :])
        mi_ = 1 if t == 1 else 0
        ao = ATTO[t % 2]
        k.begin_fill(ao)
        for kh in range(4):
            for hp in range(2):
                bank = Fb[2 + hp]
                first = True
                for g2 in range(2):
                    g = g2 * 2 + hp
                    h = 4 * kh + g
                    m = h // 2
                    for half, sl in ((0, 1 - slot), (1, slot)):
                        k.i('pe', 'matmul', reads=[qT, KT2[sl]], writes=[bank] if first else [], pwrites=[] if first else [bank],
                            out=bank[:, g2 * 256 + half * 128:g2 * 256 + (half + 1) * 128],
                            lhsT=qT[hp * 64:(hp + 1) * 64, m, :], rhs=KT2[sl][hp * 64:(hp + 1) * 64, kh, :], start=True, stop=True)
                        first = False
                k.i('dve', 'tensor_tensor', reads=[bank, maskb], writes=[SM] if hp == 0 else [], pwrites=[] if hp == 0 else [SM],
                    out=SM[:, hp:4:2, :], in0=bank[:, :].rearrange("p (a b) -> p a b", a=2),
                    in1=maskb[:, mi_, :].unsqueeze(1).to_broadcast([128, 2, 256]), op=ALU.add)
            k.i('dve', 'tensor_reduce', reads=[SM], writes=[mx], out=mx[:], in_=SM[:], axis=AX.X, op=ALU.max)
            k.i('dve', 'tensor_tensor', reads=[mx, sinkb], writes=[mx], out=mx[:], in0=mx[:], in1=sinkb[:, kh * 4:(kh + 1) * 4], op=ALU.max)
            k.i('dve', 'tensor_scalar', reads=[mx], writes=[negm], out=negm[:], in0=mx[:], scalar1=-1.0, scalar2=None, op0=ALU.mult)
            for g in range(4):
                k.i('act', 'activation', reads=[SM, negm], writes=[E, rs] if g == 0 else [], pwrites=[] if g == 0 else [E, rs],
                    out=E[:, g, :], in_=SM[:, g, :], func=AF.Exp, bias=negm[:, g:g + 1], accum_out=rs[:, g:g + 1])
            k.i('dve', 'tensor_tensor', reads=[sinkb, negm], writes=[es], out=es[:], in0=sinkb[:, kh * 4:(kh + 1) * 4], in1=negm[:], op=ALU.add)
            k.i('act', 'activation', reads=[es], writes=[es], out=es[:], in_=es[:], func=AF.Exp)
            k.i('dve', 'tensor_tensor', reads=[rs, es], writes=[rs], out=rs[:], in0=rs[:], in1=es[:], op=ALU.add)
            k.i('dve', 'reciprocal', reads=[rs], writes=[rs], out=rs[:], in_=rs[:])
            for g in range(4):
                for half in range(2):
                    i8 = g * 2 + half
                    k.i('pe', 'transpose', reads=[E, identb], writes=[HT] if i8 == 0 else [], pwrites=[] if i8 == 0 else [HT],
                        out=HT[:, i8, :], in_=E[:, g, half * 128:(half + 1) * 128], identity=identb[:])
            k.i('dve', 'tensor_copy', reads=[HT], writes=[ET], out=ET[:], in_=HT[:, :, :])
            ob = Fb[4]
            first = True
            for g in range(4):
                for half, sl in ((0, 1 - slot), (1, slot)):
                    k.i('pe', 'matmul', reads=[ET, VB[sl]], writes=[ob] if first else [], pwrites=[] if first else [ob],
                        out=ob[:, g * 64:(g + 1) * 64], lhsT=ET[:, g * 2 + half, :], rhs=VB[sl][:, kh * 64:(kh + 1) * 64],
                        start=(half == 0), stop=(half == 1))
                    first = False
            k.i('dve', 'tensor_tensor', reads=[ob, rs], pwrites=[ao], out=ao[:, kh * 256:(kh + 1) * 256].rearrange("p (a b) -> p a b", a=4),
                in0=ob[:, 0:256].rearrange("p (a b) -> p a b", a=4), in1=rs[:].unsqueeze(2).to_broadcast([128, 4, 64]), op=ALU.mult)
        AO = p.dr['ATT_O']
        k.dma('sp', AO, AO.ap()[(t - 1) * 128:t * 128, :], ao, ao[:], partial=True)


def phase_MEM(k, p):
    inp, sb = p.inp, k.sb
    memnb = p.load_ln('memn')
    xknb = p.bcast_load('xkn', 128)
    WB = sb('WB', [128, 16, 1024], BF16)
    p.load_w(WB, 'w_xkv', D, 0, 1024)
    scr = (sb('sq', [128, 1024]), sb('st', [128, 16]), sb('xn', [128, 1024]), sb('r1', [128, 16, 8]), sb('r2', [128, 16, 8]), sb('r3', [128, 16, 8]))
    uTs = [sb('uTm%d' % i, [128, 16, 128], BF16) for i in range(2)]
    mkv = [sb('mkv%d' % i, [128, 1024]) for i in range(2)]
    for t in range(2):
        uT = uTs[t]
        k.begin_fill(uT)
        p.front(inp['mem'], inp['mem'].ap()[t * 128:(t + 1) * 128, :], 128, memnb, uT, lambda half, uT=uT: uT[:, half * 8:(half + 1) * 8, :])
        uf = lambda dc, uT=uT: uT[:, dc, :]
        psk = p.linear_tm(uT, uf, 128, WB, lambda dc: WB[:, dc, 0:512], 512)
        psv = p.linear_tm(uT, uf, 128, WB, lambda dc: WB[:, dc, 512:1024], 512)
        m = mkv[t]
        head_norm(k, p, psk, psk[:, 0:512], 128, 4, 128, xknb, m, m[:, 0:512], scr)
        k.i('act', 'copy', reads=[psv], pwrites=[m], out=m[:, 512:1024], in_=psv[:, 0:512])
        k.i('act', 'copy', reads=[m], pwrites=[p.MEMV], out=p.MEMV[:, t, :], in_=m[:, 512:1024])
        kb = sb('kb%d' % t, [128, 512], BF16)
        k.i('dve', 'tensor_copy', reads=[m], writes=[kb], out=kb[:], in_=m[:, 0:512])
        HT = p.H[2]
        for hd in range(4):
            k.i('pe', 'transpose', reads=[kb, p.identb], writes=[HT] if hd == 0 else [], pwrites=[] if hd == 0 else [HT],
                out=HT[:, hd, :], in_=kb[:, hd * 128:(hd + 1) * 128], identity=p.identb[:])
        k.i('act', 'copy', reads=[HT], pwrites=[p.MEMKT], out=p.MEMKT[:, :, t * 128:(t + 1) * 128], in_=HT[:, 0:4, :])
        k.dma('sp', p.out['o_mk'], p.out['o_mk'].ap()[t * 128:(t + 1) * 128, :], m, m[:, 0:512], partial=(t > 0))
        k.dma('sp', p.out['o_mv'], p.out['o_mv'].ap()[t * 128:(t + 1) * 128, :], m, m[:, 512:1024], partial=(t > 0))


def phase_OX(k, p):
    inp, sb = p.inp, k.sb
    H, Fb, identb = p.H, p.Fb, p.identb
    ln2b = p.load_ln('ln2')
    xqnb = p.bcast_load('xqn', 128)
    WO = sb('WO', [128, 16, D], BF16)
    p.load_w(WO, 'w_out', D, 0, D)
    WQ = sb('WQ', [128, 16, 512], BF16)
    p.load_w(WQ, 'w_xq', D, 0, 512)
    WX = sb('WX', [128, 4, D], BF16)
    p.load_w(WX, 'w_xo', 512, 0, D)
    scr = (sb('sq', [128, 1024]), sb('st', [128, 16]), sb('xn', [128, 1024]), sb('r1', [128, 16, 8]), sb('r2', [128, 16, 8]), sb('r3', [128, 16, 8]))
    att_sb = [sb('att_sb%d' % i, [128, 1024], BF16) for i in range(2)]
    aT = [sb('aT%d' % i, [128, 8, 128], BF16) for i in range(2)]
    rwT = [sb('rwT%d' % i, [128, 8, 128], BF16) for i in range(2)]
    xres = [sb('xres0', [128, D])] * 2
    h1 = [sb('h1_0', [128, D])] * 2
    ub2 = sb('ub2', [128, D], BF16)
    ss2 = sb('ss2', [128, 1])
    uT2 = sb('uT2', [128, 16, 128], BF16)
    qx = sb('qx', [128, 512]); qxb = sb('qxb', [128, 512], BF16); qxT = sb('qxT', [128, 4, 128], BF16)
    Ex = sb('Ex', [128, 4, 256], BF16); ETx = sb('ETx', [128, 8, 128], BF16)
    mx = sb('mxx', [128, 4]); negm = sb('negmx', [128, 4]); rs = sb('rsx', [128, 4])
    oxb = sb('oxb', [128, 512], BF16); oxT = sb('oxT', [128, 4, 128], BF16)
    rws_sb = sb('rws_sb', [128, 1024], BF16)
    cand = [sb('cand%d' % q, [128, 8, 128], BF16) for q in range(4)]
    ohb = p.bcast_load('ohj', 4)
    HT = H[2]
    AO, RWG, H2, RWS = p.dr['ATT_O'], p.dr['RWG'], p.dr['H2'], p.dr['RWS']
    for t in range(9):
        n = 16 if t == 8 else 128
        b2 = t % 2
        xr_ = xres[b2]; a_sb = att_sb[b2]; aT_ = aT[b2]; rT = rwT[b2]; hh = h1[b2]
        xrow = 128 + t * 128
        k.dma('sp', xr_, xr_[0:n, :], inp['x_tok'], inp['x_tok'].ap()[xrow:xrow + n, :])
        k.dma('sp', a_sb, a_sb[0:n, :], AO, AO.ap()[t * 128:t * 128 + n, :])
        k.begin_fill(aT_)
        p.transpose16(a_sb, n, aT_, lambda half, aT_=aT_, n=n: aT_[:, 0:8, 0:n], nchunks=8)
        if t < 8:
            for q in range(4):
                rg = RWG[2 * q + t // 4]
                k.dma('sp', cand[q], cand[q][:], rg, rg.ap()[:, (t % 4) * 128:(t % 4 + 1) * 128].rearrange("(c p) t -> p c t", p=128))
            k.i('dve', 'tensor_scalar', reads=[cand[0], ohb], writes=[rT], out=rT[:], in0=cand[0][:], scalar1=ohb[:, 0:1], scalar2=None, op0=ALU.mult)
            for q in range(1, 4):
                k.i('dve', 'scalar_tensor_tensor', reads=[cand[q], ohb, rT], writes=[rT], out=rT[:], in0=cand[q][:], scalar=ohb[:, q:q + 1], in1=rT[:],
                    op0=ALU.mult, op1=ALU.add)
        else:
            k.dma('sp', rws_sb, rws_sb[0:16, :], RWS, RWS.ap())
            k.begin_fill(rT)
            p.transpose16(rws_sb, 16, rT, lambda half, rT=rT: rT[:, 0:8, 0:16], nchunks=8)
        for nchk in range(4):
            ps = Fb[nchk]
            for c in range(16):
                src = aT_ if c < 8 else rT
                k.i('pe', 'matmul', reads=[src, WO], writes=[ps] if c == 0 else [], pwrites=[] if c == 0 else [ps],
                    out=ps[0:n, :], lhsT=src[:, c % 8, 0:n], rhs=WO[:, c, nchk * 512:(nchk + 1) * 512], start=(c == 0), stop=(c == 15))
            k.i('dve', 'tensor_tensor', reads=[ps, xr_], writes=[hh] if nchk == 0 else [], pwrites=[] if nchk == 0 else [hh],
                out=hh[0:n, nchk * 512:(nchk + 1) * 512], in0=ps[0:n, :], in1=xr_[0:n, nchk * 512:(nchk + 1) * 512], op=ALU.add)
        k.i('act', 'activation', reads=[hh], writes=[ub2, ss2], out=ub2[0:n, :], in_=hh[0:n, :], func=AF.Square, accum_out=ss2[0:n, :])
        k.i('act', 'activation', reads=[ss2], writes=[ss2], out=ss2[0:n, :], in_=ss2[0:n, :], func=AF.Sqrt, scale=1.0 / D, bias=EPS)
        k.i('dve', 'reciprocal', reads=[ss2], writes=[ss2], out=ss2[0:n, :], in_=ss2[0:n, :])
        k.i('dve', 'scalar_tensor_tensor', reads=[hh, ss2, ln2b], writes=[ub2], out=ub2[0:n, :], in0=hh[0:n, :], scalar=ss2[0:n, 0:1],
            in1=ln2b[0:n, :], op0=ALU.mult, op1=ALU.mult)
        k.begin_fill(uT2)
        p.transpose16(ub2, n, uT2, lambda half, n=n: uT2[:, half * 8:(half + 1) * 8, 0:n])
        ps = p.linear_tm(uT2, lambda dc, n=n: uT2[:, dc, 0:n], n, WQ, lambda dc: WQ[:, dc, :], 512, ps=Fb[4])
        head_norm(k, p, ps, ps[0:n, 0:512], n, 4, 128, xqnb, qx, qx[0:n, :], scr)
        if t < 8:
            k.i('act', 'activation', reads=[qx], writes=[qxb], out=qxb[0:n, :], in_=qx[0:n, :], func=AF.Identity, scale=1.0 / math.sqrt(128.0))
            k.begin_fill(qxT)
            p.transpose16(qxb, n, qxT, lambda half, n=n: qxT[:, 0:4, 0:n], nchunks=4)
            for hp in range(2):
                bank = Fb[2 + hp]
                for h2_ in range(2):
                    hd = hp * 2 + h2_
                    k.i('pe', 'matmul', reads=[qxT, p.MEMKT], writes=[bank] if h2_ == 0 else [], pwrites=[] if h2_ == 0 else [bank],
                        out=bank[0:n, h2_ * 256:(h2_ + 1) * 256], lhsT=qxT[:, hd, 0:n], rhs=p.MEMKT[:, hd, :], start=True, stop=True)
                k.i('dve', 'tensor_reduce', reads=[bank], writes=[mx] if hp == 0 else [], pwrites=[] if hp == 0 else [mx],
                    out=mx[0:n, hp * 2:hp * 2 + 2], in_=bank[0:n, :].rearrange("p (a b) -> p a b", a=2), axis=AX.X, op=ALU.max)
            k.i('dve', 'tensor_scalar', reads=[mx], writes=[negm], out=negm[0:n, :], in0=mx[0:n, :], scalar1=-1.0, scalar2=None, op0=ALU.mult)
            for hd in range(4):
                bank = Fb[2 + hd // 2]
                k.i('act', 'activation', reads=[bank, negm], writes=[Ex, rs] if hd == 0 else [], pwrites=[] if hd == 0 else [Ex, rs],
                    out=Ex[0:n, hd, :], in_=bank[0:n, (hd % 2) * 256:(hd % 2 + 1) * 256], func=AF.Exp, bias=negm[0:n, hd:hd + 1],
                    accum_out=rs[0:n, hd:hd + 1])
            k.i('dve', 'reciprocal', reads=[rs], writes=[rs], out=rs[0:n, :], in_=rs[0:n, :])
            for hd in range(4):
                for half in range(2):
                    i8 = hd * 2 + half
                    k.i('pe', 'transpose', reads=[Ex, identb], writes=[HT] if i8 == 0 else [], pwrites=[] if i8 == 0 else [HT],
                        out=HT[:, i8, 0:n], in_=Ex[0:n, hd, half * 128:(half + 1) * 128], identity=identb[0:n, 0:n])
            k.i('dve', 'tensor_copy', reads=[HT], writes=[ETx], out=ETx[:, :, 0:n], in_=HT[:, :, 0:n])
            ob = Fb[4]
            first = True
            for hd in range(4):
                for half in range(2):
                    k.i('pe', 'matmul', reads=[ETx, p.MEMV], writes=[ob] if first else [], pwrites=[] if first else [ob],
                        out=ob[0:n, hd * 128:(hd + 1) * 128], lhsT=ETx[:, hd * 2 + half, 0:n], rhs=p.MEMV[:, half, hd * 128:(hd + 1) * 128],
                        start=(half == 0), stop=(half == 1))
                    first = False
            k.i('dve', 'tensor_tensor', reads=[ob, rs], writes=[oxb], out=oxb[0:n, :].rearrange("p (a b) -> p a b", a=4),
                in0=ob[0:n, :].rearrange("p (a b) -> p a b", a=4), in1=rs[0:n, :].unsqueeze(2).to_broadcast([n, 4, 128]), op=ALU.mult)
        else:
            k.dma('sp', p.dr['QXd'], p.dr['QXd'].ap(), qx, qx[0:16, :])
            k.dma('sp', p.H2t[8], H2.ap()[1024:1040, :], hh, hh[0:16, :])
            continue
        k.begin_fill(oxT)
        p.transpose16(oxb, n, oxT, lambda half, n=n: oxT[:, 0:4, 0:n], nchunks=4)
        for nchk in range(4):
            ps = Fb[nchk]
            for c in range(4):
                k.i('pe', 'matmul', reads=[oxT, WX], writes=[ps] if c == 0 else [], pwrites=[] if c == 0 else [ps],
                    out=ps[0:n, :], lhsT=oxT[:, c, 0:n], rhs=WX[:, c, nchk * 512:(nchk + 1) * 512], start=(c == 0), stop=(c == 3))
            k.i('dve', 'tensor_tensor', reads=[ps, hh], writes=[hh], out=hh[0:n, nchk * 512:(nchk + 1) * 512], in0=ps[0:n, :],
                in1=hh[0:n, nchk * 512:(nchk + 1) * 512], op=ALU.add)
        k.dma('sp', p.H2t[t], H2.ap()[t * 128:t * 128 + n, :], hh, hh[0:n, :])


def sample_xattn(k, p, qx, oxb):
    k.i('pool', 'memset', writes=[oxb], ap=oxb[0:16, :], constant=0.0)


def phase_M(k, p, mark0):
    inp, sb, nc = p.inp, k.sb, p.nc
    H, Fb, identb, identf = p.H, p.Fb, p.identb, p.identf
    H2 = p.dr['H2']
    CAP = 64
    U16 = sb('U16', [128, 9, D], BF16)
    GATE = sb('GATEm', [128, 9, 64]); ASG = sb('ASG', [128, 9, 64], BF16); RANK = sb('RANK', [128, 9, 64])
    iota = p.bcast_load('iota64', 64)
    onesb = sb('onesb', [128, 128], BF16)
    trib = sb('trib', [128, 128], BF16)
    k.i('pool', 'memset', writes=[onesb], ap=onesb[:], constant=1.0)
    k.i('pool', 'memset', writes=[U16], ap=U16[:], constant=0.0)
    k.i('pool', 'memset', writes=[GATE], ap=GATE[:], constant=0.0)
    k.i('pool', 'memset', writes=[ASG], ap=ASG[:], constant=0.0)
    trif = sb('trif', [128, 128])
    k.dma('sp', trif, trif[:], inp['rmask'], inp['rmask'].ap()[0])
    k.i('dve', 'tensor_copy', reads=[trif], writes=[trib], out=trib[:], in_=trif[:])
    mark1 = nc.sbuf_base
    p.alloc_front()
    fb = p.fb
    ln3b = p.load_ln('ln3')
    WR = sb('WR', [128, 16, 72])
    k.dma('sp', WR, WR[:], inp['w_rt'], inp['w_rt'].ap().rearrange("(c p) n -> p c n", p=128))
    brt = p.bcast_load('b_rt', 72)
    u32 = sb('u32', [128, D]); uT32 = sb('uT32', [128, 16, 128])
    LG = sb('LG', [128, 72])
    sm = {n: sb('m_' + n, [128, 8]) for n in ['ohg', 'e8', 'oh1', 'oh2', 'e8b', 'g8', 't8']}
    s1 = {n: sb('m1_' + n, [128, 1]) for n in ['gmax', 'ngmax', 'se', 'm1', 'm2', 'w1', 'w2']}
    sel3 = sb('sel3', [128, 8, 8])
    k.begin_fill(U16, GATE, ASG)
    for t in range(9):
        n = 16 if t == 8 else 128
        x = fb.xt[t % 2]; ss = fb.ssb[t % 4]; ubf = fb.ub[t % 2]
        k.dma('sp', x, x[0:n, :], p.H2t[t], H2.ap()[t * 128:t * 128 + n, :])
        k.i('act', 'activation', reads=[x], writes=[ubf, ss], out=ubf[0:n, :], in_=x[0:n, :], func=AF.Square, accum_out=ss[0:n, :])
        k.i('act', 'activation', reads=[ss], writes=[ss], out=ss[0:n, :], in_=ss[0:n, :], func=AF.Sqrt, scale=1.0 / D, bias=EPS)
        k.i('dve', 'reciprocal', reads=[ss], writes=[ss], out=ss[0:n, :], in_=ss[0:n, :])
        k.i('dve', 'scalar_tensor_tensor', reads=[x, ss, ln3b], writes=[u32], out=u32[0:n, :], in0=x[0:n, :], scalar=ss[0:n, 0:1], in1=ln3b[0:n, :],
            op0=ALU.mult, op1=ALU.mult)
        k.i('act', 'copy', reads=[u32], pwrites=[U16], out=U16[0:n, t, :], in_=u32[0:n, :])
        k.begin_fill(uT32)
        for q4 in range(4):
            bank = Fb[q4 % 2]
            for c4 in range(4):
                dc = q4 * 4 + c4
                k.i('pe', 'transpose', reads=[u32, identf], writes=[bank] if c4 == 0 else [], pwrites=[] if c4 == 0 else [bank],
                    out=bank[:, c4 * 128:c4 * 128 + n], in_=u32[0:n, dc * 128:(dc + 1) * 128], identity=identf[0:n, 0:n])
            k.i('act' if q4 % 2 == 0 else 'dve', 'copy' if q4 % 2 == 0 else 'tensor_copy', reads=[bank], pwrites=[uT32],
                out=uT32[:, q4 * 4:(q4 + 1) * 4, 0:n], in_=bank[:, :].rearrange("p (a b) -> p a b", a=4)[:, :, 0:n])
        ps = Fb[4]
        for dc in range(16):
            k.i('pe', 'matmul', reads=[uT32, WR], writes=[ps] if dc == 0 else [], pwrites=[] if dc == 0 else [ps],
                out=ps[0:n, 0:72], lhsT=uT32[:, dc, 0:n], rhs=WR[:, dc, :], start=(dc == 0), stop=(dc == 15))
        k.i('dve', 'tensor_tensor', reads=[ps, brt], writes=[LG], out=LG[0:n, :], in0=ps[0:n, 0:72], in1=brt[0:n, :], op=ALU.add)
        a_ = lambda r: r[0:n, :]
        gl = LG[0:n, 0:8]
        k.i('dve', 'tensor_reduce', reads=[LG], writes=[s1['gmax']], out=a_(s1['gmax']), in_=gl, axis=AX.X, op=ALU.max)
        k.i('dve', 'tensor_scalar', reads=[LG, s1['gmax']], writes=[sm['ohg']], out=a_(sm['ohg']), in0=gl, scalar1=s1['gmax'][0:n, 0:1], scalar2=None, op0=ALU.is_equal)
        k.i('dve', 'tensor_scalar', reads=[s1['gmax']], writes=[s1['ngmax']], out=a_(s1['ngmax']), in0=a_(s1['gmax']), scalar1=-1.0, scalar2=None, op0=ALU.mult)
        k.i('act', 'activation', reads=[LG, s1['ngmax']], writes=[sm['t8'], s1['se']], out=a_(sm['t8']), in_=gl, func=AF.Exp, bias=s1['ngmax'][0:n, 0:1],
            accum_out=a_(s1['se']))
        k.i('dve', 'reciprocal', reads=[s1['se']], writes=[s1['se']], out=a_(s1['se']), in_=a_(s1['se']))
        k.i('dve', 'tensor_tensor', reads=[LG, sm['ohg']], writes=[sel3], out=sel3[0:n, :, :], in0=LG[0:n, 8:72].rearrange("p (g e) -> p g e", g=8),
            in1=sm['ohg'][0:n, :].unsqueeze(2).to_broadcast([n, 8, 8]), op=ALU.mult)
        k.i('dve', 'tensor_reduce', reads=[sel3], writes=[sm['e8']], out=a_(sm['e8']), in_=sel3[0:n, :, :].rearrange("p g e -> p e g"), axis=AX.X, op=ALU.add)
        k.i('dve', 'tensor_reduce', reads=[sm['e8']], writes=[s1['m1']], out=a_(s1['m1']), in_=a_(sm['e8']), axis=AX.X, op=ALU.max)
        k.i('dve', 'tensor_scalar', reads=[sm['e8'], s1['m1']], writes=[sm['oh1']], out=a_(sm['oh1']), in0=a_(sm['e8']), scalar1=s1['m1'][0:n, 0:1], scalar2=None, op0=ALU.is_equal)
        k.i('dve', 'scalar_tensor_tensor', reads=[sm['oh1'], sm['e8']], writes=[sm['e8b']], out=a_(sm['e8b']), in0=a_(sm['oh1']), scalar=-1e30, in1=a_(sm['e8']),
            op0=ALU.mult, op1=ALU.add)
        k.i('dve', 'tensor_reduce', reads=[sm['e8b']], writes=[s1['m2']], out=a_(s1['m2']), in_=a_(sm['e8b']), axis=AX.X, op=ALU.max)
        k.i('dve', 'tensor_scalar', reads=[sm['e8b'], s1['m2']], writes=[sm['oh2']], out=a_(sm['oh2']), in0=a_(sm['e8b']), scalar1=s1['m2'][0:n, 0:1], scalar2=None, op0=ALU.is_equal)
        k.i('dve', 'tensor_tensor', reads=[s1['m2'], s1['m1']], writes=[s1['w1']], out=a_(s1['w1']), in0=a_(s1['m2']), in1=a_(s1['m1']), op=ALU.subtract)
        k.i('act', 'activation', reads=[s1['w1']], writes=[s1['w1']], out=a_(s1['w1']), in_=a_(s1['w1']), func=AF.Exp)
        k.i('dve', 'tensor_scalar', reads=[s1['w1']], writes=[s1['w1']], out=a_(s1['w1']), in0=a_(s1['w1']), scalar1=1.0, scalar2=None, op0=ALU.add)
        k.i('dve', 'reciprocal', reads=[s1['w1']], writes=[s1['w1']], out=a_(s1['w1']), in_=a_(s1['w1']))
        k.i('dve', 'tensor_scalar', reads=[s1['w1']], writes=[s1['w2']], out=a_(s1['w2']), in0=a_(s1['w1']), scalar1=-1.0, scalar2=1.0, op0=ALU.mult, op1=ALU.add)
        k.i('dve', 'tensor_tensor', reads=[s1['w1'], s1['se']], writes=[s1['w1']], out=a_(s1['w1']), in0=a_(s1['w1']), in1=a_(s1['se']), op=ALU.mult)
        k.i('dve', 'tensor_tensor', reads=[s1['w2'], s1['se']], writes=[s1['w2']], out=a_(s1['w2']), in0=a_(s1['w2']), in1=a_(s1['se']), op=ALU.mult)
        k.i('dve', 'tensor_scalar', reads=[sm['oh1'], s1['w1']], writes=[sm['g8']], out=a_(sm['g8']), in0=a_(sm['oh1']), scalar1=s1['w1'][0:n, 0:1], scalar2=None, op0=ALU.mult)
        k.i('dve', 'scalar_tensor_tensor', reads=[sm['oh2'], s1['w2'], sm['g8']], writes=[sm['g8']], out=a_(sm['g8']), in0=a_(sm['oh2']), scalar=s1['w2'][0:n, 0:1],
            in1=a_(sm['g8']), op0=ALU.mult, op1=ALU.add)
        k.i('dve', 'tensor_tensor', reads=[sm['oh1'], sm['oh2']], writes=[sm['t8']], out=a_(sm['t8']), in0=a_(sm['oh1']), in1=a_(sm['oh2']), op=ALU.add)
        k.i('dve', 'tensor_tensor', reads=[sm['ohg'], sm['g8']], pwrites=[GATE], out=GATE[0:n, t, :].rearrange("p (g e) -> p g e", g=8),
            in0=sm['ohg'][0:n, :].unsqueeze(2).to_broadcast([n, 8, 8]), in1=sm['g8'][0:n, :].unsqueeze(1).to_broadcast([n, 8, 8]), op=ALU.mult)
        k.i('dve', 'tensor_tensor', reads=[sm['ohg'], sm['t8']], pwrites=[ASG], out=ASG[0:n, t, :].rearrange("p (g e) -> p g e", g=8),
            in0=sm['ohg'][0:n, :].unsqueeze(2).to_broadcast([n, 8, 8]), in1=sm['t8'][0:n, :].unsqueeze(1).to_broadcast([n, 8, 8]), op=ALU.mult)
    k.begin_fill(RANK)
    for t in range(9):
        ps = Fb[t % 2]
        k.i('pe', 'matmul', reads=[trib, ASG], writes=[ps], out=ps[:, 0:64], lhsT=trib[:], rhs=ASG[:, t, :], start=True, stop=(t == 0))
        for t2 in range(t):
            k.i('pe', 'matmul', reads=[onesb, ASG], pwrites=[ps], out=ps[:, 0:64], lhsT=onesb[:], rhs=ASG[:, t2, :], start=False, stop=(t2 == t - 1))
        k.i('dve', 'scalar_tensor_tensor', reads=[ps, ASG], pwrites=[RANK], out=RANK[:, t, :], in0=ps[:, 0:64], scalar=1.0, in1=ASG[:, t, :], op0=ALU.add, op1=ALU.mult)
    k.i('dve', 'tensor_scalar', reads=[RANK], writes=[RANK], out=RANK[:], in0=RANK[:], scalar1=-1.0, scalar2=None, op0=ALU.add)
    k.barrier()
    nc.sbuf_base = mark1
    WG = [sb('WG%d' % i, [128, 16, 512], BF16) for i in range(2)]
    WU = [sb('WU%d' % i, [128, 16, 512], BF16) for i in range(2)]
    WD = [sb('WD%d' % i, [128, 4, D], BF16) for i in range(2)]
    SEL = sb('SEL', [128, 9, 128], BF16); SELG = sb('SELG', [128, 9, 128], BF16)
    SELGT = [sb('SELGT%d' % i, [128, 9, 128], BF16) for i in range(4)]
    UT = sb('UTp', [128, 16, 128], BF16)
    HB = sb('HB', [128, 512], BF16); HTs = sb('HTs', [128, 4, 128], BF16)
    sg = sb('sg', [128, 512])
    YG = sb('YG', [128, 4, D], BF16)
    OUTt = sb('OUTt', [128, D])
    iota3 = iota[:, :].unsqueeze(1).to_broadcast([128, 9, 64])

    def load_expert(e):
        b = e % 2
        k.dma('pool', WG[b], WG[b][:], inp['e_g'], inp['e_g'].ap()[e].rearrange("(c p) n -> p c n", p=128))
        k.dma('pool', WU[b], WU[b][:], inp['e_u'], inp['e_u'].ap()[e].rearrange("(c p) n -> p c n", p=128))
        k.dma('pool', WD[b], WD[b][:], inp['e_d'], inp['e_d'].ap()[e].rearrange("(c p) n -> p c n", p=128))
    load_expert(0)
    for grp in range(8):
        k.begin_fill(YG)
        for pr in range(4):
            e0 = grp * 8 + pr * 2
            for e2 in range(2):
                e = e0 + e2
                k.i('dve', 'tensor_tensor', reads=[iota, RANK], writes=[SEL] if e2 == 0 else [], pwrites=[] if e2 == 0 else [SEL],
                    out=SEL[:, :, e2 * 64:(e2 + 1) * 64], in0=iota3, in1=RANK[:, :, e:e + 1].to_broadcast([128, 9, 64]), op=ALU.is_equal)
                k.i('dve', 'tensor_tensor', reads=[SEL, GATE], writes=[SELG] if e2 == 0 else [], pwrites=[] if e2 == 0 else [SELG],
                    out=SELG[:, :, e2 * 64:(e2 + 1) * 64], in0=SEL[:, :, e2 * 64:(e2 + 1) * 64], in1=GATE[:, :, e:e + 1].to_broadcast([128, 9, 64]), op=ALU.mult)
            sgt = SELGT[pr]
            k.begin_fill(sgt)
            for t in range(9):
                hb_ = H[t // 8]
                k.i('pe', 'transpose', reads=[SELG, identb], writes=[hb_] if t % 8 == 0 else [], pwrites=[] if t % 8 == 0 else [hb_],
                    out=hb_[:, t % 8, :], in_=SELG[:, t, :], identity=identb[:])
            k.i('act', 'copy', reads=[H[0]], pwrites=[sgt], out=sgt[:, 0:8, :], in_=H[0][:, :, :])
            k.i('dve', 'tensor_copy', reads=[H[1]], pwrites=[sgt], out=sgt[:, 8:9, :], in_=H[1][:, 0:1, :])
            k.begin_fill(UT)
            for q4 in range(4):
                bank = Fb[q4]
                for c4 in range(4):
                    dc = q4 * 4 + c4
                    for t in range(9):
                        k.i('pe', 'matmul', reads=[U16, SEL], writes=[bank] if (c4 == 0 and t == 0) else [], pwrites=[] if (c4 == 0 and t == 0) else [bank],
                            out=bank[:, c4 * 128:(c4 + 1) * 128], lhsT=U16[:, t, dc * 128:(dc + 1) * 128], rhs=SEL[:, t, :], start=(t == 0), stop=(t == 8))
                k.i('act' if q4 % 2 == 0 else 'dve', 'copy' if q4 % 2 == 0 else 'tensor_copy', reads=[bank], pwrites=[UT],
                    out=UT[:, q4 * 4:(q4 + 1) * 4, :], in_=bank[:, :].rearrange("p (a b) -> p a b", a=4))
            for e2 in range(2):
                e = e0 + e2
                b = e % 2
                if e + 1 < 64:
                    load_expert(e + 1)
                lo, hi = e2 * 64, (e2 + 1) * 64
                tp = (0, lo)
                for (W_, bank) in ((WG[b], Fb[0]), (WU[b], Fb[1])):
                    for dc in range(16):
                        k.i('pe', 'matmul', reads=[UT, W_], writes=[bank] if dc == 0 else [], pwrites=[] if dc == 0 else [bank],
                            out=bank[lo:hi, :], lhsT=UT[:, dc, lo:hi], rhs=W_[:, dc, :], start=(dc == 0), stop=(dc == 15), tile_position=tp)
                k.i('act', 'activation', reads=[Fb[0]], writes=[sg], out=sg[lo:hi, :], in_=Fb[0][lo:hi, :], func=AF.Silu)
                k.i('dve', 'tensor_tensor', reads=[sg, Fb[1]], writes=[HB], out=HB[lo:hi, :], in0=sg[lo:hi, :], in1=Fb[1][lo:hi, :], op=ALU.mult)
                hb_ = H[2]
                for fc in range(4):
                    k.i('pe', 'transpose', reads=[HB, identb], writes=[hb_] if fc == 0 else [], pwrites=[] if fc == 0 else [hb_],
                        out=hb_[:, fc, 0:64], in_=HB[lo:hi, fc * 128:(fc + 1) * 128], identity=identb[lo:hi, lo:hi])
                k.i('act', 'copy', reads=[hb_], writes=[HTs], out=HTs[:, :, 0:64], in_=hb_[:, 0:4, 0:64])
                for half in range(2):
                    for nc2 in range(2):
                        bank = Fb[2 + nc2]
                        ncol = half * 2 + nc2
                        for fc in range(4):
                            k.i('pe', 'matmul', reads=[HTs, WD[b]], writes=[bank] if fc == 0 else [], pwrites=[] if fc == 0 else [bank],
                                out=bank[lo:hi, :], lhsT=HTs[:, fc, 0:64], rhs=WD[b][:, fc, ncol * 512:(ncol + 1) * 512], start=(fc == 0), stop=(fc == 3),
                                tile_position=tp)
                        k.i('act' if nc2 == 0 else 'dve', 'copy' if nc2 == 0 else 'tensor_copy', reads=[bank], pwrites=[YG],
                            out=YG[lo:hi, pr, ncol * 512:(ncol + 1) * 512], in_=bank[lo:hi, :])
        for t in range(9):
            n = 16 if t == 8 else 128
            k.begin_fill(OUTt)
            for nchk in range(4):
                bank = Fb[nchk]
                for pr in range(4):
                    k.i('pe', 'matmul', reads=[SELGT[pr], YG], writes=[bank] if pr == 0 else [], pwrites=[] if pr == 0 else [bank],
                        out=bank[:, :], lhsT=SELGT[pr][:, t, :], rhs=YG[:, pr, nchk * 512:(nchk + 1) * 512], start=(pr == 0), stop=(pr == 3))
                k.i('act' if nchk % 2 == 0 else 'dve', 'copy' if nchk % 2 == 0 else 'tensor_copy', reads=[bank],
                    pwrites=[OUTt], out=OUTt[:, nchk * 512:(nchk + 1) * 512], in_=bank[:, :])
            k.dma('pool', p.H2t[t], H2.ap()[t * 128:t * 128 + n, :], OUTt, OUTt[0:n, :], accum_op=ALU.add)
    for t in range(9):
        n = 16 if t == 8 else 128
        k.dma('sp', p.out['o_y'], p.out['o_y'].ap()[t * 128:t * 128 + n, :], p.H2t[t], H2.ap()[t * 128:t * 128 + n, :], partial=True)


def phase_SR(k, p):
    inp, sb = p.inp, k.sb
    Fb, H, identb, identf = p.Fb, p.H, p.identb, p.identf
    N = 16
    SRd, SV, YNd, RWS = p.dr['SRd'], p.dr['SV'], p.dr['YNd'], p.dr['RWS']
    sr = sb('s_sr', [N, RW_COLS]); pv = sb('s_prev', [N, RW_COLS]); mub = sb('s_mu', [N, RW_COLS])
    k.dma('sp', sr, sr[:], SRd, SRd.ap())
    k.dma('sp', pv, pv[:], inp['sh_s'], inp['sh_s'].ap())
    k.dma('sp', mub, mub[:], inp['mu_f'], inp['mu_f'].ap().partition_broadcast(N))
    k.i('dve', 'tensor_tensor', reads=[pv, sr], writes=[pv], out=pv[:], in0=pv[:], in1=sr[:], op=ALU.subtract)
    k.i('dve', 'tensor_tensor', reads=[pv, mub], writes=[pv], out=pv[:], in0=pv[:], in1=mub[:], op=ALU.mult)
    k.i('dve', 'tensor_tensor', reads=[pv, sr], writes=[pv], out=pv[:], in0=pv[:], in1=sr[:], op=ALU.add)
    xm = pv
    xr, xk, xv = xm[:, 0:1024], xm[:, 1024:2048], xm[:, 2048:3072]
    pvb = sb('s_pvb', [N, 7, 1024])
    k.dma('sp', pvb, pvb[:], inp['pvf'], inp['pvf'].ap().partition_broadcast(N))
    lf = sb('s_lf', [128, 4, 1024]); lwb = sb('s_lwb', [128, 4, 1024], BF16)
    k.i('pool', 'memset', writes=[lf], ap=lf[:], constant=0.0)
    k.dma('sp', lf, lf[0:64, 0, :], inp['w2_f'], inp['w2_f'].ap())
    k.dma('sp', lf, lf[0:64, 1, :], inp['a2_f'], inp['a2_f'].ap(), partial=True)
    k.dma('sp', lf, lf[:, 2, :], inp['g2_f'], inp['g2_f'].ap()[0:128, :], partial=True)
    k.dma('sp', lf, lf[0:32, 3, :], inp['g2_f'], inp['g2_f'].ap()[128:160, :], partial=True)
    k.i('dve', 'tensor_copy', reads=[lf], writes=[lwb], out=lwb[:], in_=lf[:])
    li = sb('s_li', [N, 4, 128], BF16)
    k.i('pool', 'memset', writes=[li], ap=li[:], constant=0.0)
    k.i('act', 'activation', reads=[xm], writes=[li], out=li[:, 0, 0:64], in_=xm[:, 3072:3136], func=AF.Tanh)
    k.i('act', 'copy', reads=[xm], pwrites=[li], out=li[:, 1, 0:64], in_=xm[:, 3136:3200])
    k.i('act', 'activation', reads=[xm], pwrites=[li], out=li[:, 2, :], in_=xm[:, 3200:3328], func=AF.Sigmoid)
    k.i('act', 'activation', reads=[xm], pwrites=[li], out=li[:, 3, 0:32], in_=xm[:, 3328:3360], func=AF.Sigmoid)
    liT = sb('s_liT', [128, 4, N], BF16)
    HT = H[2]
    for i in range(4):
        k.i('pe', 'transpose', reads=[li, identb], writes=[HT] if i == 0 else [], pwrites=[] if i == 0 else [HT],
            out=HT[:, i, 0:N], in_=li[:, i, :], identity=identb[0:N, 0:N])
    k.i('act', 'copy', reads=[HT], writes=[liT], out=liT[:], in_=HT[:, 0:4, 0:N])
    lw = sb('s_lw', [N, 1024]); a = sb('s_a', [N, 1024]); gt = sb('s_g', [N, 1024])
    for hh in range(2):
        cs_ = slice(hh * 512, (hh + 1) * 512)
        ps = Fb[0]
        k.i('pe', 'matmul', reads=[liT, lwb], writes=[ps], out=ps[0:N, :], lhsT=liT[0:64, 0, :], rhs=lwb[0:64, 0, cs_], start=True, stop=True)
        k.i('dve', 'tensor_tensor', reads=[ps, pvb], writes=[lw] if hh == 0 else [], pwrites=[] if hh == 0 else [lw], out=lw[:, cs_], in0=ps[0:N, :], in1=pvb[:, 0, cs_], op=ALU.add)
        ps = Fb[1]
        k.i('pe', 'matmul', reads=[liT, lwb], writes=[ps], out=ps[0:N, :], lhsT=liT[0:64, 1, :], rhs=lwb[0:64, 1, cs_], start=True, stop=True)
        k.i('dve', 'tensor_tensor', reads=[ps, pvb], writes=[a] if hh == 0 else [], pwrites=[] if hh == 0 else [a], out=a[:, cs_], in0=ps[0:N, :], in1=pvb[:, 1, cs_], op=ALU.add)
        ps = Fb[2]
        k.i('pe', 'matmul', reads=[liT, lwb], writes=[ps], out=ps[0:N, :], lhsT=liT[:, 2, :], rhs=lwb[:, 2, cs_], start=True, stop=False)
        k.i('pe', 'matmul', reads=[liT, lwb], pwrites=[ps], out=ps[0:N, :], lhsT=liT[0:32, 3, :], rhs=lwb[0:32, 3, cs_], start=False, stop=True)
        k.i('dve', 'tensor_copy', reads=[ps], writes=[gt] if hh == 0 else [], pwrites=[] if hh == 0 else [gt], out=gt[:, cs_], in_=ps[0:N, :])
    k.i('act', 'activation', reads=[lw], writes=[lw], out=lw[:], in_=lw[:], func=AF.Sigmoid)
    k.i('act', 'activation', reads=[lw], writes=[lw], out=lw[:], in_=lw[:], func=AF.Exp, scale=-C_DEC)
    k.i('act', 'activation', reads=[a], writes=[a], out=a[:], in_=a[:], func=AF.Sigmoid)
    VT = sb('s_VT', [N, 6, 1024])
    t1 = sb('s_t1', [N, 1024]); st = sb('s_st', [N, 16])
    k.begin_fill(VT)
    k.i('act', 'copy', reads=[xm], pwrites=[VT], out=VT[:, 0, :], in_=xr)
    k.i('act', 'copy', reads=[lw], pwrites=[VT], out=VT[:, 1, :], in_=lw[:])
    k.i('act', 'copy', reads=[xm], pwrites=[VT], out=VT[:, 3, :], in_=xv)
    k.i('dve', 'tensor_tensor', reads=[xm, pvb], writes=[t1], out=t1[:], in0=xk, in1=pvb[:, 2, :], op=ALU.mult)
    sq = sb('s_sq', [N, 1024])
    k.i('dve', 'tensor_tensor', reads=[t1], writes=[sq], out=sq[:], in0=t1[:], in1=t1[:], op=ALU.mult)
    k.i('dve', 'tensor_reduce', reads=[sq], writes=[st], out=st[:], in_=sq[:].rearrange("p (h n) -> p h n", h=16), axis=AX.X, op=ALU.add)
    k.i('act', 'activation', reads=[st], writes=[st], out=st[:], in_=st[:], func=AF.Sqrt)
    k.i('dve', 'tensor_scalar', reads=[st], writes=[st], out=st[:], in0=st[:], scalar1=1e-12, scalar2=None, op0=ALU.max)
    k.i('dve', 'reciprocal', reads=[st], writes=[st], out=st[:], in_=st[:])
    k.i('dve', 'tensor_tensor', reads=[t1, st], pwrites=[VT], out=VT[:, 4, :].rearrange("p (h n) -> p h n", h=16),
        in0=t1[:].rearrange("p (h n) -> p h n", h=16), in1=st[:].unsqueeze(2).to_broadcast([N, 16, 64]), op=ALU.mult)
    k.i('dve', 'tensor_scalar', reads=[a], writes=[sq], out=sq[:], in0=a[:], scalar1=-1.0, scalar2=None, op0=ALU.add)
    k.i('dve', 'tensor_tensor', reads=[sq, pvb], writes=[sq], out=sq[:], in0=sq[:], in1=pvb[:, 3, :], op=ALU.mult)
    k.i('dve', 'tensor_scalar', reads=[sq], writes=[sq], out=sq[:], in0=sq[:], scalar1=1.0, scalar2=None, op0=ALU.add)
    k.i('dve', 'tensor_tensor', reads=[sq, xm], pwrites=[VT], out=VT[:, 2, :], in0=sq[:], in1=xk, op=ALU.mult)
    k.i('dve', 'tensor_tensor', reads=[VT, a], pwrites=[VT], out=VT[:, 5, :], in0=VT[:, 4, :], in1=a[:], op=ALU.mult)
    bonus = sb('s_bonus', [N, 1024])
    k.i('dve', 'tensor_tensor', reads=[xm, VT], writes=[t1], out=t1[:], in0=xr, in1=VT[:, 2, :], op=ALU.mult)
    k.i('dve', 'tensor_tensor', reads=[t1, pvb], writes=[t1], out=t1[:], in0=t1[:], in1=pvb[:, 4, :], op=ALU.mult)
    k.i('dve', 'tensor_reduce', reads=[t1], writes=[st], out=st[:], in_=t1[:].rearrange("p (h n) -> p h n", h=16), axis=AX.X, op=ALU.add)
    k.i('dve', 'tensor_tensor', reads=[xm, st], writes=[bonus], out=bonus[:].rearrange("p (h n) -> p h n", h=16),
        in0=xv.rearrange("p (h n) -> p h n", h=16), in1=st[:].unsqueeze(2).to_broadcast([N, 16, 64]), op=ALU.mult)
    for i in range(6):
        k.dma('sp', SV, SV.ap()[i], VT, VT[:, i, :], partial=True)
    VEC = sb('s_VEC', [128, 2, 6, 64])
    k.begin_fill(VEC)
    for r in range(2):
        for i in range(6):
            k.dma('sp', VEC, VEC[:, r, i, :], SV, SV.ap()[i].rearrange("b (h n) -> (b h) n", n=64)[r * 128:(r + 1) * 128, :], partial=True)
    lnwb = sb('s_lnwb', [128, 2, 2, 64])
    k.dma('sp', lnwb, lnwb[:], inp['lnwb_s'], inp['lnwb_s'].ap().rearrange("w (r p) n -> p w r n", p=128))
    YN = sb('s_YN', [128, 2, 64])
    k.begin_fill(YN)
    wkv_in = inp['wkv_s'].ap().rearrange("b h v k -> (b h) (v k)")
    wkv_out = p.out['o_wkvs'].ap().rearrange("b h v k -> (b h) (v k)")
    S = sb('s_S', [128, 64, 64]); T1 = sb('s_T', [128, 64, 64])
    for r in range(2):
        sa = sb('s_sa%d' % r, [128, 64]); y = sb('s_y%d' % r, [128, 64]); ms = sb('s_ms%d' % r, [128, 4])
        k.dma('sp', S, S[:].rearrange("p v k -> p (v k)"), inp['wkv_s'], wkv_in[r * 128:(r + 1) * 128, :])
        vec = lambda i, r=r: VEC[:, r, i, :]
        bk = lambda i, r=r: VEC[:, r, i, :].unsqueeze(1).to_broadcast([128, 64, 64])
        bv_ = lambda ap: ap.unsqueeze(2).to_broadcast([128, 64, 64])
        eng2 = 'pool'
        k.i(eng2, 'tensor_tensor', reads=[S, VEC], writes=[T1], out=T1[:], in0=S[:], in1=bk(4), op=ALU.mult)
        k.i('dve', 'tensor_reduce', reads=[T1], writes=[sa], out=sa[:], in_=T1[:], axis=AX.X, op=ALU.add)
        k.i('dve', 'tensor_scalar', reads=[sa], writes=[sa], out=sa[:], in0=sa[:], scalar1=-1.0, scalar2=None, op0=ALU.mult)
        k.i('dve', 'tensor_tensor', reads=[S, VEC], writes=[S], out=S[:], in0=S[:], in1=bk(1), op=ALU.mult)
        k.i(eng2, 'tensor_tensor', reads=[sa, VEC], writes=[T1], out=T1[:], in0=bv_(sa[:]), in1=bk(5), op=ALU.mult)
        k.i('dve', 'tensor_tensor', reads=[S, T1], writes=[S], out=S[:], in0=S[:], in1=T1[:], op=ALU.add)
        k.i(eng2, 'tensor_tensor', reads=[VEC], writes=[T1], out=T1[:], in0=bv_(vec(3)), in1=bk(2), op=ALU.mult)
        k.i('dve', 'tensor_tensor', reads=[S, T1], writes=[S], out=S[:], in0=S[:], in1=T1[:], op=ALU.add)
        k.dma('sp', p.out['o_wkvs'], wkv_out[r * 128:(r + 1) * 128, :], S, S[:].rearrange("p v k -> p (v k)"), partial=True)
        k.i(eng2, 'tensor_tensor', reads=[S, VEC], writes=[T1], out=T1[:], in0=S[:], in1=bk(0), op=ALU.mult)
        k.i('dve', 'tensor_reduce', reads=[T1], writes=[y], out=y[:], in_=T1[:], axis=AX.X, op=ALU.add)
        k.i('dve', 'tensor_reduce', reads=[y], writes=[ms], out=ms[:, 0:1], in_=y[:], axis=AX.X, op=ALU.add)
        k.i('dve', 'tensor_scalar', reads=[ms], writes=[ms], out=ms[:, 0:1], in0=ms[:, 0:1], scalar1=-1.0 / 64, scalar2=None, op0=ALU.mult)
        k.i('dve', 'tensor_scalar', reads=[y, ms], writes=[y], out=y[:], in0=y[:], scalar1=ms[:, 0:1], scalar2=None, op0=ALU.add)
        k.i('dve', 'tensor_tensor', reads=[y], writes=[sa], out=sa[:], in0=y[:], in1=y[:], op=ALU.mult)
        k.i('dve', 'tensor_reduce', reads=[sa], writes=[ms], out=ms[:, 1:2], in_=sa[:], axis=AX.X, op=ALU.add)
        k.i('act', 'activation', reads=[ms], writes=[ms], out=ms[:, 1:2], in_=ms[:, 1:2], func=AF.Sqrt, scale=1.0 / 64, bias=GN_EPS)
        k.i('dve', 'reciprocal', reads=[ms], writes=[ms], out=ms[:, 1:2], in_=ms[:, 1:2])
        k.i('dve', 'scalar_tensor_tensor', reads=[y, ms, lnwb], writes=[y], out=y[:], in0=y[:], scalar=ms[:, 1:2], in1=lnwb[:, 0, r, :], op0=ALU.mult, op1=ALU.mult)
        k.i('dve', 'tensor_tensor', reads=[y, lnwb], pwrites=[YN], out=YN[:, r, :], in0=y[:], in1=lnwb[:, 1, r, :], op=ALU.add)
        k.dma('sp', YNd, YNd.ap()[r * 128:(r + 1) * 128, :], YN, YN[:, r, :], partial=True)
    ynt = sb('s_ynt', [N, 1024]); rwb = sb('s_rwb', [N, 1024], BF16)
    k.dma('sp', ynt, ynt[:], YNd, YNd.ap().rearrange("(b h) n -> b (h n)", h=16))
    k.i('dve', 'tensor_tensor', reads=[ynt, bonus], writes=[ynt], out=ynt[:], in0=ynt[:], in1=bonus[:], op=ALU.add)
    k.i('dve', 'tensor_tensor', reads=[ynt, gt], writes=[rwb], out=rwb[:], in0=ynt[:], in1=gt[:], op=ALU.mult)
    k.dma('sp', RWS, RWS.ap(), rwb, rwb[:])


def phase_SA(k, p):
    inp, sb = p.inp, k.sb
    QS, AO = p.dr['QS'], p.dr['ATT_O']
    q = sb('a_q', [64, 4, 64]); kn_ = sb('a_kn', [64, 64]); vn = sb('a_vn', [64, 64])
    KC = sb('a_KC', [64, 128, 64]); VC = sb('a_VC', [64, 128, 64]); TT = sb('a_T', [64, 128, 64])
    sinks = sb('a_sinks', [64, 4])
    k.dma('sp', sinks, sinks[:], inp['sinks_s'], inp['sinks_s'].ap())
    k.begin_fill(q, kn_, vn, KC, VC)
    for kh in range(4):
        ps_ = slice(kh * 16, (kh + 1) * 16)
        k.dma('sp', q, q[ps_, :, :].rearrange("p g d -> p (g d)"), QS, QS.ap()[:, kh * 256:(kh + 1) * 256], partial=True)
        k.dma('sp', kn_, kn_[ps_, :], QS, QS.ap()[:, 1024 + kh * 64:1024 + (kh + 1) * 64], partial=True)
        k.dma('sp', vn, vn[ps_, :], QS, QS.ap()[:, 1280 + kh * 64:1280 + (kh + 1) * 64], partial=True)
        k.dma('sp', KC, KC[ps_, :, :], inp['cwk'], inp['cwk'].ap()[:, :, kh * 64:(kh + 1) * 64], partial=True)
        k.dma('sp', VC, VC[ps_, :, :], inp['cwv'], inp['cwv'].ap()[:, :, kh * 64:(kh + 1) * 64], partial=True)
    sc = sb('a_sc', [64, 4, 129]); E = sb('a_E', [64, 4, 129])
    mx = sb('a_mx', [64, 4]); negm = sb('a_negm', [64, 4]); rs = sb('a_rs', [64, 4]); es = sb('a_es', [64, 4])
    t4 = sb('a_t4', [64, 4, 64])
    k.begin_fill(sc)
    for g in range(4):
        k.i('pool', 'tensor_tensor', reads=[KC, q], writes=[TT], out=TT[:], in0=KC[:], in1=q[:, g, :].unsqueeze(1).to_broadcast([64, 128, 64]), op=ALU.mult)
        k.i('dve', 'tensor_reduce', reads=[TT], pwrites=[sc], out=sc[:, g, 0:128], in_=TT[:], axis=AX.X, op=ALU.add)
    k.i('dve', 'tensor_tensor', reads=[q, kn_], writes=[t4], out=t4[:], in0=q[:], in1=kn_[:].unsqueeze(1).to_broadcast([64, 4, 64]), op=ALU.mult)
    k.i('dve', 'tensor_reduce', reads=[t4], pwrites=[sc], out=sc[:, :, 128], in_=t4[:], axis=AX.X, op=ALU.add)
    k.i('dve', 'tensor_reduce', reads=[sc], writes=[mx], out=mx[:], in_=sc[:], axis=AX.X, op=ALU.max)
    k.i('dve', 'tensor_scalar', reads=[mx], writes=[mx], out=mx[:], in0=mx[:], scalar1=0.125, scalar2=None, op0=ALU.mult)
    k.i('dve', 'tensor_tensor', reads=[mx, sinks], writes=[mx], out=mx[:], in0=mx[:], in1=sinks[:], op=ALU.max)
    k.i('dve', 'tensor_scalar', reads=[mx], writes=[negm], out=negm[:], in0=mx[:], scalar1=-1.0, scalar2=None, op0=ALU.mult)
    for g in range(4):
        k.i('act', 'activation', reads=[sc, negm], writes=[E, rs] if g == 0 else [], pwrites=[] if g == 0 else [E, rs],
            out=E[:, g, :], in_=sc[:, g, :], func=AF.Exp, scale=0.125, bias=negm[:, g:g + 1], accum_out=rs[:, g:g + 1])
    k.i('dve', 'tensor_tensor', reads=[sinks, negm], writes=[es], out=es[:], in0=sinks[:], in1=negm[:], op=ALU.add)
    k.i('act', 'activation', reads=[es], writes=[es], out=es[:], in_=es[:], func=AF.Exp)
    k.i('dve', 'tensor_tensor', reads=[rs, es], writes=[rs], out=rs[:], in0=rs[:], in1=es[:], op=ALU.add)
    k.i('dve', 'reciprocal', reads=[rs], writes=[rs], out=rs[:], in_=rs[:])
    o = sb('a_o', [64, 4, 64]); ob = sb('a_ob', [64, 4, 64], BF16)
    k.begin_fill(o)
    for g in range(4):
        k.i('pool', 'tensor_tensor', reads=[VC, E], writes=[TT], out=TT[:], in0=VC[:], in1=E[:, g, 0:128].unsqueeze(2).to_broadcast([64, 128, 64]), op=ALU.mult)
        k.i('dve', 'tensor_reduce', reads=[TT], pwrites=[o], out=o[:, g, :], in_=TT[:].rearrange("p s d -> p d s"), axis=AX.X, op=ALU.add)
        k.i('dve', 'scalar_tensor_tensor', reads=[vn, E, o], pwrites=[o], out=o[:, g, :], in0=vn[:], scalar=E[:, g, 128:129], in1=o[:, g, :], op0=ALU.mult, op1=ALU.add)
    k.i('dve', 'tensor_tensor', reads=[o, rs], writes=[ob], out=ob[:], in0=o[:], in1=rs[:].unsqueeze(2).to_broadcast([64, 4, 64]), op=ALU.mult)
    for kh in range(4):
        k.dma('sp', AO, AO.ap()[1024:1040, kh * 256:(kh + 1) * 256], ob, ob[kh * 16:(kh + 1) * 16, :, :].rearrange("p g d -> p (g d)"), partial=True)


def phase_SX(k, p):
    inp, sb = p.inp, k.sb
    H, Fb, identb = p.H, p.Fb, p.identb
    QXd, OXd, H2 = p.dr['QXd'], p.dr['OXd'], p.dr['H2']
    SC = 1.0 / math.sqrt(128.0)
    q = sb('x_q', [64, 128])
    k.begin_fill(q)
    for h in range(4):
        k.dma('sp', q, q[h * 16:(h + 1) * 16, :], QXd, QXd.ap()[:, h * 128:(h + 1) * 128], partial=True)
    KCH = [sb('x_K%d' % i, [64, 32, 128]) for i in range(2)]
    VCH = [sb('x_V%d' % i, [64, 32, 128]) for i in range(2)]
    TT = sb('x_T', [64, 32, 128])
    sc = sb('x_sc', [64, 256]); E = sb('x_E', [64, 256])
    mx = sb('x_mx', [64, 1]); rs = sb('x_rs', [64, 1])
    o = sb('x_o', [64, 128]); part = sb('x_part', [64, 128])
    k.begin_fill(sc)
    for c8 in range(8):
        kc = KCH[c8 % 2]
        k.begin_fill(kc)
        for h in range(4):
            k.dma('sp', kc, kc[h * 16:(h + 1) * 16, :, :], inp['cmk_s'], inp['cmk_s'].ap()[:, c8 * 32:(c8 + 1) * 32, h * 128:(h + 1) * 128], partial=True)
        k.i('pool', 'tensor_tensor', reads=[kc, q], writes=[TT], out=TT[:], in0=kc[:], in1=q[:].unsqueeze(1).to_broadcast([64, 32, 128]), op=ALU.mult)
        k.i('dve', 'tensor_reduce', reads=[TT], pwrites=[sc], out=sc[:, c8 * 32:(c8 + 1) * 32], in_=TT[:], axis=AX.X, op=ALU.add)
    k.i('dve', 'tensor_reduce', reads=[sc], writes=[mx], out=mx[:], in_=sc[:], axis=AX.X, op=ALU.max)
    k.i('dve', 'tensor_scalar', reads=[mx], writes=[mx], out=mx[:], in0=mx[:], scalar1=-SC, scalar2=None, op0=ALU.mult)
    k.i('act', 'activation', reads=[sc, mx], writes=[E, rs], out=E[:], in_=sc[:], func=AF.Exp, scale=SC, bias=mx[:, 0:1], accum_out=rs[:, 0:1])
    k.i('dve', 'reciprocal', reads=[rs], writes=[rs], out=rs[:], in_=rs[:])
    for c8 in range(8):
        vc = VCH[c8 % 2]
        k.begin_fill(vc)
        for h in range(4):
            k.dma('sp', vc, vc[h * 16:(h + 1) * 16, :, :], inp['cmv_s'], inp['cmv_s'].ap()[:, c8 * 32:(c8 + 1) * 32, h * 128:(h + 1) * 128], partial=True)
        k.i('pool', 'tensor_tensor', reads=[vc, E], writes=[TT], out=TT[:], in0=vc[:], in1=E[:, c8 * 32:(c8 + 1) * 32].unsqueeze(2).to_broadcast([64, 32, 128]), op=ALU.mult)
        if c8 == 0:
            k.i('dve', 'tensor_reduce', reads=[TT], writes=[o], out=o[:], in_=TT[:].rearrange("p m d -> p d m"), axis=AX.X, op=ALU.add)
        else:
            k.i('dve', 'tensor_reduce', reads=[TT], writes=[part], out=part[:], in_=TT[:].rearrange("p m d -> p d m"), axis=AX.X, op=ALU.add)
            k.i('dve', 'tensor_tensor', reads=[o, part], writes=[o], out=o[:], in0=o[:], in1=part[:], op=ALU.add)
    k.i('dve', 'tensor_scalar', reads=[o, rs], writes=[o], out=o[:], in0=o[:], scalar1=rs[:, 0:1], scalar2=None, op0=ALU.mult)
    for h in range(4):
        k.dma('sp', OXd, OXd.ap()[:, h * 128:(h + 1) * 128], o, o[h * 16:(h + 1) * 16, :], partial=True)
    WX = sb('x_WX', [128, 4, D], BF16)
    p.load_w(WX, 'w_xo', 512, 0, D)
    oxs = sb('x_oxs', [16, 512]); oxb = sb('x_oxb', [16, 512], BF16); oxT = sb('x_oxT', [128, 4, 16], BF16)
    hh = sb('x_hh', [16, D])
    k.dma('sp', oxs, oxs[:], OXd, OXd.ap())
    k.dma('sp', hh, hh[:], p.H2t[8], H2.ap()[1024:1040, :])
    k.i('act', 'copy', reads=[oxs], writes=[oxb], out=oxb[:], in_=oxs[:])
    k.begin_fill(oxT)
    p.transpose16(oxb, 16, oxT, lambda half: oxT[:, 0:4, 0:16], nchunks=4)
    for nchk in range(4):
        ps = Fb[nchk]
        for c in range(4):
            k.i('pe', 'matmul', reads=[oxT, WX], writes=[ps] if c == 0 else [], pwrites=[] if c == 0 else [ps],
                out=ps[0:16, :], lhsT=oxT[:, c, 0:16], rhs=WX[:, c, nchk * 512:(nchk + 1) * 512], start=(c == 0), stop=(c == 3))
        k.i('dve', 'tensor_tensor', reads=[ps, hh], writes=[hh], out=hh[:, nchk * 512:(nchk + 1) * 512], in0=ps[0:16, :],
            in1=hh[:, nchk * 512:(nchk + 1) * 512], op=ALU.add)
    k.dma('sp', p.H2t[8], H2.ap()[1024:1040, :], hh, hh[:])


def rope_table(pos):
    inv = (np.float32(500000.0) ** (-np.arange(8, dtype=np.float32) * np.float32(2.0) / np.float32(16.0))).astype(np.float32)
    ang = pos.astype(np.float32)[:, None] * inv[None, :]
    return np.concatenate([np.cos(ang), np.sin(ang)], axis=1).astype(np.float32)


def rmasks():
    m = np.zeros((12, 128, 128), np.float32)
    i = np.arange(128)
    tu_s = (i[:, None] < i[None, :]); tl_s = (i[:, None] > i[None, :]); tu_i = (i[:, None] <= i[None, :])
    blk = (i[:, None] // 64 == i[None, :] // 64)
    for n_, mm_ in enumerate([tu_s, tl_s, tu_s, tl_s, tu_s, tu_s, tu_i, tu_i, tu_i, tu_i, blk, blk]):
        m[n_] = mm_
    return m


def swa_masks(j):
    qi = np.arange(128)[:, None]; si = np.arange(256)[None, :]
    rel = 128 + qi - si
    band = (rel >= 0) & (rel <= 128)
    mn = np.where(band, 0.0, NEG).astype(np.float32)
    mf = np.where(band & (si >= 128), 0.0, NEG).astype(np.float32) if j == 0 else mn
    return np.stack([mn, mf])


def own_cols(j):
    cols = [np.arange(part * 1024 + 256 * j, part * 1024 + 256 * j + 256) for part in range(3)]
    cols.append(np.arange(3072, 3360))
    return np.concatenate(cols)


_INPUT_NAMES = []


def nc_input_names(nc):
    return list(_INPUT_NAMES)


import os as _os
STAGES = ('A', 'MEM', 'R', 'SR', 'SA', 'OX', 'SX', 'M')


def kernel(**inp):
    f = lambda a: np.ascontiguousarray(a, dtype=np.float32)
    x_prompt = inp['x_prompt']; x_sample = inp['x_sample']
    w_in = inp['w_in'][0]
    nc = build(STAGES)
    w_rt = f(np.concatenate([inp['router_group_w'][0], inp['router_expert_w'][0]], axis=1))
    b_rt = f(np.concatenate([inp['router_group_b'][0], inp['router_expert_b'][0]]))
    e_g = f(inp['exp_w_gate'][0]); e_u = f(inp['exp_w_up'][0]); e_d = f(inp['exp_w_down'][0])
    pvf = np.stack([inp[n][0] for n in ['rw_w0', 'rw_a0', 'rw_k_k', 'rw_k_a', 'rw_r_k', 'rw_ln_w', 'rw_ln_b']]).astype(np.float32)
    lnwb_s = np.stack([np.tile(inp['rw_ln_w'][0].reshape(16, 64), (16, 1)), np.tile(inp['rw_ln_b'][0].reshape(16, 64), (16, 1))]).astype(np.float32)
    sinks_s = np.repeat(inp['attn_sinks'][0].reshape(4, 4), 16, axis=0).astype(np.float32)
    in_maps = []
    for c in range(N_CORES):
        b, j = c // 4, c % 4
        cols = own_cols(j)
        wh = np.zeros((D, RWH), np.float32); wh[:, :cols.size] = w_in[:, ATT_COLS + cols]
        mu = np.zeros(RWH, np.float32); mu[:cols.size] = inp['rw_mu'][0][cols]
        hs = slice(256 * j, 256 * j + 256)
        pv = np.stack([inp[n][0][hs] for n in ['rw_w0', 'rw_a0', 'rw_k_k', 'rw_k_a', 'rw_r_k', 'rw_ln_w', 'rw_ln_b']]).astype(np.float32)
        x_tok = np.zeros((1168, D), np.float32)
        if j > 0:
            x_tok[0:128] = x_prompt[b, 1024 * j - 128:1024 * j]
        x_tok[128:1152] = x_prompt[b, 1024 * j:1024 * j + 1024]
        x_tok[1152:1168] = x_sample[16 * c:16 * c + 16, 0]
        pos = np.concatenate([np.arange(1024 * j - 128, 1024 * j + 1024), np.full(16, 16384)])
        m = {
            'x_tok': x_tok, 'x_seq': f(x_prompt[b]), 'mem': f(inp['mem_prompt'][b]),
            'ln1': f(inp['ln1_w'][0]), 'ln2': f(inp['ln2_w'][0]), 'ln3': f(inp['ln3_w'][0]), 'memn': f(inp['mem_norm_w'][0]),
            'qn': f(inp['q_norm_w'][0]), 'kn': f(inp['k_norm_w'][0]), 'xqn': f(inp['xq_norm_w'][0]), 'xkn': f(inp['xk_norm_w'][0]),
            'w_att': f(w_in[:, :ATT_COLS]), 'w_rw': f(w_in[:, ATT_COLS:]), 'w_rwh': wh, 'w_xkv': f(inp['xkv_w'][0]),
            'cs_all': rope_table(pos), 'ident': np.eye(128, dtype=np.float32), 'masks': swa_masks(j), 'sinks': f(inp['attn_sinks'][0]),
            'cwk': f(inp['cache_win_k'][0, 16 * c:16 * c + 16].reshape(16, 128, 256)),
            'cwv': f(inp['cache_win_v'][0, 16 * c:16 * c + 16].reshape(16, 128, 256)),
            'mu_h': mu, 'pv_h': pv, 'w2_h': f(inp['rw_w2'][0][:, hs]), 'a2_h': f(inp['rw_a2'][0][:, hs]), 'g2_h': f(inp['rw_g2'][0][:, hs]),
            'rmask': rmasks(),
            'cmk_s': f(inp['cache_mem_k'][0, 16 * c:16 * c + 16].reshape(16, 256, 512)), 'cmv_s': f(inp['cache_mem_v'][0, 16 * c:16 * c + 16].reshape(16, 256, 512)),
            'sh_s': f(inp['state_shift'][0, 16 * c:16 * c + 16]), 'wkv_s': f(inp['state_wkv'][0, 16 * c:16 * c + 16]), 'mu_f': f(inp['rw_mu'][0]),
            'pvf': pvf, 'w2_f': f(inp['rw_w2'][0]), 'a2_f': f(inp['rw_a2'][0]), 'g2_f': f(inp['rw_g2'][0]), 'lnwb_s': lnwb_s, 'sinks_s': sinks_s,
            'ohj': np.eye(4, dtype=np.float32)[j], 'w_rt': w_rt, 'b_rt': b_rt, 'e_g': e_g, 'e_u': e_u, 'e_d': e_d, 'iota64': np.arange(64, dtype=np.float32), 'w_out': f(inp['w_out'][0]), 'w_xq': f(inp['xq_w'][0]), 'w_xo': f(inp['xo_w'][0]),
        }
        in_maps.append(m)
    names = set(nc_input_names(nc))
    in_maps = [{kk: vv for kk, vv in m.items() if kk in names} for m in in_maps]
    res = run_bass_kernel_spmd(nc, in_maps, core_ids=list(range(N_CORES)))
    R = res.results
    global _DBG
    _DBG = R
    y_prompt = np.stack([np.concatenate([R[4 * b + j]['o_y'][0:1024] for j in range(4)]) for b in range(2)])
    y_sample = np.concatenate([R[c]['o_y'][1024:1040] for c in range(8)])[:, None, :]
    wkp = np.stack([R[4 * b + 3]['o_wkp'].reshape(128, 4, 64) for b in range(2)])[None]
    wvp = np.stack([R[4 * b + 3]['o_wvp'].reshape(128, 4, 64) for b in range(2)])[None]
    wkv_p = np.stack([np.concatenate([R[4 * b + j]['o_wkvp'] for j in range(4)]) for b in range(2)])[None]
    shp = np.zeros((1, 2, RW_COLS), np.float32)
    for b in range(2):
        for j in range(4):
            o = R[4 * b + j]['o_shp']
            for part in range(3):
                shp[0, b, part * 1024 + 256 * j: part * 1024 + 256 * j + 256] = o[part * 256:(part + 1) * 256]
            shp[0, b, 3072:3360] = o[768:768 + 288]
    mk = np.stack([R[4 * b]['o_mk'].reshape(256, 4, 128) for b in range(2)])[None]
    mv = np.stack([R[4 * b]['o_mv'].reshape(256, 4, 128) for b in range(2)])[None]
    swk = np.concatenate([R[c]['o_swk'].reshape(16, 128, 4, 64) for c in range(8)])[None]
    swv = np.concatenate([R[c]['o_swv'].reshape(16, 128, 4, 64) for c in range(8)])[None]
    wkv_s = np.concatenate([R[c]['o_wkvs'] for c in range(8)])[None]
    shs = np.concatenate([R[c]['o_shs'] for c in range(8)])[None]
    return (y_prompt, y_sample, wkp, wvp, wkv_p, shp, mk, mv, swk, swv, wkv_s, shs)
```

```python
import numpy as np
import ml_dtypes
import concourse.bass as bass
import concourse.mybir as mybir

F32 = mybir.dt.float32
BF16 = mybir.dt.bfloat16
I32 = mybir.dt.int32
ALU = mybir.AluOpType
AF = mybir.ActivationFunctionType
AX = mybir.AxisListType


class Res:
    def __init__(self, name, h, kind):
        self.name = name
        self.h = h
        self.kind = kind
        self.w = {}
        self.r = {}
        self.prev = {}
        self.dsem = None
        self.dcnt = 0

    def __getitem__(self, idx):
        return self.h[idx]

    def ap(self):
        return self.h.ap() if self.kind in ('dram', 'in', 'out') else self.h[:]


def _merge(dst, src):
    for k, v in src.items():
        if dst.get(k, 0) < v:
            dst[k] = v


class K:
    ENG = ('pe', 'act', 'dve', 'pool', 'sp')

    def __init__(self, nc):
        self.nc = nc
        self.prog = {e: [] for e in self.ENG}
        self.sems = {}
        self.cnt = {}
        for e in self.ENG:
            self.sems[e] = nc.alloc_semaphore('s_' + e)
            self.cnt[e] = 0
        self.waited = {e: {} for e in self.ENG}
        self.out_tickets = {}
        self.n_dsem = 0
        self.nres = 0
        self.dtot = {}
        self.ring_i = 0
        self.NRING = 64

    def sb(self, name, shape, dt=F32):
        self.nres += 1
        name = '%s_%d' % (name, self.nres)
        return Res(name, self.nc.alloc_sbuf_tensor(name, list(shape), dt), 'sb')

    def ps(self, name, shape, dt=F32):
        return Res(name, self.nc.alloc_psum_tensor(name, list(shape), dt), 'ps')

    def dram(self, name, shape, dt=F32):
        return Res(name, self.nc.dram_tensor(name, list(shape), dt), 'dram')

    def inp(self, name, shape, dt=F32):
        return Res(name, self.nc.dram_tensor(name, list(shape), dt, kind='ExternalInput'), 'in')

    def outp(self, name, shape, dt=F32):
        return Res(name, self.nc.dram_tensor(name, list(shape), dt, kind='ExternalOutput'), 'out')

    def _wait(self, e, deps):
        for key, val in deps.items():
            if key == 'pe' and e == 'pe':
                continue
            if self.waited[e].get(key, 0) >= val:
                continue
            self.waited[e][key] = val
            sem = self.sems[key]
            self.prog[e].append(lambda eng, sem=sem, val=val: eng.wait_ge(sem, val))

    def begin_fill(self, *ress):
        for b in ress:
            b.prev = {}
            _merge(b.prev, b.w)
            _merge(b.prev, b.r)
            b.w = {}
            b.r = {}

    def op(self, e, fn, reads=(), writes=(), pwrites=()):
        deps = {}
        for b in reads:
            _merge(deps, b.w)
        for b in writes:
            _merge(deps, b.w)
            _merge(deps, b.r)
        for b in pwrites:
            _merge(deps, b.prev)
        self._wait(e, deps)
        self.cnt[e] += 1
        t = {e: self.cnt[e]}
        sem = self.sems[e]
        self.prog[e].append(lambda eng, fn=fn, sem=sem: fn(eng).then_inc(sem, 1))
        for b in reads:
            _merge(b.r, t)
        for b in writes:
            b.w = dict(t)
            b.r = {}
        for b in pwrites:
            _merge(b.w, t)
        return t

    def dma(self, q, dst, dst_ap, src, src_ap, partial=False, **kw):
        deps = {}
        _merge(deps, src.w)
        if partial:
            _merge(deps, dst.prev)
        else:
            _merge(deps, dst.w)
            _merge(deps, dst.r)
        self._wait(q, deps)
        slot = self.ring_i % self.NRING
        self.ring_i += 1
        key = 'r%d' % slot
        if key not in self.sems:
            self.sems[key] = self.nc.alloc_semaphore(key)
            self.dtot[key] = 0
        self._wait(q, {key: self.dtot[key]})
        self.dtot[key] += 16
        t = {key: self.dtot[key]}
        sem = self.sems[key]
        self.prog[q].append(
            lambda eng, o=dst_ap, i=src_ap, sem=sem, kw=kw: eng.dma_start(out=o, in_=i, **kw).then_inc(sem, 16))
        _merge(src.r, t)
        if partial:
            _merge(dst.w, t)
        else:
            dst.w = dict(t)
            dst.r = {}
        if dst.kind == 'out':
            _merge(self.out_tickets, t)
        return t

    def custom(self, e, fn, inc, semkey_res, reads=(), writes=()):
        deps = {}
        for b in reads:
            _merge(deps, b.w)
        for b in writes:
            _merge(deps, b.w)
            _merge(deps, b.r)
        self._wait(e, deps)
        sres = semkey_res
        if sres.dsem is None:
            key = 'd%d' % self.n_dsem
            self.n_dsem += 1
            self.sems[key] = self.nc.alloc_semaphore(key)
            sres.dsem = key
        sres.dcnt += inc
        t = {sres.dsem: sres.dcnt}
        self.dtot[sres.dsem] = sres.dcnt
        sem = self.sems[sres.dsem]
        self.prog[e].append(lambda eng, fn=fn, sem=sem, inc=inc: fn(eng).then_inc(sem, inc))
        for b in reads:
            _merge(b.r, t)
        for b in writes:
            b.w = dict(t)
            b.r = {}
        return t

    def finish(self):
        self._wait('sp', self.out_tickets)
        allt = {e: self.cnt[e] for e in self.ENG if self.cnt[e] > 0}
        self._wait('sp', allt)
        nc = self.nc
        prog = self.prog
        with nc.Block() as block:
            @block.sync
            def _(eng):
                for f in prog['sp']:
                    f(eng)

            @block.tensor
            def _(eng):
                for f in prog['pe']:
                    f(eng)

            @block.scalar
            def _(eng):
                for f in prog['act']:
                    f(eng)

            @block.vector
            def _(eng):
                for f in prog['dve']:
                    f(eng)

            @block.gpsimd
            def _(eng):
                for f in prog['pool']:
                    f(eng)
        return nc


def _k_i(self, e, meth, reads=(), writes=(), pwrites=(), **kw):
    return self.op(e, lambda eng, meth=meth, kw=kw: getattr(eng, meth)(**kw), reads=reads, writes=writes, pwrites=pwrites)


K.i = _k_i


class ResView:
    def __init__(self, parent, ap, name=None):
        self.p = parent
        self.h = ap
        self.name = name or parent.name + '_v'
        self.kind = parent.kind

    def __getitem__(self, idx):
        return self.h[idx]

    w = property(lambda s: s.p.w, lambda s, v: setattr(s.p, 'w', v))
    r = property(lambda s: s.p.r, lambda s, v: setattr(s.p, 'r', v))
    prev = property(lambda s: s.p.prev, lambda s, v: setattr(s.p, 'prev', v))
    dsem = property(lambda s: s.p.dsem, lambda s, v: setattr(s.p, 'dsem', v))
    dcnt = property(lambda s: s.p.dcnt, lambda s, v: setattr(s.p, 'dcnt', v))


def _k_barrier(self):
    allt = {e: self.cnt[e] for e in self.ENG if self.cnt[e] > 0}
    for key, v in self.dtot.items():
        allt[key] = v
    for e in self.ENG:
        self._wait(e, allt)


K.barrier = _k_barrier


class P:
    pass

C_DEC = 0.6065306597126334
GN_EPS = 64e-5


def rwkv_consts(k, p, inp):
    c = P()
    p.rc = c
    c.mu = k.sb('r_mu', [128, 9])
    c.omm = k.sb('r_omm', [128, 9])
    k.dma('sp', c.mu, c.mu[:], inp['mu_h'], inp['mu_h'].ap().rearrange("(c p) -> p c", p=128), allow_slow_non_contiguous=True)
    k.i('dve', 'tensor_scalar', reads=[c.mu], writes=[c.omm], out=c.omm[:], in0=c.mu[:], scalar1=-1.0, scalar2=1.0, op0=ALU.mult, op1=ALU.add)
    c.pv = k.sb('r_pv', [128, 7, 2])
    k.dma('sp', c.pv, c.pv[:], inp['pv_h'], inp['pv_h'].ap().rearrange("v (g p) -> p v g", p=128), allow_slow_non_contiguous=True)
    c.omka = k.sb('r_omka', [128, 2])
    k.i('dve', 'tensor_scalar', reads=[c.pv], writes=[c.omka], out=c.omka[:], in0=c.pv[:, 3, :], scalar1=-1.0, scalar2=1.0, op0=ALU.mult, op1=ALU.add)
    lf = k.sb('r_lf', [128, 3, 256])
    c.lw = k.sb('r_lw', [128, 3, 256], BF16)
    k.i('pool', 'memset', writes=[lf], ap=lf[:], constant=0.0)
    k.dma('sp', lf, lf[0:64, 0, :], inp['w2_h'], inp['w2_h'].ap())
    k.dma('sp', lf, lf[64:128, 0, :], inp['a2_h'], inp['a2_h'].ap(), partial=True)
    k.dma('sp', lf, lf[:, 1, :], inp['g2_h'], inp['g2_h'].ap()[0:128, :], partial=True)
    k.dma('sp', lf, lf[0:32, 2, :], inp['g2_h'], inp['g2_h'].ap()[128:160, :], partial=True)
    k.i('dve', 'tensor_copy', reads=[lf], writes=[c.lw], out=c.lw[:], in_=lf[:])
    mf = k.sb('r_mf', [128, 12, 128])
    k.dma('sp', mf, mf[:], inp['rmask'], inp['rmask'].ap().rearrange("m p n -> p m n"))
    c.mf = mf
    c.mb = k.sb('r_mb', [128, 12, 128], BF16)
    k.i('dve', 'tensor_copy', reads=[mf], writes=[c.mb], out=c.mb[:], in_=mf[:])
    return c


def rwkv_phase(k, p, inp, T, x_src, x_ap_fn, WH, lnb, front, out_rw, out_state, out_shift, after_st=None):
    c = p.rc
    identb, identf = p.identb, p.identf
    NST = T // 512
    sb = k.sb
    uTall = sb('rk_uT', [128, 16, 512], BF16)
    PR = sb('rk_PR', [128, 9, 513])
    XM = sb('rk_XM', [128, 9, 512])
    k.i('pool', 'memset', writes=[PR], ap=PR[:], constant=0.0)
    f32t = {n: sb('rk_' + n, [128, 512]) for n in ['lw', 'a', 'kk', 'kmod', 'bv', 'cum', 't1', 't2']}
    b16t = {n: sb('rk_' + n, [128, 512], BF16) for n in ['tw', 'sg7', 'sg8']}
    bd = {n: sb('rk_bd_' + n, [128, 2, 8, 128], BF16) for n in ['kkt', 'rt', 'bh', 'kh', 'v']}
    gC = sb('rk_gC', [128, 2, 8])
    GATE = sb('rk_gate', [128, 2, 512])
    BONUS = sb('rk_bonus', [128, 2, 512])
    YV = sb('rk_yv', [128, 2, 512])
    S32 = sb('rk_S32', [128, 2, 128])
    S16 = sb('rk_S16', [128, 2, 128], BF16)
    S0g = sb('rk_S0g', [128, 2, 128])
    k.i('pool', 'memset', writes=[S32], ap=S32[:], constant=0.0)
    k.i('pool', 'memset', writes=[S16], ap=S16[:], constant=0.0)
    NA = [sb('rk_NA%d' % i, [128, 4, 128], BF16) for i in range(2)]
    AB = [sb('rk_AB%d' % i, [128, 4, 128], BF16) for i in range(2)]
    KR = [sb('rk_KR%d' % i, [128, 2, 128], BF16) for i in range(2)]
    TM = [sb('rk_TM%d' % i, [128, 6, 128], BF16) for i in range(2)]
    PQ = [sb('rk_PQ%d' % i, [128, 4, 128], BF16) for i in range(2)]
    TT = [sb('rk_TT%d' % i, [128, 2, 128], BF16) for i in range(2)]
    TTF = [sb('rk_TTF%d' % i, [128, 2, 128], BF16) for i in range(2)]
    U0 = sb('rk_U0', [128, 2, 128], BF16)
    UU = sb('rk_U', [128, 2, 128], BF16)
    rwo = [sb('rk_rwo%d' % i, [128, 512], BF16) for i in range(2)]
    pW = p.pM
    pC = p.pC
    pH = p.pH
    st = P()
    st.q = 0
    st.ev = 0

    def nb():
        s_ = pC[st.q % len(pC)]
        st.q += 1
        return s_

    def mmg(ps, slot_, terms, first_in_bank):
        n = len(terms)
        for i, (L, Lap, Rr, Rap) in enumerate(terms):
            fresh = first_in_bank and i == 0
            k.i('pe', 'matmul', reads=[L, Rr], writes=[ps] if fresh else [], pwrites=[] if fresh else [ps],
                out=ps[:, slot_, :], lhsT=Lap, rhs=Rap, start=(i == 0), stop=(i == n - 1))

    def cp(dst, dst_ap, ps, ps_ap, scale=None):
        st.ev += 1
        if scale is not None:
            k.i('act', 'activation', reads=[ps], writes=[dst], out=dst_ap, in_=ps_ap, func=AF.Identity, scale=scale)
        elif st.ev % 2 == 0:
            k.i('act', 'copy', reads=[ps], writes=[dst], out=dst_ap, in_=ps_ap)
        else:
            k.i('dve', 'tensor_copy', reads=[ps], writes=[dst], out=dst_ap, in_=ps_ap)

    BONES = c.mf[:, 10, :]
    BM = c.mb[:, 11, :].rearrange("p (j s) -> p j s", j=2).unsqueeze(1).to_broadcast([128, 8, 2, 64])
    for stile in range(NST):
        k.begin_fill(uTall)
        for tt in range(4):
            front(x_src, x_ap_fn(stile * 4 + tt), 128, lnb, uTall,
                  lambda half, tt=tt: uTall[:, half * 8:(half + 1) * 8, tt * 128:(tt + 1) * 128])
        if stile > 0:
            k.i('act', 'copy', reads=[PR], writes=[PR], out=PR[:, :, 0:1], in_=PR[:, :, 512:513])
        k.begin_fill(PR)
        for cc in range(9):
            ps = pW[p.mi % len(pW)]
            p.mi += 1
            for dc in range(16):
                k.i('pe', 'matmul', reads=[uTall, WH], writes=[ps] if dc == 0 else [], pwrites=[] if dc == 0 else [ps],
                    out=ps[:, :], lhsT=WH[:, dc, cc * 128:(cc + 1) * 128], rhs=uTall[:, dc, :], start=(dc == 0), stop=(dc == 15))
            if cc % 2 == 0:
                k.i('act', 'copy', reads=[ps], pwrites=[PR], out=PR[:, cc, 1:513], in_=ps[:, :])
            else:
                k.i('dve', 'tensor_copy', reads=[ps], pwrites=[PR], out=PR[:, cc, 1:513], in_=ps[:, :])
        if stile == NST - 1:
            k.dma('sp', out_shift, out_shift.ap().rearrange("(c p) -> p c", p=128), PR, PR[:, :, 512], allow_slow_non_contiguous=True)
        k.begin_fill(XM)
        for cc in range(9):
            eng = 'dve' if cc % 2 == 0 else 'pool'
            t1 = f32t['t1'] if cc % 2 == 0 else f32t['t2']
            k.i(eng, 'tensor_scalar', reads=[PR, c.mu], writes=[t1], out=t1[:], in0=PR[:, cc, 0:512], scalar1=c.mu[:, cc:cc + 1],
                scalar2=None, op0=ALU.mult)
            k.i('dve', 'scalar_tensor_tensor', reads=[PR, c.omm, t1], pwrites=[XM], out=XM[:, cc, :], in0=PR[:, cc, 1:513],
                scalar=c.omm[:, cc:cc + 1], in1=t1[:], op0=ALU.mult, op1=ALU.add)
        tw, sg7, sg8 = b16t['tw'], b16t['sg7'], b16t['sg8']
        k.i('act', 'activation', reads=[XM], writes=[tw], out=tw[0:64, :], in_=XM[0:64, 6, :], func=AF.Tanh)
        k.i('act', 'copy', reads=[XM], pwrites=[tw], out=tw[64:128, :], in_=XM[64:128, 6, :])
        k.i('act', 'activation', reads=[XM], writes=[sg7], out=sg7[:], in_=XM[:, 7, :], func=AF.Sigmoid)
        k.i('act', 'activation', reads=[XM], writes=[sg8], out=sg8[0:32, :], in_=XM[0:32, 8, :], func=AF.Sigmoid)
        k.begin_fill(GATE, BONUS, gC, *bd.values())
        for g in range(2):
            gs = slice(g * 128, (g + 1) * 128)
            lw, a, kk, kmod, bv, cum, t1, t2 = [f32t[n] for n in ['lw', 'a', 'kk', 'kmod', 'bv', 'cum', 't1', 't2']]
            xr, xk, xv = XM[:, 0 + g, :], XM[:, 2 + g, :], XM[:, 4 + g, :]
            pvg = lambda i, g=g: c.pv[:, i, g:g + 1]
            ps = pW[p.mi % len(pW)]; p.mi += 1
            k.i('pe', 'matmul', reads=[c.lw, tw], writes=[ps], out=ps[:, :], lhsT=c.lw[0:64, 0, gs], rhs=tw[0:64, :], start=True, stop=True)
            k.i('act', 'activation', reads=[ps, c.pv], writes=[lw], out=lw[:], in_=ps[:, :], func=AF.Sigmoid, bias=pvg(0))
            k.i('dve', 'tensor_scalar', reads=[lw], writes=[lw], out=lw[:], in0=lw[:], scalar1=-C_DEC, scalar2=None, op0=ALU.mult)
            ps = pW[p.mi % len(pW)]; p.mi += 1
            k.i('pe', 'matmul', reads=[c.lw, tw], writes=[ps], out=ps[:, :], lhsT=c.lw[64:128, 0, gs], rhs=tw[64:128, :], start=True, stop=True)
            k.i('act', 'activation', reads=[ps, c.pv], writes=[a], out=a[:], in_=ps[:, :], func=AF.Sigmoid, bias=pvg(1))
            ps = pW[p.mi % len(pW)]; p.mi += 1
            k.i('pe', 'matmul', reads=[c.lw, sg7], writes=[ps], out=ps[:, :], lhsT=c.lw[:, 1, gs], rhs=sg7[:], start=True, stop=False)
            k.i('pe', 'matmul', reads=[c.lw, sg8], pwrites=[ps], out=ps[:, :], lhsT=c.lw[0:32, 2, gs], rhs=sg8[0:32, :], start=False, stop=True)
            k.i('act', 'copy', reads=[ps], pwrites=[GATE], out=GATE[:, g, :], in_=ps[:, :])
            k.i('dve', 'tensor_scalar', reads=[XM, c.pv], writes=[kk], out=kk[:], in0=xk, scalar1=pvg(2), scalar2=None, op0=ALU.mult)
            k.i('pool', 'tensor_tensor', reads=[kk], writes=[t1], out=t1[:], in0=kk[:], in1=kk[:], op=ALU.mult)
            ps = pW[p.mi % len(pW)]; p.mi += 1
            k.i('pe', 'matmul', reads=[c.mf, t1], writes=[ps], out=ps[:, :], lhsT=BONES, rhs=t1[:], start=True, stop=True)
            k.i('act', 'activation', reads=[ps], writes=[t2], out=t2[:], in_=ps[:, :], func=AF.Sqrt)
            k.i('dve', 'tensor_scalar', reads=[t2], writes=[t2], out=t2[:], in0=t2[:], scalar1=1e-12, scalar2=None, op0=ALU.max)
            k.i('dve', 'reciprocal', reads=[t2], writes=[t2], out=t2[:], in_=t2[:])
            k.i('dve', 'tensor_tensor', reads=[kk, t2], writes=[kk], out=kk[:], in0=kk[:], in1=t2[:], op=ALU.mult)
            k.i('dve', 'tensor_scalar', reads=[a, c.pv, c.omka], writes=[t1], out=t1[:], in0=a[:], scalar1=pvg(3), scalar2=c.omka[:, g:g + 1],
                op0=ALU.mult, op1=ALU.add)
            k.i('pool', 'tensor_tensor', reads=[XM, t1], writes=[kmod], out=kmod[:], in0=xk, in1=t1[:], op=ALU.mult)
            k.i('pool', 'tensor_tensor', reads=[kk, a], writes=[bv], out=bv[:], in0=kk[:], in1=a[:], op=ALU.mult)
            k.i('dve', 'scalar_tensor_tensor', reads=[XM, kmod, c.pv], writes=[t1], out=t1[:], in0=xr, scalar=pvg(4), in1=kmod[:], op0=ALU.mult, op1=ALU.mult)
            ps = pW[p.mi % len(pW)]; p.mi += 1
            k.i('pe', 'matmul', reads=[c.mf, t1], writes=[ps], out=ps[:, :], lhsT=BONES, rhs=t1[:], start=True, stop=True)
            k.i('dve', 'tensor_tensor', reads=[ps, XM], pwrites=[BONUS], out=BONUS[:, g, :], in0=ps[:, :], in1=xv, op=ALU.mult)
            for ch in range(8):
                cs_ = slice(ch * 64, (ch + 1) * 64)
                k.i('dve', 'tensor_tensor_scan', reads=[lw, p.ones64], writes=[] if ch else [cum], pwrites=[cum] if ch else [],
                    out=cum[:, cs_], data0=p.ones64[:, :], data1=lw[:, cs_], initial=0.0, op0=ALU.mult, op1=ALU.add)
            k.i('act', 'activation', reads=[cum], pwrites=[gC], out=gC[:, g, :], in_=cum[:].rearrange("p (c s) -> p c s", s=64)[:, :, 63], func=AF.Exp)
            k.i('act', 'activation', reads=[cum], writes=[t1], out=t1[:], in_=cum[:], func=AF.Exp)
            k.i('act', 'activation', reads=[cum], writes=[t2], out=t2[:], in_=cum[:], func=AF.Exp, scale=-1.0)
            k.i('dve', 'tensor_tensor', reads=[cum, lw], writes=[lw], out=lw[:], in0=cum[:], in1=lw[:], op=ALU.subtract)
            k.i('act', 'activation', reads=[lw], writes=[lw], out=lw[:], in_=lw[:], func=AF.Exp)

            def mk_bd(dstn, a_res, a_ap, b_res, b_ap, g=g, cum=cum):
                k.i('dve', 'tensor_tensor', reads=[a_res, b_res], writes=[cum], out=cum[:], in0=a_ap, in1=b_ap, op=ALU.mult)
                d = bd[dstn]
                k.i('dve', 'tensor_tensor', reads=[cum, c.mb], pwrites=[d],
                    out=d[:, g, :, :].rearrange("p c (j s) -> p c j s", j=2),
                    in0=cum[:].rearrange("p (c s) -> p c s", s=64).unsqueeze(2).to_broadcast([128, 8, 2, 64]), in1=BM, op=ALU.mult)
            mk_bd('kkt', kk, kk[:], lw, lw[:])
            mk_bd('rt', XM, xr, t1, t1[:])
            mk_bd('bh', bv, bv[:], t2, t2[:])
            mk_bd('kh', kmod, kmod[:], t2, t2[:])
            k.i('dve', 'tensor_tensor', reads=[XM, c.mb], pwrites=[bd['v']],
                out=bd['v'][:, g, :, :].rearrange("p c (j s) -> p c j s", j=2),
                in0=xv.rearrange("p (c s) -> p c s", s=64).unsqueeze(2).to_broadcast([128, 8, 2, 64]), in1=BM, op=ALU.mult)
        kkt, rt, bh, kh, vb = [bd[n] for n in ['kkt', 'rt', 'bh', 'kh', 'v']]
        k.begin_fill(YV)
        def dep_pieces(ch, stile=stile):
            par = ch % 2
            na, ab, kr, tm = NA[par], AB[par], KR[par], TM[par]
            cur = TTF[par]
            bank = {}

            def p0():
                for g in range(2):
                    k.i('act', 'activation', reads=[S32, gC], writes=[S0g] if g == 0 else [], pwrites=[] if g == 0 else [S0g],
                        out=S0g[:, g, :], in_=S32[:, g, :], func=AF.Identity, scale=gC[:, g, ch:ch + 1])
                ps = nb()
                for g in range(2):
                    mmg(ps, g, [(kkt, kkt[:, g, ch, :], S16, S16[:, g, :]), (ab, ab[:, g, :], tm, tm[:, g * 3, :])], g == 0)
                cp(U0, U0[:], ps, ps[:, 0:2, :], scale=-1.0)

            def p1():
                ps = nb()
                for g in range(2):
                    mmg(ps, g, [(cur, cur[:, g, :], U0, U0[:, g, :])], g == 0)
                cp(UU, UU[:], ps, ps[:, 0:2, :])

            def p2():
                ps = nb()
                bank['ps'] = ps
                for g in range(2):
                    mmg(ps, g, [(S16, S16[:, g, :], rt, rt[:, g, ch, :]), (UU, UU[:, g, :], ab, ab[:, 2 + g, :]),
                                (tm, tm[:, g * 3, :], kr, kr[:, g, :])], g == 0)
                for g in range(2):
                    mmg(ps, 2 + g, [(tm, tm[:, g * 3 + 1, :], UU, UU[:, g, :]), (tm, tm[:, g * 3 + 2, :], tm, tm[:, g * 3, :])], False)

            def p3():
                ps = bank['ps']
                k.i('act', 'copy', reads=[ps], pwrites=[YV], out=YV[0:64, :, ch * 64:(ch + 1) * 64], in_=ps[0:64, 0:2, 0:64])
                k.i('dve', 'tensor_copy', reads=[ps], pwrites=[YV], out=YV[64:128, :, ch * 64:(ch + 1) * 64], in_=ps[64:128, 0:2, 64:128])
                for g in range(2):
                    k.i('dve', 'scalar_tensor_tensor', reads=[ps, gC, S0g], writes=[S32] if g == 0 else [], pwrites=[] if g == 0 else [S32],
                        out=S32[:, g, :], in0=ps[:, 2 + g, :], scalar=gC[:, g, ch:ch + 1], in1=S0g[:, g, :], op0=ALU.mult, op1=ALU.add)

            def p4():
                k.i('act', 'copy', reads=[S32], writes=[S16], out=S16[:], in_=S32[:])
            return [p0, p1, p2, p3, p4]

        pend = []
        for ch in range(8):
            par = ch % 2
            na, ab, kr, tm = NA[par], AB[par], KR[par], TM[par]
            ps = nb()
            for g in range(2):
                mmg(ps, 2 * g, [(bh, bh[:, g, ch, :], kkt, kkt[:, g, ch, :])], g == 0)
                mmg(ps, 2 * g + 1, [(kkt, kkt[:, g, ch, :], bh, bh[:, g, ch, :])], False)
            k.i('dve', 'tensor_tensor', reads=[ps, c.mb], writes=[na], out=na[:], in0=ps[:, :, :], in1=c.mb[:, 0:4, :], op=ALU.mult)
            ps = nb()
            for g in range(2):
                mmg(ps, g, [(kh, kh[:, g, ch, :], kkt, kkt[:, g, ch, :])], g == 0)
            for g in range(2):
                mmg(ps, 2 + g, [(bh, bh[:, g, ch, :], rt, rt[:, g, ch, :])], False)
            k.i('dve', 'tensor_tensor', reads=[ps, c.mb], writes=[ab], out=ab[:], in0=ps[:, :, :], in1=c.mb[:, 4:8, :], op=ALU.mult)
            ps = nb()
            for g in range(2):
                mmg(ps, g, [(kh, kh[:, g, ch, :], rt, rt[:, g, ch, :])], g == 0)
            k.i('dve', 'tensor_tensor', reads=[ps, c.mb], writes=[kr], out=kr[:], in0=ps[:, 0:2, :], in1=c.mb[:, 8:10, :], op=ALU.mult)
            first = True
            for g in range(2):
                for j_, src in enumerate((vb, bh, kh)):
                    k.i('pe', 'transpose', reads=[src, identb], writes=[pH] if first else [], pwrites=[] if first else [pH],
                        out=pH[:, g * 3 + j_, :], in_=src[:, g, ch, :], identity=identb[:])
                    first = False
            cp(tm, tm[:], pH, pH[:, 0:6, :])
            cur = TT[0]
            k.i('pool', 'tensor_tensor', reads=[identb, na], writes=[cur], out=cur[:], in0=identb[:].unsqueeze(1).to_broadcast([128, 2, 128]),
                in1=na[:, 0:4:2, :], op=ALU.subtract)
            Pc = [(na, na[:, 0, :]), (na, na[:, 2, :])]
            Qc = [(na, na[:, 1, :]), (na, na[:, 3, :])]
            for lev in range(5):
                pq = PQ[lev % 2]
                ps = nb()
                for g in range(2):
                    if lev < 4:
                        mmg(ps, 2 * g, [(Qc[g][0], Qc[g][1], Pc[g][0], Pc[g][1])], g == 0)
                    mmg(ps, 2 * g + 1, [(Pc[g][0], Pc[g][1], Qc[g][0], Qc[g][1])], (g == 0 and lev == 4))
                if lev < 4:
                    cp(pq, pq[:], ps, ps[:, :, :])
                else:
                    cp(pq, pq[:, 1:4:2, :], ps, ps[:, 1:4:2, :])
                Pn = [(pq, pq[:, 0, :]), (pq, pq[:, 2, :])]
                Qn = [(pq, pq[:, 1, :]), (pq, pq[:, 3, :])]
                nxt = TT[(lev + 1) % 2] if lev < 4 else TTF[par]
                ps = nb()
                for g in range(2):
                    mmg(ps, g, [(identb, identb[:], cur, cur[:, g, :]), (Qn[g][0], Qn[g][1], cur, cur[:, g, :])], g == 0)
                cp(nxt, nxt[:], ps, ps[:, 0:2, :])
                cur = nxt
                Pc, Qc = Pn, Qn
                if pend:
                    pend.pop(0)()
            while pend:
                pend.pop(0)()
            pend = dep_pieces(ch)
        while pend:
            pend.pop(0)()
        for g in range(2):
            t1, t2 = f32t['t1'], f32t['t2']
            pvg = lambda i, g=g: c.pv[:, i, g:g + 1]
            ps = pW[p.mi % len(pW)]; p.mi += 1
            k.i('pe', 'matmul', reads=[c.mf, YV], writes=[ps], out=ps[:, :], lhsT=BONES, rhs=YV[:, g, :], start=True, stop=True)
            k.i('dve', 'scalar_tensor_tensor', reads=[ps, YV], writes=[t1], out=t1[:], in0=ps[:, :], scalar=-1.0 / 64, in1=YV[:, g, :], op0=ALU.mult, op1=ALU.add)
            k.i('pool', 'tensor_tensor', reads=[t1], writes=[t2], out=t2[:], in0=t1[:], in1=t1[:], op=ALU.mult)
            ps = pW[p.mi % len(pW)]; p.mi += 1
            k.i('pe', 'matmul', reads=[c.mf, t2], writes=[ps], out=ps[:, :], lhsT=BONES, rhs=t2[:], start=True, stop=True)
            k.i('act', 'activation', reads=[ps], writes=[t2], out=t2[:], in_=ps[:, :], func=AF.Sqrt, scale=1.0 / 64, bias=GN_EPS)
            k.i('dve', 'reciprocal', reads=[t2], writes=[t2], out=t2[:], in_=t2[:])
            k.i('dve', 'tensor_tensor', reads=[t1, t2], writes=[t1], out=t1[:], in0=t1[:], in1=t2[:], op=ALU.mult)
            k.i('dve', 'tensor_scalar', reads=[t1, c.pv], writes=[t1], out=t1[:], in0=t1[:], scalar1=pvg(5), scalar2=pvg(6), op0=ALU.mult, op1=ALU.add)
            k.i('pool', 'tensor_tensor', reads=[t1, BONUS], writes=[t1], out=t1[:], in0=t1[:], in1=BONUS[:, g, :], op=ALU.add)
            ro = rwo[g]
            k.i('dve', 'tensor_tensor', reads=[t1, GATE], writes=[ro], out=ro[:], in0=t1[:], in1=GATE[:, g, :], op=ALU.mult)
            ow = out_rw[stile]
            k.dma('sp', ow, ow.ap()[g * 128:(g + 1) * 128, :], ro, ro[:], partial=True)
        if after_st is not None:
            after_st(stile)
    so = S0g
    ps = nb()
    for g in range(2):
        k.i('pe', 'transpose', reads=[S32, identf], writes=[ps] if g == 0 else [], pwrites=[] if g == 0 else [ps],
            out=ps[:, g, :], in_=S32[:, g, :], identity=identf[:])
    k.i('dve', 'tensor_copy', reads=[ps], writes=[so], out=so[:], in_=ps[:, 0:2, :])
    for g in range(2):
        for j2 in range(2):
            k.dma('sp', out_state, out_state.ap()[g * 2 + j2], so, so[j2 * 64:(j2 + 1) * 64, g, j2 * 64:(j2 + 1) * 64], partial=True)

from concourse.bass_utils import run_bass_kernel_spmd
import math

D = 2048
EPS = 1e-6
ATT_COLS = 1536
RW_COLS = 3360
RWH = 1152
N_CORES = 8
T_SEQ = 4096
NEG = -30000.0


class P:
    pass


def build(stages):
    nc = bass.Bass("TRN2", target_bir_lowering=False)
    k = K(nc)
    p = P()
    p.k = k
    inp = {}

    del _INPUT_NAMES[:]

    def I(name, shape, dt=F32):
        inp[name] = k.inp(name, shape, dt)
        _INPUT_NAMES.append(name)
        return inp[name]

    I('x_tok', [1168, D]); I('x_seq', [T_SEQ, D]); I('mem', [256, D])
    USED_LN = ('ln1', 'memn') + (('ln2',) if 'OX' in stages else ()) + (('ln3',) if 'M' in stages else ())
    for n in USED_LN:
        I(n, [D])
    I('qn', [64]); I('kn', [64]); I('xkn', [128])
    if 'OX' in stages:
        I('xqn', [128])
    I('w_att', [D, ATT_COLS]); I('w_rw', [D, RW_COLS]); I('w_rwh', [D, RWH]); I('w_xkv', [D, 1024])
    I('cs_all', [1168, 16]); I('ident', [128, 128]); I('masks', [2, 128, 256]); I('sinks', [16])
    I('cwk', [16, 128, 256]); I('cwv', [16, 128, 256])
    if 'SR' in stages:
        I('sh_s', [16, RW_COLS]); I('wkv_s', [16, 16, 64, 64]); I('mu_f', [RW_COLS]); I('pvf', [7, 1024])
        I('w2_f', [64, 1024]); I('a2_f', [64, 1024]); I('g2_f', [160, 1024]); I('lnwb_s', [2, 256, 64])
    if 'SA' in stages:
        I('sinks_s', [64, 4])
    if 'SX' in stages:
        I('cmk_s', [16, 256, 512]); I('cmv_s', [16, 256, 512])
    if 'M' in stages:
        I('w_rt', [D, 72]); I('b_rt', [72]); I('e_g', [64, D, 512]); I('e_u', [64, D, 512]); I('e_d', [64, 512, D]); I('iota64', [64])
    if 'OX' in stages:
        I('w_out', [D, D]); I('w_xq', [D, 512]); I('w_xo', [512, D]); I('ohj', [4])
    I('mu_h', [RWH]); I('pv_h', [7, 256]); I('w2_h', [64, 256]); I('a2_h', [64, 256]); I('g2_h', [160, 256]); I('rmask', [12, 128, 128])
    o_y = k.outp('o_y', [1040, D])
    o_wkp = k.outp('o_wkp', [128, 256]); o_wvp = k.outp('o_wvp', [128, 256])
    o_shp = k.outp('o_shp', [RWH]); o_wkvp = k.outp('o_wkvp', [4, 64, 64])
    o_mk = k.outp('o_mk', [256, 512]); o_mv = k.outp('o_mv', [256, 512])
    o_swk = k.outp('o_swk', [16, 128, 256]); o_swv = k.outp('o_swv', [16, 128, 256])
    o_wkvs = k.outp('o_wkvs', [16, 16, 64, 64]); o_shs = k.outp('o_shs', [16, RW_COLS])
    ATT_O = k.dram('ATT_O', [1152, 1024], BF16)
    RWIN = [k.dram('RWIN%d' % i, [256, 512], BF16) for i in range(8)]
    RWG = [k.dram('RWG%d' % i, [1024, 512], BF16) for i in range(8)]
    QS = k.dram('QS', [16, 1536])
    H2 = k.dram('H2d', [1152, D])
    RWS = k.dram('RWS', [16, 1024], BF16)
    SRd = k.dram('SRd', [16, RW_COLS])
    SV = k.dram('SV', [6, 16, 1024])
    YNd = k.dram('YNd', [256, 64])
    QXd = k.dram('QXd', [16, 512])
    OXd = k.dram('OXd', [16, 512])
    H = [k.ps('H%d' % i, [128, 8, 128], BF16) for i in range(3)]
    Fb = [k.ps('F%d' % i, [128, 512]) for i in range(5)]
    p.pM = Fb[0:2]
    p.pC = [ResView(Fb[2 + i], Fb[2 + i].h[:, :].rearrange("p (a b) -> p a b", a=4)) for i in range(3)]
    p.pH = H[2]
    pT = H[0:2]
    p.identf = k.sb('identf', [128, 128]); p.identb = k.sb('identb', [128, 128], BF16)
    identb = p.identb
    k.dma('sp', p.identf, p.identf[:], inp['ident'], inp['ident'].ap())
    k.i('dve', 'tensor_copy', reads=[p.identf], writes=[identb], out=identb[:], in_=p.identf[:])
    p.ones64 = k.sb('ones64', [128, 64])
    k.i('pool', 'memset', writes=[p.ones64], ap=p.ones64[:], constant=1.0)
    k.dma('sp', o_swk, o_swk.ap()[:, 0:127, :], inp['cwk'], inp['cwk'].ap()[:, 1:128, :])
    k.dma('sp', o_swv, o_swv.ap()[:, 0:127, :], inp['cwv'], inp['cwv'].ap()[:, 1:128, :])
    k.begin_fill(o_swk, o_swv)

    fb = P()

    def alloc_front():
        fb.xt = [k.sb('xt%d' % i, [128, D]) for i in range(2)]
        fb.ub = [k.sb('ub%d' % i, [128, D], BF16) for i in range(2)]
        fb.ssb = [k.sb('ss%d' % i, [128, 1]) for i in range(4)]
        fb.lnA = k.sb('lnA', [128, D])
    p.alloc_front = alloc_front
    p.fb = fb
    p.ti = 0
    p.mi = 0

    def load_ln(name):
        k.dma('sp', fb.lnA, fb.lnA[:], inp[name], inp[name].ap().partition_broadcast(128))
        return fb.lnA

    def bcast_load(name, n):
        t = k.sb('bc_' + name, [128, n])
        k.dma('sp', t, t[:], inp[name], inp[name].ap().partition_broadcast(128))
        return t

    def front(src, src_ap, n, lnb, uT, uT_ap_fn, x_keep=None):
        i = p.ti; p.ti += 1
        x = fb.xt[i % 2] if x_keep is None else x_keep
        u = fb.ub[i % 2]; ss = fb.ssb[i % 4]
        k.dma('sp', x, x[0:n, :], src, src_ap)
        k.i('act', 'activation', reads=[x], writes=[u, ss], out=u[0:n, :], in_=x[0:n, :], func=AF.Square, accum_out=ss[0:n, :])
        k.i('act', 'activation', reads=[ss], writes=[ss], out=ss[0:n, :], in_=ss[0:n, :], func=AF.Sqrt, scale=1.0 / D, bias=EPS)
        k.i('dve', 'reciprocal', reads=[ss], writes=[ss], out=ss[0:n, :], in_=ss[0:n, :])
        k.i('dve', 'scalar_tensor_tensor', reads=[x, ss, lnb], writes=[u], out=u[0:n, :], in0=x[0:n, :], scalar=ss[0:n, 0:1], in1=lnb[0:n, :],
            op0=ALU.mult, op1=ALU.mult)
        transpose16(u, n, uT, uT_ap_fn)

    def transpose16(u, n, uT, uT_ap_fn, nchunks=16):
        for half in range((nchunks + 7) // 8):
            pt = pT[half % 2]
            m = min(8, nchunks - half * 8)
            for cc in range(m):
                dc = half * 8 + cc
                k.i('pe', 'transpose', reads=[u, identb], writes=[pt] if cc == 0 else [], pwrites=[] if cc == 0 else [pt],
                    out=pt[:, cc, 0:n], in_=u[0:n, dc * 128:(dc + 1) * 128], identity=identb[0:n, 0:n])
            dst = uT_ap_fn(half)
            if half % 2 == 0:
                k.i('act', 'copy', reads=[pt], pwrites=[uT], out=dst, in_=pt[:, 0:m, 0:n])
            else:
                k.i('dve', 'tensor_copy', reads=[pt], pwrites=[uT], out=dst, in_=pt[:, 0:m, 0:n])

    def linear_tm(uT, uT_ap_fn, n, W, W_ap_fn, ncols, nk=16, ps=None):
        if ps is None:
            ps = p.pM[p.mi % len(p.pM)]
            p.mi += 1
        for dc in range(nk):
            k.i('pe', 'matmul', reads=[uT, W], writes=[ps] if dc == 0 else [], pwrites=[] if dc == 0 else [ps],
                out=ps[0:n, 0:ncols], lhsT=uT_ap_fn(dc), rhs=W_ap_fn(dc), start=(dc == 0), stop=(dc == nk - 1))
        return ps

    def load_w(dst, src_name, rows, c0, c1, step=512, q='pool'):
        k.begin_fill(dst)
        for c in range(c0, c1, step):
            ce = min(c + step, c1)
            k.dma(q, dst, dst[:, :, c - c0:ce - c0], inp[src_name],
                  inp[src_name].ap()[:, c:ce].rearrange("(c p) n -> p c n", p=128), partial=True)

    p.front = front; p.transpose16 = transpose16; p.linear_tm = linear_tm; p.load_w = load_w
    p.inp = inp; p.nc = nc; p.H = H; p.Fb = Fb; p.pT = pT; p.load_ln = load_ln; p.bcast_load = bcast_load
    p.out = dict(o_y=o_y, o_wkp=o_wkp, o_wvp=o_wvp, o_shp=o_shp, o_wkvp=o_wkvp, o_mk=o_mk, o_mv=o_mv, o_swk=o_swk, o_swv=o_swv,
                 o_wkvs=o_wkvs, o_shs=o_shs)
    p.H2t = [ResView(Res('H2t%d' % i, H2.h, 'dram'), H2.h) for i in range(9)]
    p.dr = dict(ATT_O=ATT_O, RWIN=RWIN, RWG=RWG, QS=QS, H2=H2, RWS=RWS, SRd=SRd, SV=SV, YNd=YNd, QXd=QXd, OXd=OXd)

    p.MEMKT = k.sb('MEMKT', [128, 4, 256], BF16)
    p.MEMV = k.sb('MEMV', [128, 2, 512], BF16)
    k.begin_fill(p.MEMKT, p.MEMV)
    mark0 = nc.sbuf_base

    def phase_end():
        k.barrier()
        nc.sbuf_base = mark0

    if 'A' in stages:
        alloc_front()
        phase_A(k, p)
        phase_end()
    if 'MEM' in stages:
        alloc_front()
        phase_MEM(k, p)
        phase_end()
    if 'R' in stages:
        alloc_front()
        ln1b = load_ln('ln1')
        WH = k.sb('WH', [128, 16, RWH], BF16)
        load_w(WH, 'w_rwh', D, 0, RWH, step=384)
        rwkv_consts(k, p, inp)
        def gather_st(st_):
            k.custom('pool', lambda e, st_=st_: e.collective_compute("AllGather", ALU.bypass, replica_groups=[[0, 1, 2, 3], [4, 5, 6, 7]],
                                                                   ins=[RWIN[st_].h.ap().opt()], outs=[RWG[st_].h.ap().opt()]),
                     1, RWG[st_], reads=[RWIN[st_]], writes=[RWG[st_]])
        rwkv_phase(k, p, inp, T_SEQ, inp['x_seq'], lambda t: inp['x_seq'].ap()[t * 128:(t + 1) * 128, :], WH, ln1b, front,
                   RWIN, o_wkvp, o_shp, after_st=gather_st)
        phase_end()
    if 'SR' in stages:
        phase_SR(k, p)
        phase_end()
    if 'SA' in stages:
        phase_SA(k, p)
        phase_end()
    if 'OX' in stages:
        alloc_front()
        phase_OX(k, p)
        phase_end()
    if 'MT' in stages:
        h2in = I('h2_in', [1152, D])
        for t_ in range(9):
            k.dma('sp', p.H2t[t_], H2.ap()[t_ * 128:(t_ + 1) * 128, :], h2in, h2in.ap()[t_ * 128:(t_ + 1) * 128, :])
    if 'SX' in stages:
        phase_SX(k, p)
        phase_end()
    if 'M' in stages:
        phase_M(k, p, mark0)
        phase_end()
    if 'DBG' in stages:
        o_dbg = k.outp('o_dbg', [1152, 1024], BF16)
        k.dma('sp', o_dbg, o_dbg.ap(), ATT_O, ATT_O.ap())
        o_dbg2 = k.outp('o_dbg2', [1024, T_SEQ], BF16)
        for i_ in range(8):
            k.dma('sp', o_dbg2, o_dbg2.ap()[:, i_ * 512:(i_ + 1) * 512], RWG[i_], RWG[i_].ap(), partial=True)
        o_dbg4 = k.outp('o_dbg4', [16, 1024], BF16)
        k.dma('sp', o_dbg4, o_dbg4.ap(), RWS, RWS.ap())
        o_dbg3 = k.outp('o_dbg3', [1152, D])
        k.dma('sp', o_dbg3, o_dbg3.ap(), H2, H2.ap())
    k.finish()
    print('instr counts', {e: len(k.prog[e]) for e in k.prog}, 'dma sems', k.n_dsem, flush=True)
    return nc


def head_norm(k, p, src, src2d, n, nh, hd, nwb, dst, dst2d, scr, cs=None):
    sq, st, xn, r1, r2, r3 = scr
    w = nh * hd
    k.i('act', 'activation', reads=[src], writes=[sq], out=sq[0:n, 0:w], in_=src2d, func=AF.Square)
    k.i('dve', 'tensor_reduce', reads=[sq], writes=[st], out=st[0:n, 0:nh], in_=sq[0:n, 0:w].rearrange("p (a b) -> p a b", a=nh), axis=AX.X, op=ALU.add)
    k.i('act', 'activation', reads=[st], writes=[st], out=st[0:n, 0:nh], in_=st[0:n, 0:nh], func=AF.Sqrt, scale=1.0 / hd, bias=EPS)
    k.i('dve', 'reciprocal', reads=[st], writes=[st], out=st[0:n, 0:nh], in_=st[0:n, 0:nh])
    k.i('dve', 'tensor_tensor', reads=[src, st], writes=[xn], out=xn[0:n, 0:w].rearrange("p (a b) -> p a b", a=nh),
        in0=src2d.rearrange("p (a b) -> p a b", a=nh), in1=st[0:n, 0:nh].unsqueeze(2).to_broadcast([n, nh, hd]), op=ALU.mult)
    d3 = dst2d.rearrange("p (a b) -> p a b", a=nh)
    k.i('dve', 'tensor_tensor', reads=[xn, nwb], writes=[dst], out=d3, in0=xn[0:n, 0:w].rearrange("p (a b) -> p a b", a=nh),
        in1=nwb[0:n, 0:hd].unsqueeze(1).to_broadcast([n, nh, hd]), op=ALU.mult)
    if cs is not None:
        x1 = d3[:, :, 0:8]; x2 = d3[:, :, 8:16]
        cosb = cs[0:n, 0:8].unsqueeze(1).to_broadcast([n, nh, 8])
        sinb = cs[0:n, 8:16].unsqueeze(1).to_broadcast([n, nh, 8])
        a1, a2, a3 = r1[0:n, 0:nh, :], r2[0:n, 0:nh, :], r3[0:n, 0:nh, :]
        k.i('dve', 'tensor_tensor', reads=[dst, cs], writes=[r1], out=a1, in0=x1, in1=cosb, op=ALU.mult)
        k.i('dve', 'tensor_tensor', reads=[dst, cs], writes=[r2], out=a2, in0=x2, in1=sinb, op=ALU.mult)
        k.i('dve', 'tensor_tensor', reads=[dst, cs], writes=[r3], out=a3, in0=x1, in1=sinb, op=ALU.mult)
        k.i('dve', 'tensor_tensor', reads=[r1, r2], writes=[r1], out=a1, in0=a1, in1=a2, op=ALU.subtract)
        k.i('dve', 'tensor_tensor', reads=[dst, cs], writes=[r2], out=a2, in0=x2, in1=cosb, op=ALU.mult)
        k.i('dve', 'tensor_tensor', reads=[r2, r3], writes=[dst], out=x2, in0=a2, in1=a3, op=ALU.add)
        k.i('dve', 'tensor_copy', reads=[r1], writes=[dst], out=x1, in_=a1)


def phase_A(k, p):
    inp, sb = p.inp, k.sb
    H, Fb = p.H, p.Fb
    identb = p.identb
    ln1b = p.load_ln('ln1')
    qnb = p.bcast_load('qn', 64); knb = p.bcast_load('kn', 64); sinkb = p.bcast_load('sinks', 16)
    maskb = sb('maskb', [128, 2, 256])
    k.dma('sp', maskb, maskb[:], inp['masks'], inp['masks'].ap().rearrange("m p n -> p m n"))
    WA = sb('WA', [128, 16, ATT_COLS], BF16)
    p.load_w(WA, 'w_att', D, 0, ATT_COLS)
    WS = sb('WSr', [128, 16, 480], BF16)
    scr = (sb('sq', [128, 1024]), sb('st', [128, 16]), sb('xn', [128, 1024]), sb('r1', [128, 16, 8]), sb('r2', [128, 16, 8]), sb('r3', [128, 16, 8]))
    uTs = [sb('uT%d' % i, [128, 16, 128], BF16) for i in range(2)]
    cst = [sb('cst%d' % i, [128, 16]) for i in range(2)]
    QF = sb('QF', [128, 1024]); QB = sb('QB', [128, 1024], BF16)
    KF = [sb('KF%d' % i, [128, 512]) for i in range(2)]
    kdup = sb('kdup', [128, 4, 2, 64], BF16)
    KT2 = [sb('KT2_%d' % i, [128, 4, 128], BF16) for i in range(2)]
    VB = [sb('VB%d' % i, [128, 256], BF16) for i in range(2)]
    qT = sb('qT', [128, 8, 128], BF16)
    SM = sb('SM', [128, 4, 256]); E = sb('E', [128, 4, 256], BF16); ET = sb('ET', [128, 8, 128], BF16)
    mx = sb('mx', [128, 4]); negm = sb('negm', [128, 4]); rs = sb('rs', [128, 4]); es = sb('es', [128, 4])
    ATTO = [sb('ATTO%d' % i, [128, 1024], BF16) for i in range(2)]
    srs = [sb('srs%d' % i, [16, 480]) for i in range(2)]
    HT = H[2]
    for t in range(10):
        n = 16 if t == 9 else 128
        r0 = t * 128
        slot = t % 2
        uT = uTs[t % 2]
        k.begin_fill(uT)
        cs = cst[t % 2]
        k.dma('sp', cs, cs[0:n, :], inp['cs_all'], inp['cs_all'].ap()[r0:r0 + n, :])
        p.front(inp['x_tok'], inp['x_tok'].ap()[r0:r0 + n, :], n, ln1b, uT, lambda half, uT=uT, n=n: uT[:, half * 8:(half + 1) * 8, 0:n])
        uf = lambda dc, uT=uT, n=n: uT[:, dc, 0:n]
        kf = KF[slot]
        if t >= 1:
            for hq in range(2):
                ps = p.linear_tm(uT, uf, n, WA, lambda dc, hq=hq: WA[:, dc, hq * 512:(hq + 1) * 512], 512)
                head_norm(k, p, ps, ps[0:n, 0:512], n, 8, 64, qnb, QF, QF[0:n, hq * 512:(hq + 1) * 512], scr, cs=cs)
            k.i('act', 'activation', reads=[QF], writes=[QB], out=QB[0:n, :], in_=QF[0:n, :], func=AF.Identity, scale=0.125)
        ps = p.linear_tm(uT, uf, n, WA, lambda dc: WA[:, dc, 1024:1536], 512)
        head_norm(k, p, ps, ps[0:n, 0:256], n, 4, 64, knb, kf, kf[0:n, 0:256], scr, cs=cs)
        k.i('dve', 'tensor_copy', reads=[ps], writes=[], pwrites=[kf], out=kf[0:n, 256:512], in_=ps[0:n, 256:512])
        if t == 8:
            k.dma('sp', p.out['o_wkp'], p.out['o_wkp'].ap(), kf, kf[:, 0:256])
            k.dma('sp', p.out['o_wvp'], p.out['o_wvp'].ap(), kf, kf[:, 256:512])
        if t == 9:
            k.dma('sp', p.out['o_swk'], p.out['o_swk'].ap()[:, 127, :], kf, kf[0:16, 0:256], partial=True)
            k.dma('sp', p.out['o_swv'], p.out['o_swv'].ap()[:, 127, :], kf, kf[0:16, 256:512], partial=True)
            QSd = p.dr['QS']
            k.dma('sp', QSd, QSd.ap()[:, 0:1024], QF, QF[0:16, :])
            k.dma('sp', QSd, QSd.ap()[:, 1024:1536], kf, kf[0:16, :], partial=True)
            for c in range(7):
                k.dma('pool', WS, WS[:], inp['w_rw'], inp['w_rw'].ap()[:, c * 480:(c + 1) * 480].rearrange("(c p) n -> p c n", p=128))
                ps = p.linear_tm(uT, uf, 16, WS, lambda dc: WS[:, dc, :], 480)
                sr = srs[c % 2]
                k.i('act' if c % 2 == 0 else 'dve', 'copy' if c % 2 == 0 else 'tensor_copy', reads=[ps], writes=[sr], out=sr[0:16, :], in_=ps[0:16, 0:480])
                k.dma('sp', p.out['o_shs'], p.out['o_shs'].ap()[:, c * 480:(c + 1) * 480], sr, sr[:], partial=(c > 0))
                k.dma('sp', p.dr['SRd'], p.dr['SRd'].ap()[:, c * 480:(c + 1) * 480], sr, sr[:], partial=True)
            continue
        k.i('dve', 'tensor_copy', reads=[kf], writes=[kdup], out=kdup[:],
            in_=kf[:, 0:256].rearrange("p (a b) -> p a b", a=4).unsqueeze(2).to_broadcast([128, 4, 2, 64]))
        k.i('act', 'copy', reads=[kf], writes=[VB[slot]], out=VB[slot][:], in_=kf[:, 256:512])
        for kh in range(4):
            k.i('pe', 'transpose', reads=[kdup, identb], writes=[HT] if kh == 0 else [], pwrites=[] if kh == 0 else [HT],
                out=HT[:, kh, :], in_=kdup[:, kh, :, :].rearrange("p a b -> p (a b)"), identity=identb[:])
        k.i('act', 'copy', reads=[HT], writes=[KT2[slot]], out=KT2[slot][:], in_=HT[:, 0:4, :])
        if t == 0:
            continue
        for m in range(8):
            k.i('pe', 'transpose', reads=[QB, identb], writes=[HT] if m == 0 else [], pwrites=[] if m == 0 else [HT],
                out=HT[:, m, :], in_=QB[:, m * 128:(m + 1) * 128], identity=identb[:])
        k.i('dve', 'tensor_copy', reads=[HT], writes=[qT], out=qT[:], in_=HT[:, :, :])
        mi_ = 1 if t == 1 else 0
        ao = ATTO[t % 2]
        k.begin_fill(ao)
        for kh in range(4):
            for hp in range(2):
                bank = Fb[2 + hp]
                first = True
                for g2 in range(2):
                    g = g2 * 2 + hp
                    h = 4 * kh + g
                    m = h // 2
                    for half, sl in ((0, 1 - slot), (1, slot)):
                        k.i('pe', 'matmul', reads=[qT, KT2[sl]], writes=[bank] if first else [], pwrites=[] if first else [bank],
                            out=bank[:, g2 * 256 + half * 128:g2 * 256 + (half + 1) * 128],
                            lhsT=qT[hp * 64:(hp + 1) * 64, m, :], rhs=KT2[sl][hp * 64:(hp + 1) * 64, kh, :], start=True, stop=True)
                        first = False
                k.i('dve', 'tensor_tensor', reads=[bank, maskb], writes=[SM] if hp == 0 else [], pwrites=[] if hp == 0 else [SM],
                    out=SM[:, hp:4:2, :], in0=bank[:, :].rearrange("p (a b) -> p a b", a=2),
                    in1=maskb[:, mi_, :].unsqueeze(1).to_broadcast([128, 2, 256]), op=ALU.add)
            k.i('dve', 'tensor_reduce', reads=[SM], writes=[mx], out=mx[:], in_=SM[:], axis=AX.X, op=ALU.max)
            k.i('dve', 'tensor_tensor', reads=[mx, sinkb], writes=[mx], out=mx[:], in0=mx[:], in1=sinkb[:, kh * 4:(kh + 1) * 4], op=ALU.max)
            k.i('dve', 'tensor_scalar', reads=[mx], writes=[negm], out=negm[:], in0=mx[:], scalar1=-1.0, scalar2=None, op0=ALU.mult)
            for g in range(4):
                k.i('act', 'activation', reads=[SM, negm], writes=[E, rs] if g == 0 else [], pwrites=[] if g == 0 else [E, rs],
                    out=E[:, g, :], in_=SM[:, g, :], func=AF.Exp, bias=negm[:, g:g + 1], accum_out=rs[:, g:g + 1])
            k.i('dve', 'tensor_tensor', reads=[sinkb, negm], writes=[es], out=es[:], in0=sinkb[:, kh * 4:(kh + 1) * 4], in1=negm[:], op=ALU.add)
            k.i('act', 'activation', reads=[es], writes=[es], out=es[:], in_=es[:], func=AF.Exp)
            k.i('dve', 'tensor_tensor', reads=[rs, es], writes=[rs], out=rs[:], in0=rs[:], in1=es[:], op=ALU.add)
            k.i('dve', 'reciprocal', reads=[rs], writes=[rs], out=rs[:], in_=rs[:])
            for g in range(4):
                for half in range(2):
                    i8 = g * 2 + half
                    k.i('pe', 'transpose', reads=[E, identb], writes=[HT] if i8 == 0 else [], pwrites=[] if i8 == 0 else [HT],
                        out=HT[:, i8, :], in_=E[:, g, half * 128:(half + 1) * 128], identity=identb[:])
            k.i('dve', 'tensor_copy', reads=[HT], writes=[ET], out=ET[:], in_=HT[:, :, :])
            ob = Fb[4]
            first = True
            for g in range(4):
                for half, sl in ((0, 1 - slot), (1, slot)):
                    k.i('pe', 'matmul', reads=[ET, VB[sl]], writes=[ob] if first else [], pwrites=[] if first else [ob],
                        out=ob[:, g * 64:(g + 1) * 64], lhsT=ET[:, g * 2 + half, :], rhs=VB[sl][:, kh * 64:(kh + 1) * 64],
                        start=(half == 0), stop=(half == 1))
                    first = False
            k.i('dve', 'tensor_tensor', reads=[ob, rs], pwrites=[ao], out=ao[:, kh * 256:(kh + 1) * 256].rearrange("p (a b) -> p a b", a=4),
                in0=ob[:, 0:256].rearrange("p (a b) -> p a b", a=4), in1=rs[:].unsqueeze(2).to_broadcast([128, 4, 64]), op=ALU.mult)
        AO = p.dr['ATT_O']
        k.dma('sp', AO, AO.ap()[(t - 1) * 128:t * 128, :], ao, ao[:], partial=True)


def phase_MEM(k, p):
    inp, sb = p.inp, k.sb
    memnb = p.load_ln('memn')
    xknb = p.bcast_load('xkn', 128)
    WB = sb('WB', [128, 16, 1024], BF16)
    p.load_w(WB, 'w_xkv', D, 0, 1024)
    scr = (sb('sq', [128, 1024]), sb('st', [128, 16]), sb('xn', [128, 1024]), sb('r1', [128, 16, 8]), sb('r2', [128, 16, 8]), sb('r3', [128, 16, 8]))
    uTs = [sb('uTm%d' % i, [128, 16, 128], BF16) for i in range(2)]
    mkv = [sb('mkv%d' % i, [128, 1024]) for i in range(2)]
    for t in range(2):
        uT = uTs[t]
        k.begin_fill(uT)
        p.front(inp['mem'], inp['mem'].ap()[t * 128:(t + 1) * 128, :], 128, memnb, uT, lambda half, uT=uT: uT[:, half * 8:(half + 1) * 8, :])
        uf = lambda dc, uT=uT: uT[:, dc, :]
        psk = p.linear_tm(uT, uf, 128, WB, lambda dc: WB[:, dc, 0:512], 512)
        psv = p.linear_tm(uT, uf, 128, WB, lambda dc: WB[:, dc, 512:1024], 512)
        m = mkv[t]
        head_norm(k, p, psk, psk[:, 0:512], 128, 4, 128, xknb, m, m[:, 0:512], scr)
        k.i('act', 'copy', reads=[psv], pwrites=[m], out=m[:, 512:1024], in_=psv[:, 0:512])
        k.i('act', 'copy', reads=[m], pwrites=[p.MEMV], out=p.MEMV[:, t, :], in_=m[:, 512:1024])
        kb = sb('kb%d' % t, [128, 512], BF16)
        k.i('dve', 'tensor_copy', reads=[m], writes=[kb], out=kb[:], in_=m[:, 0:512])
        HT = p.H[2]
        for hd in range(4):
            k.i('pe', 'transpose', reads=[kb, p.identb], writes=[HT] if hd == 0 else [], pwrites=[] if hd == 0 else [HT],
                out=HT[:, hd, :], in_=kb[:, hd * 128:(hd + 1) * 128], identity=p.identb[:])
        k.i('act', 'copy', reads=[HT], pwrites=[p.MEMKT], out=p.MEMKT[:, :, t * 128:(t + 1) * 128], in_=HT[:, 0:4, :])
        k.dma('sp', p.out['o_mk'], p.out['o_mk'].ap()[t * 128:(t + 1) * 128, :], m, m[:, 0:512], partial=(t > 0))
        k.dma('sp', p.out['o_mv'], p.out['o_mv'].ap()[t * 128:(t + 1) * 128, :], m, m[:, 512:1024], partial=(t > 0))


def phase_OX(k, p):
    inp, sb = p.inp, k.sb
    H, Fb, identb = p.H, p.Fb, p.identb
    ln2b = p.load_ln('ln2')
    xqnb = p.bcast_load('xqn', 128)
    WO = sb('WO', [128, 16, D], BF16)
    p.load_w(WO, 'w_out', D, 0, D)
    WQ = sb('WQ', [128, 16, 512], BF16)
    p.load_w(WQ, 'w_xq', D, 0, 512)
    WX = sb('WX', [128, 4, D], BF16)
    p.load_w(WX, 'w_xo', 512, 0, D)
    scr = (sb('sq', [128, 1024]), sb('st', [128, 16]), sb('xn', [128, 1024]), sb('r1', [128, 16, 8]), sb('r2', [128, 16, 8]), sb('r3', [128, 16, 8]))
    att_sb = [sb('att_sb%d' % i, [128, 1024], BF16) for i in range(2)]
    aT = [sb('aT%d' % i, [128, 8, 128], BF16) for i in range(2)]
    rwT = [sb('rwT%d' % i, [128, 8, 128], BF16) for i in range(2)]
    xres = [sb('xres0', [128, D])] * 2
    h1 = [sb('h1_0', [128, D])] * 2
    ub2 = sb('ub2', [128, D], BF16)
    ss2 = sb('ss2', [128, 1])
    uT2 = sb('uT2', [128, 16, 128], BF16)
    qx = sb('qx', [128, 512]); qxb = sb('qxb', [128, 512], BF16); qxT = sb('qxT', [128, 4, 128], BF16)
    Ex = sb('Ex', [128, 4, 256], BF16); ETx = sb('ETx', [128, 8, 128], BF16)
    mx = sb('mxx', [128, 4]); negm = sb('negmx', [128, 4]); rs = sb('rsx', [128, 4])
    oxb = sb('oxb', [128, 512], BF16); oxT = sb('oxT', [128, 4, 128], BF16)
    rws_sb = sb('rws_sb', [128, 1024], BF16)
    cand = [sb('cand%d' % q, [128, 8, 128], BF16) for q in range(4)]
    ohb = p.bcast_load('ohj', 4)
    HT = H[2]
    AO, RWG, H2, RWS = p.dr['ATT_O'], p.dr['RWG'], p.dr['H2'], p.dr['RWS']
    for t in range(9):
        n = 16 if t == 8 else 128
        b2 = t % 2
        xr_ = xres[b2]; a_sb = att_sb[b2]; aT_ = aT[b2]; rT = rwT[b2]; hh = h1[b2]
        xrow = 128 + t * 128
        k.dma('sp', xr_, xr_[0:n, :], inp['x_tok'], inp['x_tok'].ap()[xrow:xrow + n, :])
        k.dma('sp', a_sb, a_sb[0:n, :], AO, AO.ap()[t * 128:t * 128 + n, :])
        k.begin_fill(aT_)
        p.transpose16(a_sb, n, aT_, lambda half, aT_=aT_, n=n: aT_[:, 0:8, 0:n], nchunks=8)
        if t < 8:
            for q in range(4):
                rg = RWG[2 * q + t // 4]
                k.dma('sp', cand[q], cand[q][:], rg, rg.ap()[:, (t % 4) * 128:(t % 4 + 1) * 128].rearrange("(c p) t -> p c t", p=128))
            k.i('dve', 'tensor_scalar', reads=[cand[0], ohb], writes=[rT], out=rT[:], in0=cand[0][:], scalar1=ohb[:, 0:1], scalar2=None, op0=ALU.mult)
            for q in range(1, 4):
                k.i('dve', 'scalar_tensor_tensor', reads=[cand[q], ohb, rT], writes=[rT], out=rT[:], in0=cand[q][:], scalar=ohb[:, q:q + 1], in1=rT[:],
                    op0=ALU.mult, op1=ALU.add)
        else:
            k.dma('sp', rws_sb, rws_sb[0:16, :], RWS, RWS.ap())
            k.begin_fill(rT)
            p.transpose16(rws_sb, 16, rT, lambda half, rT=rT: rT[:, 0:8, 0:16], nchunks=8)
        for nchk in range(4):
            ps = Fb[nchk]
            for c in range(16):
                src = aT_ if c < 8 else rT
                k.i('pe', 'matmul', reads=[src, WO], writes=[ps] if c == 0 else [], pwrites=[] if c == 0 else [ps],
                    out=ps[0:n, :], lhsT=src[:, c % 8, 0:n], rhs=WO[:, c, nchk * 512:(nchk + 1) * 512], start=(c == 0), stop=(c == 15))
            k.i('dve', 'tensor_tensor', reads=[ps, xr_], writes=[hh] if nchk == 0 else [], pwrites=[] if nchk == 0 else [hh],
                out=hh[0:n, nchk * 512:(nchk + 1) * 512], in0=ps[0:n, :], in1=xr_[0:n, nchk * 512:(nchk + 1) * 512], op=ALU.add)
        k.i('act', 'activation', reads=[hh], writes=[ub2, ss2], out=ub2[0:n, :], in_=hh[0:n, :], func=AF.Square, accum_out=ss2[0:n, :])
        k.i('act', 'activation', reads=[ss2], writes=[ss2], out=ss2[0:n, :], in_=ss2[0:n, :], func=AF.Sqrt, scale=1.0 / D, bias=EPS)
        k.i('dve', 'reciprocal', reads=[ss2], writes=[ss2], out=ss2[0:n, :], in_=ss2[0:n, :])
        k.i('dve', 'scalar_tensor_tensor', reads=[hh, ss2, ln2b], writes=[ub2], out=ub2[0:n, :], in0=hh[0:n, :], scalar=ss2[0:n, 0:1],
            in1=ln2b[0:n, :], op0=ALU.mult, op1=ALU.mult)
        k.begin_fill(uT2)
        p.transpose16(ub2, n, uT2, lambda half, n=n: uT2[:, half * 8:(half + 1) * 8, 0:n])
        ps = p.linear_tm(uT2, lambda dc, n=n: uT2[:, dc, 0:n], n, WQ, lambda dc: WQ[:, dc, :], 512, ps=Fb[4])
        head_norm(k, p, ps, ps[0:n, 0:512], n, 4, 128, xqnb, qx, qx[0:n, :], scr)
        if t < 8:
            k.i('act', 'activation', reads=[qx], writes=[qxb], out=qxb[0:n, :], in_=qx[0:n, :], func=AF.Identity, scale=1.0 / math.sqrt(128.0))
            k.begin_fill(qxT)
            p.transpose16(qxb, n, qxT, lambda half, n=n: qxT[:, 0:4, 0:n], nchunks=4)
            for hp in range(2):
                bank = Fb[2 + hp]
                for h2_ in range(2):
                    hd = hp * 2 + h2_
                    k.i('pe', 'matmul', reads=[qxT, p.MEMKT], writes=[bank] if h2_ == 0 else [], pwrites=[] if h2_ == 0 else [bank],
                        out=bank[0:n, h2_ * 256:(h2_ + 1) * 256], lhsT=qxT[:, hd, 0:n], rhs=p.MEMKT[:, hd, :], start=True, stop=True)
                k.i('dve', 'tensor_reduce', reads=[bank], writes=[mx] if hp == 0 else [], pwrites=[] if hp == 0 else [mx],
                    out=mx[0:n, hp * 2:hp * 2 + 2], in_=bank[0:n, :].rearrange("p (a b) -> p a b", a=2), axis=AX.X, op=ALU.max)
            k.i('dve', 'tensor_scalar', reads=[mx], writes=[negm], out=negm[0:n, :], in0=mx[0:n, :], scalar1=-1.0, scalar2=None, op0=ALU.mult)
            for hd in range(4):
                bank = Fb[2 + hd // 2]
                k.i('act', 'activation', reads=[bank, negm], writes=[Ex, rs] if hd == 0 else [], pwrites=[] if hd == 0 else [Ex, rs],
                    out=Ex[0:n, hd, :], in_=bank[0:n, (hd % 2) * 256:(hd % 2 + 1) * 256], func=AF.Exp, bias=negm[0:n, hd:hd + 1],
                    accum_out=rs[0:n, hd:hd + 1])
            k.i('dve', 'reciprocal', reads=[rs], writes=[rs], out=rs[0:n, :], in_=rs[0:n, :])
            for hd in range(4):
                for half in range(2):
                    i8 = hd * 2 + half
                    k.i('pe', 'transpose', reads=[Ex, identb], writes=[HT] if i8 == 0 else [], pwrites=[] if i8 == 0 else [HT],
                        out=HT[:, i8, 0:n], in_=Ex[0:n, hd, half * 128:(half + 1) * 128], identity=identb[0:n, 0:n])
            k.i('dve', 'tensor_copy', reads=[HT], writes=[ETx], out=ETx[:, :, 0:n], in_=HT[:, :, 0:n])
            ob = Fb[4]
            first = True
            for hd in range(4):
                for half in range(2):
                    k.i('pe', 'matmul', reads=[ETx, p.MEMV], writes=[ob] if first else [], pwrites=[] if first else [ob],
                        out=ob[0:n, hd * 128:(hd + 1) * 128], lhsT=ETx[:, hd * 2 + half, 0:n], rhs=p.MEMV[:, half, hd * 128:(hd + 1) * 128],
                        start=(half == 0), stop=(half == 1))
                    first = False
            k.i('dve', 'tensor_tensor', reads=[ob, rs], writes=[oxb], out=oxb[0:n, :].rearrange("p (a b) -> p a b", a=4),
                in0=ob[0:n, :].rearrange("p (a b) -> p a b", a=4), in1=rs[0:n, :].unsqueeze(2).to_broadcast([n, 4, 128]), op=ALU.mult)
        else:
            k.dma('sp', p.dr['QXd'], p.dr['QXd'].ap(), qx, qx[0:16, :])
            k.dma('sp', p.H2t[8], H2.ap()[1024:1040, :], hh, hh[0:16, :])
            continue
        k.begin_fill(oxT)
        p.transpose16(oxb, n, oxT, lambda half, n=n: oxT[:, 0:4, 0:n], nchunks=4)
        for nchk in range(4):
            ps = Fb[nchk]
            for c in range(4):
                k.i('pe', 'matmul', reads=[oxT, WX], writes=[ps] if c == 0 else [], pwrites=[] if c == 0 else [ps],
                    out=ps[0:n, :], lhsT=oxT[:, c, 0:n], rhs=WX[:, c, nchk * 512:(nchk + 1) * 512], start=(c == 0), stop=(c == 3))
            k.i('dve', 'tensor_tensor', reads=[ps, hh], writes=[hh], out=hh[0:n, nchk * 512:(nchk + 1) * 512], in0=ps[0:n, :],
                in1=hh[0:n, nchk * 512:(nchk + 1) * 512], op=ALU.add)
        k.dma('sp', p.H2t[t], H2.ap()[t * 128:t * 128 + n, :], hh, hh[0:n, :])


def sample_xattn(k, p, qx, oxb):
    k.i('pool', 'memset', writes=[oxb], ap=oxb[0:16, :], constant=0.0)


def phase_M(k, p, mark0):
    inp, sb, nc = p.inp, k.sb, p.nc
    H, Fb, identb, identf = p.H, p.Fb, p.identb, p.identf
    H2 = p.dr['H2']
    CAP = 64
    U16 = sb('U16', [128, 9, D], BF16)
    GATE = sb('GATEm', [128, 9, 64]); ASG = sb('ASG', [128, 9, 64], BF16); RANK = sb('RANK', [128, 9, 64])
    iota = p.bcast_load('iota64', 64)
    onesb = sb('onesb', [128, 128], BF16)
    trib = sb('trib', [128, 128], BF16)
    k.i('pool', 'memset', writes=[onesb], ap=onesb[:], constant=1.0)
    k.i('pool', 'memset', writes=[U16], ap=U16[:], constant=0.0)
    k.i('pool', 'memset', writes=[GATE], ap=GATE[:], constant=0.0)
    k.i('pool', 'memset', writes=[ASG], ap=ASG[:], constant=0.0)
    trif = sb('trif', [128, 128])
    k.dma('sp', trif, trif[:], inp['rmask'], inp['rmask'].ap()[0])
    k.i('dve', 'tensor_copy', reads=[trif], writes=[trib], out=trib[:], in_=trif[:])
    mark1 = nc.sbuf_base
    p.alloc_front()
    fb = p.fb
    ln3b = p.load_ln('ln3')
    WR = sb('WR', [128, 16, 72])
    k.dma('sp', WR, WR[:], inp['w_rt'], inp['w_rt'].ap().rearrange("(c p) n -> p c n", p=128))
    brt = p.bcast_load('b_rt', 72)
    u32 = sb('u32', [128, D]); uT32 = sb('uT32', [128, 16, 128])
    LG = sb('LG', [128, 72])
    sm = {n: sb('m_' + n, [128, 8]) for n in ['ohg', 'e8', 'oh1', 'oh2', 'e8b', 'g8', 't8']}
    s1 = {n: sb('m1_' + n, [128, 1]) for n in ['gmax', 'ngmax', 'se', 'm1', 'm2', 'w1', 'w2']}
    sel3 = sb('sel3', [128, 8, 8])
    k.begin_fill(U16, GATE, ASG)
    for t in range(9):
        n = 16 if t == 8 else 128
        x = fb.xt[t % 2]; ss = fb.ssb[t % 4]; ubf = fb.ub[t % 2]
        k.dma('sp', x, x[0:n, :], p.H2t[t], H2.ap()[t * 128:t * 128 + n, :])
        k.i('act', 'activation', reads=[x], writes=[ubf, ss], out=ubf[0:n, :], in_=x[0:n, :], func=AF.Square, accum_out=ss[0:n, :])
        k.i('act', 'activation', reads=[ss], writes=[ss], out=ss[0:n, :], in_=ss[0:n, :], func=AF.Sqrt, scale=1.0 / D, bias=EPS)
        k.i('dve', 'reciprocal', reads=[ss], writes=[ss], out=ss[0:n, :], in_=ss[0:n, :])
        k.i('dve', 'scalar_tensor_tensor', reads=[x, ss, ln3b], writes=[u32], out=u32[0:n, :], in0=x[0:n, :], scalar=ss[0:n, 0:1], in1=ln3b[0:n, :],
            op0=ALU.mult, op1=ALU.mult)
        k.i('act', 'copy', reads=[u32], pwrites=[U16], out=U16[0:n, t, :], in_=u32[0:n, :])
        k.begin_fill(uT32)
        for q4 in range(4):
            bank = Fb[q4 % 2]
            for c4 in range(4):
                dc = q4 * 4 + c4
                k.i('pe', 'transpose', reads=[u32, identf], writes=[bank] if c4 == 0 else [], pwrites=[] if c4 == 0 else [bank],
                    out=bank[:, c4 * 128:c4 * 128 + n], in_=u32[0:n, dc * 128:(dc + 1) * 128], identity=identf[0:n, 0:n])
            k.i('act' if q4 % 2 == 0 else 'dve', 'copy' if q4 % 2 == 0 else 'tensor_copy', reads=[bank], pwrites=[uT32],
                out=uT32[:, q4 * 4:(q4 + 1) * 4, 0:n], in_=bank[:, :].rearrange("p (a b) -> p a b", a=4)[:, :, 0:n])
        ps = Fb[4]
        for dc in range(16):
            k.i('pe', 'matmul', reads=[uT32, WR], writes=[ps] if dc == 0 else [], pwrites=[] if dc == 0 else [ps],
                out=ps[0:n, 0:72], lhsT=uT32[:, dc, 0:n], rhs=WR[:, dc, :], start=(dc == 0), stop=(dc == 15))
        k.i('dve', 'tensor_tensor', reads=[ps, brt], writes=[LG], out=LG[0:n, :], in0=ps[0:n, 0:72], in1=brt[0:n, :], op=ALU.add)
        a_ = lambda r: r[0:n, :]
        gl = LG[0:n, 0:8]
        k.i('dve', 'tensor_reduce', reads=[LG], writes=[s1['gmax']], out=a_(s1['gmax']), in_=gl, axis=AX.X, op=ALU.max)
        k.i('dve', 'tensor_scalar', reads=[LG, s1['gmax']], writes=[sm['ohg']], out=a_(sm['ohg']), in0=gl, scalar1=s1['gmax'][0:n, 0:1], scalar2=None, op0=ALU.is_equal)
        k.i('dve', 'tensor_scalar', reads=[s1['gmax']], writes=[s1['ngmax']], out=a_(s1['ngmax']), in0=a_(s1['gmax']), scalar1=-1.0, scalar2=None, op0=ALU.mult)
        k.i('act', 'activation', reads=[LG, s1['ngmax']], writes=[sm['t8'], s1['se']], out=a_(sm['t8']), in_=gl, func=AF.Exp, bias=s1['ngmax'][0:n, 0:1],
            accum_out=a_(s1['se']))
        k.i('dve', 'reciprocal', reads=[s1['se']], writes=[s1['se']], out=a_(s1['se']), in_=a_(s1['se']))
        k.i('dve', 'tensor_tensor', reads=[LG, sm['ohg']], writes=[sel3], out=sel3[0:n, :, :], in0=LG[0:n, 8:72].rearrange("p (g e) -> p g e", g=8),
            in1=sm['ohg'][0:n, :].unsqueeze(2).to_broadcast([n, 8, 8]), op=ALU.mult)
        k.i('dve', 'tensor_reduce', reads=[sel3], writes=[sm['e8']], out=a_(sm['e8']), in_=sel3[0:n, :, :].rearrange("p g e -> p e g"), axis=AX.X, op=ALU.add)
        k.i('dve', 'tensor_reduce', reads=[sm['e8']], writes=[s1['m1']], out=a_(s1['m1']), in_=a_(sm['e8']), axis=AX.X, op=ALU.max)
        k.i('dve', 'tensor_scalar', reads=[sm['e8'], s1['m1']], writes=[sm['oh1']], out=a_(sm['oh1']), in0=a_(sm['e8']), scalar1=s1['m1'][0:n, 0:1], scalar2=None, op0=ALU.is_equal)
        k.i('dve', 'scalar_tensor_tensor', reads=[sm['oh1'], sm['e8']], writes=[sm['e8b']], out=a_(sm['e8b']), in0=a_(sm['oh1']), scalar=-1e30, in1=a_(sm['e8']),
            op0=ALU.mult, op1=ALU.add)
        k.i('dve', 'tensor_reduce', reads=[sm['e8b']], writes=[s1['m2']], out=a_(s1['m2']), in_=a_(sm['e8b']), axis=AX.X, op=ALU.max)
        k.i('dve', 'tensor_scalar', reads=[sm['e8b'], s1['m2']], writes=[sm['oh2']], out=a_(sm['oh2']), in0=a_(sm['e8b']), scalar1=s1['m2'][0:n, 0:1], scalar2=None, op0=ALU.is_equal)
        k.i('dve', 'tensor_tensor', reads=[s1['m2'], s1['m1']], writes=[s1['w1']], out=a_(s1['w1']), in0=a_(s1['m2']), in1=a_(s1['m1']), op=ALU.subtract)
        k.i('act', 'activation', reads=[s1['w1']], writes=[s1['w1']], out=a_(s1['w1']), in_=a_(s1['w1']), func=AF.Exp)
        k.i('dve', 'tensor_scalar', reads=[s1['w1']], writes=[s1['w1']], out=a_(s1['w1']), in0=a_(s1['w1']), scalar1=1.0, scalar2=None, op0=ALU.add)
        k.i('dve', 'reciprocal', reads=[s1['w1']], writes=[s1['w1']], out=a_(s1['w1']), in_=a_(s1['w1']))
        k.i('dve', 'tensor_scalar', reads=[s1['w1']], writes=[s1['w2']], out=a_(s1['w2']), in0=a_(s1['w1']), scalar1=-1.0, scalar2=1.0, op0=ALU.mult, op1=ALU.add)
        k.i('dve', 'tensor_tensor', reads=[s1['w1'], s1['se']], writes=[s1['w1']], out=a_(s1['w1']), in0=a_(s1['w1']), in1=a_(s1['se']), op=ALU.mult)
        k.i('dve', 'tensor_tensor', reads=[s1['w2'], s1['se']], writes=[s1['w2']], out=a_(s1['w2']), in0=a_(s1['w2']), in1=a_(s1['se']), op=ALU.mult)
        k.i('dve', 'tensor_scalar', reads=[sm['oh1'], s1['w1']], writes=[sm['g8']], out=a_(sm['g8']), in0=a_(sm['oh1']), scalar1=s1['w1'][0:n, 0:1], scalar2=None, op0=ALU.mult)
        k.i('dve', 'scalar_tensor_tensor', reads=[sm['oh2'], s1['w2'], sm['g8']], writes=[sm['g8']], out=a_(sm['g8']), in0=a_(sm['oh2']), scalar=s1['w2'][0:n, 0:1],
            in1=a_(sm['g8']), op0=ALU.mult, op1=ALU.add)
        k.i('dve', 'tensor_tensor', reads=[sm['oh1'], sm['oh2']], writes=[sm['t8']], out=a_(sm['t8']), in0=a_(sm['oh1']), in1=a_(sm['oh2']), op=ALU.add)
        k.i('dve', 'tensor_tensor', reads=[sm['ohg'], sm['g8']], pwrites=[GATE], out=GATE[0:n, t, :].rearrange("p (g e) -> p g e", g=8),
            in0=sm['ohg'][0:n, :].unsqueeze(2).to_broadcast([n, 8, 8]), in1=sm['g8'][0:n, :].unsqueeze(1).to_broadcast([n, 8, 8]), op=ALU.mult)
        k.i('dve', 'tensor_tensor', reads=[sm['ohg'], sm['t8']], pwrites=[ASG], out=ASG[0:n, t, :].rearrange("p (g e) -> p g e", g=8),
            in0=sm['ohg'][0:n, :].unsqueeze(2).to_broadcast([n, 8, 8]), in1=sm['t8'][0:n, :].unsqueeze(1).to_broadcast([n, 8, 8]), op=ALU.mult)
    k.begin_fill(RANK)
    for t in range(9):
        ps = Fb[t % 2]
        k.i('pe', 'matmul', reads=[trib, ASG], writes=[ps], out=ps[:, 0:64], lhsT=trib[:], rhs=ASG[:, t, :], start=True, stop=(t == 0))
        for t2 in range(t):
            k.i('pe', 'matmul', reads=[onesb, ASG], pwrites=[ps], out=ps[:, 0:64], lhsT=onesb[:], rhs=ASG[:, t2, :], start=False, stop=(t2 == t - 1))
        k.i('dve', 'scalar_tensor_tensor', reads=[ps, ASG], pwrites=[RANK], out=RANK[:, t, :], in0=ps[:, 0:64], scalar=1.0, in1=ASG[:, t, :], op0=ALU.add, op1=ALU.mult)
    k.i('dve', 'tensor_scalar', reads=[RANK], writes=[RANK], out=RANK[:], in0=RANK[:], scalar1=-1.0, scalar2=None, op0=ALU.add)
    k.barrier()
    nc.sbuf_base = mark1
    WG = [sb('WG%d' % i, [128, 16, 512], BF16) for i in range(2)]
    WU = [sb('WU%d' % i, [128, 16, 512], BF16) for i in range(2)]
    WD = [sb('WD%d' % i, [128, 4, D], BF16) for i in range(2)]
    SEL = sb('SEL', [128, 9, 128], BF16); SELG = sb('SELG', [128, 9, 128], BF16)
    SELGT = [sb('SELGT%d' % i, [128, 9, 128], BF16) for i in range(4)]
    UT = sb('UTp', [128, 16, 128], BF16)
    HB = sb('HB', [128, 512], BF16); HTs = sb('HTs', [128, 4, 128], BF16)
    sg = sb('sg', [128, 512])
    YG = sb('YG', [128, 4, D], BF16)
    OUTt = sb('OUTt', [128, D])
    iota3 = iota[:, :].unsqueeze(1).to_broadcast([128, 9, 64])

    def load_expert(e):
        b = e % 2
        k.dma('pool', WG[b], WG[b][:], inp['e_g'], inp['e_g'].ap()[e].rearrange("(p c) n -> p c n", c=16))
        k.dma('pool', WU[b], WU[b][:], inp['e_u'], inp['e_u'].ap()[e].rearrange("(p c) n -> p c n", c=16))
        k.dma('pool', WD[b], WD[b][:], inp['e_d'], inp['e_d'].ap()[e].rearrange("(p c) n -> p c n", c=4))
    load_expert(0)
    for grp in range(8):
        k.begin_fill(YG)
        for pr in range(4):
            e0 = grp * 8 + pr * 2
            for e2 in range(2):
                e = e0 + e2
                k.i('dve', 'tensor_tensor', reads=[iota, RANK], writes=[SEL] if e2 == 0 else [], pwrites=[] if e2 == 0 else [SEL],
                    out=SEL[:, :, e2 * 64:(e2 + 1) * 64], in0=iota3, in1=RANK[:, :, e:e + 1].to_broadcast([128, 9, 64]), op=ALU.is_equal)
                k.i('dve', 'tensor_tensor', reads=[SEL, GATE], writes=[SELG] if e2 == 0 else [], pwrites=[] if e2 == 0 else [SELG],
                    out=SELG[:, :, e2 * 64:(e2 + 1) * 64], in0=SEL[:, :, e2 * 64:(e2 + 1) * 64], in1=GATE[:, :, e:e + 1].to_broadcast([128, 9, 64]), op=ALU.mult)
            sgt = SELGT[pr]
            k.begin_fill(sgt)
            for t in range(9):
                hb_ = H[t // 8]
                k.i('pe', 'transpose', reads=[SELG, identb], writes=[hb_] if t % 8 == 0 else [], pwrites=[] if t % 8 == 0 else [hb_],
                    out=hb_[:, t % 8, :], in_=SELG[:, t, :], identity=identb[:])
            k.i('act', 'copy', reads=[H[0]], pwrites=[sgt], out=sgt[:, 0:8, :], in_=H[0][:, :, :])
            k.i('dve', 'tensor_copy', reads=[H[1]], pwrites=[sgt], out=sgt[:, 8:9, :], in_=H[1][:, 0:1, :])
            k.begin_fill(UT)
            for q4 in range(4):
                bank = Fb[q4]
                for c4 in range(4):
                    dc = q4 * 4 + c4
                    for t in range(9):
                        k.i('pe', 'matmul', reads=[U16, SEL], writes=[bank] if (c4 == 0 and t == 0) else [], pwrites=[] if (c4 == 0 and t == 0) else [bank],
                            out=bank[:, c4 * 128:(c4 + 1) * 128], lhsT=U16[:, t, :].rearrange("q (p c) -> q c p", c=16)[:, dc, :], rhs=SEL[:, t, :], start=(t == 0), stop=(t == 8))
                k.i('act' if q4 % 2 == 0 else 'dve', 'copy' if q4 % 2 == 0 else 'tensor_copy', reads=[bank], pwrites=[UT],
                    out=UT[:, q4 * 4:(q4 + 1) * 4, :], in_=bank[:, :].rearrange("p (a b) -> p a b", a=4))
            for e2 in range(2):
                e = e0 + e2
                b = e % 2
                if e + 1 < 64:
                    load_expert(e + 1)
                lo, hi = e2 * 64, (e2 + 1) * 64
                tp = (0, lo)
                for (W_, bank) in ((WG[b], Fb[0]), (WU[b], Fb[1])):
                    for dc in range(16):
                        k.i('pe', 'matmul', reads=[UT, W_], writes=[bank] if dc == 0 else [], pwrites=[] if dc == 0 else [bank],
                            out=bank[lo:hi, :], lhsT=UT[:, dc, lo:hi], rhs=W_[:, dc, :], start=(dc == 0), stop=(dc == 15), tile_position=tp)
                k.i('act', 'activation', reads=[Fb[0]], writes=[sg], out=sg[lo:hi, :], in_=Fb[0][lo:hi, :], func=AF.Silu)
                k.i('dve', 'tensor_tensor', reads=[sg, Fb[1]], writes=[HB], out=HB[lo:hi, :], in0=sg[lo:hi, :], in1=Fb[1][lo:hi, :], op=ALU.mult)
                hb_ = H[2]
                for fc in range(4):
                    k.i('pe', 'transpose', reads=[HB, identb], writes=[hb_] if fc == 0 else [], pwrites=[] if fc == 0 else [hb_],
                        out=hb_[:, fc, 0:64], in_=HB[lo:hi, :].rearrange("q (p c) -> q c p", c=4)[:, fc, :], identity=identb[lo:hi, lo:hi])
                k.i('act', 'copy', reads=[hb_], writes=[HTs], out=HTs[:, :, 0:64], in_=hb_[:, 0:4, 0:64])
                for half in range(2):
                    for nc2 in range(2):
                        bank = Fb[2 + nc2]
                        ncol = half * 2 + nc2
                        for fc in range(4):
                            k.i('pe', 'matmul', reads=[HTs, WD[b]], writes=[bank] if fc == 0 else [], pwrites=[] if fc == 0 else [bank],
                                out=bank[lo:hi, :], lhsT=HTs[:, fc, 0:64], rhs=WD[b][:, fc, ncol * 512:(ncol + 1) * 512], start=(fc == 0), stop=(fc == 3),
                                tile_position=tp)
                        k.i('act' if nc2 == 0 else 'dve', 'copy' if nc2 == 0 else 'tensor_copy', reads=[bank], pwrites=[YG],
                            out=YG[lo:hi, pr, ncol * 512:(ncol + 1) * 512], in_=bank[lo:hi, :])
        for t in range(9):
            n = 16 if t == 8 else 128
            k.begin_fill(OUTt)
            for nchk in range(4):
                bank = Fb[nchk]
                for pr in range(4):
                    k.i('pe', 'matmul', reads=[SELGT[pr], YG], writes=[bank] if pr == 0 else [], pwrites=[] if pr == 0 else [bank],
                        out=bank[:, :], lhsT=SELGT[pr][:, t, :], rhs=YG[:, pr, nchk * 512:(nchk + 1) * 512], start=(pr == 0), stop=(pr == 3))
                k.i('act' if nchk % 2 == 0 else 'dve', 'copy' if nchk % 2 == 0 else 'tensor_copy', reads=[bank],
                    pwrites=[OUTt], out=OUTt[:, nchk * 512:(nchk + 1) * 512], in_=bank[:, :])
            k.dma('pool', p.H2t[t], H2.ap()[t * 128:t * 128 + n, :], OUTt, OUTt[0:n, :], accum_op=ALU.add)
    for t in range(9):
        n = 16 if t == 8 else 128
        k.dma('sp', p.out['o_y'], p.out['o_y'].ap()[t * 128:t * 128 + n, :], p.H2t[t], H2.ap()[t * 128:t * 128 + n, :], partial=True)


def phase_SR(k, p):
    inp, sb = p.inp, k.sb
    Fb, H, identb, identf = p.Fb, p.H, p.identb, p.identf
    N = 16
    SRd, SV, YNd, RWS = p.dr['SRd'], p.dr['SV'], p.dr['YNd'], p.dr['RWS']
    sr = sb('s_sr', [N, RW_COLS]); pv = sb('s_prev', [N, RW_COLS]); mub = sb('s_mu', [N, RW_COLS])
    k.dma('sp', sr, sr[:], SRd, SRd.ap())
    k.dma('sp', pv, pv[:], inp['sh_s'], inp['sh_s'].ap())
    k.dma('sp', mub, mub[:], inp['mu_f'], inp['mu_f'].ap().partition_broadcast(N))
    k.i('dve', 'tensor_tensor', reads=[pv, sr], writes=[pv], out=pv[:], in0=pv[:], in1=sr[:], op=ALU.subtract)
    k.i('dve', 'tensor_tensor', reads=[pv, mub], writes=[pv], out=pv[:], in0=pv[:], in1=mub[:], op=ALU.mult)
    k.i('dve', 'tensor_tensor', reads=[pv, sr], writes=[pv], out=pv[:], in0=pv[:], in1=sr[:], op=ALU.add)
    xm = pv
    xr, xk, xv = xm[:, 0:1024], xm[:, 1024:2048], xm[:, 2048:3072]
    pvb = sb('s_pvb', [N, 7, 1024])
    k.dma('sp', pvb, pvb[:], inp['pvf'], inp['pvf'].ap().partition_broadcast(N))
    lf = sb('s_lf', [128, 4, 1024]); lwb = sb('s_lwb', [128, 4, 1024], BF16)
    k.i('pool', 'memset', writes=[lf], ap=lf[:], constant=0.0)
    k.dma('sp', lf, lf[0:64, 0, :], inp['w2_f'], inp['w2_f'].ap())
    k.dma('sp', lf, lf[0:64, 1, :], inp['a2_f'], inp['a2_f'].ap(), partial=True)
    k.dma('sp', lf, lf[:, 2, :], inp['g2_f'], inp['g2_f'].ap()[0:128, :], partial=True)
    k.dma('sp', lf, lf[0:32, 3, :], inp['g2_f'], inp['g2_f'].ap()[128:160, :], partial=True)
    k.i('dve', 'tensor_copy', reads=[lf], writes=[lwb], out=lwb[:], in_=lf[:])
    li = sb('s_li', [N, 4, 128], BF16)
    k.i('pool', 'memset', writes=[li], ap=li[:], constant=0.0)
    k.i('act', 'activation', reads=[xm], writes=[li], out=li[:, 0, 0:64], in_=xm[:, 3072:3136], func=AF.Tanh)
    k.i('act', 'copy', reads=[xm], pwrites=[li], out=li[:, 1, 0:64], in_=xm[:, 3136:3200])
    k.i('act', 'activation', reads=[xm], pwrites=[li], out=li[:, 2, :], in_=xm[:, 3200:3328], func=AF.Sigmoid)
    k.i('act', 'activation', reads=[xm], pwrites=[li], out=li[:, 3, 0:32], in_=xm[:, 3328:3360], func=AF.Sigmoid)
    liT = sb('s_liT', [128, 4, N], BF16)
    HT = H[2]
    for i in range(4):
        k.i('pe', 'transpose', reads=[li, identb], writes=[HT] if i == 0 else [], pwrites=[] if i == 0 else [HT],
            out=HT[:, i, 0:N], in_=li[:, i, :], identity=identb[0:N, 0:N])
    k.i('act', 'copy', reads=[HT], writes=[liT], out=liT[:], in_=HT[:, 0:4, 0:N])
    lw = sb('s_lw', [N, 1024]); a = sb('s_a', [N, 1024]); gt = sb('s_g', [N, 1024])
    for hh in range(2):
        cs_ = slice(hh * 512, (hh + 1) * 512)
        ps = Fb[0]
        k.i('pe', 'matmul', reads=[liT, lwb], writes=[ps], out=ps[0:N, :], lhsT=liT[0:64, 0, :], rhs=lwb[0:64, 0, cs_], start=True, stop=True)
        k.i('dve', 'tensor_tensor', reads=[ps, pvb], writes=[lw] if hh == 0 else [], pwrites=[] if hh == 0 else [lw], out=lw[:, cs_], in0=ps[0:N, :], in1=pvb[:, 0, cs_], op=ALU.add)
        ps = Fb[1]
        k.i('pe', 'matmul', reads=[liT, lwb], writes=[ps], out=ps[0:N, :], lhsT=liT[0:64, 1, :], rhs=lwb[0:64, 1, cs_], start=True, stop=True)
        k.i('dve', 'tensor_tensor', reads=[ps, pvb], writes=[a] if hh == 0 else [], pwrites=[] if hh == 0 else [a], out=a[:, cs_], in0=ps[0:N, :], in1=pvb[:, 1, cs_], op=ALU.add)
        ps = Fb[2]
        k.i('pe', 'matmul', reads=[liT, lwb], writes=[ps], out=ps[0:N, :], lhsT=liT[:, 2, :], rhs=lwb[:, 2, cs_], start=True, stop=False)
        k.i('pe', 'matmul', reads=[liT, lwb], pwrites=[ps], out=ps[0:N, :], lhsT=liT[0:32, 3, :], rhs=lwb[0:32, 3, cs_], start=False, stop=True)
        k.i('dve', 'tensor_copy', reads=[ps], writes=[gt] if hh == 0 else [], pwrites=[] if hh == 0 else [gt], out=gt[:, cs_], in_=ps[0:N, :])
    k.i('act', 'activation', reads=[lw], writes=[lw], out=lw[:], in_=lw[:], func=AF.Sigmoid)
    k.i('act', 'activation', reads=[lw], writes=[lw], out=lw[:], in_=lw[:], func=AF.Exp, scale=-C_DEC)
    k.i('act', 'activation', reads=[a], writes=[a], out=a[:], in_=a[:], func=AF.Sigmoid)
    VT = sb('s_VT', [N, 6, 1024])
    t1 = sb('s_t1', [N, 1024]); st = sb('s_st', [N, 16])
    k.begin_fill(VT)
    k.i('act', 'copy', reads=[xm], pwrites=[VT], out=VT[:, 0, :], in_=xr)
    k.i('act', 'copy', reads=[lw], pwrites=[VT], out=VT[:, 1, :], in_=lw[:])
    k.i('act', 'copy', reads=[xm], pwrites=[VT], out=VT[:, 3, :], in_=xv)
    k.i('dve', 'tensor_tensor', reads=[xm, pvb], writes=[t1], out=t1[:], in0=xk, in1=pvb[:, 2, :], op=ALU.mult)
    sq = sb('s_sq', [N, 1024])
    k.i('dve', 'tensor_tensor', reads=[t1], writes=[sq], out=sq[:], in0=t1[:], in1=t1[:], op=ALU.mult)
    k.i('dve', 'tensor_reduce', reads=[sq], writes=[st], out=st[:], in_=sq[:].rearrange("p (h n) -> p h n", h=16), axis=AX.X, op=ALU.add)
    k.i('act', 'activation', reads=[st], writes=[st], out=st[:], in_=st[:], func=AF.Sqrt)
    k.i('dve', 'tensor_scalar', reads=[st], writes=[st], out=st[:], in0=st[:], scalar1=1e-12, scalar2=None, op0=ALU.max)
    k.i('dve', 'reciprocal', reads=[st], writes=[st], out=st[:], in_=st[:])
    k.i('dve', 'tensor_tensor', reads=[t1, st], pwrites=[VT], out=VT[:, 4, :].rearrange("p (h n) -> p h n", h=16),
        in0=t1[:].rearrange("p (h n) -> p h n", h=16), in1=st[:].unsqueeze(2).to_broadcast([N, 16, 64]), op=ALU.mult)
    k.i('dve', 'tensor_scalar', reads=[a], writes=[sq], out=sq[:], in0=a[:], scalar1=-1.0, scalar2=None, op0=ALU.add)
    k.i('dve', 'tensor_tensor', reads=[sq, pvb], writes=[sq], out=sq[:], in0=sq[:], in1=pvb[:, 3, :], op=ALU.mult)
    k.i('dve', 'tensor_scalar', reads=[sq], writes=[sq], out=sq[:], in0=sq[:], scalar1=1.0, scalar2=None, op0=ALU.add)
    k.i('dve', 'tensor_tensor', reads=[sq, xm], pwrites=[VT], out=VT[:, 2, :], in0=sq[:], in1=xk, op=ALU.mult)
    k.i('dve', 'tensor_tensor', reads=[VT, a], pwrites=[VT], out=VT[:, 5, :], in0=VT[:, 4, :], in1=a[:], op=ALU.mult)
    bonus = sb('s_bonus', [N, 1024])
    k.i('dve', 'tensor_tensor', reads=[xm, VT], writes=[t1], out=t1[:], in0=xr, in1=VT[:, 2, :], op=ALU.mult)
    k.i('dve', 'tensor_tensor', reads=[t1, pvb], writes=[t1], out=t1[:], in0=t1[:], in1=pvb[:, 4, :], op=ALU.mult)
    k.i('dve', 'tensor_reduce', reads=[t1], writes=[st], out=st[:], in_=t1[:].rearrange("p (h n) -> p h n", h=16), axis=AX.X, op=ALU.add)
    k.i('dve', 'tensor_tensor', reads=[xm, st], writes=[bonus], out=bonus[:].rearrange("p (h n) -> p h n", h=16),
        in0=xv.rearrange("p (h n) -> p h n", h=16), in1=st[:].unsqueeze(2).to_broadcast([N, 16, 64]), op=ALU.mult)
    for i in range(6):
        k.dma('sp', SV, SV.ap()[i], VT, VT[:, i, :], partial=True)
    VEC = sb('s_VEC', [128, 2, 6, 64])
    k.begin_fill(VEC)
    for r in range(2):
        for i in range(6):
            k.dma('sp', VEC, VEC[:, r, i, :], SV, SV.ap()[i].rearrange("b (h n) -> (b h) n", n=64)[r * 128:(r + 1) * 128, :], partial=True)
    lnwb = sb('s_lnwb', [128, 2, 2, 64])
    k.dma('sp', lnwb, lnwb[:], inp['lnwb_s'], inp['lnwb_s'].ap().rearrange("w (r p) n -> p w r n", p=128))
    YN = sb('s_YN', [128, 2, 64])
    k.begin_fill(YN)
    wkv_in = inp['wkv_s'].ap().rearrange("b h v k -> (b h) (v k)")
    wkv_out = p.out['o_wkvs'].ap().rearrange("b h v k -> (b h) (v k)")
    S = sb('s_S', [128, 64, 64]); T1 = sb('s_T', [128, 64, 64])
    for r in range(2):
        sa = sb('s_sa%d' % r, [128, 64]); y = sb('s_y%d' % r, [128, 64]); ms = sb('s_ms%d' % r, [128, 4])
        k.dma('sp', S, S[:].rearrange("p v k -> p (v k)"), inp['wkv_s'], wkv_in[r * 128:(r + 1) * 128, :])
        vec = lambda i, r=r: VEC[:, r, i, :]
        bk = lambda i, r=r: VEC[:, r, i, :].unsqueeze(1).to_broadcast([128, 64, 64])
        bv_ = lambda ap: ap.unsqueeze(2).to_broadcast([128, 64, 64])
        eng2 = 'pool'
        k.i(eng2, 'tensor_tensor', reads=[S, VEC], writes=[T1], out=T1[:], in0=S[:], in1=bk(4), op=ALU.mult)
        k.i('dve', 'tensor_reduce', reads=[T1], writes=[sa], out=sa[:], in_=T1[:], axis=AX.X, op=ALU.add)
        k.i('dve', 'tensor_scalar', reads=[sa], writes=[sa], out=sa[:], in0=sa[:], scalar1=-1.0, scalar2=None, op0=ALU.mult)
        k.i('dve', 'tensor_tensor', reads=[S, VEC], writes=[S], out=S[:], in0=S[:], in1=bk(1), op=ALU.mult)
        k.i(eng2, 'tensor_tensor', reads=[sa, VEC], writes=[T1], out=T1[:], in0=bv_(sa[:]), in1=bk(5), op=ALU.mult)
        k.i('dve', 'tensor_tensor', reads=[S, T1], writes=[S], out=S[:], in0=S[:], in1=T1[:], op=ALU.add)
        k.i(eng2, 'tensor_tensor', reads=[VEC], writes=[T1], out=T1[:], in0=bv_(vec(3)), in1=bk(2), op=ALU.mult)
        k.i('dve', 'tensor_tensor', reads=[S, T1], writes=[S], out=S[:], in0=S[:], in1=T1[:], op=ALU.add)
        k.dma('sp', p.out['o_wkvs'], wkv_out[r * 128:(r + 1) * 128, :], S, S[:].rearrange("p v k -> p (v k)"), partial=True)
        k.i(eng2, 'tensor_tensor', reads=[S, VEC], writes=[T1], out=T1[:], in0=S[:], in1=bk(0), op=ALU.mult)
        k.i('dve', 'tensor_reduce', reads=[T1], writes=[y], out=y[:], in_=T1[:], axis=AX.X, op=ALU.add)
        k.i('dve', 'tensor_reduce', reads=[y], writes=[ms], out=ms[:, 0:1], in_=y[:], axis=AX.X, op=ALU.add)
        k.i('dve', 'tensor_scalar', reads=[ms], writes=[ms], out=ms[:, 0:1], in0=ms[:, 0:1], scalar1=-1.0 / 64, scalar2=None, op0=ALU.mult)
        k.i('dve', 'tensor_scalar', reads=[y, ms], writes=[y], out=y[:], in0=y[:], scalar1=ms[:, 0:1], scalar2=None, op0=ALU.add)
        k.i('dve', 'tensor_tensor', reads=[y], writes=[sa], out=sa[:], in0=y[:], in1=y[:], op=ALU.mult)
        k.i('dve', 'tensor_reduce', reads=[sa], writes=[ms], out=ms[:, 1:2], in_=sa[:], axis=AX.X, op=ALU.add)
        k.i('act', 'activation', reads=[ms], writes=[ms], out=ms[:, 1:2], in_=ms[:, 1:2], func=AF.Sqrt, scale=1.0 / 64, bias=GN_EPS)
        k.i('dve', 'reciprocal', reads=[ms], writes=[ms], out=ms[:, 1:2], in_=ms[:, 1:2])
        k.i('dve', 'scalar_tensor_tensor', reads=[y, ms, lnwb], writes=[y], out=y[:], in0=y[:], scalar=ms[:, 1:2], in1=lnwb[:, 0, r, :], op0=ALU.mult, op1=ALU.mult)
        k.i('dve', 'tensor_tensor', reads=[y, lnwb], pwrites=[YN], out=YN[:, r, :], in0=y[:], in1=lnwb[:, 1, r, :], op=ALU.add)
        k.dma('sp', YNd, YNd.ap()[r * 128:(r + 1) * 128, :], YN, YN[:, r, :], partial=True)
    ynt = sb('s_ynt', [N, 1024]); rwb = sb('s_rwb', [N, 1024], BF16)
    k.dma('sp', ynt, ynt[:], YNd, YNd.ap().rearrange("(b h) n -> b (h n)", h=16))
    k.i('dve', 'tensor_tensor', reads=[ynt, bonus], writes=[ynt], out=ynt[:], in0=ynt[:], in1=bonus[:], op=ALU.add)
    k.i('dve', 'tensor_tensor', reads=[ynt, gt], writes=[rwb], out=rwb[:], in0=ynt[:], in1=gt[:], op=ALU.mult)
    k.dma('sp', RWS, RWS.ap(), rwb, rwb[:])


def phase_SA(k, p):
    inp, sb = p.inp, k.sb
    QS, AO = p.dr['QS'], p.dr['ATT_O']
    q = sb('a_q', [64, 4, 64]); kn_ = sb('a_kn', [64, 64]); vn = sb('a_vn', [64, 64])
    KC = sb('a_KC', [64, 128, 64]); VC = sb('a_VC', [64, 128, 64]); TT = sb('a_T', [64, 128, 64])
    sinks = sb('a_sinks', [64, 4])
    k.dma('sp', sinks, sinks[:], inp['sinks_s'], inp['sinks_s'].ap())
    k.begin_fill(q, kn_, vn, KC, VC)
    for kh in range(4):
        ps_ = slice(kh * 16, (kh + 1) * 16)
        k.dma('sp', q, q[ps_, :, :].rearrange("p g d -> p (g d)"), QS, QS.ap()[:, kh * 256:(kh + 1) * 256], partial=True)
        k.dma('sp', kn_, kn_[ps_, :], QS, QS.ap()[:, 1024 + kh * 64:1024 + (kh + 1) * 64], partial=True)
        k.dma('sp', vn, vn[ps_, :], QS, QS.ap()[:, 1280 + kh * 64:1280 + (kh + 1) * 64], partial=True)
        k.dma('sp', KC, KC[ps_, :, :], inp['cwk'], inp['cwk'].ap()[:, :, kh * 64:(kh + 1) * 64], partial=True)
        k.dma('sp', VC, VC[ps_, :, :], inp['cwv'], inp['cwv'].ap()[:, :, kh * 64:(kh + 1) * 64], partial=True)
    sc = sb('a_sc', [64, 4, 129]); E = sb('a_E', [64, 4, 129])
    mx = sb('a_mx', [64, 4]); negm = sb('a_negm', [64, 4]); rs = sb('a_rs', [64, 4]); es = sb('a_es', [64, 4])
    t4 = sb('a_t4', [64, 4, 64])
    k.begin_fill(sc)
    for g in range(4):
        k.i('pool', 'tensor_tensor', reads=[KC, q], writes=[TT], out=TT[:], in0=KC[:], in1=q[:, g, :].unsqueeze(1).to_broadcast([64, 128, 64]), op=ALU.mult)
        k.i('dve', 'tensor_reduce', reads=[TT], pwrites=[sc], out=sc[:, g, 0:128], in_=TT[:], axis=AX.X, op=ALU.add)
    k.i('dve', 'tensor_tensor', reads=[q, kn_], writes=[t4], out=t4[:], in0=q[:], in1=kn_[:].unsqueeze(1).to_broadcast([64, 4, 64]), op=ALU.mult)
    k.i('dve', 'tensor_reduce', reads=[t4], pwrites=[sc], out=sc[:, :, 128], in_=t4[:], axis=AX.X, op=ALU.add)
    k.i('dve', 'tensor_reduce', reads=[sc], writes=[mx], out=mx[:], in_=sc[:], axis=AX.X, op=ALU.max)
    k.i('dve', 'tensor_scalar', reads=[mx], writes=[mx], out=mx[:], in0=mx[:], scalar1=0.125, scalar2=None, op0=ALU.mult)
    k.i('dve', 'tensor_tensor', reads=[mx, sinks], writes=[mx], out=mx[:], in0=mx[:], in1=sinks[:], op=ALU.max)
    k.i('dve', 'tensor_scalar', reads=[mx], writes=[negm], out=negm[:], in0=mx[:], scalar1=-1.0, scalar2=None, op0=ALU.mult)
    for g in range(4):
        k.i('act', 'activation', reads=[sc, negm], writes=[E, rs] if g == 0 else [], pwrites=[] if g == 0 else [E, rs],
            out=E[:, g, :], in_=sc[:, g, :], func=AF.Exp, scale=0.125, bias=negm[:, g:g + 1], accum_out=rs[:, g:g + 1])
    k.i('dve', 'tensor_tensor', reads=[sinks, negm], writes=[es], out=es[:], in0=sinks[:], in1=negm[:], op=ALU.add)
    k.i('act', 'activation', reads=[es], writes=[es], out=es[:], in_=es[:], func=AF.Exp)
    k.i('dve', 'tensor_tensor', reads=[rs, es], writes=[rs], out=rs[:], in0=rs[:], in1=es[:], op=ALU.add)
    k.i('dve', 'reciprocal', reads=[rs], writes=[rs], out=rs[:], in_=rs[:])
    o = sb('a_o', [64, 4, 64]); ob = sb('a_ob', [64, 4, 64], BF16)
    k.begin_fill(o)
    for g in range(4):
        k.i('pool', 'tensor_tensor', reads=[VC, E], writes=[TT], out=TT[:], in0=VC[:], in1=E[:, g, 0:128].unsqueeze(2).to_broadcast([64, 128, 64]), op=ALU.mult)
        k.i('dve', 'tensor_reduce', reads=[TT], pwrites=[o], out=o[:, g, :], in_=TT[:].rearrange("p s d -> p d s"), axis=AX.X, op=ALU.add)
        k.i('dve', 'scalar_tensor_tensor', reads=[vn, E, o], pwrites=[o], out=o[:, g, :], in0=vn[:], scalar=E[:, g, 128:129], in1=o[:, g, :], op0=ALU.mult, op1=ALU.add)
    k.i('dve', 'tensor_tensor', reads=[o, rs], writes=[ob], out=ob[:], in0=o[:], in1=rs[:].unsqueeze(2).to_broadcast([64, 4, 64]), op=ALU.mult)
    for kh in range(4):
        k.dma('sp', AO, AO.ap()[1024:1040, kh * 256:(kh + 1) * 256], ob, ob[kh * 16:(kh + 1) * 16, :, :].rearrange("p g d -> p (g d)"), partial=True)


def phase_SX(k, p):
    inp, sb = p.inp, k.sb
    H, Fb, identb = p.H, p.Fb, p.identb
    QXd, OXd, H2 = p.dr['QXd'], p.dr['OXd'], p.dr['H2']
    SC = 1.0 / math.sqrt(128.0)
    q = sb('x_q', [64, 128])
    k.begin_fill(q)
    for h in range(4):
        k.dma('sp', q, q[h * 16:(h + 1) * 16, :], QXd, QXd.ap()[:, h * 128:(h + 1) * 128], partial=True)
    KCH = [sb('x_K%d' % i, [64, 32, 128]) for i in range(2)]
    VCH = [sb('x_V%d' % i, [64, 32, 128]) for i in range(2)]
    TT = sb('x_T', [64, 32, 128])
    sc = sb('x_sc', [64, 256]); E = sb('x_E', [64, 256])
    mx = sb('x_mx', [64, 1]); rs = sb('x_rs', [64, 1])
    o = sb('x_o', [64, 128]); part = sb('x_part', [64, 128])
    k.begin_fill(sc)
    for c8 in range(8):
        kc = KCH[c8 % 2]
        k.begin_fill(kc)
        for h in range(4):
            k.dma('sp', kc, kc[h * 16:(h + 1) * 16, :, :], inp['cmk_s'], inp['cmk_s'].ap()[:, c8 * 32:(c8 + 1) * 32, h * 128:(h + 1) * 128], partial=True)
        k.i('pool', 'tensor_tensor', reads=[kc, q], writes=[TT], out=TT[:], in0=kc[:], in1=q[:].unsqueeze(1).to_broadcast([64, 32, 128]), op=ALU.mult)
        k.i('dve', 'tensor_reduce', reads=[TT], pwrites=[sc], out=sc[:, c8 * 32:(c8 + 1) * 32], in_=TT[:], axis=AX.X, op=ALU.add)
    k.i('dve', 'tensor_reduce', reads=[sc], writes=[mx], out=mx[:], in_=sc[:], axis=AX.X, op=ALU.max)
    k.i('dve', 'tensor_scalar', reads=[mx], writes=[mx], out=mx[:], in0=mx[:], scalar1=-SC, scalar2=None, op0=ALU.mult)
    k.i('act', 'activation', reads=[sc, mx], writes=[E, rs], out=E[:], in_=sc[:], func=AF.Exp, scale=SC, bias=mx[:, 0:1], accum_out=rs[:, 0:1])
    k.i('dve', 'reciprocal', reads=[rs], writes=[rs], out=rs[:], in_=rs[:])
    for c8 in range(8):
        vc = VCH[c8 % 2]
        k.begin_fill(vc)
        for h in range(4):
            k.dma('sp', vc, vc[h * 16:(h + 1) * 16, :, :], inp['cmv_s'], inp['cmv_s'].ap()[:, c8 * 32:(c8 + 1) * 32, h * 128:(h + 1) * 128], partial=True)
        k.i('pool', 'tensor_tensor', reads=[vc, E], writes=[TT], out=TT[:], in0=vc[:], in1=E[:, c8 * 32:(c8 + 1) * 32].unsqueeze(2).to_broadcast([64, 32, 128]), op=ALU.mult)
        if c8 == 0:
            k.i('dve', 'tensor_reduce', reads=[TT], writes=[o], out=o[:], in_=TT[:].rearrange("p m d -> p d m"), axis=AX.X, op=ALU.add)
        else:
            k.i('dve', 'tensor_reduce', reads=[TT], writes=[part], out=part[:], in_=TT[:].rearrange("p m d -> p d m"), axis=AX.X, op=ALU.add)
            k.i('dve', 'tensor_tensor', reads=[o, part], writes=[o], out=o[:], in0=o[:], in1=part[:], op=ALU.add)
    k.i('dve', 'tensor_scalar', reads=[o, rs], writes=[o], out=o[:], in0=o[:], scalar1=rs[:, 0:1], scalar2=None, op0=ALU.mult)
    for h in range(4):
        k.dma('sp', OXd, OXd.ap()[:, h * 128:(h + 1) * 128], o, o[h * 16:(h + 1) * 16, :], partial=True)
    WX = sb('x_WX', [128, 4, D], BF16)
    p.load_w(WX, 'w_xo', 512, 0, D)
    oxs = sb('x_oxs', [16, 512]); oxb = sb('x_oxb', [16, 512], BF16); oxT = sb('x_oxT', [128, 4, 16], BF16)
    hh = sb('x_hh', [16, D])
    k.dma('sp', oxs, oxs[:], OXd, OXd.ap())
    k.dma('sp', hh, hh[:], p.H2t[8], H2.ap()[1024:1040, :])
    k.i('act', 'copy', reads=[oxs], writes=[oxb], out=oxb[:], in_=oxs[:])
    k.begin_fill(oxT)
    p.transpose16(oxb, 16, oxT, lambda half: oxT[:, 0:4, 0:16], nchunks=4)
    for nchk in range(4):
        ps = Fb[nchk]
        for c in range(4):
            k.i('pe', 'matmul', reads=[oxT, WX], writes=[ps] if c == 0 else [], pwrites=[] if c == 0 else [ps],
                out=ps[0:16, :], lhsT=oxT[:, c, 0:16], rhs=WX[:, c, nchk * 512:(nchk + 1) * 512], start=(c == 0), stop=(c == 3))
        k.i('dve', 'tensor_tensor', reads=[ps, hh], writes=[hh], out=hh[:, nchk * 512:(nchk + 1) * 512], in0=ps[0:16, :],
            in1=hh[:, nchk * 512:(nchk + 1) * 512], op=ALU.add)
    k.dma('sp', p.H2t[8], H2.ap()[1024:1040, :], hh, hh[:])


def rope_table(pos):
    inv = (np.float32(500000.0) ** (-np.arange(8, dtype=np.float32) * np.float32(2.0) / np.float32(16.0))).astype(np.float32)
    ang = pos.astype(np.float32)[:, None] * inv[None, :]
    return np.concatenate([np.cos(ang), np.sin(ang)], axis=1).astype(np.float32)


def rmasks():
    m = np.zeros((12, 128, 128), np.float32)
    i = np.arange(128)
    tu_s = (i[:, None] < i[None, :]); tl_s = (i[:, None] > i[None, :]); tu_i = (i[:, None] <= i[None, :])
    blk = (i[:, None] // 64 == i[None, :] // 64)
    for n_, mm_ in enumerate([tu_s, tl_s, tu_s, tl_s, tu_s, tu_s, tu_i, tu_i, tu_i, tu_i, blk, blk]):
        m[n_] = mm_
    return m


def swa_masks(j):
    qi = np.arange(128)[:, None]; si = np.arange(256)[None, :]
    rel = 128 + qi - si
    band = (rel >= 0) & (rel <= 128)
    mn = np.where(band, 0.0, NEG).astype(np.float32)
    mf = np.where(band & (si >= 128), 0.0, NEG).astype(np.float32) if j == 0 else mn
    return np.stack([mn, mf])


def own_cols(j):
    cols = [np.arange(part * 1024 + 256 * j, part * 1024 + 256 * j + 256) for part in range(3)]
    cols.append(np.arange(3072, 3360))
    return np.concatenate(cols)


_INPUT_NAMES = []


def nc_input_names(nc):
    return list(_INPUT_NAMES)


import os as _os
STAGES = ('A', 'MEM', 'R', 'SR', 'SA', 'OX', 'SX', 'M')


def kernel(**inp):
    f = lambda a: np.ascontiguousarray(a, dtype=np.float32)
    x_prompt = inp['x_prompt']; x_sample = inp['x_sample']
    w_in = inp['w_in'][0]
    nc = build(STAGES)
    w_rt = f(np.concatenate([inp['router_group_w'][0], inp['router_expert_w'][0]], axis=1))
    b_rt = f(np.concatenate([inp['router_group_b'][0], inp['router_expert_b'][0]]))
    e_g = f(inp['exp_w_gate'][0]); e_u = f(inp['exp_w_up'][0]); e_d = f(inp['exp_w_down'][0])
    pvf = np.stack([inp[n][0] for n in ['rw_w0', 'rw_a0', 'rw_k_k', 'rw_k_a', 'rw_r_k', 'rw_ln_w', 'rw_ln_b']]).astype(np.float32)
    lnwb_s = np.stack([np.tile(inp['rw_ln_w'][0].reshape(16, 64), (16, 1)), np.tile(inp['rw_ln_b'][0].reshape(16, 64), (16, 1))]).astype(np.float32)
    sinks_s = np.repeat(inp['attn_sinks'][0].reshape(4, 4), 16, axis=0).astype(np.float32)
    in_maps = []
    for c in range(N_CORES):
        b, j = c // 4, c % 4
        cols = own_cols(j)
        wh = np.zeros((D, RWH), np.float32); wh[:, :cols.size] = w_in[:, ATT_COLS + cols]
        mu = np.zeros(RWH, np.float32); mu[:cols.size] = inp['rw_mu'][0][cols]
        hs = slice(256 * j, 256 * j + 256)
        pv = np.stack([inp[n][0][hs] for n in ['rw_w0', 'rw_a0', 'rw_k_k', 'rw_k_a', 'rw_r_k', 'rw_ln_w', 'rw_ln_b']]).astype(np.float32)
        x_tok = np.zeros((1168, D), np.float32)
        if j > 0:
            x_tok[0:128] = x_prompt[b, 1024 * j - 128:1024 * j]
        x_tok[128:1152] = x_prompt[b, 1024 * j:1024 * j + 1024]
        x_tok[1152:1168] = x_sample[16 * c:16 * c + 16, 0]
        pos = np.concatenate([np.arange(1024 * j - 128, 1024 * j + 1024), np.full(16, 16384)])
        m = {
            'x_tok': x_tok, 'x_seq': f(x_prompt[b]), 'mem': f(inp['mem_prompt'][b]),
            'ln1': f(inp['ln1_w'][0]), 'ln2': f(inp['ln2_w'][0]), 'ln3': f(inp['ln3_w'][0]), 'memn': f(inp['mem_norm_w'][0]),
            'qn': f(inp['q_norm_w'][0]), 'kn': f(inp['k_norm_w'][0]), 'xqn': f(inp['xq_norm_w'][0]), 'xkn': f(inp['xk_norm_w'][0]),
            'w_att': f(w_in[:, :ATT_COLS]), 'w_rw': f(w_in[:, ATT_COLS:]), 'w_rwh': wh, 'w_xkv': f(inp['xkv_w'][0]),
            'cs_all': rope_table(pos), 'ident': np.eye(128, dtype=np.float32), 'masks': swa_masks(j), 'sinks': f(inp['attn_sinks'][0]),
            'cwk': f(inp['cache_win_k'][0, 16 * c:16 * c + 16].reshape(16, 128, 256)),
            'cwv': f(inp['cache_win_v'][0, 16 * c:16 * c + 16].reshape(16, 128, 256)),
            'mu_h': mu, 'pv_h': pv, 'w2_h': f(inp['rw_w2'][0][:, hs]), 'a2_h': f(inp['rw_a2'][0][:, hs]), 'g2_h': f(inp['rw_g2'][0][:, hs]),
            'rmask': rmasks(),
            'cmk_s': f(inp['cache_mem_k'][0, 16 * c:16 * c + 16].reshape(16, 256, 512)), 'cmv_s': f(inp['cache_mem_v'][0, 16 * c:16 * c + 16].reshape(16, 256, 512)),
            'sh_s': f(inp['state_shift'][0, 16 * c:16 * c + 16]), 'wkv_s': f(inp['state_wkv'][0, 16 * c:16 * c + 16]), 'mu_f': f(inp['rw_mu'][0]),
            'pvf': pvf, 'w2_f': f(inp['rw_w2'][0]), 'a2_f': f(inp['rw_a2'][0]), 'g2_f': f(inp['rw_g2'][0]), 'lnwb_s': lnwb_s, 'sinks_s': sinks_s,
            'ohj': np.eye(4, dtype=np.float32)[j], 'w_rt': w_rt, 'b_rt': b_rt, 'e_g': e_g, 'e_u': e_u, 'e_d': e_d, 'iota64': np.arange(64, dtype=np.float32), 'w_out': f(inp['w_out'][0]), 'w_xq': f(inp['xq_w'][0]), 'w_xo': f(inp['xo_w'][0]),
        }
        in_maps.append(m)
    names = set(nc_input_names(nc))
    in_maps = [{kk: vv for kk, vv in m.items() if kk in names} for m in in_maps]
    res = run_bass_kernel_spmd(nc, in_maps, core_ids=list(range(N_CORES)))
    R = res.results
    global _DBG
    _DBG = R
    y_prompt = np.stack([np.concatenate([R[4 * b + j]['o_y'][0:1024] for j in range(4)]) for b in range(2)])
    y_sample = np.concatenate([R[c]['o_y'][1024:1040] for c in range(8)])[:, None, :]
    wkp = np.stack([R[4 * b + 3]['o_wkp'].reshape(128, 4, 64) for b in range(2)])[None]
    wvp = np.stack([R[4 * b + 3]['o_wvp'].reshape(128, 4, 64) for b in range(2)])[None]
    wkv_p = np.stack([np.concatenate([R[4 * b + j]['o_wkvp'] for j in range(4)]) for b in range(2)])[None]
    shp = np.zeros((1, 2, RW_COLS), np.float32)
    for b in range(2):
        for j in range(4):
            o = R[4 * b + j]['o_shp']
            for part in range(3):
                shp[0, b, part * 1024 + 256 * j: part * 1024 + 256 * j + 256] = o[part * 256:(part + 1) * 256]
            shp[0, b, 3072:3360] = o[768:768 + 288]
    mk = np.stack([R[4 * b]['o_mk'].reshape(256, 4, 128) for b in range(2)])[None]
    mv = np.stack([R[4 * b]['o_mv'].reshape(256, 4, 128) for b in range(2)])[None]
    swk = np.concatenate([R[c]['o_swk'].reshape(16, 128, 4, 64) for c in range(8)])[None]
    swv = np.concatenate([R[c]['o_swv'].reshape(16, 128, 4, 64) for c in range(8)])[None]
    wkv_s = np.concatenate([R[c]['o_wkvs'] for c in range(8)])[None]
    shs = np.concatenate([R[c]['o_shs'] for c in range(8)])[None]
    return (y_prompt, y_sample, wkp, wvp, wkv_p, shp, mk, mv, swk, swv, wkv_s, shs)
```

```python
import numpy as np
import ml_dtypes
import concourse.bass as bass
import concourse.mybir as mybir

F32 = mybir.dt.float32
BF16 = mybir.dt.bfloat16
I32 = mybir.dt.int32
ALU = mybir.AluOpType
AF = mybir.ActivationFunctionType
AX = mybir.AxisListType


class Res:
    def __init__(self, name, h, kind):
        self.name = name
        self.h = h
        self.kind = kind
        self.w = {}
        self.r = {}
        self.prev = {}
        self.dsem = None
        self.dcnt = 0

    def __getitem__(self, idx):
        return self.h[idx]

    def ap(self):
        return self.h.ap() if self.kind in ('dram', 'in', 'out') else self.h[:]


def _merge(dst, src):
    for k, v in src.items():
        if dst.get(k, 0) < v:
            dst[k] = v


class K:
    ENG = ('pe', 'act', 'dve', 'pool', 'sp')

    def __init__(self, nc):
        self.nc = nc
        self.prog = {e: [] for e in self.ENG}
        self.sems = {}
        self.cnt = {}
        for e in self.ENG:
            self.sems[e] = nc.alloc_semaphore('s_' + e)
            self.cnt[e] = 0
        self.waited = {e: {} for e in self.ENG}
        self.out_tickets = {}
        self.n_dsem = 0
        self.nres = 0
        self.dtot = {}
        self.ring_i = 0
        self.NRING = 64

    def sb(self, name, shape, dt=F32):
        self.nres += 1
        name = '%s_%d' % (name, self.nres)
        return Res(name, self.nc.alloc_sbuf_tensor(name, list(shape), dt), 'sb')

    def ps(self, name, shape, dt=F32):
        return Res(name, self.nc.alloc_psum_tensor(name, list(shape), dt), 'ps')

    def dram(self, name, shape, dt=F32):
        return Res(name, self.nc.dram_tensor(name, list(shape), dt), 'dram')

    def inp(self, name, shape, dt=F32):
        return Res(name, self.nc.dram_tensor(name, list(shape), dt, kind='ExternalInput'), 'in')

    def outp(self, name, shape, dt=F32):
        return Res(name, self.nc.dram_tensor(name, list(shape), dt, kind='ExternalOutput'), 'out')

    def _wait(self, e, deps):
        for key, val in deps.items():
            if key == 'pe' and e == 'pe':
                continue
            if self.waited[e].get(key, 0) >= val:
                continue
            self.waited[e][key] = val
            sem = self.sems[key]
            self.prog[e].append(lambda eng, sem=sem, val=val: eng.wait_ge(sem, val))

    def begin_fill(self, *ress):
        for b in ress:
            b.prev = {}
            _merge(b.prev, b.w)
            _merge(b.prev, b.r)
            b.w = {}
            b.r = {}

    def op(self, e, fn, reads=(), writes=(), pwrites=()):
        deps = {}
        for b in reads:
            _merge(deps, b.w)
        for b in writes:
            _merge(deps, b.w)
            _merge(deps, b.r)
        for b in pwrites:
            _merge(deps, b.prev)
        self._wait(e, deps)
        self.cnt[e] += 1
        t = {e: self.cnt[e]}
        sem = self.sems[e]
        self.prog[e].append(lambda eng, fn=fn, sem=sem: fn(eng).then_inc(sem, 1))
        for b in reads:
            _merge(b.r, t)
        for b in writes:
            b.w = dict(t)
            b.r = {}
        for b in pwrites:
            _merge(b.w, t)
        return t

    def dma(self, q, dst, dst_ap, src, src_ap, partial=False, **kw):
        deps = {}
        _merge(deps, src.w)
        if partial:
            _merge(deps, dst.prev)
        else:
            _merge(deps, dst.w)
            _merge(deps, dst.r)
        self._wait(q, deps)
        slot = self.ring_i % self.NRING
        self.ring_i += 1
        key = 'r%d' % slot
        if key not in self.sems:
            self.sems[key] = self.nc.alloc_semaphore(key)
            self.dtot[key] = 0
        self._wait(q, {key: self.dtot[key]})
        self.dtot[key] += 16
        t = {key: self.dtot[key]}
        sem = self.sems[key]
        self.prog[q].append(
            lambda eng, o=dst_ap, i=src_ap, sem=sem, kw=kw: eng.dma_start(out=o, in_=i, **kw).then_inc(sem, 16))
        _merge(src.r, t)
        if partial:
            _merge(dst.w, t)
        else:
            dst.w = dict(t)
            dst.r = {}
        if dst.kind == 'out':
            _merge(self.out_tickets, t)
        return t

    def custom(self, e, fn, inc, semkey_res, reads=(), writes=()):
        deps = {}
        for b in reads:
            _merge(deps, b.w)
        for b in writes:
            _merge(deps, b.w)
            _merge(deps, b.r)
        self._wait(e, deps)
        sres = semkey_res
        if sres.dsem is None:
            key = 'd%d' % self.n_dsem
            self.n_dsem += 1
            self.sems[key] = self.nc.alloc_semaphore(key)
            sres.dsem = key
        sres.dcnt += inc
        t = {sres.dsem: sres.dcnt}
        self.dtot[sres.dsem] = sres.dcnt
        sem = self.sems[sres.dsem]
        self.prog[e].append(lambda eng, fn=fn, sem=sem, inc=inc: fn(eng).then_inc(sem, inc))
        for b in reads:
            _merge(b.r, t)
        for b in writes:
            b.w = dict(t)
            b.r = {}
        return t

    def finish(self):
        self._wait('sp', self.out_tickets)
        allt = {e: self.cnt[e] for e in self.ENG if self.cnt[e] > 0}
        self._wait('sp', allt)
        nc = self.nc
        prog = self.prog
        with nc.Block() as block:
            @block.sync
            def _(eng):
                for f in prog['sp']:
                    f(eng)

            @block.tensor
            def _(eng):
                for f in prog['pe']:
                    f(eng)

            @block.scalar
            def _(eng):
                for f in prog['act']:
                    f(eng)

            @block.vector
            def _(eng):
                for f in prog['dve']:
                    f(eng)

            @block.gpsimd
            def _(eng):
                for f in prog['pool']:
                    f(eng)
        return nc


def _k_i(self, e, meth, reads=(), writes=(), pwrites=(), **kw):
    return self.op(e, lambda eng, meth=meth, kw=kw: getattr(eng, meth)(**kw), reads=reads, writes=writes, pwrites=pwrites)


K.i = _k_i


class ResView:
    def __init__(self, parent, ap, name=None):
        self.p = parent
        self.h = ap
        self.name = name or parent.name + '_v'
        self.kind = parent.kind

    def __getitem__(self, idx):
        return self.h[idx]

    w = property(lambda s: s.p.w, lambda s, v: setattr(s.p, 'w', v))
    r = property(lambda s: s.p.r, lambda s, v: setattr(s.p, 'r', v))
    prev = property(lambda s: s.p.prev, lambda s, v: setattr(s.p, 'prev', v))
    dsem = property(lambda s: s.p.dsem, lambda s, v: setattr(s.p, 'dsem', v))
    dcnt = property(lambda s: s.p.dcnt, lambda s, v: setattr(s.p, 'dcnt', v))


def _k_barrier(self):
    allt = {e: self.cnt[e] for e in self.ENG if self.cnt[e] > 0}
    for key, v in self.dtot.items():
        allt[key] = v
    for e in self.ENG:
        self._wait(e, allt)


K.barrier = _k_barrier


class P:
    pass

C_DEC = 0.6065306597126334
GN_EPS = 64e-5


def rwkv_consts(k, p, inp):
    c = P()
    p.rc = c
    c.mu = k.sb('r_mu', [128, 9])
    c.omm = k.sb('r_omm', [128, 9])
    k.dma('sp', c.mu, c.mu[:], inp['mu_h'], inp['mu_h'].ap().rearrange("(c p) -> p c", p=128), allow_slow_non_contiguous=True)
    k.i('dve', 'tensor_scalar', reads=[c.mu], writes=[c.omm], out=c.omm[:], in0=c.mu[:], scalar1=-1.0, scalar2=1.0, op0=ALU.mult, op1=ALU.add)
    c.pv = k.sb('r_pv', [128, 7, 2])
    k.dma('sp', c.pv, c.pv[:], inp['pv_h'], inp['pv_h'].ap().rearrange("v (g p) -> p v g", p=128), allow_slow_non_contiguous=True)
    c.omka = k.sb('r_omka', [128, 2])
    k.i('dve', 'tensor_scalar', reads=[c.pv], writes=[c.omka], out=c.omka[:], in0=c.pv[:, 3, :], scalar1=-1.0, scalar2=1.0, op0=ALU.mult, op1=ALU.add)
    lf = k.sb('r_lf', [128, 3, 256])
    c.lw = k.sb('r_lw', [128, 3, 256], BF16)
    k.i('pool', 'memset', writes=[lf], ap=lf[:], constant=0.0)
    k.dma('sp', lf, lf[0:64, 0, :], inp['w2_h'], inp['w2_h'].ap())
    k.dma('sp', lf, lf[64:128, 0, :], inp['a2_h'], inp['a2_h'].ap(), partial=True)
    k.dma('sp', lf, lf[:, 1, :], inp['g2_h'], inp['g2_h'].ap()[0:128, :], partial=True)
    k.dma('sp', lf, lf[0:32, 2, :], inp['g2_h'], inp['g2_h'].ap()[128:160, :], partial=True)
    k.i('dve', 'tensor_copy', reads=[lf], writes=[c.lw], out=c.lw[:], in_=lf[:])
    mf = k.sb('r_mf', [128, 12, 128])
    k.dma('sp', mf, mf[:], inp['rmask'], inp['rmask'].ap().rearrange("m p n -> p m n"))
    c.mf = mf
    c.mb = k.sb('r_mb', [128, 12, 128], BF16)
    k.i('dve', 'tensor_copy', reads=[mf], writes=[c.mb], out=c.mb[:], in_=mf[:])
    return c


def rwkv_phase(k, p, inp, T, x_src, x_ap_fn, WH, lnb, front, out_rw, out_state, out_shift, after_st=None):
    c = p.rc
    identb, identf = p.identb, p.identf
    NST = T // 512
    sb = k.sb
    uTall = sb('rk_uT', [128, 16, 512], BF16)
    PR = sb('rk_PR', [128, 9, 513])
    XM = sb('rk_XM', [128, 9, 512])
    k.i('pool', 'memset', writes=[PR], ap=PR[:], constant=0.0)
    f32t = {n: sb('rk_' + n, [128, 512]) for n in ['lw', 'a', 'kk', 'kmod', 'bv', 'cum', 't1', 't2']}
    b16t = {n: sb('rk_' + n, [128, 512], BF16) for n in ['tw', 'sg7', 'sg8']}
    bd = {n: sb('rk_bd_' + n, [128, 2, 8, 128], BF16) for n in ['kkt', 'rt', 'bh', 'kh', 'v']}
    gC = sb('rk_gC', [128, 2, 8])
    GATE = sb('rk_gate', [128, 2, 512])
    BONUS = sb('rk_bonus', [128, 2, 512])
    YV = sb('rk_yv', [128, 2, 512])
    S32 = sb('rk_S32', [128, 2, 128])
    S16 = sb('rk_S16', [128, 2, 128], BF16)
    S0g = sb('rk_S0g', [128, 2, 128])
    k.i('pool', 'memset', writes=[S32], ap=S32[:], constant=0.0)
    k.i('pool', 'memset', writes=[S16], ap=S16[:], constant=0.0)
    NA = [sb('rk_NA%d' % i, [128, 4, 128], BF16) for i in range(2)]
    AB = [sb('rk_AB%d' % i, [128, 4, 128], BF16) for i in range(2)]
    KR = [sb('rk_KR%d' % i, [128, 2, 128], BF16) for i in range(2)]
    TM = [sb('rk_TM%d' % i, [128, 6, 128], BF16) for i in range(2)]
    PQ = [sb('rk_PQ%d' % i, [128, 4, 128], BF16) for i in range(2)]
    TT = [sb('rk_TT%d' % i, [128, 2, 128], BF16) for i in range(2)]
    TTF = [sb('rk_TTF%d' % i, [128, 2, 128], BF16) for i in range(2)]
    U0 = sb('rk_U0', [128, 2, 128], BF16)
    UU = sb('rk_U', [128, 2, 128], BF16)
    rwo = [sb('rk_rwo%d' % i, [128, 512], BF16) for i in range(2)]
    pW = p.pM
    pC = p.pC
    pH = p.pH
    st = P()
    st.q = 0
    st.ev = 0

    def nb():
        s_ = pC[st.q % len(pC)]
        st.q += 1
        return s_

    def mmg(ps, slot_, terms, first_in_bank):
        n = len(terms)
        for i, (L, Lap, Rr, Rap) in enumerate(terms):
            fresh = first_in_bank and i == 0
            k.i('pe', 'matmul', reads=[L, Rr], writes=[ps] if fresh else [], pwrites=[] if fresh else [ps],
                out=ps[:, slot_, :], lhsT=Lap, rhs=Rap, start=(i == 0), stop=(i == n - 1))

    def cp(dst, dst_ap, ps, ps_ap, scale=None):
        st.ev += 1
        if scale is not None:
            k.i('act', 'activation', reads=[ps], writes=[dst], out=dst_ap, in_=ps_ap, func=AF.Identity, scale=scale)
        elif st.ev % 2 == 0:
            k.i('act', 'copy', reads=[ps], writes=[dst], out=dst_ap, in_=ps_ap)
        else:
            k.i('dve', 'tensor_copy', reads=[ps], writes=[dst], out=dst_ap, in_=ps_ap)

    BONES = c.mf[:, 10, :]
    BM = c.mb[:, 11, :].rearrange("p (j s) -> p j s", j=2).unsqueeze(1).to_broadcast([128, 8, 2, 64])
    for stile in range(NST):
        k.begin_fill(uTall)
        for tt in range(4):
            front(x_src, x_ap_fn(stile * 4 + tt), 128, lnb, uTall,
                  lambda half, tt=tt: uTall[:, half * 8:(half + 1) * 8, tt * 128:(tt + 1) * 128])
        if stile > 0:
            k.i('act', 'copy', reads=[PR], writes=[PR], out=PR[:, :, 0:1], in_=PR[:, :, 512:513])
        k.begin_fill(PR)
        for cc in range(9):
            ps = pW[p.mi % len(pW)]
            p.mi += 1
            for dc in range(16):
                k.i('pe', 'matmul', reads=[uTall, WH], writes=[ps] if dc == 0 else [], pwrites=[] if dc == 0 else [ps],
                    out=ps[:, :], lhsT=WH[:, dc, cc * 128:(cc + 1) * 128], rhs=uTall[:, dc, :], start=(dc == 0), stop=(dc == 15))
            if cc % 2 == 0:
                k.i('act', 'copy', reads=[ps], pwrites=[PR], out=PR[:, cc, 1:513], in_=ps[:, :])
            else:
                k.i('dve', 'tensor_copy', reads=[ps], pwrites=[PR], out=PR[:, cc, 1:513], in_=ps[:, :])
        if stile == NST - 1:
            k.dma('sp', out_shift, out_shift.ap().rearrange("(c p) -> p c", p=128), PR, PR[:, :, 512], allow_slow_non_contiguous=True)
        k.begin_fill(XM)
        for cc in range(9):
            eng = 'dve' if cc % 2 == 0 else 'pool'
            t1 = f32t['t1'] if cc % 2 == 0 else f32t['t2']
            k.i(eng, 'tensor_scalar', reads=[PR, c.mu], writes=[t1], out=t1[:], in0=PR[:, cc, 0:512], scalar1=c.mu[:, cc:cc + 1],
                scalar2=0.0, op0=ALU.mult, op1=ALU.add)
            k.i('dve', 'scalar_tensor_tensor', reads=[PR, c.omm, t1], pwrites=[XM], out=XM[:, cc, :], in0=PR[:, cc, 1:513],
                scalar=c.omm[:, cc:cc + 1], in1=t1[:], op0=ALU.mult, op1=ALU.add)
        tw, sg7, sg8 = b16t['tw'], b16t['sg7'], b16t['sg8']
        k.i('act', 'activation', reads=[XM], writes=[tw], out=tw[0:64, :], in_=XM[0:64, 6, :], func=AF.Tanh)
        k.i('act', 'copy', reads=[XM], pwrites=[tw], out=tw[64:128, :], in_=XM[64:128, 6, :])
        k.i('act', 'activation', reads=[XM], writes=[sg7], out=sg7[:], in_=XM[:, 7, :], func=AF.Sigmoid)
        k.i('act', 'activation', reads=[XM], writes=[sg8], out=sg8[0:32, :], in_=XM[0:32, 8, :], func=AF.Sigmoid)
        k.begin_fill(GATE, BONUS, gC, *bd.values())
        for g in range(2):
            gs = slice(g * 128, (g + 1) * 128)
            lw, a, kk, kmod, bv, cum, t1, t2 = [f32t[n] for n in ['lw', 'a', 'kk', 'kmod', 'bv', 'cum', 't1', 't2']]
            xr, xk, xv = XM[:, 0 + g, :], XM[:, 2 + g, :], XM[:, 4 + g, :]
            pvg = lambda i, g=g: c.pv[:, i, g:g + 1]
            ps = pW[p.mi % len(pW)]; p.mi += 1
            k.i('pe', 'matmul', reads=[c.lw, tw], writes=[ps], out=ps[:, :], lhsT=c.lw[0:64, 0, gs], rhs=tw[0:64, :], start=True, stop=True)
            k.i('act', 'activation', reads=[ps, c.pv], writes=[lw], out=lw[:], in_=ps[:, :], func=AF.Sigmoid, bias=pvg(0))
            k.i('dve', 'tensor_scalar', reads=[lw], writes=[lw], out=lw[:], in0=lw[:], scalar1=-C_DEC, scalar2=None, op0=ALU.mult)
            ps = pW[p.mi % len(pW)]; p.mi += 1
            k.i('pe', 'matmul', reads=[c.lw, tw], writes=[ps], out=ps[:, :], lhsT=c.lw[64:128, 0, gs], rhs=tw[64:128, :], start=True, stop=True)
            k.i('act', 'activation', reads=[ps, c.pv], writes=[a], out=a[:], in_=ps[:, :], func=AF.Sigmoid, bias=pvg(1))
            ps = pW[p.mi % len(pW)]; p.mi += 1
            k.i('pe', 'matmul', reads=[c.lw, sg7], writes=[ps], out=ps[:, :], lhsT=c.lw[:, 1, gs], rhs=sg7[:], start=True, stop=False)
            k.i('pe', 'matmul', reads=[c.lw, sg8], pwrites=[ps], out=ps[:, :], lhsT=c.lw[0:32, 2, gs], rhs=sg8[0:32, :], start=False, stop=True)
            k.i('act', 'copy', reads=[ps], pwrites=[GATE], out=GATE[:, g, :], in_=ps[:, :])
            k.i('dve', 'tensor_scalar', reads=[XM, c.pv], writes=[kk], out=kk[:], in0=xk, scalar1=pvg(2), scalar2=None, op0=ALU.mult)
            k.i('pool', 'tensor_tensor', reads=[kk], writes=[t1], out=t1[:], in0=kk[:], in1=kk[:], op=ALU.mult)
            ps = pW[p.mi % len(pW)]; p.mi += 1
            k.i('pe', 'matmul', reads=[c.mf, t1], writes=[ps], out=ps[:, :], lhsT=BONES, rhs=t1[:], start=True, stop=True)
            k.i('act', 'activation', reads=[ps], writes=[t2], out=t2[:], in_=ps[:, :], func=AF.Sqrt)
            k.i('dve', 'tensor_scalar', reads=[t2], writes=[t2], out=t2[:], in0=t2[:], scalar1=1e-12, scalar2=None, op0=ALU.max)
            k.i('dve', 'reciprocal', reads=[t2], writes=[t2], out=t2[:], in_=t2[:])
            k.i('dve', 'tensor_tensor', reads=[kk, t2], writes=[kk], out=kk[:], in0=kk[:], in1=t2[:], op=ALU.mult)
            k.i('dve', 'tensor_scalar', reads=[a, c.pv, c.omka], writes=[t1], out=t1[:], in0=a[:], scalar1=pvg(3), scalar2=c.omka[:, g:g + 1],
                op0=ALU.mult, op1=ALU.add)
            k.i('pool', 'tensor_tensor', reads=[XM, t1], writes=[kmod], out=kmod[:], in0=xk, in1=t1[:], op=ALU.mult)
            k.i('pool', 'tensor_tensor', reads=[kk, a], writes=[bv], out=bv[:], in0=kk[:], in1=a[:], op=ALU.mult)
            k.i('dve', 'scalar_tensor_tensor', reads=[XM, kmod, c.pv], writes=[t1], out=t1[:], in0=xr, scalar=pvg(4), in1=kmod[:], op0=ALU.mult, op1=ALU.mult)
            ps = pW[p.mi % len(pW)]; p.mi += 1
            k.i('pe', 'matmul', reads=[c.mf, t1], writes=[ps], out=ps[:, :], lhsT=BONES, rhs=t1[:], start=True, stop=True)
            k.i('dve', 'tensor_tensor', reads=[ps, XM], pwrites=[BONUS], out=BONUS[:, g, :], in0=ps[:, :], in1=xv, op=ALU.mult)
            for ch in range(8):
                cs_ = slice(ch * 64, (ch + 1) * 64)
                k.i('dve', 'tensor_tensor_scan', reads=[lw, p.ones64], writes=[] if ch else [cum], pwrites=[cum] if ch else [],
                    out=cum[:, cs_], data0=p.ones64[:, :], data1=lw[:, cs_], initial=0.0, op0=ALU.mult, op1=ALU.add)
            k.i('act', 'activation', reads=[cum], pwrites=[gC], out=gC[:, g, :], in_=cum[:].rearrange("p (c s) -> p c s", s=64)[:, :, 63], func=AF.Exp)
            k.i('act', 'activation', reads=[cum], writes=[t1], out=t1[:], in_=cum[:], func=AF.Exp)
            k.i('act', 'activation', reads=[cum], writes=[t2], out=t2[:], in_=cum[:], func=AF.Exp, scale=-1.0)
            k.i('dve', 'tensor_tensor', reads=[cum, lw], writes=[lw], out=lw[:], in0=cum[:], in1=lw[:], op=ALU.subtract)
            k.i('act', 'activation', reads=[lw], writes=[lw], out=lw[:], in_=lw[:], func=AF.Exp)

            def mk_bd(dstn, a_res, a_ap, b_res, b_ap, g=g, cum=cum):
                k.i('dve', 'tensor_tensor', reads=[a_res, b_res], writes=[cum], out=cum[:], in0=a_ap, in1=b_ap, op=ALU.mult)
                d = bd[dstn]
                k.i('dve', 'tensor_tensor', reads=[cum, c.mb], pwrites=[d],
                    out=d[:, g, :, :].rearrange("p c (j s) -> p c j s", j=2),
                    in0=cum[:].rearrange("p (c s) -> p c s", s=64).unsqueeze(2).to_broadcast([128, 8, 2, 64]), in1=BM, op=ALU.mult)
            mk_bd('kkt', kk, kk[:], lw, lw[:])
            mk_bd('rt', XM, xr, t1, t1[:])
            mk_bd('bh', bv, bv[:], t2, t2[:])
            mk_bd('kh', kmod, kmod[:], t2, t2[:])
            k.i('dve', 'tensor_tensor', reads=[XM, c.mb], pwrites=[bd['v']],
                out=bd['v'][:, g, :, :].rearrange("p c (j s) -> p c j s", j=2),
                in0=xv.rearrange("p (c s) -> p c s", s=64).unsqueeze(2).to_broadcast([128, 8, 2, 64]), in1=BM, op=ALU.mult)
        kkt, rt, bh, kh, vb = [bd[n] for n in ['kkt', 'rt', 'bh', 'kh', 'v']]
        k.begin_fill(YV)
        def dep_pieces(ch, stile=stile):
            par = ch % 2
            na, ab, kr, tm = NA[par], AB[par], KR[par], TM[par]
            cur = TTF[par]
            bank = {}

            def p0():
                for g in range(2):
                    k.i('act', 'activation', reads=[S32, gC], writes=[S0g] if g == 0 else [], pwrites=[] if g == 0 else [S0g],
                        out=S0g[:, g, :], in_=S32[:, g, :], func=AF.Identity, scale=gC[:, g, ch:ch + 1])
                ps = nb()
                for g in range(2):
                    mmg(ps, g, [(kkt, kkt[:, g, ch, :], S16, S16[:, g, :]), (ab, ab[:, g, :], tm, tm[:, g * 3, :])], g == 0)
                cp(U0, U0[:], ps, ps[:, 0:2, :], scale=-1.0)

            def p1():
                ps = nb()
                for g in range(2):
                    mmg(ps, g, [(cur, cur[:, g, :], U0, U0[:, g, :])], g == 0)
                cp(UU, UU[:], ps, ps[:, 0:2, :])

            def p2():
                ps = nb()
                bank['ps'] = ps
                for g in range(2):
                    mmg(ps, g, [(S16, S16[:, g, :], rt, rt[:, g, ch, :]), (UU, UU[:, g, :], ab, ab[:, 2 + g, :]),
                                (tm, tm[:, g * 3, :], kr, kr[:, g, :])], g == 0)
                for g in range(2):
                    mmg(ps, 2 + g, [(tm, tm[:, g * 3 + 1, :], UU, UU[:, g, :]), (tm, tm[:, g * 3 + 2, :], tm, tm[:, g * 3, :])], False)

            def p3():
                ps = bank['ps']
                k.i('act', 'copy', reads=[ps], pwrites=[YV], out=YV[0:64, :, ch * 64:(ch + 1) * 64], in_=ps[0:64, 0:2, 0:64])
                k.i('dve', 'tensor_copy', reads=[ps], pwrites=[YV], out=YV[64:128, :, ch * 64:(ch + 1) * 64], in_=ps[64:128, 0:2, 64:128])
                for g in range(2):
                    k.i('dve', 'scalar_tensor_tensor', reads=[ps, gC, S0g], writes=[S32] if g == 0 else [], pwrites=[] if g == 0 else [S32],
                        out=S32[:, g, :], in0=ps[:, 2 + g, :], scalar=gC[:, g, ch:ch + 1], in1=S0g[:, g, :], op0=ALU.mult, op1=ALU.add)

            def p4():
                k.i('act', 'copy', reads=[S32], writes=[S16], out=S16[:], in_=S32[:])
            return [p0, p1, p2, p3, p4]

        pend = []
        for ch in range(8):
            par = ch % 2
            na, ab, kr, tm = NA[par], AB[par], KR[par], TM[par]
            ps = nb()
            for g in range(2):
                mmg(ps, 2 * g, [(bh, bh[:, g, ch, :], kkt, kkt[:, g, ch, :])], g == 0)
                mmg(ps, 2 * g + 1, [(kkt, kkt[:, g, ch, :], bh, bh[:, g, ch, :])], False)
            k.i('dve', 'tensor_tensor', reads=[ps, c.mb], writes=[na], out=na[:], in0=ps[:, :, :], in1=c.mb[:, 0:4, :], op=ALU.mult)
            ps = nb()
            for g in range(2):
                mmg(ps, g, [(kh, kh[:, g, ch, :], kkt, kkt[:, g, ch, :])], g == 0)
            for g in range(2):
                mmg(ps, 2 + g, [(bh, bh[:, g, ch, :], rt, rt[:, g, ch, :])], False)
            k.i('dve', 'tensor_tensor', reads=[ps, c.mb], writes=[ab], out=ab[:], in0=ps[:, :, :], in1=c.mb[:, 4:8, :], op=ALU.mult)
            ps = nb()
            for g in range(2):
                mmg(ps, g, [(kh, kh[:, g, ch, :], rt, rt[:, g, ch, :])], g == 0)
            k.i('dve', 'tensor_tensor', reads=[ps, c.mb], writes=[kr], out=kr[:], in0=ps[:, 0:2, :], in1=c.mb[:, 8:10, :], op=ALU.mult)
            first = True
            for g in range(2):
                for j_, src in enumerate((vb, bh, kh)):
                    k.i('pe', 'transpose', reads=[src, identb], writes=[pH] if first else [], pwrites=[] if first else [pH],
                        out=pH[:, g * 3 + j_, :], in_=src[:, g, ch, :], identity=identb[:])
                    first = False
            cp(tm, tm[:], pH, pH[:, 0:6, :])
            cur = TT[0]
            k.i('pool', 'tensor_tensor', reads=[identb, na], writes=[cur], out=cur[:], in0=identb[:].unsqueeze(1).to_broadcast([128, 2, 128]),
                in1=na[:, 0:4:2, :], op=ALU.subtract)
            Pc = [(na, na[:, 0, :]), (na, na[:, 2, :])]
            Qc = [(na, na[:, 1, :]), (na, na[:, 3, :])]
            for lev in range(5):
                pq = PQ[lev % 2]
                ps = nb()
                for g in range(2):
                    if lev < 4:
                        mmg(ps, 2 * g, [(Qc[g][0], Qc[g][1], Pc[g][0], Pc[g][1])], g == 0)
                    mmg(ps, 2 * g + 1, [(Pc[g][0], Pc[g][1], Qc[g][0], Qc[g][1])], (g == 0 and lev == 4))
                if lev < 4:
                    cp(pq, pq[:], ps, ps[:, :, :])
                else:
                    cp(pq, pq[:, 1:4:2, :], ps, ps[:, 1:4:2, :])
                Pn = [(pq, pq[:, 0, :]), (pq, pq[:, 2, :])]
                Qn = [(pq, pq[:, 1, :]), (pq, pq[:, 3, :])]
                nxt = TT[(lev + 1) % 2] if lev < 4 else TTF[par]
                ps = nb()
                for g in range(2):
                    mmg(ps, g, [(identb, identb[:], cur, cur[:, g, :]), (Qn[g][0], Qn[g][1], cur, cur[:, g, :])], g == 0)
                cp(nxt, nxt[:], ps, ps[:, 0:2, :])
                cur = nxt
                Pc, Qc = Pn, Qn
                if pend:
                    pend.pop(0)()
            while pend:
                pend.pop(0)()
            pend = dep_pieces(ch)
        while pend:
            pend.pop(0)()
        for g in range(2):
            t1, t2 = f32t['t1'], f32t['t2']
            pvg = lambda i, g=g: c.pv[:, i, g:g + 1]
            ps = pW[p.mi % len(pW)]; p.mi += 1
            k.i('pe', 'matmul', reads=[c.mf, YV], writes=[ps], out=ps[:, :], lhsT=BONES, rhs=YV[:, g, :], start=True, stop=True)
            k.i('dve', 'scalar_tensor_tensor', reads=[ps, YV], writes=[t1], out=t1[:], in0=ps[:, :], scalar=-1.0 / 64, in1=YV[:, g, :], op0=ALU.mult, op1=ALU.add)
            k.i('pool', 'tensor_tensor', reads=[t1], writes=[t2], out=t2[:], in0=t1[:], in1=t1[:], op=ALU.mult)
            ps = pW[p.mi % len(pW)]; p.mi += 1
            k.i('pe', 'matmul', reads=[c.mf, t2], writes=[ps], out=ps[:, :], lhsT=BONES, rhs=t2[:], start=True, stop=True)
            k.i('act', 'activation', reads=[ps], writes=[t2], out=t2[:], in_=ps[:, :], func=AF.Sqrt, scale=1.0 / 64, bias=GN_EPS)
            k.i('dve', 'reciprocal', reads=[t2], writes=[t2], out=t2[:], in_=t2[:])
            k.i('dve', 'tensor_tensor', reads=[t1, t2], writes=[t1], out=t1[:], in0=t1[:], in1=t2[:], op=ALU.mult)
            k.i('dve', 'tensor_scalar', reads=[t1, c.pv], writes=[t1], out=t1[:], in0=t1[:], scalar1=pvg(5), scalar2=pvg(6), op0=ALU.mult, op1=ALU.add)
            k.i('pool', 'tensor_tensor', reads=[t1, BONUS], writes=[t1], out=t1[:], in0=t1[:], in1=BONUS[:, g, :], op=ALU.add)
            ro = rwo[g]
            k.i('dve', 'tensor_tensor', reads=[t1, GATE], writes=[ro], out=ro[:], in0=t1[:], in1=GATE[:, g, :], op=ALU.mult)
            ow = out_rw[stile]
            k.dma('sp', ow, ow.ap()[g * 128:(g + 1) * 128, :], ro, ro[:], partial=True)
        if after_st is not None:
            after_st(stile)
    so = S0g
    ps = nb()
    for g in range(2):
        k.i('pe', 'transpose', reads=[S32, identf], writes=[ps] if g == 0 else [], pwrites=[] if g == 0 else [ps],
            out=ps[:, g, :], in_=S32[:, g, :], identity=identf[:])
    k.i('dve', 'tensor_copy', reads=[ps], writes=[so], out=so[:], in_=ps[:, 0:2, :])
    for g in range(2):
        for j2 in range(2):
            k.dma('sp', out_state, out_state.ap()[g * 2 + j2], so, so[j2 * 64:(j2 + 1) * 64, g, j2 * 64:(j2 + 1) * 64], partial=True)

from concourse.bass_utils import run_bass_kernel_spmd
import math

D = 2048
EPS = 1e-6
ATT_COLS = 1536
RW_COLS = 3360
RWH = 1152
N_CORES = 8
T_SEQ = 4096
NEG = -30000.0


class P:
    pass


def build(stages):
    nc = bass.Bass("TRN2", target_bir_lowering=False)
    k = K(nc)
    p = P()
    p.k = k
    inp = {}

    del _INPUT_NAMES[:]

    def I(name, shape, dt=F32):
        inp[name] = k.inp(name, shape, dt)
        _INPUT_NAMES.append(name)
        return inp[name]

    I('x_tok', [1168, D]); I('x_seq', [T_SEQ, D]); I('mem', [256, D])
    USED_LN = ('ln1', 'memn') + (('ln2',) if 'OX' in stages else ()) + (('ln3',) if 'M' in stages else ())
    for n in USED_LN:
        I(n, [D])
    I('qn', [64]); I('kn', [64]); I('xkn', [128])
    if 'OX' in stages:
        I('xqn', [128])
    I('w_att', [D, ATT_COLS]); I('w_rw', [D, RW_COLS]); I('w_rwh', [D, RWH]); I('w_xkv', [D, 1024])
    I('cs_all', [1168, 16]); I('ident', [128, 128]); I('masks', [2, 128, 256]); I('sinks', [16])
    I('cwk', [16, 128, 256]); I('cwv', [16, 128, 256])
    if 'SR' in stages:
        I('sh_s', [16, RW_COLS]); I('wkv_s', [16, 16, 64, 64]); I('mu_f', [RW_COLS]); I('pvf', [7, 1024])
        I('w2_f', [64, 1024]); I('a2_f', [64, 1024]); I('g2_f', [160, 1024]); I('lnwb_s', [2, 256, 64])
    if 'SA' in stages:
        I('sinks_s', [64, 4])
    if 'SX' in stages:
        I('cmk_s', [16, 256, 512]); I('cmv_s', [16, 256, 512])
    if 'M' in stages:
        I('w_rt', [D, 72]); I('b_rt', [72]); I('e_g', [64, D, 512]); I('e_u', [64, D, 512]); I('e_d', [64, 512, D]); I('iota64', [64])
    if 'OX' in stages:
        I('w_out', [D, D]); I('w_xq', [D, 512]); I('w_xo', [512, D]); I('ohj', [4])
    I('mu_h', [RWH]); I('pv_h', [7, 256]); I('w2_h', [64, 256]); I('a2_h', [64, 256]); I('g2_h', [160, 256]); I('rmask', [12, 128, 128])
    o_y = k.outp('o_y', [1040, D])
    o_wkp = k.outp('o_wkp', [128, 256]); o_wvp = k.outp('o_wvp', [128, 256])
    o_shp = k.outp('o_shp', [RWH]); o_wkvp = k.outp('o_wkvp', [4, 64, 64])
    o_mk = k.outp('o_mk', [256, 512]); o_mv = k.outp('o_mv', [256, 512])
    o_swk = k.outp('o_swk', [16, 128, 256]); o_swv = k.outp('o_swv', [16, 128, 256])
    o_wkvs = k.outp('o_wkvs', [16, 16, 64, 64]); o_shs = k.outp('o_shs', [16, RW_COLS])
    ATT_O = k.dram('ATT_O', [1152, 1024], BF16)
    RWIN = [k.dram('RWIN%d' % i, [256, 512], BF16) for i in range(8)]
    RWG = [k.dram('RWG%d' % i, [1024, 512], BF16) for i in range(8)]
    QS = k.dram('QS', [16, 1536])
    H2 = k.dram('H2d', [1152, D])
    RWS = k.dram('RWS', [16, 1024], BF16)
    SRd = k.dram('SRd', [16, RW_COLS])
    SV = k.dram('SV', [6, 16, 1024])
    YNd = k.dram('YNd', [256, 64])
    QXd = k.dram('QXd', [16, 512])
    OXd = k.dram('OXd', [16, 512])
    H = [k.ps('H%d' % i, [128, 8, 128], BF16) for i in range(3)]
    Fb = [k.ps('F%d' % i, [128, 512]) for i in range(5)]
    p.pM = Fb[0:2]
    p.pC = [ResView(Fb[2 + i], Fb[2 + i].h[:, :].rearrange("p (a b) -> p a b", a=4)) for i in range(3)]
    p.pH = H[2]
    pT = H[0:2]
    p.identf = k.sb('identf', [128, 128]); p.identb = k.sb('identb', [128, 128], BF16)
    identb = p.identb
    k.dma('sp', p.identf, p.identf[:], inp['ident'], inp['ident'].ap())
    k.i('dve', 'tensor_copy', reads=[p.identf], writes=[identb], out=identb[:], in_=p.identf[:])
    p.ones64 = k.sb('ones64', [128, 64])
    k.i('pool', 'memset', writes=[p.ones64], ap=p.ones64[:], constant=1.0)
    k.dma('sp', o_swk, o_swk.ap()[:, 0:127, :], inp['cwk'], inp['cwk'].ap()[:, 1:128, :])
    k.dma('sp', o_swv, o_swv.ap()[:, 0:127, :], inp['cwv'], inp['cwv'].ap()[:, 1:128, :])
    k.begin_fill(o_swk, o_swv)

    fb = P()

    def alloc_front():
        fb.xt = [k.sb('xt%d' % i, [128, D]) for i in range(2)]
        fb.ub = [k.sb('ub%d' % i, [128, D], BF16) for i in range(2)]
        fb.ssb = [k.sb('ss%d' % i, [128, 1]) for i in range(4)]
        fb.lnA = k.sb('lnA', [128, D])
    p.alloc_front = alloc_front
    p.fb = fb
    p.ti = 0
    p.mi = 0

    def load_ln(name):
        k.dma('sp', fb.lnA, fb.lnA[:], inp[name], inp[name].ap().partition_broadcast(128))
        return fb.lnA

    def bcast_load(name, n):
        t = k.sb('bc_' + name, [128, n])
        k.dma('sp', t, t[:], inp[name], inp[name].ap().partition_broadcast(128))
        return t

    def front(src, src_ap, n, lnb, uT, uT_ap_fn, x_keep=None):
        i = p.ti; p.ti += 1
        x = fb.xt[i % 2] if x_keep is None else x_keep
        u = fb.ub[i % 2]; ss = fb.ssb[i % 4]
        k.dma('sp', x, x[0:n, :], src, src_ap)
        k.i('act', 'activation', reads=[x], writes=[u, ss], out=u[0:n, :], in_=x[0:n, :], func=AF.Square, accum_out=ss[0:n, :])
        k.i('act', 'activation', reads=[ss], writes=[ss], out=ss[0:n, :], in_=ss[0:n, :], func=AF.Sqrt, scale=1.0 / D, bias=EPS)
        k.i('dve', 'reciprocal', reads=[ss], writes=[ss], out=ss[0:n, :], in_=ss[0:n, :])
        k.i('dve', 'scalar_tensor_tensor', reads=[x, ss, lnb], writes=[u], out=u[0:n, :], in0=x[0:n, :], scalar=ss[0:n, 0:1], in1=lnb[0:n, :],
            op0=ALU.mult, op1=ALU.mult)
        transpose16(u, n, uT, uT_ap_fn)

    def transpose16(u, n, uT, uT_ap_fn, nchunks=16):
        for half in range((nchunks + 7) // 8):
            pt = pT[half % 2]
            m = min(8, nchunks - half * 8)
            for cc in range(m):
                dc = half * 8 + cc
                k.i('pe', 'transpose', reads=[u, identb], writes=[pt] if cc == 0 else [], pwrites=[] if cc == 0 else [pt],
                    out=pt[:, cc, 0:n], in_=u[0:n, dc * 128:(dc + 1) * 128], identity=identb[0:n, 0:n])
            dst = uT_ap_fn(half)
            if half % 2 == 0:
                k.i('act', 'copy', reads=[pt], pwrites=[uT], out=dst, in_=pt[:, 0:m, 0:n])
            else:
                k.i('dve', 'tensor_copy', reads=[pt], pwrites=[uT], out=dst, in_=pt[:, 0:m, 0:n])

    def linear_tm(uT, uT_ap_fn, n, W, W_ap_fn, ncols, nk=16, ps=None):
        if ps is None:
            ps = p.pM[p.mi % len(p.pM)]
            p.mi += 1
        for dc in range(nk):
            k.i('pe', 'matmul', reads=[uT, W], writes=[ps] if dc == 0 else [], pwrites=[] if dc == 0 else [ps],
                out=ps[0:n, 0:ncols], lhsT=uT_ap_fn(dc), rhs=W_ap_fn(dc), start=(dc == 0), stop=(dc == nk - 1))
        return ps

    def load_w(dst, src_name, rows, c0, c1, step=512, q='pool'):
        k.begin_fill(dst)
        for c in range(c0, c1, step):
            ce = min(c + step, c1)
            k.dma(q, dst, dst[:, :, c - c0:ce - c0], inp[src_name],
                  inp[src_name].ap()[:, c:ce].rearrange("(c p) n -> p c n", p=128), partial=True)

    p.front = front; p.transpose16 = transpose16; p.linear_tm = linear_tm; p.load_w = load_w
    p.inp = inp; p.nc = nc; p.H = H; p.Fb = Fb; p.pT = pT; p.load_ln = load_ln; p.bcast_load = bcast_load
    p.out = dict(o_y=o_y, o_wkp=o_wkp, o_wvp=o_wvp, o_shp=o_shp, o_wkvp=o_wkvp, o_mk=o_mk, o_mv=o_mv, o_swk=o_swk, o_swv=o_swv,
                 o_wkvs=o_wkvs, o_shs=o_shs)
    p.H2t = [ResView(Res('H2t%d' % i, H2.h, 'dram'), H2.h) for i in range(9)]
    p.dr = dict(ATT_O=ATT_O, RWIN=RWIN, RWG=RWG, QS=QS, H2=H2, RWS=RWS, SRd=SRd, SV=SV, YNd=YNd, QXd=QXd, OXd=OXd)

    p.MEMKT = k.sb('MEMKT', [128, 4, 256], BF16)
    p.MEMV = k.sb('MEMV', [128, 2, 512], BF16)
    k.begin_fill(p.MEMKT, p.MEMV)
    mark0 = nc.sbuf_base

    def phase_end():
        k.barrier()
        nc.sbuf_base = mark0

    if 'A' in stages:
        alloc_front()
        phase_A(k, p)
        phase_end()
    if 'MEM' in stages:
        alloc_front()
        phase_MEM(k, p)
        phase_end()
    if 'R' in stages:
        alloc_front()
        ln1b = load_ln('ln1')
        WH = k.sb('WH', [128, 16, RWH], BF16)
        load_w(WH, 'w_rwh', D, 0, RWH, step=384)
        rwkv_consts(k, p, inp)
        def gather_st(st_):
            k.custom('pool', lambda e, st_=st_: e.collective_compute("AllGather", ALU.bypass, replica_groups=[[0, 1, 2, 3], [4, 5, 6, 7]],
                                                                   ins=[RWIN[st_].h.ap().opt()], outs=[RWG[st_].h.ap().opt()]),
                     1, RWG[st_], reads=[RWIN[st_]], writes=[RWG[st_]])
        rwkv_phase(k, p, inp, T_SEQ, inp['x_seq'], lambda t: inp['x_seq'].ap()[t * 128:(t + 1) * 128, :], WH, ln1b, front,
                   RWIN, o_wkvp, o_shp, after_st=gather_st)
        phase_end()
    if 'SR' in stages:
        phase_SR(k, p)
        phase_end()
    if 'SA' in stages:
        phase_SA(k, p)
        phase_end()
    if 'OX' in stages:
        alloc_front()
        phase_OX(k, p)
        phase_end()
    if 'MT' in stages:
        h2in = I('h2_in', [1152, D])
        for t_ in range(9):
            k.dma('sp', p.H2t[t_], H2.ap()[t_ * 128:(t_ + 1) * 128, :], h2in, h2in.ap()[t_ * 128:(t_ + 1) * 128, :])
    if 'SX' in stages:
        phase_SX(k, p)
        phase_end()
    if 'M' in stages:
        phase_M(k, p, mark0)
        phase_end()
    if 'DBG' in stages:
        o_dbg = k.outp('o_dbg', [1152, 1024], BF16)
        k.dma('sp', o_dbg, o_dbg.ap(), ATT_O, ATT_O.ap())
        o_dbg2 = k.outp('o_dbg2', [1024, T_SEQ], BF16)
        for i_ in range(8):
            k.dma('sp', o_dbg2, o_dbg2.ap()[:, i_ * 512:(i_ + 1) * 512], RWG[i_], RWG[i_].ap(), partial=True)
        o_dbg4 = k.outp('o_dbg4', [16, 1024], BF16)
        k.dma('sp', o_dbg4, o_dbg4.ap(), RWS, RWS.ap())
        o_dbg3 = k.outp('o_dbg3', [1152, D])
        k.dma('sp', o_dbg3, o_dbg3.ap(), H2, H2.ap())
    k.finish()
    print('instr counts', {e: len(k.prog[e]) for e in k.prog}, 'dma sems', k.n_dsem, flush=True)
    return nc


def head_norm(k, p, src, src2d, n, nh, hd, nwb, dst, dst2d, scr, cs=None):
    sq, st, xn, r1, r2, r3 = scr
    w = nh * hd
    k.i('act', 'activation', reads=[src], writes=[sq], out=sq[0:n, 0:w], in_=src2d, func=AF.Square)
    k.i('dve', 'tensor_reduce', reads=[sq], writes=[st], out=st[0:n, 0:nh], in_=sq[0:n, 0:w].rearrange("p (a b) -> p a b", a=nh), axis=AX.X, op=ALU.add)
    k.i('act', 'activation', reads=[st], writes=[st], out=st[0:n, 0:nh], in_=st[0:n, 0:nh], func=AF.Sqrt, scale=1.0 / hd, bias=EPS)
    k.i('dve', 'reciprocal', reads=[st], writes=[st], out=st[0:n, 0:nh], in_=st[0:n, 0:nh])
    k.i('dve', 'tensor_tensor', reads=[src, st], writes=[xn], out=xn[0:n, 0:w].rearrange("p (a b) -> p a b", a=nh),
        in0=src2d.rearrange("p (a b) -> p a b", a=nh), in1=st[0:n, 0:nh].unsqueeze(2).to_broadcast([n, nh, hd]), op=ALU.mult)
    d3 = dst2d.rearrange("p (a b) -> p a b", a=nh)
    k.i('dve', 'tensor_tensor', reads=[xn, nwb], writes=[dst], out=d3, in0=xn[0:n, 0:w].rearrange("p (a b) -> p a b", a=nh),
        in1=nwb[0:n, 0:hd].unsqueeze(1).to_broadcast([n, nh, hd]), op=ALU.mult)
    if cs is not None:
        x1 = d3[:, :, 0:8]; x2 = d3[:, :, 8:16]
        cosb = cs[0:n, 0:8].unsqueeze(1).to_broadcast([n, nh, 8])
        sinb = cs[0:n, 8:16].unsqueeze(1).to_broadcast([n, nh, 8])
        a1, a2, a3 = r1[0:n, 0:nh, :], r2[0:n, 0:nh, :], r3[0:n, 0:nh, :]
        k.i('dve', 'tensor_tensor', reads=[dst, cs], writes=[r1], out=a1, in0=x1, in1=cosb, op=ALU.mult)
        k.i('dve', 'tensor_tensor', reads=[dst, cs], writes=[r2], out=a2, in0=x2, in1=sinb, op=ALU.mult)
        k.i('dve', 'tensor_tensor', reads=[dst, cs], writes=[r3], out=a3, in0=x1, in1=sinb, op=ALU.mult)
        k.i('dve', 'tensor_tensor', reads=[r1, r2], writes=[r1], out=a1, in0=a1, in1=a2, op=ALU.subtract)
        k.i('dve', 'tensor_tensor', reads=[dst, cs], writes=[r2], out=a2, in0=x2, in1=cosb, op=ALU.mult)
        k.i('dve', 'tensor_tensor', reads=[r2, r3], writes=[dst], out=x2, in0=a2, in1=a3, op=ALU.add)
        k.i('dve', 'tensor_copy', reads=[r1], writes=[dst], out=x1, in_=a1)


def phase_A(k, p):
    inp, sb = p.inp, k.sb
    H, Fb = p.H, p.Fb
    identb = p.identb
    ln1b = p.load_ln('ln1')
    qnb = p.bcast_load('qn', 64); knb = p.bcast_load('kn', 64); sinkb = p.bcast_load('sinks', 16)
    maskb = sb('maskb', [128, 2, 256])
    k.dma('sp', maskb, maskb[:], inp['masks'], inp['masks'].ap().rearrange("m p n -> p m n"))
    WA = sb('WA', [128, 16, ATT_COLS], BF16)
    p.load_w(WA, 'w_att', D, 0, ATT_COLS)
    WS = sb('WSr', [128, 16, 480], BF16)
    scr = (sb('sq', [128, 1024]), sb('st', [128, 16]), sb('xn', [128, 1024]), sb('r1', [128, 16, 8]), sb('r2', [128, 16, 8]), sb('r3', [128, 16, 8]))
    uTs = [sb('uT%d' % i, [128, 16, 128], BF16) for i in range(2)]
    cst = [sb('cst%d' % i, [128, 16]) for i in range(2)]
    QF = sb('QF', [128, 1024]); QB = sb('QB', [128, 1024], BF16)
    KF = [sb('KF%d' % i, [128, 512]) for i in range(2)]
    kdup = sb('kdup', [128, 4, 2, 64], BF16)
    KT2 = [sb('KT2_%d' % i, [128, 4, 128], BF16) for i in range(2)]
    VB = [sb('VB%d' % i, [128, 256], BF16) for i in range(2)]
    qT = sb('qT', [128, 8, 128], BF16)
    SM = sb('SM', [128, 4, 256]); E = sb('E', [128, 4, 256], BF16); ET = sb('ET', [128, 8, 128], BF16)
    mx = sb('mx', [128, 4]); negm = sb('negm', [128, 4]); rs = sb('rs', [128, 4]); es = sb('es', [128, 4])
    ATTO = [sb('ATTO%d' % i, [128, 1024], BF16) for i in range(2)]
    srs = [sb('srs%d' % i, [16, 480]) for i in range(2)]
    HT = H[2]
    for t in range(10):
        n = 16 if t == 9 else 128
        r0 = t * 128
        slot = t % 2
        uT = uTs[t % 2]
        k.begin_fill(uT)
        cs = cst[t % 2]
        k.dma('sp', cs, cs[0:n, :], inp['cs_all'], inp['cs_all'].ap()[r0:r0 + n, :])
        p.front(inp['x_tok'], inp['x_tok'].ap()[r0:r0 + n, :], n, ln1b, uT, lambda half, uT=uT, n=n: uT[:, half * 8:(half + 1) * 8, 0:n])
        uf = lambda dc, uT=uT, n=n: uT[:, dc, 0:n]
        kf = KF[slot]
        if t >= 1:
            for hq in range(2):
                ps = p.linear_tm(uT, uf, n, WA, lambda dc, hq=hq: WA[:, dc, hq * 512:(hq + 1) * 512], 512)
                head_norm(k, p, ps, ps[0:n, 0:512], n, 8, 64, qnb, QF, QF[0:n, hq * 512:(hq + 1) * 512], scr, cs=cs)
            k.i('act', 'activation', reads=[QF], writes=[QB], out=QB[0:n, :], in_=QF[0:n, :], func=AF.Identity, scale=0.125)
        ps = p.linear_tm(uT, uf, n, WA, lambda dc: WA[:, dc, 1024:1536], 512)
        head_norm(k, p, ps, ps[0:n, 0:256], n, 4, 64, knb, kf, kf[0:n, 0:256], scr, cs=cs)
        k.i('dve', 'tensor_copy', reads=[ps], writes=[], pwrites=[kf], out=kf[0:n, 256:512], in_=ps[0:n, 256:512])
        if t == 8:
            k.dma('sp', p.out['o_wkp'], p.out['o_wkp'].ap(), kf, kf[:, 0:256])
            k.dma('sp', p.out['o_wvp'], p.out['o_wvp'].ap(), kf, kf[:, 256:512])
        if t == 9:
            k.dma('sp', p.out['o_swk'], p.out['o_swk'].ap()[:, 127, :], kf, kf[0:16, 0:256], partial=True)
            k.dma('sp', p.out['o_swv'], p.out['o_swv'].ap()[:, 127, :], kf, kf[0:16, 256:512], partial=True)
            QSd = p.dr['QS']
            k.dma('sp', QSd, QSd.ap()[:, 0:1024], QF, QF[0:16, :])
            k.dma('sp', QSd, QSd.ap()[:, 1024:1536], kf, kf[0:16, :], partial=True)
            for c in range(7):
                k.dma('pool', WS, WS[:], inp['w_rw'], inp['w_rw'].ap()[:, c * 480:(c + 1) * 480].rearrange("(c p) n -> p c n", p=128))
                ps = p.linear_tm(uT, uf, 16, WS, lambda dc: WS[:, dc, :], 480)
                sr = srs[c % 2]
                k.i('act' if c % 2 == 0 else 'dve', 'copy' if c % 2 == 0 else 'tensor_copy', reads=[ps], writes=[sr], out=sr[0:16, :], in_=ps[0:16, 0:480])
                k.dma('sp', p.out['o_shs'], p.out['o_shs'].ap()[:, c * 480:(c + 1) * 480], sr, sr[:], partial=(c > 0))
                k.dma('sp', p.dr['SRd'], p.dr['SRd'].ap()[:, c * 480:(c + 1) * 480], sr, sr[:], partial=True)
            continue
        k.i('dve', 'tensor_copy', reads=[kf], writes=[kdup], out=kdup[:],
            in_=kf[:, 0:256].rearrange("p (a b) -> p a b", a=4).unsqueeze(2).to_broadcast([128, 4, 2, 64]))
        k.i('act', 'copy', reads=[kf], writes=[VB[slot]], out=VB[slot][:], in_=kf[:, 256:512])
        for kh in range(4):
            k.i('pe', 'transpose', reads=[kdup, identb], writes=[HT] if kh == 0 else [], pwrites=[] if kh == 0 else [HT],
                out=HT[:, kh, :], in_=kdup[:, kh, :, :].rearrange("p a b -> p (a b)"), identity=identb[:])
        k.i('act', 'copy', reads=[HT], writes=[KT2[slot]], out=KT2[slot][:], in_=HT[:, 0:4, :])
        if t == 0:
            continue
        for m in range(8):
            k.i('pe', 'transpose', reads=[QB, identb], writes=[HT] if m == 0 else [], pwrites=[] if m == 0 else [HT],
                out=HT[:, m, :], in_=QB[:, m * 128:(m + 1) * 128], identity=identb[:])
        k.i('dve', 'tensor_copy', reads=[HT], writes=[qT], out=qT[:], in_=HT[:, :, :])
        mi_ = 1 if t == 1 else 0
        ao = ATTO[t % 2]
        k.begin_fill(ao)
        for kh in range(4):
            for hp in range(2):
                bank = Fb[2 + hp]
                first = True
                for g2 in range(2):
                    g = g2 * 2 + hp
                    h = 4 * kh + g
                    m = h // 2
                    for half, sl in ((0, 1 - slot), (1, slot)):
                        k.i('pe', 'matmul', reads=[qT, KT2[sl]], writes=[bank] if first else [], pwrites=[] if first else [bank],
                            out=bank[:, g2 * 256 + half * 128:g2 * 256 + (half + 1) * 128],
                            lhsT=qT[hp * 64:(hp + 1) * 64, m, :], rhs=KT2[sl][hp * 64:(hp + 1) * 64, kh, :], start=True, stop=True)
                        first = False
                k.i('dve', 'tensor_tensor', reads=[bank, maskb], writes=[SM] if hp == 0 else [], pwrites=[] if hp == 0 else [SM],
                    out=SM[:, hp:4:2, :], in0=bank[:, :].rearrange("p (a b) -> p a b", a=2),
                    in1=maskb[:, mi_, :].unsqueeze(1).to_broadcast([128, 2, 256]), op=ALU.add)
            k.i('dve', 'tensor_reduce', reads=[SM], writes=[mx], out=mx[:], in_=SM[:], axis=AX.X, op=ALU.max)
            k.i('dve', 'tensor_tensor', reads=[mx, sinkb], writes=[mx], out=mx[:], in0=mx[:], in1=sinkb[:, kh * 4:(kh + 1) * 4], op=ALU.max)
            k.i('dve', 'tensor_scalar', reads=[mx], writes=[negm], out=negm[:], in0=mx[:], scalar1=-1.0, scalar2=None, op0=ALU.mult)
            for g in range(4):
                k.i('act', 'activation', reads=[SM, negm], writes=[E, rs] if g == 0 else [], pwrites=[] if g == 0 else [E, rs],
                    out=E[:, g, :], in_=SM[:, g, :], func=AF.Exp, bias=negm[:, g:g + 1], accum_out=rs[:, g:g + 1])
            k.i('dve', 'tensor_tensor', reads=[sinkb, negm], writes=[es], out=es[:], in0=sinkb[:, kh * 4:(kh + 1) * 4], in1=negm[:], op=ALU.add)
            k.i('act', 'activation', reads=[es], writes=[es], out=es[:], in_=es[:], func=AF.Exp)
            k.i('dve', 'tensor_tensor', reads=[rs, es], writes=[rs], out=rs[:], in0=rs[:], in1=es[:], op=ALU.add)
            k.i('dve', 'reciprocal', reads=[rs], writes=[rs], out=rs[:], in_=rs[:])
            for g in range(4):
                for half in range(2):
                    i8 = g * 2 + half
                    k.i('pe', 'transpose', reads=[E, identb], writes=[HT] if i8 == 0 else [], pwrites=[] if i8 == 0 else [HT],
                        out=HT[:, i8, :], in_=E[:, g, half * 128:(half + 1) * 128], identity=identb[:])
            k.i('dve', 'tensor_copy', reads=[HT], writes=[ET], out=ET[:], in_=HT[:, :, :])
            ob = Fb[4]
            first = True
            for g in range(4):
                for half, sl in ((0, 1 - slot), (1, slot)):
                    k.i('pe', 'matmul', reads=[ET, VB[sl]], writes=[ob] if first else [], pwrites=[] if first else [ob],
                        out=ob[:, g * 64:(g + 1) * 64], lhsT=ET[:, g * 2 + half, :], rhs=VB[sl][:, kh * 64:(kh + 1) * 64],
                        start=(half == 0), stop=(half == 1))
                    first = False
            k.i('dve', 'tensor_tensor', reads=[ob, rs], pwrites=[ao], out=ao[:, kh * 256:(kh + 1) * 256].rearrange("p (a b) -> p a b", a=4),
                in0=ob[:, 0:256].rearrange("p (a b) -> p a b", a=4), in1=rs[:].unsqueeze(2).to_broadcast([128, 4, 64]), op=ALU.mult)
        AO = p.dr['ATT_O']
        k.dma('sp', AO, AO.ap()[(t - 1) * 128:t * 128, :], ao, ao[:], partial=True)


def phase_MEM(k, p):
    inp, sb = p.inp, k.sb
    memnb = p.load_ln('memn')
    xknb = p.bcast_load('xkn', 128)
    WB = sb('WB', [128, 16, 1024], BF16)
    p.load_w(WB, 'w_xkv', D, 0, 1024)
    scr = (sb('sq', [128, 1024]), sb('st', [128, 16]), sb('xn', [128, 1024]), sb('r1', [128, 16, 8]), sb('r2', [128, 16, 8]), sb('r3', [128, 16, 8]))
    uTs = [sb('uTm%d' % i, [128, 16, 128], BF16) for i in range(2)]
    mkv = [sb('mkv%d' % i, [128, 1024]) for i in range(2)]
    for t in range(2):
        uT = uTs[t]
        k.begin_fill(uT)
        p.front(inp['mem'], inp['mem'].ap()[t * 128:(t + 1) * 128, :], 128, memnb, uT, lambda half, uT=uT: uT[:, half * 8:(half + 1) * 8, :])
        uf = lambda dc, uT=uT: uT[:, dc, :]
        psk = p.linear_tm(uT, uf, 128, WB, lambda dc: WB[:, dc, 0:512], 512)
        psv = p.linear_tm(uT, uf, 128, WB, lambda dc: WB[:, dc, 512:1024], 512)
        m = mkv[t]
        head_norm(k, p, psk, psk[:, 0:512], 128, 4, 128, xknb, m, m[:, 0:512], scr)
        k.i('act', 'copy', reads=[psv], pwrites=[m], out=m[:, 512:1024], in_=psv[:, 0:512])
        k.i('act', 'copy', reads=[m], pwrites=[p.MEMV], out=p.MEMV[:, t, :], in_=m[:, 512:1024])
        kb = sb('kb%d' % t, [128, 512], BF16)
        k.i('dve', 'tensor_copy', reads=[m], writes=[kb], out=kb[:], in_=m[:, 0:512])
        HT = p.H[2]
        for hd in range(4):
            k.i('pe', 'transpose', reads=[kb, p.identb], writes=[HT] if hd == 0 else [], pwrites=[] if hd == 0 else [HT],
                out=HT[:, hd, :], in_=kb[:, hd * 128:(hd + 1) * 128], identity=p.identb[:])
        k.i('act', 'copy', reads=[HT], pwrites=[p.MEMKT], out=p.MEMKT[:, :, t * 128:(t + 1) * 128], in_=HT[:, 0:4, :])
        k.dma('sp', p.out['o_mk'], p.out['o_mk'].ap()[t * 128:(t + 1) * 128, :], m, m[:, 0:512], partial=(t > 0))
        k.dma('sp', p.out['o_mv'], p.out['o_mv'].ap()[t * 128:(t + 1) * 128, :], m, m[:, 512:1024], partial=(t > 0))


def phase_OX(k, p):
    inp, sb = p.inp, k.sb
    H, Fb, identb = p.H, p.Fb, p.identb
    ln2b = p.load_ln('ln2')
    xqnb = p.bcast_load('xqn', 128)
    WO = sb('WO', [128, 16, D], BF16)
    p.load_w(WO, 'w_out', D, 0, D)
    WQ = sb('WQ', [128, 16, 512], BF16)
    p.load_w(WQ, 'w_xq', D, 0, 512)
    WX = sb('WX', [128, 4, D], BF16)
    p.load_w(WX, 'w_xo', 512, 0, D)
    scr = (sb('sq', [128, 1024]), sb('st', [128, 16]), sb('xn', [128, 1024]), sb('r1', [128, 16, 8]), sb('r2', [128, 16, 8]), sb('r3', [128, 16, 8]))
    att_sb = [sb('att_sb%d' % i, [128, 1024], BF16) for i in range(2)]
    aT = [sb('aT%d' % i, [128, 8, 128], BF16) for i in range(2)]
    rwT = [sb('rwT%d' % i, [128, 8, 128], BF16) for i in range(2)]
    xres = [sb('xres0', [128, D])] * 2
    h1 = [sb('h1_0', [128, D])] * 2
    ub2 = sb('ub2', [128, D], BF16)
    ss2 = sb('ss2', [128, 1])
    uT2 = sb('uT2', [128, 16, 128], BF16)
    qx = sb('qx', [128, 512]); qxb = sb('qxb', [128, 512], BF16); qxT = sb('qxT', [128, 4, 128], BF16)
    Ex = sb('Ex', [128, 4, 256], BF16); ETx = sb('ETx', [128, 8, 128], BF16)
    mx = sb('mxx', [128, 4]); negm = sb('negmx', [128, 4]); rs = sb('rsx', [128, 4])
    oxb = sb('oxb', [128, 512], BF16); oxT = sb('oxT', [128, 4, 128], BF16)
    rws_sb = sb('rws_sb', [128, 1024], BF16)
    cand = [sb('cand%d' % q, [128, 8, 128], BF16) for q in range(4)]
    ohb = p.bcast_load('ohj', 4)
    HT = H[2]
    AO, RWG, H2, RWS = p.dr['ATT_O'], p.dr['RWG'], p.dr['H2'], p.dr['RWS']
    for t in range(9):
        n = 16 if t == 8 else 128
        b2 = t % 2
        xr_ = xres[b2]; a_sb = att_sb[b2]; aT_ = aT[b2]; rT = rwT[b2]; hh = h1[b2]
        xrow = 128 + t * 128
        k.dma('sp', xr_, xr_[0:n, :], inp['x_tok'], inp['x_tok'].ap()[xrow:xrow + n, :])
        k.dma('sp', a_sb, a_sb[0:n, :], AO, AO.ap()[t * 128:t * 128 + n, :])
        k.begin_fill(aT_)
        p.transpose16(a_sb, n, aT_, lambda half, aT_=aT_, n=n: aT_[:, 0:8, 0:n], nchunks=8)
        if t < 8:
            for q in range(4):
                rg = RWG[2 * q + t // 4]
                k.dma('sp', cand[q], cand[q][:], rg, rg.ap()[:, (t % 4) * 128:(t % 4 + 1) * 128].rearrange("(c p) t -> p c t", p=128))
            k.i('dve', 'tensor_scalar', reads=[cand[0], ohb], writes=[rT], out=rT[:], in0=cand[0][:], scalar1=ohb[:, 0:1], scalar2=None, op0=ALU.mult)
            for q in range(1, 4):
                k.i('dve', 'scalar_tensor_tensor', reads=[cand[q], ohb, rT], writes=[rT], out=rT[:], in0=cand[q][:], scalar=ohb[:, q:q + 1], in1=rT[:],
                    op0=ALU.mult, op1=ALU.add)
        else:
            k.dma('sp', rws_sb, rws_sb[0:16, :], RWS, RWS.ap())
            k.begin_fill(rT)
            p.transpose16(rws_sb, 16, rT, lambda half, rT=rT: rT[:, 0:8, 0:16], nchunks=8)
        for nchk in range(4):
            ps = Fb[nchk]
            for c in range(16):
                src = aT_ if c < 8 else rT
                k.i('pe', 'matmul', reads=[src, WO], writes=[ps] if c == 0 else [], pwrites=[] if c == 0 else [ps],
                    out=ps[0:n, :], lhsT=src[:, c % 8, 0:n], rhs=WO[:, c, nchk * 512:(nchk + 1) * 512], start=(c == 0), stop=(c == 15))
            k.i('dve', 'tensor_tensor', reads=[ps, xr_], writes=[hh] if nchk == 0 else [], pwrites=[] if nchk == 0 else [hh],
                out=hh[0:n, nchk * 512:(nchk + 1) * 512], in0=ps[0:n, :], in1=xr_[0:n, nchk * 512:(nchk + 1) * 512], op=ALU.add)
        k.i('act', 'activation', reads=[hh], writes=[ub2, ss2], out=ub2[0:n, :], in_=hh[0:n, :], func=AF.Square, accum_out=ss2[0:n, :])
        k.i('act', 'activation', reads=[ss2], writes=[ss2], out=ss2[0:n, :], in_=ss2[0:n, :], func=AF.Sqrt, scale=1.0 / D, bias=EPS)
        k.i('dve', 'reciprocal', reads=[ss2], writes=[ss2], out=ss2[0:n, :], in_=ss2[0:n, :])
        k.i('dve', 'scalar_tensor_tensor', reads=[hh, ss2, ln2b], writes=[ub2], out=ub2[0:n, :], in0=hh[0:n, :], scalar=ss2[0:n, 0:1],
            in1=ln2b[0:n, :], op0=ALU.mult, op1=ALU.mult)
        k.begin_fill(uT2)
        p.transpose16(ub2, n, uT2, lambda half, n=n: uT2[:, half * 8:(half + 1) * 8, 0:n])
        ps = p.linear_tm(uT2, lambda dc, n=n: uT2[:, dc, 0:n], n, WQ, lambda dc: WQ[:, dc, :], 512, ps=Fb[4])
        head_norm(k, p, ps, ps[0:n, 0:512], n, 4, 128, xqnb, qx, qx[0:n, :], scr)
        if t < 8:
            k.i('act', 'activation', reads=[qx], writes=[qxb], out=qxb[0:n, :], in_=qx[0:n, :], func=AF.Identity, scale=1.0 / math.sqrt(128.0))
            k.begin_fill(qxT)
            p.transpose16(qxb, n, qxT, lambda half, n=n: qxT[:, 0:4, 0:n], nchunks=4)
            for hp in range(2):
                bank = Fb[2 + hp]
                for h2_ in range(2):
                    hd = hp * 2 + h2_
                    k.i('pe', 'matmul', reads=[qxT, p.MEMKT], writes=[bank] if h2_ == 0 else [], pwrites=[] if h2_ == 0 else [bank],
                        out=bank[0:n, h2_ * 256:(h2_ + 1) * 256], lhsT=qxT[:, hd, 0:n], rhs=p.MEMKT[:, hd, :], start=True, stop=True)
                k.i('dve', 'tensor_reduce', reads=[bank], writes=[mx] if hp == 0 else [], pwrites=[] if hp == 0 else [mx],
                    out=mx[0:n, hp * 2:hp * 2 + 2], in_=bank[0:n, :].rearrange("p (a b) -> p a b", a=2), axis=AX.X, op=ALU.max)
            k.i('dve', 'tensor_scalar', reads=[mx], writes=[negm], out=negm[0:n, :], in0=mx[0:n, :], scalar1=-1.0, scalar2=None, op0=ALU.mult)
            for hd in range(4):
                bank = Fb[2 + hd // 2]
                k.i('act', 'activation', reads=[bank, negm], writes=[Ex, rs] if hd == 0 else [], pwrites=[] if hd == 0 else [Ex, rs],
                    out=Ex[0:n, hd, :], in_=bank[0:n, (hd % 2) * 256:(hd % 2 + 1) * 256], func=AF.Exp, bias=negm[0:n, hd:hd + 1],
                    accum_out=rs[0:n, hd:hd + 1])
            k.i('dve', 'reciprocal', reads=[rs], writes=[rs], out=rs[0:n, :], in_=rs[0:n, :])
            for hd in range(4):
                for half in range(2):
                    i8 = hd * 2 + half
                    k.i('pe', 'transpose', reads=[Ex, identb], writes=[HT] if i8 == 0 else [], pwrites=[] if i8 == 0 else [HT],
                        out=HT[:, i8, 0:n], in_=Ex[0:n, hd, half * 128:(half + 1) * 128], identity=identb[0:n, 0:n])
            k.i('dve', 'tensor_copy', reads=[HT], writes=[ETx], out=ETx[:, :, 0:n], in_=HT[:, :, 0:n])
            ob = Fb[4]
            first = True
            for hd in range(4):
                for half in range(2):
                    k.i('pe', 'matmul', reads=[ETx, p.MEMV], writes=[ob] if first else [], pwrites=[] if first else [ob],
                        out=ob[0:n, hd * 128:(hd + 1) * 128], lhsT=ETx[:, hd * 2 + half, 0:n], rhs=p.MEMV[:, half, hd * 128:(hd + 1) * 128],
                        start=(half == 0), stop=(half == 1))
                    first = False
            k.i('dve', 'tensor_tensor', reads=[ob, rs], writes=[oxb], out=oxb[0:n, :].rearrange("p (a b) -> p a b", a=4),
                in0=ob[0:n, :].rearrange("p (a b) -> p a b", a=4), in1=rs[0:n, :].unsqueeze(2).to_broadcast([n, 4, 128]), op=ALU.mult)
        else:
            k.dma('sp', p.dr['QXd'], p.dr['QXd'].ap(), qx, qx[0:16, :])
            k.dma('sp', p.H2t[8], H2.ap()[1024:1040, :], hh, hh[0:16, :])
            continue
        k.begin_fill(oxT)
        p.transpose16(oxb, n, oxT, lambda half, n=n: oxT[:, 0:4, 0:n], nchunks=4)
        for nchk in range(4):
            ps = Fb[nchk]
            for c in range(4):
                k.i('pe', 'matmul', reads=[oxT, WX], writes=[ps] if c == 0 else [], pwrites=[] if c == 0 else [ps],
                    out=ps[0:n, :], lhsT=oxT[:, c, 0:n], rhs=WX[:, c, nchk * 512:(nchk + 1) * 512], start=(c == 0), stop=(c == 3))
            k.i('dve', 'tensor_tensor', reads=[ps, hh], writes=[hh], out=hh[0:n, nchk * 512:(nchk + 1) * 512], in0=ps[0:n, :],
                in1=hh[0:n, nchk * 512:(nchk + 1) * 512], op=ALU.add)
        k.dma('sp', p.H2t[t], H2.ap()[t * 128:t * 128 + n, :], hh, hh[0:n, :])


def sample_xattn(k, p, qx, oxb):
    k.i('pool', 'memset', writes=[oxb], ap=oxb[0:16, :], constant=0.0)


def phase_M(k, p, mark0):
    inp, sb, nc = p.inp, k.sb, p.nc
    H, Fb, identb, identf = p.H, p.Fb, p.identb, p.identf
    H2 = p.dr['H2']
    CAP = 64
    U16 = sb('U16', [128, 9, D], BF16)
    GATE = sb('GATEm', [128, 9, 64]); ASG = sb('ASG', [128, 9, 64], BF16); RANK = sb('RANK', [128, 9, 64])
    iota = p.bcast_load('iota64', 64)
    onesb = sb('onesb', [128, 128], BF16)
    trib = sb('trib', [128, 128], BF16)
    k.i('pool', 'memset', writes=[onesb], ap=onesb[:], constant=1.0)
    k.i('pool', 'memset', writes=[U16], ap=U16[:], constant=0.0)
    k.i('pool', 'memset', writes=[GATE], ap=GATE[:], constant=0.0)
    k.i('pool', 'memset', writes=[ASG], ap=ASG[:], constant=0.0)
    trif = sb('trif', [128, 128])
    k.dma('sp', trif, trif[:], inp['rmask'], inp['rmask'].ap()[0])
    k.i('dve', 'tensor_copy', reads=[trif], writes=[trib], out=trib[:], in_=trif[:])
    mark1 = nc.sbuf_base
    p.alloc_front()
    fb = p.fb
    ln3b = p.load_ln('ln3')
    WR = sb('WR', [128, 16, 72])
    k.dma('sp', WR, WR[:], inp['w_rt'], inp['w_rt'].ap().rearrange("(c p) n -> p c n", p=128))
    brt = p.bcast_load('b_rt', 72)
    u32 = sb('u32', [128, D]); uT32 = sb('uT32', [128, 16, 128])
    LG = sb('LG', [128, 72])
    sm = {n: sb('m_' + n, [128, 8]) for n in ['ohg', 'e8', 'oh1', 'oh2', 'e8b', 'g8', 't8']}
    s1 = {n: sb('m1_' + n, [128, 1]) for n in ['gmax', 'ngmax', 'se', 'm1', 'm2', 'w1', 'w2']}
    sel3 = sb('sel3', [128, 8, 8])
    k.begin_fill(U16, GATE, ASG)
    for t in range(9):
        n = 16 if t == 8 else 128
        x = fb.xt[t % 2]; ss = fb.ssb[t % 4]; ubf = fb.ub[t % 2]
        k.dma('sp', x, x[0:n, :], p.H2t[t], H2.ap()[t * 128:t * 128 + n, :])
        k.i('act', 'activation', reads=[x], writes=[ubf, ss], out=ubf[0:n, :], in_=x[0:n, :], func=AF.Square, accum_out=ss[0:n, :])
        k.i('act', 'activation', reads=[ss], writes=[ss], out=ss[0:n, :], in_=ss[0:n, :], func=AF.Sqrt, scale=1.0 / D, bias=EPS)
        k.i('dve', 'reciprocal', reads=[ss], writes=[ss], out=ss[0:n, :], in_=ss[0:n, :])
        k.i('dve', 'scalar_tensor_tensor', reads=[x, ss, ln3b], writes=[u32], out=u32[0:n, :], in0=x[0:n, :], scalar=ss[0:n, 0:1], in1=ln3b[0:n, :],
            op0=ALU.mult, op1=ALU.mult)
        k.i('act', 'copy', reads=[u32], pwrites=[U16], out=U16[0:n, t, :], in_=u32[0:n, :])
        k.begin_fill(uT32)
        for q4 in range(4):
            bank = Fb[q4 % 2]
            for c4 in range(4):
                dc = q4 * 4 + c4
                k.i('pe', 'transpose', reads=[u32, identf], writes=[bank] if c4 == 0 else [], pwrites=[] if c4 == 0 else [bank],
                    out=bank[:, c4 * 128:c4 * 128 + n], in_=u32[0:n, dc * 128:(dc + 1) * 128], identity=identf[0:n, 0:n])
            k.i('act' if q4 % 2 == 0 else 'dve', 'copy' if q4 % 2 == 0 else 'tensor_copy', reads=[bank], pwrites=[uT32],
                out=uT32[:, q4 * 4:(q4 + 1) * 4, 0:n], in_=bank[:, :].rearrange("p (a b) -> p a b", a=4)[:, :, 0:n])
        ps = Fb[4]
        for dc in range(16):
            k.i('pe', 'matmul', reads=[uT32, WR], writes=[ps] if dc == 0 else [], pwrites=[] if dc == 0 else [ps],
                out=ps[0:n, 0:72], lhsT=uT32[:, dc, 0:n], rhs=WR[:, dc, :], start=(dc == 0), stop=(dc == 15))
        k.i('dve', 'tensor_tensor', reads=[ps, brt], writes=[LG], out=LG[0:n, :], in0=ps[0:n, 0:72], in1=brt[0:n, :], op=ALU.add)
        a_ = lambda r: r[0:n, :]
        gl = LG[0:n, 0:8]
        k.i('dve', 'tensor_reduce', reads=[LG], writes=[s1['gmax']], out=a_(s1['gmax']), in_=gl, axis=AX.X, op=ALU.max)
        k.i('dve', 'tensor_scalar', reads=[LG, s1['gmax']], writes=[sm['ohg']], out=a_(sm['ohg']), in0=gl, scalar1=s1['gmax'][0:n, 0:1], scalar2=None, op0=ALU.is_equal)
        k.i('dve', 'tensor_scalar', reads=[s1['gmax']], writes=[s1['ngmax']], out=a_(s1['ngmax']), in0=a_(s1['gmax']), scalar1=-1.0, scalar2=None, op0=ALU.mult)
        k.i('act', 'activation', reads=[LG, s1['ngmax']], writes=[sm['t8'], s1['se']], out=a_(sm['t8']), in_=gl, func=AF.Exp, bias=s1['ngmax'][0:n, 0:1],
            accum_out=a_(s1['se']))
        k.i('dve', 'reciprocal', reads=[s1['se']], writes=[s1['se']], out=a_(s1['se']), in_=a_(s1['se']))
        k.i('dve', 'tensor_tensor', reads=[LG, sm['ohg']], writes=[sel3], out=sel3[0:n, :, :], in0=LG[0:n, 8:72].rearrange("p (g e) -> p g e", g=8),
            in1=sm['ohg'][0:n, :].unsqueeze(2).to_broadcast([n, 8, 8]), op=ALU.mult)
        k.i('dve', 'tensor_reduce', reads=[sel3], writes=[sm['e8']], out=a_(sm['e8']), in_=sel3[0:n, :, :].rearrange("p g e -> p e g"), axis=AX.X, op=ALU.add)
        k.i('dve', 'tensor_reduce', reads=[sm['e8']], writes=[s1['m1']], out=a_(s1['m1']), in_=a_(sm['e8']), axis=AX.X, op=ALU.max)
        k.i('dve', 'tensor_scalar', reads=[sm['e8'], s1['m1']], writes=[sm['oh1']], out=a_(sm['oh1']), in0=a_(sm['e8']), scalar1=s1['m1'][0:n, 0:1], scalar2=None, op0=ALU.is_equal)
        k.i('dve', 'scalar_tensor_tensor', reads=[sm['oh1'], sm['e8']], writes=[sm['e8b']], out=a_(sm['e8b']), in0=a_(sm['oh1']), scalar=-1e30, in1=a_(sm['e8']),
            op0=ALU.mult, op1=ALU.add)
        k.i('dve', 'tensor_reduce', reads=[sm['e8b']], writes=[s1['m2']], out=a_(s1['m2']), in_=a_(sm['e8b']), axis=AX.X, op=ALU.max)
        k.i('dve', 'tensor_scalar', reads=[sm['e8b'], s1['m2']], writes=[sm['oh2']], out=a_(sm['oh2']), in0=a_(sm['e8b']), scalar1=s1['m2'][0:n, 0:1], scalar2=None, op0=ALU.is_equal)
        k.i('dve', 'tensor_tensor', reads=[s1['m2'], s1['m1']], writes=[s1['w1']], out=a_(s1['w1']), in0=a_(s1['m2']), in1=a_(s1['m1']), op=ALU.subtract)
        k.i('act', 'activation', reads=[s1['w1']], writes=[s1['w1']], out=a_(s1['w1']), in_=a_(s1['w1']), func=AF.Exp)
        k.i('dve', 'tensor_scalar', reads=[s1['w1']], writes=[s1['w1']], out=a_(s1['w1']), in0=a_(s1['w1']), scalar1=1.0, scalar2=None, op0=ALU.add)
        k.i('dve', 'reciprocal', reads=[s1['w1']], writes=[s1['w1']], out=a_(s1['w1']), in_=a_(s1['w1']))
        k.i('dve', 'tensor_scalar', reads=[s1['w1']], writes=[s1['w2']], out=a_(s1['w2']), in0=a_(s1['w1']), scalar1=-1.0, scalar2=1.0, op0=ALU.mult, op1=ALU.add)
        k.i('dve', 'tensor_tensor', reads=[s1['w1'], s1['se']], writes=[s1['w1']], out=a_(s1['w1']), in0=a_(s1['w1']), in1=a_(s1['se']), op=ALU.mult)
        k.i('dve', 'tensor_tensor', reads=[s1['w2'], s1['se']], writes=[s1['w2']], out=a_(s1['w2']), in0=a_(s1['w2']), in1=a_(s1['se']), op=ALU.mult)
        k.i('dve', 'tensor_scalar', reads=[sm['oh1'], s1['w1']], writes=[sm['g8']], out=a_(sm['g8']), in0=a_(sm['oh1']), scalar1=s1['w1'][0:n, 0:1], scalar2=None, op0=ALU.mult)
        k.i('dve', 'scalar_tensor_tensor', reads=[sm['oh2'], s1['w2'], sm['g8']], writes=[sm['g8']], out=a_(sm['g8']), in0=a_(sm['oh2']), scalar=s1['w2'][0:n, 0:1],
            in1=a_(sm['g8']), op0=ALU.mult, op1=ALU.add)
        k.i('dve', 'tensor_tensor', reads=[sm['oh1'], sm['oh2']], writes=[sm['t8']], out=a_(sm['t8']), in0=a_(sm['oh1']), in1=a_(sm['oh2']), op=ALU.add)
        k.i('dve', 'tensor_tensor', reads=[sm['ohg'], sm['g8']], pwrites=[GATE], out=GATE[0:n, t, :].rearrange("p (g e) -> p g e", g=8),
            in0=sm['ohg'][0:n, :].unsqueeze(2).to_broadcast([n, 8, 8]), in1=sm['g8'][0:n, :].unsqueeze(1).to_broadcast([n, 8, 8]), op=ALU.mult)
        k.i('dve', 'tensor_tensor', reads=[sm['ohg'], sm['t8']], pwrites=[ASG], out=ASG[0:n, t, :].rearrange("p (g e) -> p g e", g=8),
            in0=sm['ohg'][0:n, :].unsqueeze(2).to_broadcast([n, 8, 8]), in1=sm['t8'][0:n, :].unsqueeze(1).to_broadcast([n, 8, 8]), op=ALU.mult)
    k.begin_fill(RANK)
    for t in range(9):
        ps = Fb[t % 2]
        k.i('pe', 'matmul', reads=[trib, ASG], writes=[ps], out=ps[:, 0:64], lhsT=trib[:], rhs=ASG[:, t, :], start=True, stop=(t == 0))
        for t2 in range(t):
            k.i('pe', 'matmul', reads=[onesb, ASG], pwrites=[ps], out=ps[:, 0:64], lhsT=onesb[:], rhs=ASG[:, t2, :], start=False, stop=(t2 == t - 1))
        k.i('dve', 'scalar_tensor_tensor', reads=[ps, ASG], pwrites=[RANK], out=RANK[:, t, :], in0=ps[:, 0:64], scalar=1.0, in1=ASG[:, t, :], op0=ALU.add, op1=ALU.mult)
    k.i('dve', 'tensor_scalar', reads=[RANK], writes=[RANK], out=RANK[:], in0=RANK[:], scalar1=-1.0, scalar2=None, op0=ALU.add)
    k.barrier()
    nc.sbuf_base = mark1
    WG = [sb('WG%d' % i, [128, 16, 512], BF16) for i in range(2)]
    WU = [sb('WU%d' % i, [128, 16, 512], BF16) for i in range(2)]
    WD = [sb('WD%d' % i, [128, 4, D], BF16) for i in range(2)]
    SEL = sb('SEL', [128, 9, 128], BF16); SELG = sb('SELG', [128, 9, 128], BF16)
    SELGT = [sb('SELGT%d' % i, [128, 9, 128], BF16) for i in range(4)]
    UT = sb('UTp', [128, 16, 128], BF16)
    HB = sb('HB', [128, 512], BF16); HTs = sb('HTs', [128, 4, 128], BF16)
    sg = sb('sg', [128, 512])
    YG = sb('YG', [128, 4, D], BF16)
    OUTt = sb('OUTt', [128, D])
    iota3 = iota[:, :].unsqueeze(1).to_broadcast([128, 9, 64])

    def load_expert(e):
        b = e % 2
        k.dma('pool', WG[b], WG[b][:], inp['e_g'], inp['e_g'].ap()[e].rearrange("(p c) n -> p c n", c=16))
        k.dma('pool', WU[b], WU[b][:], inp['e_u'], inp['e_u'].ap()[e].rearrange("(p c) n -> p c n", c=16))
        k.dma('pool', WD[b], WD[b][:], inp['e_d'], inp['e_d'].ap()[e].rearrange("(p c) n -> p c n", c=4))
    load_expert(0)
    for grp in range(8):
        k.begin_fill(YG)
        for pr in range(4):
            e0 = grp * 8 + pr * 2
            for e2 in range(2):
                e = e0 + e2
                k.i('dve', 'tensor_tensor', reads=[iota, RANK], writes=[SEL] if e2 == 0 else [], pwrites=[] if e2 == 0 else [SEL],
                    out=SEL[:, :, e2 * 64:(e2 + 1) * 64], in0=iota3, in1=RANK[:, :, e:e + 1].to_broadcast([128, 9, 64]), op=ALU.is_equal)
                k.i('dve', 'tensor_tensor', reads=[SEL, GATE], writes=[SELG] if e2 == 0 else [], pwrites=[] if e2 == 0 else [SELG],
                    out=SELG[:, :, e2 * 64:(e2 + 1) * 64], in0=SEL[:, :, e2 * 64:(e2 + 1) * 64], in1=GATE[:, :, e:e + 1].to_broadcast([128, 9, 64]), op=ALU.mult)
            sgt = SELGT[pr]
            k.begin_fill(sgt)
            for t in range(9):
                hb_ = H[t // 8]
                k.i('pe', 'transpose', reads=[SELG, identb], writes=[hb_] if t % 8 == 0 else [], pwrites=[] if t % 8 == 0 else [hb_],
                    out=hb_[:, t % 8, :], in_=SELG[:, t, :], identity=identb[:])
            k.i('act', 'copy', reads=[H[0]], pwrites=[sgt], out=sgt[:, 0:8, :], in_=H[0][:, :, :])
            k.i('dve', 'tensor_copy', reads=[H[1]], pwrites=[sgt], out=sgt[:, 8:9, :], in_=H[1][:, 0:1, :])
            k.begin_fill(UT)
            for q4 in range(4):
                bank = Fb[q4]
                for c4 in range(4):
                    dc = q4 * 4 + c4
                    for t in range(9):
                        k.i('pe', 'matmul', reads=[U16, SEL], writes=[bank] if (c4 == 0 and t == 0) else [], pwrites=[] if (c4 == 0 and t == 0) else [bank],
                            out=bank[:, c4 * 128:(c4 + 1) * 128], lhsT=U16[:, t, :].rearrange("q (p c) -> q c p", c=16)[:, dc, :], rhs=SEL[:, t, :], start=(t == 0), stop=(t == 8))
                k.i('act' if q4 % 2 == 0 else 'dve', 'copy' if q4 % 2 == 0 else 'tensor_copy', reads=[bank], pwrites=[UT],
                    out=UT[:, q4 * 4:(q4 + 1) * 4, :], in_=bank[:, :].rearrange("p (a b) -> p a b", a=4))
            for e2 in range(2):
                e = e0 + e2
                b = e % 2
                if e + 1 < 64:
                    load_expert(e + 1)
                lo, hi = e2 * 64, (e2 + 1) * 64
                tp = (0, lo)
                for (W_, bank) in ((WG[b], Fb[0]), (WU[b], Fb[1])):
                    for dc in range(16):
                        k.i('pe', 'matmul', reads=[UT, W_], writes=[bank] if dc == 0 else [], pwrites=[] if dc == 0 else [bank],
                            out=bank[lo:hi, :], lhsT=UT[:, dc, lo:hi], rhs=W_[:, dc, :], start=(dc == 0), stop=(dc == 15), tile_position=tp)
                k.i('act', 'activation', reads=[Fb[0]], writes=[sg], out=sg[lo:hi, :], in_=Fb[0][lo:hi, :], func=AF.Silu)
                k.i('dve', 'tensor_tensor', reads=[sg, Fb[1]], writes=[HB], out=HB[lo:hi, :], in0=sg[lo:hi, :], in1=Fb[1][lo:hi, :], op=ALU.mult)
                hb_ = H[2]
                for fc in range(4):
                    k.i('pe', 'transpose', reads=[HB, identb], writes=[hb_] if fc == 0 else [], pwrites=[] if fc == 0 else [hb_],
                        out=hb_[:, fc, 0:64], in_=HB[lo:hi, :].rearrange("q (p c) -> q c p", c=4)[:, fc, :], identity=identb[lo:hi, lo:hi])
                k.i('act', 'copy', reads=[hb_], writes=[HTs], out=HTs[:, :, 0:64], in_=hb_[:, 0:4, 0:64])
                for half in range(2):
                    for nc2 in range(2):
                        bank = Fb[2 + nc2]
                        ncol = half * 2 + nc2
                        for fc in range(4):
                            k.i('pe', 'matmul', reads=[HTs, WD[b]], writes=[bank] if fc == 0 else [], pwrites=[] if fc == 0 else [bank],
                                out=bank[lo:hi, :], lhsT=HTs[:, fc, 0:64], rhs=WD[b][:, fc, ncol * 512:(ncol + 1) * 512], start=(fc == 0), stop=(fc == 3),
                                tile_position=tp)
                        k.i('act' if nc2 == 0 else 'dve', 'copy' if nc2 == 0 else 'tensor_copy', reads=[bank], pwrites=[YG],
                            out=YG[lo:hi, pr, ncol * 512:(ncol + 1) * 512], in_=bank[lo:hi, :])
        for t in range(9):
            n = 16 if t == 8 else 128
            k.begin_fill(OUTt)
            for nchk in range(4):
                bank = Fb[nchk]
                for pr in range(4):
                    k.i('pe', 'matmul', reads=[SELGT[pr], YG], writes=[bank] if pr == 0 else [], pwrites=[] if pr == 0 else [bank],
                        out=bank[:, :], lhsT=SELGT[pr][:, t, :], rhs=YG[:, pr, nchk * 512:(nchk + 1) * 512], start=(pr == 0), stop=(pr == 3))
                k.i('act' if nchk % 2 == 0 else 'dve', 'copy' if nchk % 2 == 0 else 'tensor_copy', reads=[bank],
                    pwrites=[OUTt], out=OUTt[:, nchk * 512:(nchk + 1) * 512], in_=bank[:, :])
            k.dma('pool', p.H2t[t], H2.ap()[t * 128:t * 128 + n, :], OUTt, OUTt[0:n, :], accum_op=ALU.add)
    for t in range(9):
        n = 16 if t == 8 else 128
        k.dma('sp', p.out['o_y'], p.out['o_y'].ap()[t * 128:t * 128 + n, :], p.H2t[t], H2.ap()[t * 128:t * 128 + n, :], partial=True)


def phase_SR(k, p):
    inp, sb = p.inp, k.sb
    Fb, H, identb, identf = p.Fb, p.H, p.identb, p.identf
    N = 16
    SRd, SV, YNd, RWS = p.dr['SRd'], p.dr['SV'], p.dr['YNd'], p.dr['RWS']
    sr = sb('s_sr', [N, RW_COLS]); pv = sb('s_prev', [N, RW_COLS]); mub = sb('s_mu', [N, RW_COLS])
    k.dma('sp', sr, sr[:], SRd, SRd.ap())
    k.dma('sp', pv, pv[:], inp['sh_s'], inp['sh_s'].ap())
    k.dma('sp', mub, mub[:], inp['mu_f'], inp['mu_f'].ap().partition_broadcast(N))
    k.i('dve', 'tensor_tensor', reads=[pv, sr], writes=[pv], out=pv[:], in0=pv[:], in1=sr[:], op=ALU.subtract)
    k.i('dve', 'tensor_tensor', reads=[pv, mub], writes=[pv], out=pv[:], in0=pv[:], in1=mub[:], op=ALU.mult)
    k.i('dve', 'tensor_tensor', reads=[pv, sr], writes=[pv], out=pv[:], in0=pv[:], in1=sr[:], op=ALU.add)
    xm = pv
    xr, xk, xv = xm[:, 0:1024], xm[:, 1024:2048], xm[:, 2048:3072]
    pvb = sb('s_pvb', [N, 7, 1024])
    k.dma('sp', pvb, pvb[:], inp['pvf'], inp['pvf'].ap().partition_broadcast(N))
    lf = sb('s_lf', [128, 4, 1024]); lwb = sb('s_lwb', [128, 4, 1024], BF16)
    k.i('pool', 'memset', writes=[lf], ap=lf[:], constant=0.0)
    k.dma('sp', lf, lf[0:64, 0, :], inp['w2_f'], inp['w2_f'].ap())
    k.dma('sp', lf, lf[0:64, 1, :], inp['a2_f'], inp['a2_f'].ap(), partial=True)
    k.dma('sp', lf, lf[:, 2, :], inp['g2_f'], inp['g2_f'].ap()[0:128, :], partial=True)
    k.dma('sp', lf, lf[0:32, 3, :], inp['g2_f'], inp['g2_f'].ap()[128:160, :], partial=True)
    k.i('dve', 'tensor_copy', reads=[lf], writes=[lwb], out=lwb[:], in_=lf[:])
    li = sb('s_li', [N, 4, 128], BF16)
    k.i('pool', 'memset', writes=[li], ap=li[:], constant=0.0)
    k.i('act', 'activation', reads=[xm], writes=[li], out=li[:, 0, 0:64], in_=xm[:, 3072:3136], func=AF.Tanh)
    k.i('act', 'copy', reads=[xm], pwrites=[li], out=li[:, 1, 0:64], in_=xm[:, 3136:3200])
    k.i('act', 'activation', reads=[xm], pwrites=[li], out=li[:, 2, :], in_=xm[:, 3200:3328], func=AF.Sigmoid)
    k.i('act', 'activation', reads=[xm], pwrites=[li], out=li[:, 3, 0:32], in_=xm[:, 3328:3360], func=AF.Sigmoid)
    liT = sb('s_liT', [128, 4, N], BF16)
    HT = H[2]
    for i in range(4):
        k.i('pe', 'transpose', reads=[li, identb], writes=[HT] if i == 0 else [], pwrites=[] if i == 0 else [HT],
            out=HT[:, i, 0:N], in_=li[:, i, :], identity=identb[0:N, 0:N])
    k.i('act', 'copy', reads=[HT], writes=[liT], out=liT[:], in_=HT[:, 0:4, 0:N])
    lw = sb('s_lw', [N, 1024]); a = sb('s_a', [N, 1024]); gt = sb('s_g', [N, 1024])
    for hh in range(2):
        cs_ = slice(hh * 512, (hh + 1) * 512)
        ps = Fb[0]
        k.i('pe', 'matmul', reads=[liT, lwb], writes=[ps], out=ps[0:N, :], lhsT=liT[0:64, 0, :], rhs=lwb[0:64, 0, cs_], start=True, stop=True)
        k.i('dve', 'tensor_tensor', reads=[ps, pvb], writes=[lw] if hh == 0 else [], pwrites=[] if hh == 0 else [lw], out=lw[:, cs_], in0=ps[0:N, :], in1=pvb[:, 0, cs_], op=ALU.add)
        ps = Fb[1]
        k.i('pe', 'matmul', reads=[liT, lwb], writes=[ps], out=ps[0:N, :], lhsT=liT[0:64, 1, :], rhs=lwb[0:64, 1, cs_], start=True, stop=True)
        k.i('dve', 'tensor_tensor', reads=[ps, pvb], writes=[a] if hh == 0 else [], pwrites=[] if hh == 0 else [a], out=a[:, cs_], in0=ps[0:N, :], in1=pvb[:, 1, cs_], op=ALU.add)
        ps = Fb[2]
        k.i('pe', 'matmul', reads=[liT, lwb], writes=[ps], out=ps[0:N, :], lhsT=liT[:, 2, :], rhs=lwb[:, 2, cs_], start=True, stop=False)
        k.i('pe', 'matmul', reads=[liT, lwb], pwrites=[ps], out=ps[0:N, :], lhsT=liT[0:32, 3, :], rhs=lwb[0:32, 3, cs_], start=False, stop=True)
        k.i('dve', 'tensor_copy', reads=[ps], writes=[gt] if hh == 0 else [], pwrites=[] if hh == 0 else [gt], out=gt[:, cs_], in_=ps[0:N, :])
    k.i('act', 'activation', reads=[lw], writes=[lw], out=lw[:], in_=lw[:], func=AF.Sigmoid)
    k.i('act', 'activation', reads=[lw], writes=[lw], out=lw[:], in_=lw[:], func=AF.Exp, scale=-C_DEC)
    k.i('act', 'activation', reads=[a], writes=[a], out=a[:], in_=a[:], func=AF.Sigmoid)
    VT = sb('s_VT', [N, 6, 1024])
    t1 = sb('s_t1', [N, 1024]); st = sb('s_st', [N, 16])
    k.begin_fill(VT)
    k.i('act', 'copy', reads=[xm], pwrites=[VT], out=VT[:, 0, :], in_=xr)
    k.i('act', 'copy', reads=[lw], pwrites=[VT], out=VT[:, 1, :], in_=lw[:])
    k.i('act', 'copy', reads=[xm], pwrites=[VT], out=VT[:, 3, :], in_=xv)
    k.i('dve', 'tensor_tensor', reads=[xm, pvb], writes=[t1], out=t1[:], in0=xk, in1=pvb[:, 2, :], op=ALU.mult)
    sq = sb('s_sq', [N, 1024])
    k.i('dve', 'tensor_tensor', reads=[t1], writes=[sq], out=sq[:], in0=t1[:], in1=t1[:], op=ALU.mult)
    k.i('dve', 'tensor_reduce', reads=[sq], writes=[st], out=st[:], in_=sq[:].rearrange("p (h n) -> p h n", h=16), axis=AX.X, op=ALU.add)
    k.i('act', 'activation', reads=[st], writes=[st], out=st[:], in_=st[:], func=AF.Sqrt)
    k.i('dve', 'tensor_scalar', reads=[st], writes=[st], out=st[:], in0=st[:], scalar1=1e-12, scalar2=None, op0=ALU.max)
    k.i('dve', 'reciprocal', reads=[st], writes=[st], out=st[:], in_=st[:])
    k.i('dve', 'tensor_tensor', reads=[t1, st], pwrites=[VT], out=VT[:, 4, :].rearrange("p (h n) -> p h n", h=16),
        in0=t1[:].rearrange("p (h n) -> p h n", h=16), in1=st[:].unsqueeze(2).to_broadcast([N, 16, 64]), op=ALU.mult)
    k.i('dve', 'tensor_scalar', reads=[a], writes=[sq], out=sq[:], in0=a[:], scalar1=-1.0, scalar2=None, op0=ALU.add)
    k.i('dve', 'tensor_tensor', reads=[sq, pvb], writes=[sq], out=sq[:], in0=sq[:], in1=pvb[:, 3, :], op=ALU.mult)
    k.i('dve', 'tensor_scalar', reads=[sq], writes=[sq], out=sq[:], in0=sq[:], scalar1=1.0, scalar2=None, op0=ALU.add)
    k.i('dve', 'tensor_tensor', reads=[sq, xm], pwrites=[VT], out=VT[:, 2, :], in0=sq[:], in1=xk, op=ALU.mult)
    k.i('dve', 'tensor_tensor', reads=[VT, a], pwrites=[VT], out=VT[:, 5, :], in0=VT[:, 4, :], in1=a[:], op=ALU.mult)
    bonus = sb('s_bonus', [N, 1024])
    k.i('dve', 'tensor_tensor', reads=[xm, VT], writes=[t1], out=t1[:], in0=xr, in1=VT[:, 2, :], op=ALU.mult)
    k.i('dve', 'tensor_tensor', reads=[t1, pvb], writes=[t1], out=t1[:], in0=t1[:], in1=pvb[:, 4, :], op=ALU.mult)
    k.i('dve', 'tensor_reduce', reads=[t1], writes=[st], out=st[:], in_=t1[:].rearrange("p (h n) -> p h n", h=16), axis=AX.X, op=ALU.add)
    k.i('dve', 'tensor_tensor', reads=[xm, st], writes=[bonus], out=bonus[:].rearrange("p (h n) -> p h n", h=16),
        in0=xv.rearrange("p (h n) -> p h n", h=16), in1=st[:].unsqueeze(2).to_broadcast([N, 16, 64]), op=ALU.mult)
    for i in range(6):
        k.dma('sp', SV, SV.ap()[i], VT, VT[:, i, :], partial=True)
    VEC = sb('s_VEC', [128, 2, 6, 64])
    k.begin_fill(VEC)
    for r in range(2):
        for i in range(6):
            k.dma('sp', VEC, VEC[:, r, i, :], SV, SV.ap()[i].rearrange("b (h n) -> (b h) n", n=64)[r * 128:(r + 1) * 128, :], partial=True)
    lnwb = sb('s_lnwb', [128, 2, 2, 64])
    k.dma('sp', lnwb, lnwb[:], inp['lnwb_s'], inp['lnwb_s'].ap().rearrange("w (r p) n -> p w r n", p=128))
    YN = sb('s_YN', [128, 2, 64])
    k.begin_fill(YN)
    wkv_in = inp['wkv_s'].ap().rearrange("b h v k -> (b h) (v k)")
    wkv_out = p.out['o_wkvs'].ap().rearrange("b h v k -> (b h) (v k)")
    S = sb('s_S', [128, 64, 64]); T1 = sb('s_T', [128, 64, 64])
    for r in range(2):
        sa = sb('s_sa%d' % r, [128, 64]); y = sb('s_y%d' % r, [128, 64]); ms = sb('s_ms%d' % r, [128, 4])
        k.dma('sp', S, S[:].rearrange("p v k -> p (v k)"), inp['wkv_s'], wkv_in[r * 128:(r + 1) * 128, :])
        vec = lambda i, r=r: VEC[:, r, i, :]
        bk = lambda i, r=r: VEC[:, r, i, :].unsqueeze(1).to_broadcast([128, 64, 64])
        bv_ = lambda ap: ap.unsqueeze(2).to_broadcast([128, 64, 64])
        eng2 = 'pool'
        k.i(eng2, 'tensor_tensor', reads=[S, VEC], writes=[T1], out=T1[:], in0=S[:], in1=bk(4), op=ALU.mult)
        k.i('dve', 'tensor_reduce', reads=[T1], writes=[sa], out=sa[:], in_=T1[:], axis=AX.X, op=ALU.add)
        k.i('dve', 'tensor_scalar', reads=[sa], writes=[sa], out=sa[:], in0=sa[:], scalar1=-1.0, scalar2=None, op0=ALU.mult)
        k.i('dve', 'tensor_tensor', reads=[S, VEC], writes=[S], out=S[:], in0=S[:], in1=bk(1), op=ALU.mult)
        k.i(eng2, 'tensor_tensor', reads=[sa, VEC], writes=[T1], out=T1[:], in0=bv_(sa[:]), in1=bk(5), op=ALU.mult)
        k.i('dve', 'tensor_tensor', reads=[S, T1], writes=[S], out=S[:], in0=S[:], in1=T1[:], op=ALU.add)
        k.i(eng2, 'tensor_tensor', reads=[VEC], writes=[T1], out=T1[:], in0=bv_(vec(3)), in1=bk(2), op=ALU.mult)
        k.i('dve', 'tensor_tensor', reads=[S, T1], writes=[S], out=S[:], in0=S[:], in1=T1[:], op=ALU.add)
        k.dma('sp', p.out['o_wkvs'], wkv_out[r * 128:(r + 1) * 128, :], S, S[:].rearrange("p v k -> p (v k)"), partial=True)
        k.i(eng2, 'tensor_tensor', reads=[S, VEC], writes=[T1], out=T1[:], in0=S[:], in1=bk(0), op=ALU.mult)
        k.i('dve', 'tensor_reduce', reads=[T1], writes=[y], out=y[:], in_=T1[:], axis=AX.X, op=ALU.add)
        k.i('dve', 'tensor_reduce', reads=[y], writes=[ms], out=ms[:, 0:1], in_=y[:], axis=AX.X, op=ALU.add)
        k.i('dve', 'tensor_scalar', reads=[ms], writes=[ms], out=ms[:, 0:1], in0=ms[:, 0:1], scalar1=-1.0 / 64, scalar2=None, op0=ALU.mult)
        k.i('dve', 'tensor_scalar', reads=[y, ms], writes=[y], out=y[:], in0=y[:], scalar1=ms[:, 0:1], scalar2=None, op0=ALU.add)
        k.i('dve', 'tensor_tensor', reads=[y], writes=[sa], out=sa[:], in0=y[:], in1=y[:], op=ALU.mult)
        k.i('dve', 'tensor_reduce', reads=[sa], writes=[ms], out=ms[:, 1:2], in_=sa[:], axis=AX.X, op=ALU.add)
        k.i('act', 'activation', reads=[ms], writes=[ms], out=ms[:, 1:2], in_=ms[:, 1:2], func=AF.Sqrt, scale=1.0 / 64, bias=GN_EPS)
        k.i('dve', 'reciprocal', reads=[ms], writes=[ms], out=ms[:, 1:2], in_=ms[:, 1:2])
        k.i('dve', 'scalar_tensor_tensor', reads=[y, ms, lnwb], writes=[y], out=y[:], in0=y[:], scalar=ms[:, 1:2], in1=lnwb[:, 0, r, :], op0=ALU.mult, op1=ALU.mult)
        k.i('dve', 'tensor_tensor', reads=[y, lnwb], pwrites=[YN], out=YN[:, r, :], in0=y[:], in1=lnwb[:, 1, r, :], op=ALU.add)
        k.dma('sp', YNd, YNd.ap()[r * 128:(r + 1) * 128, :], YN, YN[:, r, :], partial=True)
    ynt = sb('s_ynt', [N, 1024]); rwb = sb('s_rwb', [N, 1024], BF16)
    k.dma('sp', ynt, ynt[:], YNd, YNd.ap().rearrange("(b h) n -> b (h n)", h=16))
    k.i('dve', 'tensor_tensor', reads=[ynt, bonus], writes=[ynt], out=ynt[:], in0=ynt[:], in1=bonus[:], op=ALU.add)
    k.i('dve', 'tensor_tensor', reads=[ynt, gt], writes=[rwb], out=rwb[:], in0=ynt[:], in1=gt[:], op=ALU.mult)
    k.dma('sp', RWS, RWS.ap(), rwb, rwb[:])


def phase_SA(k, p):
    inp, sb = p.inp, k.sb
    QS, AO = p.dr['QS'], p.dr['ATT_O']
    q = sb('a_q', [64, 4, 64]); kn_ = sb('a_kn', [64, 64]); vn = sb('a_vn', [64, 64])
    KC = sb('a_KC', [64, 128, 64]); VC = sb('a_VC', [64, 128, 64]); TT = sb('a_T', [64, 128, 64])
    sinks = sb('a_sinks', [64, 4])
    k.dma('sp', sinks, sinks[:], inp['sinks_s'], inp['sinks_s'].ap())
    k.begin_fill(q, kn_, vn, KC, VC)
    for kh in range(4):
        ps_ = slice(kh * 16, (kh + 1) * 16)
        k.dma('sp', q, q[ps_, :, :].rearrange("p g d -> p (g d)"), QS, QS.ap()[:, kh * 256:(kh + 1) * 256], partial=True)
        k.dma('sp', kn_, kn_[ps_, :], QS, QS.ap()[:, 1024 + kh * 64:1024 + (kh + 1) * 64], partial=True)
        k.dma('sp', vn, vn[ps_, :], QS, QS.ap()[:, 1280 + kh * 64:1280 + (kh + 1) * 64], partial=True)
        k.dma('sp', KC, KC[ps_, :, :], inp['cwk'], inp['cwk'].ap()[:, :, kh * 64:(kh + 1) * 64], partial=True)
        k.dma('sp', VC, VC[ps_, :, :], inp['cwv'], inp['cwv'].ap()[:, :, kh * 64:(kh + 1) * 64], partial=True)
    sc = sb('a_sc', [64, 4, 129]); E = sb('a_E', [64, 4, 129])
    mx = sb('a_mx', [64, 4]); negm = sb('a_negm', [64, 4]); rs = sb('a_rs', [64, 4]); es = sb('a_es', [64, 4])
    t4 = sb('a_t4', [64, 4, 64])
    k.begin_fill(sc)
    for g in range(4):
        k.i('pool', 'tensor_tensor', reads=[KC, q], writes=[TT], out=TT[:], in0=KC[:], in1=q[:, g, :].unsqueeze(1).to_broadcast([64, 128, 64]), op=ALU.mult)
        k.i('dve', 'tensor_reduce', reads=[TT], pwrites=[sc], out=sc[:, g, 0:128], in_=TT[:], axis=AX.X, op=ALU.add)
    k.i('dve', 'tensor_tensor', reads=[q, kn_], writes=[t4], out=t4[:], in0=q[:], in1=kn_[:].unsqueeze(1).to_broadcast([64, 4, 64]), op=ALU.mult)
    k.i('dve', 'tensor_reduce', reads=[t4], pwrites=[sc], out=sc[:, :, 128], in_=t4[:], axis=AX.X, op=ALU.add)
    k.i('dve', 'tensor_reduce', reads=[sc], writes=[mx], out=mx[:], in_=sc[:], axis=AX.X, op=ALU.max)
    k.i('dve', 'tensor_scalar', reads=[mx], writes=[mx], out=mx[:], in0=mx[:], scalar1=0.125, scalar2=None, op0=ALU.mult)
    k.i('dve', 'tensor_tensor', reads=[mx, sinks], writes=[mx], out=mx[:], in0=mx[:], in1=sinks[:], op=ALU.max)
    k.i('dve', 'tensor_scalar', reads=[mx], writes=[negm], out=negm[:], in0=mx[:], scalar1=-1.0, scalar2=None, op0=ALU.mult)
    for g in range(4):
        k.i('act', 'activation', reads=[sc, negm], writes=[E, rs] if g == 0 else [], pwrites=[] if g == 0 else [E, rs],
            out=E[:, g, :], in_=sc[:, g, :], func=AF.Exp, scale=0.125, bias=negm[:, g:g + 1], accum_out=rs[:, g:g + 1])
    k.i('dve', 'tensor_tensor', reads=[sinks, negm], writes=[es], out=es[:], in0=sinks[:], in1=negm[:], op=ALU.add)
    k.i('act', 'activation', reads=[es], writes=[es], out=es[:], in_=es[:], func=AF.Exp)
    k.i('dve', 'tensor_tensor', reads=[rs, es], writes=[rs], out=rs[:], in0=rs[:], in1=es[:], op=ALU.add)
    k.i('dve', 'reciprocal', reads=[rs], writes=[rs], out=rs[:], in_=rs[:])
    o = sb('a_o', [64, 4, 64]); ob = sb('a_ob', [64, 4, 64], BF16)
    k.begin_fill(o)
    for g in range(4):
        k.i('pool', 'tensor_tensor', reads=[VC, E], writes=[TT], out=TT[:], in0=VC[:], in1=E[:, g, 0:128].unsqueeze(2).to_broadcast([64, 128, 64]), op=ALU.mult)
        k.i('dve', 'tensor_reduce', reads=[TT], pwrites=[o], out=o[:, g, :], in_=TT[:].rearrange("p s d -> p d s"), axis=AX.X, op=ALU.add)
        k.i('dve', 'scalar_tensor_tensor', reads=[vn, E, o], pwrites=[o], out=o[:, g, :], in0=vn[:], scalar=E[:, g, 128:129], in1=o[:, g, :], op0=ALU.mult, op1=ALU.add)
    k.i('dve', 'tensor_tensor', reads=[o, rs], writes=[ob], out=ob[:], in0=o[:], in1=rs[:].unsqueeze(2).to_broadcast([64, 4, 64]), op=ALU.mult)
    for kh in range(4):
        k.dma('sp', AO, AO.ap()[1024:1040, kh * 256:(kh + 1) * 256], ob, ob[kh * 16:(kh + 1) * 16, :, :].rearrange("p g d -> p (g d)"), partial=True)


def phase_SX(k, p):
    inp, sb = p.inp, k.sb
    H, Fb, identb = p.H, p.Fb, p.identb
    QXd, OXd, H2 = p.dr['QXd'], p.dr['OXd'], p.dr['H2']
    SC = 1.0 / math.sqrt(128.0)
    q = sb('x_q', [64, 128])
    k.begin_fill(q)
    for h in range(4):
        k.dma('sp', q, q[h * 16:(h + 1) * 16, :], QXd, QXd.ap()[:, h * 128:(h + 1) * 128], partial=True)
    KCH = [sb('x_K%d' % i, [64, 32, 128]) for i in range(2)]
    VCH = [sb('x_V%d' % i, [64, 32, 128]) for i in range(2)]
    TT = sb('x_T', [64, 32, 128])
    sc = sb('x_sc', [64, 256]); E = sb('x_E', [64, 256])
    mx = sb('x_mx', [64, 1]); rs = sb('x_rs', [64, 1])
    o = sb('x_o', [64, 128]); part = sb('x_part', [64, 128])
    k.begin_fill(sc)
    for c8 in range(8):
        kc = KCH[c8 % 2]
        k.begin_fill(kc)
        for h in range(4):
            k.dma('sp', kc, kc[h * 16:(h + 1) * 16, :, :], inp['cmk_s'], inp['cmk_s'].ap()[:, c8 * 32:(c8 + 1) * 32, h * 128:(h + 1) * 128], partial=True)
        k.i('pool', 'tensor_tensor', reads=[kc, q], writes=[TT], out=TT[:], in0=kc[:], in1=q[:].unsqueeze(1).to_broadcast([64, 32, 128]), op=ALU.mult)
        k.i('dve', 'tensor_reduce', reads=[TT], pwrites=[sc], out=sc[:, c8 * 32:(c8 + 1) * 32], in_=TT[:], axis=AX.X, op=ALU.add)
    k.i('dve', 'tensor_reduce', reads=[sc], writes=[mx], out=mx[:], in_=sc[:], axis=AX.X, op=ALU.max)
    k.i('dve', 'tensor_scalar', reads=[mx], writes=[mx], out=mx[:], in0=mx[:], scalar1=-SC, scalar2=None, op0=ALU.mult)
    k.i('act', 'activation', reads=[sc, mx], writes=[E, rs], out=E[:], in_=sc[:], func=AF.Exp, scale=SC, bias=mx[:, 0:1], accum_out=rs[:, 0:1])
    k.i('dve', 'reciprocal', reads=[rs], writes=[rs], out=rs[:], in_=rs[:])
    for c8 in range(8):
        vc = VCH[c8 % 2]
        k.begin_fill(vc)
        for h in range(4):
            k.dma('sp', vc, vc[h * 16:(h + 1) * 16, :, :], inp['cmv_s'], inp['cmv_s'].ap()[:, c8 * 32:(c8 + 1) * 32, h * 128:(h + 1) * 128], partial=True)
        k.i('pool', 'tensor_tensor', reads=[vc, E], writes=[TT], out=TT[:], in0=vc[:], in1=E[:, c8 * 32:(c8 + 1) * 32].unsqueeze(2).to_broadcast([64, 32, 128]), op=ALU.mult)
        if c8 == 0:
            k.i('dve', 'tensor_reduce', reads=[TT], writes=[o], out=o[:], in_=TT[:].rearrange("p m d -> p d m"), axis=AX.X, op=ALU.add)
        else:
            k.i('dve', 'tensor_reduce', reads=[TT], writes=[part], out=part[:], in_=TT[:].rearrange("p m d -> p d m"), axis=AX.X, op=ALU.add)
            k.i('dve', 'tensor_tensor', reads=[o, part], writes=[o], out=o[:], in0=o[:], in1=part[:], op=ALU.add)
    k.i('dve', 'tensor_scalar', reads=[o, rs], writes=[o], out=o[:], in0=o[:], scalar1=rs[:, 0:1], scalar2=None, op0=ALU.mult)
    for h in range(4):
        k.dma('sp', OXd, OXd.ap()[:, h * 128:(h + 1) * 128], o, o[h * 16:(h + 1) * 16, :], partial=True)
    WX = sb('x_WX', [128, 4, D], BF16)
    p.load_w(WX, 'w_xo', 512, 0, D)
    oxs = sb('x_oxs', [16, 512]); oxb = sb('x_oxb', [16, 512], BF16); oxT = sb('x_oxT', [128, 4, 16], BF16)
    hh = sb('x_hh', [16, D])
    k.dma('sp', oxs, oxs[:], OXd, OXd.ap())
    k.dma('sp', hh, hh[:], p.H2t[8], H2.ap()[1024:1040, :])
    k.i('act', 'copy', reads=[oxs], writes=[oxb], out=oxb[:], in_=oxs[:])
    k.begin_fill(oxT)
    p.transpose16(oxb, 16, oxT, lambda half: oxT[:, 0:4, 0:16], nchunks=4)
    for nchk in range(4):
        ps = Fb[nchk]
        for c in range(4):
            k.i('pe', 'matmul', reads=[oxT, WX], writes=[ps] if c == 0 else [], pwrites=[] if c == 0 else [ps],
                out=ps[0:16, :], lhsT=oxT[:, c, 0:16], rhs=WX[:, c, nchk * 512:(nchk + 1) * 512], start=(c == 0), stop=(c == 3))
        k.i('dve', 'tensor_tensor', reads=[ps, hh], writes=[hh], out=hh[:, nchk * 512:(nchk + 1) * 512], in0=ps[0:16, :],
            in1=hh[:, nchk * 512:(nchk + 1) * 512], op=ALU.add)
    k.dma('sp', p.H2t[8], H2.ap()[1024:1040, :], hh, hh[:])


def rope_table(pos):
    inv = (np.float32(500000.0) ** (-np.arange(8, dtype=np.float32) * np.float32(2.0) / np.float32(16.0))).astype(np.float32)
    ang = pos.astype(np.float32)[:, None] * inv[None, :]
    return np.concatenate([np.cos(ang), np.sin(ang)], axis=1).astype(np.float32)


def rmasks():
    m = np.zeros((12, 128, 128), np.float32)
    i = np.arange(128)
    tu_s = (i[:, None] < i[None, :]); tl_s = (i[:, None] > i[None, :]); tu_i = (i[:, None] <= i[None, :])
    blk = (i[:, None] // 64 == i[None, :] // 64)
    for n_, mm_ in enumerate([tu_s, tl_s, tu_s, tl_s, tu_s, tu_s, tu_i, tu_i, tu_i, tu_i, blk, blk]):
        m[n_] = mm_
    return m


def swa_masks(j):
    qi = np.arange(128)[:, None]; si = np.arange(256)[None, :]
    rel = 128 + qi - si
    band = (rel >= 0) & (rel <= 128)
    mn = np.where(band, 0.0, NEG).astype(np.float32)
    mf = np.where(band & (si >= 128), 0.0, NEG).astype(np.float32) if j == 0 else mn
    return np.stack([mn, mf])


def own_cols(j):
    cols = [np.arange(part * 1024 + 256 * j, part * 1024 + 256 * j + 256) for part in range(3)]
    cols.append(np.arange(3072, 3360))
    return np.concatenate(cols)


_INPUT_NAMES = []


def nc_input_names(nc):
    return list(_INPUT_NAMES)


import os as _os
STAGES = ('A', 'MEM', 'R', 'SR', 'SA', 'OX', 'SX', 'M')


def kernel(**inp):
    f = lambda a: np.ascontiguousarray(a, dtype=np.float32)
    x_prompt = inp['x_prompt']; x_sample = inp['x_sample']
    w_in = inp['w_in'][0]
    nc = build(STAGES)
    w_rt = f(np.concatenate([inp['router_group_w'][0], inp['router_expert_w'][0]], axis=1))
    b_rt = f(np.concatenate([inp['router_group_b'][0], inp['router_expert_b'][0]]))
    e_g = f(inp['exp_w_gate'][0]); e_u = f(inp['exp_w_up'][0]); e_d = f(inp['exp_w_down'][0])
    pvf = np.stack([inp[n][0] for n in ['rw_w0', 'rw_a0', 'rw_k_k', 'rw_k_a', 'rw_r_k', 'rw_ln_w', 'rw_ln_b']]).astype(np.float32)
    lnwb_s = np.stack([np.tile(inp['rw_ln_w'][0].reshape(16, 64), (16, 1)), np.tile(inp['rw_ln_b'][0].reshape(16, 64), (16, 1))]).astype(np.float32)
    sinks_s = np.repeat(inp['attn_sinks'][0].reshape(4, 4), 16, axis=0).astype(np.float32)
    in_maps = []
    for c in range(N_CORES):
        b, j = c // 4, c % 4
        cols = own_cols(j)
        wh = np.zeros((D, RWH), np.float32); wh[:, :cols.size] = w_in[:, ATT_COLS + cols]
        mu = np.zeros(RWH, np.float32); mu[:cols.size] = inp['rw_mu'][0][cols]
        hs = slice(256 * j, 256 * j + 256)
        pv = np.stack([inp[n][0][hs] for n in ['rw_w0', 'rw_a0', 'rw_k_k', 'rw_k_a', 'rw_r_k', 'rw_ln_w', 'rw_ln_b']]).astype(np.float32)
        x_tok = np.zeros((1168, D), np.float32)
        if j > 0:
            x_tok[0:128] = x_prompt[b, 1024 * j - 128:1024 * j]
        x_tok[128:1152] = x_prompt[b, 1024 * j:1024 * j + 1024]
        x_tok[1152:1168] = x_sample[16 * c:16 * c + 16, 0]
        pos = np.concatenate([np.arange(1024 * j - 128, 1024 * j + 1024), np.full(16, 16384)])
        m = {
            'x_tok': x_tok, 'x_seq': f(x_prompt[b]), 'mem': f(inp['mem_prompt'][b]),
            'ln1': f(inp['ln1_w'][0]), 'ln2': f(inp['ln2_w'][0]), 'ln3': f(inp['ln3_w'][0]), 'memn': f(inp['mem_norm_w'][0]),
            'qn': f(inp['q_norm_w'][0]), 'kn': f(inp['k_norm_w'][0]), 'xqn': f(inp['xq_norm_w'][0]), 'xkn': f(inp['xk_norm_w'][0]),
            'w_att': f(w_in[:, :ATT_COLS]), 'w_rw': f(w_in[:, ATT_COLS:]), 'w_rwh': wh, 'w_xkv': f(inp['xkv_w'][0]),
            'cs_all': rope_table(pos), 'ident': np.eye(128, dtype=np.float32), 'masks': swa_masks(j), 'sinks': f(inp['attn_sinks'][0]),
            'cwk': f(inp['cache_win_k'][0, 16 * c:16 * c + 16].reshape(16, 128, 256)),
            'cwv': f(inp['cache_win_v'][0, 16 * c:16 * c + 16].reshape(16, 128, 256)),
            'mu_h': mu, 'pv_h': pv, 'w2_h': f(inp['rw_w2'][0][:, hs]), 'a2_h': f(inp['rw_a2'][0][:, hs]), 'g2_h': f(inp['rw_g2'][0][:, hs]),
            'rmask': rmasks(),
            'cmk_s': f(inp['cache_mem_k'][0, 16 * c:16 * c + 16].reshape(16, 256, 512)), 'cmv_s': f(inp['cache_mem_v'][0, 16 * c:16 * c + 16].reshape(16, 256, 512)),
            'sh_s': f(inp['state_shift'][0, 16 * c:16 * c + 16]), 'wkv_s': f(inp['state_wkv'][0, 16 * c:16 * c + 16]), 'mu_f': f(inp['rw_mu'][0]),
            'pvf': pvf, 'w2_f': f(inp['rw_w2'][0]), 'a2_f': f(inp['rw_a2'][0]), 'g2_f': f(inp['rw_g2'][0]), 'lnwb_s': lnwb_s, 'sinks_s': sinks_s,
            'ohj': np.eye(4, dtype=np.float32)[j], 'w_rt': w_rt, 'b_rt': b_rt, 'e_g': e_g, 'e_u': e_u, 'e_d': e_d, 'iota64': np.arange(64, dtype=np.float32), 'w_out': f(inp['w_out'][0]), 'w_xq': f(inp['xq_w'][0]), 'w_xo': f(inp['xo_w'][0]),
        }
        in_maps.append(m)
    names = set(nc_input_names(nc))
    in_maps = [{kk: vv for kk, vv in m.items() if kk in names} for m in in_maps]
    res = run_bass_kernel_spmd(nc, in_maps, core_ids=list(range(N_CORES)))
    R = res.results
    global _DBG
    _DBG = R
    y_prompt = np.stack([np.concatenate([R[4 * b + j]['o_y'][0:1024] for j in range(4)]) for b in range(2)])
    y_sample = np.concatenate([R[c]['o_y'][1024:1040] for c in range(8)])[:, None, :]
    wkp = np.stack([R[4 * b + 3]['o_wkp'].reshape(128, 4, 64) for b in range(2)])[None]
    wvp = np.stack([R[4 * b + 3]['o_wvp'].reshape(128, 4, 64) for b in range(2)])[None]
    wkv_p = np.stack([np.concatenate([R[4 * b + j]['o_wkvp'] for j in range(4)]) for b in range(2)])[None]
    shp = np.zeros((1, 2, RW_COLS), np.float32)
    for b in range(2):
        for j in range(4):
            o = R[4 * b + j]['o_shp']
            for part in range(3):
                shp[0, b, part * 1024 + 256 * j: part * 1024 + 256 * j + 256] = o[part * 256:(part + 1) * 256]
            shp[0, b, 3072:3360] = o[768:768 + 288]
    mk = np.stack([R[4 * b]['o_mk'].reshape(256, 4, 128) for b in range(2)])[None]
    mv = np.stack([R[4 * b]['o_mv'].reshape(256, 4, 128) for b in range(2)])[None]
    swk = np.concatenate([R[c]['o_swk'].reshape(16, 128, 4, 64) for c in range(8)])[None]
    swv = np.concatenate([R[c]['o_swv'].reshape(16, 128, 4, 64) for c in range(8)])[None]
    wkv_s = np.concatenate([R[c]['o_wkvs'] for c in range(8)])[None]
    shs = np.concatenate([R[c]['o_shs'] for c in range(8)])[None]
    return (y_prompt, y_sample, wkp, wvp, wkv_p, shp, mk, mv, swk, swv, wkv_s, shs)
```

```python
import numpy as np
import ml_dtypes
import concourse.bass as bass
import concourse.mybir as mybir

F32 = mybir.dt.float32
BF16 = mybir.dt.bfloat16
I32 = mybir.dt.int32
ALU = mybir.AluOpType
AF = mybir.ActivationFunctionType
AX = mybir.AxisListType


class Res:
    def __init__(self, name, h, kind):
        self.name = name
        self.h = h
        self.kind = kind
        self.w = {}
        self.r = {}
        self.prev = {}
        self.dsem = None
        self.dcnt = 0

    def __getitem__(self, idx):
        return self.h[idx]

    def ap(self):
        return self.h.ap() if self.kind in ('dram', 'in', 'out') else self.h[:]


def _merge(dst, src):
    for k, v in src.items():
        if dst.get(k, 0) < v:
            dst[k] = v


class K:
    ENG = ('pe', 'act', 'dve', 'pool', 'sp')

    def __init__(self, nc):
        self.nc = nc
        self.prog = {e: [] for e in self.ENG}
        self.sems = {}
        self.cnt = {}
        for e in self.ENG:
            self.sems[e] = nc.alloc_semaphore('s_' + e)
            self.cnt[e] = 0
        self.waited = {e: {} for e in self.ENG}
        self.out_tickets = {}
        self.n_dsem = 0
        self.nres = 0
        self.dtot = {}
        self.ring_i = 0
        self.NRING = 64

    def sb(self, name, shape, dt=F32):
        self.nres += 1
        name = '%s_%d' % (name, self.nres)
        return Res(name, self.nc.alloc_sbuf_tensor(name, list(shape), dt), 'sb')

    def ps(self, name, shape, dt=F32):
        return Res(name, self.nc.alloc_psum_tensor(name, list(shape), dt), 'ps')

    def dram(self, name, shape, dt=F32):
        return Res(name, self.nc.dram_tensor(name, list(shape), dt), 'dram')

    def inp(self, name, shape, dt=F32):
        return Res(name, self.nc.dram_tensor(name, list(shape), dt, kind='ExternalInput'), 'in')

    def outp(self, name, shape, dt=F32):
        return Res(name, self.nc.dram_tensor(name, list(shape), dt, kind='ExternalOutput'), 'out')

    def _wait(self, e, deps):
        for key, val in deps.items():
            if key == 'pe' and e == 'pe':
                continue
            if self.waited[e].get(key, 0) >= val:
                continue
            self.waited[e][key] = val
            sem = self.sems[key]
            self.prog[e].append(lambda eng, sem=sem, val=val: eng.wait_ge(sem, val))

    def begin_fill(self, *ress):
        for b in ress:
            b.prev = {}
            _merge(b.prev, b.w)
            _merge(b.prev, b.r)
            b.w = {}
            b.r = {}

    def op(self, e, fn, reads=(), writes=(), pwrites=()):
        deps = {}
        for b in reads:
            _merge(deps, b.w)
        for b in writes:
            _merge(deps, b.w)
            _merge(deps, b.r)
        for b in pwrites:
            _merge(deps, b.prev)
        self._wait(e, deps)
        self.cnt[e] += 1
        t = {e: self.cnt[e]}
        sem = self.sems[e]
        self.prog[e].append(lambda eng, fn=fn, sem=sem: fn(eng).then_inc(sem, 1))
        for b in reads:
            _merge(b.r, t)
        for b in writes:
            b.w = dict(t)
            b.r = {}
        for b in pwrites:
            _merge(b.w, t)
        return t

    def dma(self, q, dst, dst_ap, src, src_ap, partial=False, **kw):
        deps = {}
        _merge(deps, src.w)
        if partial:
            _merge(deps, dst.prev)
        else:
            _merge(deps, dst.w)
            _merge(deps, dst.r)
        self._wait(q, deps)
        slot = self.ring_i % self.NRING
        self.ring_i += 1
        key = 'r%d' % slot
        if key not in self.sems:
            self.sems[key] = self.nc.alloc_semaphore(key)
            self.dtot[key] = 0
        self._wait(q, {key: self.dtot[key]})
        self.dtot[key] += 16
        t = {key: self.dtot[key]}
        sem = self.sems[key]
        self.prog[q].append(
            lambda eng, o=dst_ap, i=src_ap, sem=sem, kw=kw: eng.dma_start(out=o, in_=i, **kw).then_inc(sem, 16))
        _merge(src.r, t)
        if partial:
            _merge(dst.w, t)
        else:
            dst.w = dict(t)
            dst.r = {}
        if dst.kind == 'out':
            _merge(self.out_tickets, t)
        return t

    def custom(self, e, fn, inc, semkey_res, reads=(), writes=()):
        deps = {}
        for b in reads:
            _merge(deps, b.w)
        for b in writes:
            _merge(deps, b.w)
            _merge(deps, b.r)
        self._wait(e, deps)
        sres = semkey_res
        if sres.dsem is None:
            key = 'd%d' % self.n_dsem
            self.n_dsem += 1
            self.sems[key] = self.nc.alloc_semaphore(key)
            sres.dsem = key
        sres.dcnt += inc
        t = {sres.dsem: sres.dcnt}
        self.dtot[sres.dsem] = sres.dcnt
        sem = self.sems[sres.dsem]
        self.prog[e].append(lambda eng, fn=fn, sem=sem, inc=inc: fn(eng).then_inc(sem, inc))
        for b in reads:
            _merge(b.r, t)
        for b in writes:
            b.w = dict(t)
            b.r = {}
        return t

    def finish(self):
        self._wait('sp', self.out_tickets)
        allt = {e: self.cnt[e] for e in self.ENG if self.cnt[e] > 0}
        self._wait('sp', allt)
        nc = self.nc
        prog = self.prog
        with nc.Block() as block:
            @block.sync
            def _(eng):
                for f in prog['sp']:
                    f(eng)

            @block.tensor
            def _(eng):
                for f in prog['pe']:
                    f(eng)

            @block.scalar
            def _(eng):
                for f in prog['act']:
                    f(eng)

            @block.vector
            def _(eng):
                for f in prog['dve']:
                    f(eng)

            @block.gpsimd
            def _(eng):
                for f in prog['pool']:
                    f(eng)
        return nc


def _k_i(self, e, meth, reads=(), writes=(), pwrites=(), **kw):
    return self.op(e, lambda eng, meth=meth, kw=kw: getattr(eng, meth)(**kw), reads=reads, writes=writes, pwrites=pwrites)


K.i = _k_i


class ResView:
    def __init__(self, parent, ap, name=None):
        self.p = parent
        self.h = ap
        self.name = name or parent.name + '_v'
        self.kind = parent.kind

    def __getitem__(self, idx):
        return self.h[idx]

    w = property(lambda s: s.p.w, lambda s, v: setattr(s.p, 'w', v))
    r = property(lambda s: s.p.r, lambda s, v: setattr(s.p, 'r', v))
    prev = property(lambda s: s.p.prev, lambda s, v: setattr(s.p, 'prev', v))
    dsem = property(lambda s: s.p.dsem, lambda s, v: setattr(s.p, 'dsem', v))
    dcnt = property(lambda s: s.p.dcnt, lambda s, v: setattr(s.p, 'dcnt', v))


def _k_barrier(self):
    allt = {e: self.cnt[e] for e in self.ENG if self.cnt[e] > 0}
    for key, v in self.dtot.items():
        allt[key] = v
    for e in self.ENG:
        self._wait(e, allt)


K.barrier = _k_barrier


class P:
    pass

C_DEC = 0.6065306597126334
GN_EPS = 64e-5


def rwkv_consts(k, p, inp):
    c = P()
    p.rc = c
    c.mu = k.sb('r_mu', [128, 9])
    c.omm = k.sb('r_omm', [128, 9])
    k.dma('sp', c.mu, c.mu[:], inp['mu_h'], inp['mu_h'].ap().rearrange("(c p) -> p c", p=128), allow_slow_non_contiguous=True)
    k.i('dve', 'tensor_scalar', reads=[c.mu], writes=[c.omm], out=c.omm[:], in0=c.mu[:], scalar1=-1.0, scalar2=1.0, op0=ALU.mult, op1=ALU.add)
    c.pv = k.sb('r_pv', [128, 7, 2])
    k.dma('sp', c.pv, c.pv[:], inp['pv_h'], inp['pv_h'].ap().rearrange("v (g p) -> p v g", p=128), allow_slow_non_contiguous=True)
    c.omka = k.sb('r_omka', [128, 2])
    k.i('dve', 'tensor_scalar', reads=[c.pv], writes=[c.omka], out=c.omka[:], in0=c.pv[:, 3, :], scalar1=-1.0, scalar2=1.0, op0=ALU.mult, op1=ALU.add)
    lf = k.sb('r_lf', [128, 3, 256])
    c.lw = k.sb('r_lw', [128, 3, 256], BF16)
    k.i('pool', 'memset', writes=[lf], ap=lf[:], constant=0.0)
    k.dma('sp', lf, lf[0:64, 0, :], inp['w2_h'], inp['w2_h'].ap())
    k.dma('sp', lf, lf[64:128, 0, :], inp['a2_h'], inp['a2_h'].ap(), partial=True)
    k.dma('sp', lf, lf[:, 1, :], inp['g2_h'], inp['g2_h'].ap()[0:128, :], partial=True)
    k.dma('sp', lf, lf[0:32, 2, :], inp['g2_h'], inp['g2_h'].ap()[128:160, :], partial=True)
    k.i('dve', 'tensor_copy', reads=[lf], writes=[c.lw], out=c.lw[:], in_=lf[:])
    mf = k.sb('r_mf', [128, 12, 128])
    k.dma('sp', mf, mf[:], inp['rmask'], inp['rmask'].ap().rearrange("m p n -> p m n"))
    c.mf = mf
    c.mb = k.sb('r_mb', [128, 12, 128], BF16)
    k.i('dve', 'tensor_copy', reads=[mf], writes=[c.mb], out=c.mb[:], in_=mf[:])
    return c


def rwkv_phase(k, p, inp, T, x_src, x_ap_fn, WH, lnb, front, out_rw, out_state, out_shift, after_st=None):
    c = p.rc
    identb, identf = p.identb, p.identf
    NST = T // 512
    sb = k.sb
    uTall = sb('rk_uT', [128, 16, 512], BF16)
    PR = sb('rk_PR', [128, 9, 513])
    XM = sb('rk_XM', [128, 9, 512])
    k.i('pool', 'memset', writes=[PR], ap=PR[:], constant=0.0)
    f32t = {n: sb('rk_' + n, [128, 512]) for n in ['lw', 'a', 'kk', 'kmod', 'bv', 'cum', 't1', 't2']}
    b16t = {n: sb('rk_' + n, [128, 512], BF16) for n in ['tw', 'sg7', 'sg8']}
    bd = {n: sb('rk_bd_' + n, [128, 2, 8, 128], BF16) for n in ['kkt', 'rt', 'bh', 'kh', 'v']}
    gC = sb('rk_gC', [128, 2, 8])
    GATE = sb('rk_gate', [128, 2, 512])
    BONUS = sb('rk_bonus', [128, 2, 512])
    YV = sb('rk_yv', [128, 2, 512])
    S32 = sb('rk_S32', [128, 2, 128])
    S16 = sb('rk_S16', [128, 2, 128], BF16)
    S0g = sb('rk_S0g', [128, 2, 128])
    k.i('pool', 'memset', writes=[S32], ap=S32[:], constant=0.0)
    k.i('pool', 'memset', writes=[S16], ap=S16[:], constant=0.0)
    NA = [sb('rk_NA%d' % i, [128, 4, 128], BF16) for i in range(2)]
    AB = [sb('rk_AB%d' % i, [128, 4, 128], BF16) for i in range(2)]
    KR = [sb('rk_KR%d' % i, [128, 2, 128], BF16) for i in range(2)]
    TM = [sb('rk_TM%d' % i, [128, 6, 128], BF16) for i in range(2)]
    PQ = [sb('rk_PQ%d' % i, [128, 4, 128], BF16) for i in range(2)]
    TT = [sb('rk_TT%d' % i, [128, 2, 128], BF16) for i in range(2)]
    TTF = [sb('rk_TTF%d' % i, [128, 2, 128], BF16) for i in range(2)]
    U0 = sb('rk_U0', [128, 2, 128], BF16)
    UU = sb('rk_U', [128, 2, 128], BF16)
    rwo = [sb('rk_rwo%d' % i, [128, 512], BF16) for i in range(2)]
    pW = p.pM
    pC = p.pC
    pH = p.pH
    st = P()
    st.q = 0
    st.ev = 0

    def nb():
        s_ = pC[st.q % len(pC)]
        st.q += 1
        return s_

    def mmg(ps, slot_, terms, first_in_bank):
        n = len(terms)
        for i, (L, Lap, Rr, Rap) in enumerate(terms):
            fresh = first_in_bank and i == 0
            k.i('pe', 'matmul', reads=[L, Rr], writes=[ps] if fresh else [], pwrites=[] if fresh else [ps],
                out=ps[:, slot_, :], lhsT=Lap, rhs=Rap, start=(i == 0), stop=(i == n - 1))

    def cp(dst, dst_ap, ps, ps_ap, scale=None):
        st.ev += 1
        if scale is not None:
            k.i('act', 'activation', reads=[ps], writes=[dst], out=dst_ap, in_=ps_ap, func=AF.Identity, scale=scale)
        elif st.ev % 2 == 0:
            k.i('act', 'copy', reads=[ps], writes=[dst], out=dst_ap, in_=ps_ap)
        else:
            k.i('dve', 'tensor_copy', reads=[ps], writes=[dst], out=dst_ap, in_=ps_ap)

    BONES = c.mf[:, 10, :]
    BM = c.mb[:, 11, :].rearrange("p (j s) -> p j s", j=2).unsqueeze(1).to_broadcast([128, 8, 2, 64])
    for stile in range(NST):
        k.begin_fill(uTall)
        for tt in range(4):
            front(x_src, x_ap_fn(stile * 4 + tt), 128, lnb, uTall,
                  lambda half, tt=tt: uTall[:, half * 8:(half + 1) * 8, tt * 128:(tt + 1) * 128])
        if stile > 0:
            k.i('act', 'copy', reads=[PR], writes=[PR], out=PR[:, :, 0:1], in_=PR[:, :, 512:513])
        k.begin_fill(PR)
        for cc in range(9):
            ps = pW[p.mi % len(pW)]
            p.mi += 1
            for dc in range(16):
                k.i('pe', 'matmul', reads=[uTall, WH], writes=[ps] if dc == 0 else [], pwrites=[] if dc == 0 else [ps],
                    out=ps[:, :], lhsT=WH[:, dc, cc * 128:(cc + 1) * 128], rhs=uTall[:, dc, :], start=(dc == 0), stop=(dc == 15))
            if cc % 2 == 0:
                k.i('act', 'copy', reads=[ps], pwrites=[PR], out=PR[:, cc, 1:513], in_=ps[:, :])
            else:
                k.i('dve', 'tensor_copy', reads=[ps], pwrites=[PR], out=PR[:, cc, 1:513], in_=ps[:, :])
        if stile == NST - 1:
            k.dma('sp', out_shift, out_shift.ap().rearrange("(c p) -> p c", p=128), PR, PR[:, :, 512], allow_slow_non_contiguous=True)
        k.begin_fill(XM)
        for cc in range(9):
            eng = 'dve' if cc % 2 == 0 else 'pool'
            t1 = f32t['t1'] if cc % 2 == 0 else f32t['t2']
            k.i(eng, 'tensor_scalar', reads=[PR, c.mu], writes=[t1], out=t1[:], in0=PR[:, cc, 0:512], scalar1=c.mu[:, cc:cc + 1],
                scalar2=0.0, op0=ALU.mult, op1=ALU.add)
            k.i('dve', 'scalar_tensor_tensor', reads=[PR, c.omm, t1], pwrites=[XM], out=XM[:, cc, :], in0=PR[:, cc, 1:513],
                scalar=c.omm[:, cc:cc + 1], in1=t1[:], op0=ALU.mult, op1=ALU.add)
        tw, sg7, sg8 = b16t['tw'], b16t['sg7'], b16t['sg8']
        k.i('act', 'activation', reads=[XM], writes=[tw], out=tw[0:64, :], in_=XM[0:64, 6, :], func=AF.Tanh)
        k.i('act', 'copy', reads=[XM], pwrites=[tw], out=tw[64:128, :], in_=XM[64:128, 6, :])
        k.i('act', 'activation', reads=[XM], writes=[sg7], out=sg7[:], in_=XM[:, 7, :], func=AF.Sigmoid)
        k.i('act', 'activation', reads=[XM], writes=[sg8], out=sg8[0:32, :], in_=XM[0:32, 8, :], func=AF.Sigmoid)
        k.begin_fill(GATE, BONUS, gC, *bd.values())
        for g in range(2):
            gs = slice(g * 128, (g + 1) * 128)
            lw, a, kk, kmod, bv, cum, t1, t2 = [f32t[n] for n in ['lw', 'a', 'kk', 'kmod', 'bv', 'cum', 't1', 't2']]
            xr, xk, xv = XM[:, 0 + g, :], XM[:, 2 + g, :], XM[:, 4 + g, :]
            pvg = lambda i, g=g: c.pv[:, i, g:g + 1]
            ps = pW[p.mi % len(pW)]; p.mi += 1
            k.i('pe', 'matmul', reads=[c.lw, tw], writes=[ps], out=ps[:, :], lhsT=c.lw[0:64, 0, gs], rhs=tw[0:64, :], start=True, stop=True)
            k.i('act', 'activation', reads=[ps, c.pv], writes=[lw], out=lw[:], in_=ps[:, :], func=AF.Sigmoid, bias=pvg(0))
            k.i('dve', 'tensor_scalar', reads=[lw], writes=[lw], out=lw[:], in0=lw[:], scalar1=-C_DEC, scalar2=None, op0=ALU.mult)
            ps = pW[p.mi % len(pW)]; p.mi += 1
            k.i('pe', 'matmul', reads=[c.lw, tw], writes=[ps], out=ps[:, :], lhsT=c.lw[64:128, 0, gs], rhs=tw[64:128, :], start=True, stop=True)
            k.i('act', 'activation', reads=[ps, c.pv], writes=[a], out=a[:], in_=ps[:, :], func=AF.Sigmoid, bias=pvg(1))
            ps = pW[p.mi % len(pW)]; p.mi += 1
            k.i('pe', 'matmul', reads=[c.lw, sg7], writes=[ps], out=ps[:, :], lhsT=c.lw[:, 1, gs], rhs=sg7[:], start=True, stop=False)
            k.i('pe', 'matmul', reads=[c.lw, sg8], pwrites=[ps], out=ps[:, :], lhsT=c.lw[0:32, 2, gs], rhs=sg8[0:32, :], start=False, stop=True)
            k.i('act', 'copy', reads=[ps], pwrites=[GATE], out=GATE[:, g, :], in_=ps[:, :])
            k.i('dve', 'tensor_scalar', reads=[XM, c.pv], writes=[kk], out=kk[:], in0=xk, scalar1=pvg(2), scalar2=None, op0=ALU.mult)
            k.i('pool', 'tensor_tensor', reads=[kk], writes=[t1], out=t1[:], in0=kk[:], in1=kk[:], op=ALU.mult)
            ps = pW[p.mi % len(pW)]; p.mi += 1
            k.i('pe', 'matmul', reads=[c.mf, t1], writes=[ps], out=ps[:, :], lhsT=BONES, rhs=t1[:], start=True, stop=True)
            k.i('act', 'activation', reads=[ps], writes=[t2], out=t2[:], in_=ps[:, :], func=AF.Sqrt)
            k.i('dve', 'tensor_scalar', reads=[t2], writes=[t2], out=t2[:], in0=t2[:], scalar1=1e-12, scalar2=None, op0=ALU.max)
            k.i('dve', 'reciprocal', reads=[t2], writes=[t2], out=t2[:], in_=t2[:])
            k.i('dve', 'tensor_tensor', reads=[kk, t2], writes=[kk], out=kk[:], in0=kk[:], in1=t2[:], op=ALU.mult)
            k.i('dve', 'tensor_scalar', reads=[a, c.pv, c.omka], writes=[t1], out=t1[:], in0=a[:], scalar1=pvg(3), scalar2=c.omka[:, g:g + 1],
                op0=ALU.mult, op1=ALU.add)
            k.i('pool', 'tensor_tensor', reads=[XM, t1], writes=[kmod], out=kmod[:], in0=xk, in1=t1[:], op=ALU.mult)
            k.i('pool', 'tensor_tensor', reads=[kk, a], writes=[bv], out=bv[:], in0=kk[:], in1=a[:], op=ALU.mult)
            k.i('dve', 'scalar_tensor_tensor', reads=[XM, kmod, c.pv], writes=[t1], out=t1[:], in0=xr, scalar=pvg(4), in1=kmod[:], op0=ALU.mult, op1=ALU.mult)
            ps = pW[p.mi % len(pW)]; p.mi += 1
            k.i('pe', 'matmul', reads=[c.mf, t1], writes=[ps], out=ps[:, :], lhsT=BONES, rhs=t1[:], start=True, stop=True)
            k.i('dve', 'tensor_tensor', reads=[ps, XM], pwrites=[BONUS], out=BONUS[:, g, :], in0=ps[:, :], in1=xv, op=ALU.mult)
            for ch in range(8):
                cs_ = slice(ch * 64, (ch + 1) * 64)
                k.i('dve', 'tensor_tensor_scan', reads=[lw, p.ones64], writes=[] if ch else [cum], pwrites=[cum] if ch else [],
                    out=cum[:, cs_], data0=p.ones64[:, :], data1=lw[:, cs_], initial=0.0, op0=ALU.mult, op1=ALU.add)
            k.i('act', 'activation', reads=[cum], pwrites=[gC], out=gC[:, g, :], in_=cum[:].rearrange("p (c s) -> p c s", s=64)[:, :, 63], func=AF.Exp)
            k.i('act', 'activation', reads=[cum], writes=[t1], out=t1[:], in_=cum[:], func=AF.Exp)
            k.i('act', 'activation', reads=[cum], writes=[t2], out=t2[:], in_=cum[:], func=AF.Exp, scale=-1.0)
            k.i('dve', 'tensor_tensor', reads=[cum, lw], writes=[lw], out=lw[:], in0=cum[:], in1=lw[:], op=ALU.subtract)
            k.i('act', 'activation', reads=[lw], writes=[lw], out=lw[:], in_=lw[:], func=AF.Exp)

            def mk_bd(dstn, a_res, a_ap, b_res, b_ap, g=g, cum=cum):
                k.i('dve', 'tensor_tensor', reads=[a_res, b_res], writes=[cum], out=cum[:], in0=a_ap, in1=b_ap, op=ALU.mult)
                d = bd[dstn]
                k.i('dve', 'tensor_tensor', reads=[cum, c.mb], pwrites=[d],
                    out=d[:, g, :, :].rearrange("p c (j s) -> p c j s", j=2),
                    in0=cum[:].rearrange("p (c s) -> p c s", s=64).unsqueeze(2).to_broadcast([128, 8, 2, 64]), in1=BM, op=ALU.mult)
            mk_bd('kkt', kk, kk[:], lw, lw[:])
            mk_bd('rt', XM, xr, t1, t1[:])
            mk_bd('bh', bv, bv[:], t2, t2[:])
            mk_bd('kh', kmod, kmod[:], t2, t2[:])
            k.i('dve', 'tensor_tensor', reads=[XM, c.mb], pwrites=[bd['v']],
                out=bd['v'][:, g, :, :].rearrange("p c (j s) -> p c j s", j=2),
                in0=xv.rearrange("p (c s) -> p c s", s=64).unsqueeze(2).to_broadcast([128, 8, 2, 64]), in1=BM, op=ALU.mult)
        kkt, rt, bh, kh, vb = [bd[n] for n in ['kkt', 'rt', 'bh', 'kh', 'v']]
        k.begin_fill(YV)
        def dep_pieces(ch, stile=stile):
            par = ch % 2
            na, ab, kr, tm = NA[par], AB[par], KR[par], TM[par]
            cur = TTF[par]
            bank = {}

            def p0():
                for g in range(2):
                    k.i('act', 'activation', reads=[S32, gC], writes=[S0g] if g == 0 else [], pwrites=[] if g == 0 else [S0g],
                        out=S0g[:, g, :], in_=S32[:, g, :], func=AF.Identity, scale=gC[:, g, ch:ch + 1])
                ps = nb()
                for g in range(2):
                    mmg(ps, g, [(kkt, kkt[:, g, ch, :], S16, S16[:, g, :]), (ab, ab[:, g, :], tm, tm[:, g * 3, :])], g == 0)
                cp(U0, U0[:], ps, ps[:, 0:2, :], scale=-1.0)

            def p1():
                ps = nb()
                for g in range(2):
                    mmg(ps, g, [(cur, cur[:, g, :], U0, U0[:, g, :])], g == 0)
                cp(UU, UU[:], ps, ps[:, 0:2, :])

            def p2():
                ps = nb()
                bank['ps'] = ps
                for g in range(2):
                    mmg(ps, g, [(S16, S16[:, g, :], rt, rt[:, g, ch, :]), (UU, UU[:, g, :], ab, ab[:, 2 + g, :]),
                                (tm, tm[:, g * 3, :], kr, kr[:, g, :])], g == 0)
                for g in range(2):
                    mmg(ps, 2 + g, [(tm, tm[:, g * 3 + 1, :], UU, UU[:, g, :]), (tm, tm[:, g * 3 + 2, :], tm, tm[:, g * 3, :])], False)

            def p3():
                ps = bank['ps']
                k.i('act', 'copy', reads=[ps], pwrites=[YV], out=YV[0:64, :, ch * 64:(ch + 1) * 64], in_=ps[0:64, 0:2, 0:64])
                k.i('dve', 'tensor_copy', reads=[ps], pwrites=[YV], out=YV[64:128, :, ch * 64:(ch + 1) * 64], in_=ps[64:128, 0:2, 64:128])
                for g in range(2):
                    k.i('dve', 'scalar_tensor_tensor', reads=[ps, gC, S0g], writes=[S32] if g == 0 else [], pwrites=[] if g == 0 else [S32],
                        out=S32[:, g, :], in0=ps[:, 2 + g, :], scalar=gC[:, g, ch:ch + 1], in1=S0g[:, g, :], op0=ALU.mult, op1=ALU.add)

            def p4():
                k.i('act', 'copy', reads=[S32], writes=[S16], out=S16[:], in_=S32[:])
            return [p0, p1, p2, p3, p4]

        pend = []
        for ch in range(8):
            par = ch % 2
            na, ab, kr, tm = NA[par], AB[par], KR[par], TM[par]
            ps = nb()
            for g in range(2):
                mmg(ps, 2 * g, [(bh, bh[:, g, ch, :], kkt, kkt[:, g, ch, :])], g == 0)
                mmg(ps, 2 * g + 1, [(kkt, kkt[:, g, ch, :], bh, bh[:, g, ch, :])], False)
            k.i('dve', 'tensor_tensor', reads=[ps, c.mb], writes=[na], out=na[:], in0=ps[:, :, :], in1=c.mb[:, 0:4, :], op=ALU.mult)
            ps = nb()
            for g in range(2):
                mmg(ps, g, [(kh, kh[:, g, ch, :], kkt, kkt[:, g, ch, :])], g == 0)
            for g in range(2):
                mmg(ps, 2 + g, [(bh, bh[:, g, ch, :], rt, rt[:, g, ch, :])], False)
            k.i('dve', 'tensor_tensor', reads=[ps, c.mb], writes=[ab], out=ab[:], in0=ps[:, :, :], in1=c.mb[:, 4:8, :], op=ALU.mult)
            ps = nb()
            for g in range(2):
                mmg(ps, g, [(kh, kh[:, g, ch, :], rt, rt[:, g, ch, :])], g == 0)
            k.i('dve', 'tensor_tensor', reads=[ps, c.mb], writes=[kr], out=kr[:], in0=ps[:, 0:2, :], in1=c.mb[:, 8:10, :], op=ALU.mult)
            first = True
            for g in range(2):
                for j_, src in enumerate((vb, bh, kh)):
                    k.i('pe', 'transpose', reads=[src, identb], writes=[pH] if first else [], pwrites=[] if first else [pH],
                        out=pH[:, g * 3 + j_, :], in_=src[:, g, ch, :], identity=identb[:])
                    first = False
            cp(tm, tm[:], pH, pH[:, 0:6, :])
            cur = TT[0]
            k.i('pool', 'tensor_tensor', reads=[identb, na], writes=[cur], out=cur[:], in0=identb[:].unsqueeze(1).to_broadcast([128, 2, 128]),
                in1=na[:, 0:4:2, :], op=ALU.subtract)
            Pc = [(na, na[:, 0, :]), (na, na[:, 2, :])]
            Qc = [(na, na[:, 1, :]), (na, na[:, 3, :])]
            for lev in range(5):
                pq = PQ[lev % 2]
                ps = nb()
                for g in range(2):
                    if lev < 4:
                        mmg(ps, 2 * g, [(Qc[g][0], Qc[g][1], Pc[g][0], Pc[g][1])], g == 0)
                    mmg(ps, 2 * g + 1, [(Pc[g][0], Pc[g][1], Qc[g][0], Qc[g][1])], (g == 0 and lev == 4))
                if lev < 4:
                    cp(pq, pq[:], ps, ps[:, :, :])
                else:
                    cp(pq, pq[:, 1:4:2, :], ps, ps[:, 1:4:2, :])
                Pn = [(pq, pq[:, 0, :]), (pq, pq[:, 2, :])]
                Qn = [(pq, pq[:, 1, :]), (pq, pq[:, 3, :])]
                nxt = TT[(lev + 1) % 2] if lev < 4 else TTF[par]
                ps = nb()
                for g in range(2):
                    mmg(ps, g, [(identb, identb[:], cur, cur[:, g, :]), (Qn[g][0], Qn[g][1], cur, cur[:, g, :])], g == 0)
                cp(nxt, nxt[:], ps, ps[:, 0:2, :])
                cur = nxt
                Pc, Qc = Pn, Qn
                if pend:
                    pend.pop(0)()
            while pend:
                pend.pop(0)()
            pend = dep_pieces(ch)
        while pend:
            pend.pop(0)()
        for g in range(2):
            t1, t2 = f32t['t1'], f32t['t2']
            pvg = lambda i, g=g: c.pv[:, i, g:g + 1]
            ps = pW[p.mi % len(pW)]; p.mi += 1
            k.i('pe', 'matmul', reads=[c.mf, YV], writes=[ps], out=ps[:, :], lhsT=BONES, rhs=YV[:, g, :], start=True, stop=True)
            k.i('dve', 'scalar_tensor_tensor', reads=[ps, YV], writes=[t1], out=t1[:], in0=ps[:, :], scalar=-1.0 / 64, in1=YV[:, g, :], op0=ALU.mult, op1=ALU.add)
            k.i('pool', 'tensor_tensor', reads=[t1], writes=[t2], out=t2[:], in0=t1[:], in1=t1[:], op=ALU.mult)
            ps = pW[p.mi % len(pW)]; p.mi += 1
            k.i('pe', 'matmul', reads=[c.mf, t2], writes=[ps], out=ps[:, :], lhsT=BONES, rhs=t2[:], start=True, stop=True)
            k.i('act', 'activation', reads=[ps], writes=[t2], out=t2[:], in_=ps[:, :], func=AF.Sqrt, scale=1.0 / 64, bias=GN_EPS)
            k.i('dve', 'reciprocal', reads=[t2], writes=[t2], out=t2[:], in_=t2[:])
            k.i('dve', 'tensor_tensor', reads=[t1, t2], writes=[t1], out=t1[:], in0=t1[:], in1=t2[:], op=ALU.mult)
            k.i('dve', 'tensor_scalar', reads=[t1, c.pv], writes=[t1], out=t1[:], in0=t1[:], scalar1=pvg(5), scalar2=pvg(6), op0=ALU.mult, op1=ALU.add)
            k.i('pool', 'tensor_tensor', reads=[t1, BONUS], writes=[t1], out=t1[:], in0=t1[:], in1=BONUS[:, g, :], op=ALU.add)
            ro = rwo[g]
            k.i('dve', 'tensor_tensor', reads=[t1, GATE], writes=[ro], out=ro[:], in0=t1[:], in1=GATE[:, g, :], op=ALU.mult)
            ow = out_rw[stile]
            k.dma('sp', ow, ow.ap()[g * 128:(g + 1) * 128, :], ro, ro[:], partial=True)
        if after_st is not None:
            after_st(stile)
    so = S0g
    ps = nb()
    for g in range(2):
        k.i('pe', 'transpose', reads=[S32, identf], writes=[ps] if g == 0 else [], pwrites=[] if g == 0 else [ps],
            out=ps[:, g, :], in_=S32[:, g, :], identity=identf[:])
    k.i('dve', 'tensor_copy', reads=[ps], writes=[so], out=so[:], in_=ps[:, 0:2, :])
    for g in range(2):
        for j2 in range(2):
            k.dma('sp', out_state, out_state.ap()[g * 2 + j2], so, so[j2 * 64:(j2 + 1) * 64, g, j2 * 64:(j2 + 1) * 64], partial=True)

from concourse.bass_utils import run_bass_kernel_spmd
import math

D = 2048
EPS = 1e-6
ATT_COLS = 1536
RW_COLS = 3360
RWH = 1152
N_CORES = 8
T_SEQ = 4096
NEG = -30000.0


class P:
    pass


def build(stages):
    nc = bass.Bass("TRN2", target_bir_lowering=False)
    k = K(nc)
    p = P()
    p.k = k
    inp = {}

    del _INPUT_NAMES[:]

    def I(name, shape, dt=F32):
        inp[name] = k.inp(name, shape, dt)
        _INPUT_NAMES.append(name)
        return inp[name]

    I('x_tok', [1168, D]); I('x_seq', [T_SEQ, D]); I('mem', [256, D])
    USED_LN = ('ln1', 'memn') + (('ln2',) if 'OX' in stages else ()) + (('ln3',) if 'M' in stages else ())
    for n in USED_LN:
        I(n, [D])
    I('qn', [64]); I('kn', [64]); I('xkn', [128])
    if 'OX' in stages:
        I('xqn', [128])
    I('w_att', [D, ATT_COLS]); I('w_rw', [D, RW_COLS]); I('w_rwh', [D, RWH]); I('w_xkv', [D, 1024])
    I('cs_all', [1168, 16]); I('ident', [128, 128]); I('masks', [2, 128, 256]); I('sinks', [16])
    I('cwk', [16, 128, 256]); I('cwv', [16, 128, 256])
    if 'SR' in stages:
        I('sh_s', [16, RW_COLS]); I('wkv_s', [16, 16, 64, 64]); I('mu_f', [RW_COLS]); I('pvf', [7, 1024])
        I('w2_f', [64, 1024]); I('a2_f', [64, 1024]); I('g2_f', [160, 1024]); I('lnwb_s', [2, 256, 64])
    if 'SA' in stages:
        I('sinks_s', [64, 4])
    if 'SX' in stages:
        I('cmk_s', [16, 256, 512]); I('cmv_s', [16, 256, 512])
    if 'M' in stages:
        I('w_rt', [D, 72]); I('b_rt', [72]); I('e_g', [64, D, 512]); I('e_u', [64, D, 512]); I('e_d', [64, 512, D]); I('iota64', [64])
    if 'OX' in stages:
        I('w_out', [D, D]); I('w_xq', [D, 512]); I('w_xo', [512, D]); I('ohj', [4])
    I('mu_h', [RWH]); I('pv_h', [7, 256]); I('w2_h', [64, 256]); I('a2_h', [64, 256]); I('g2_h', [160, 256]); I('rmask', [12, 128, 128])
    o_y = k.outp('o_y', [1040, D])
    o_wkp = k.outp('o_wkp', [128, 256]); o_wvp = k.outp('o_wvp', [128, 256])
    o_shp = k.outp('o_shp', [RWH]); o_wkvp = k.outp('o_wkvp', [4, 64, 64])
    o_mk = k.outp('o_mk', [256, 512]); o_mv = k.outp('o_mv', [256, 512])
    o_swk = k.outp('o_swk', [16, 128, 256]); o_swv = k.outp('o_swv', [16, 128, 256])
    o_wkvs = k.outp('o_wkvs', [16, 16, 64, 64]); o_shs = k.outp('o_shs', [16, RW_COLS])
    ATT_O = k.dram('ATT_O', [1152, 1024], BF16)
    RWIN = [k.dram('RWIN%d' % i, [256, 512], BF16) for i in range(8)]
    RWG = [k.dram('RWG%d' % i, [1024, 512], BF16) for i in range(8)]
    QS = k.dram('QS', [16, 1536])
    H2 = k.dram('H2d', [1152, D])
    RWS = k.dram('RWS', [16, 1024], BF16)
    SRd = k.dram('SRd', [16, RW_COLS])
    SV = k.dram('SV', [6, 16, 1024])
    YNd = k.dram('YNd', [256, 64])
    QXd = k.dram('QXd', [16, 512])
    OXd = k.dram('OXd', [16, 512])
    H = [k.ps('H%d' % i, [128, 8, 128], BF16) for i in range(3)]
    Fb = [k.ps('F%d' % i, [128, 512]) for i in range(5)]
    p.pM = Fb[0:2]
    p.pC = [ResView(Fb[2 + i], Fb[2 + i].h[:, :].rearrange("p (a b) -> p a b", a=4)) for i in range(3)]
    p.pH = H[2]
    pT = H[0:2]
    p.identf = k.sb('identf', [128, 128]); p.identb = k.sb('identb', [128, 128], BF16)
    identb = p.identb
    k.dma('sp', p.identf, p.identf[:], inp['ident'], inp['ident'].ap())
    k.i('dve', 'tensor_copy', reads=[p.identf], writes=[identb], out=identb[:], in_=p.identf[:])
    p.ones64 = k.sb('ones64', [128, 64])
    k.i('pool', 'memset', writes=[p.ones64], ap=p.ones64[:], constant=1.0)
    k.dma('sp', o_swk, o_swk.ap()[:, 0:127, :], inp['cwk'], inp['cwk'].ap()[:, 1:128, :])
    k.dma('sp', o_swv, o_swv.ap()[:, 0:127, :], inp['cwv'], inp['cwv'].ap()[:, 1:128, :])
    k.begin_fill(o_swk, o_swv)

    fb = P()

    def alloc_front():
        fb.xt = [k.sb('xt%d' % i, [128, D]) for i in range(2)]
        fb.ub = [k.sb('ub%d' % i, [128, D], BF16) for i in range(2)]
        fb.ssb = [k.sb('ss%d' % i, [128, 1]) for i in range(4)]
        fb.lnA = k.sb('lnA', [128, D])
    p.alloc_front = alloc_front
    p.fb = fb
    p.ti = 0
    p.mi = 0

    def load_ln(name):
        k.dma('sp', fb.lnA, fb.lnA[:], inp[name], inp[name].ap().partition_broadcast(128))
        return fb.lnA

    def bcast_load(name, n):
        t = k.sb('bc_' + name, [128, n])
        k.dma('sp', t, t[:], inp[name], inp[name].ap().partition_broadcast(128))
        return t

    def front(src, src_ap, n, lnb, uT, uT_ap_fn, x_keep=None):
        i = p.ti; p.ti += 1
        x = fb.xt[i % 2] if x_keep is None else x_keep
        u = fb.ub[i % 2]; ss = fb.ssb[i % 4]
        k.dma('sp', x, x[0:n, :], src, src_ap)
        k.i('act', 'activation', reads=[x], writes=[u, ss], out=u[0:n, :], in_=x[0:n, :], func=AF.Square, accum_out=ss[0:n, :])
        k.i('act', 'activation', reads=[ss], writes=[ss], out=ss[0:n, :], in_=ss[0:n, :], func=AF.Sqrt, scale=1.0 / D, bias=EPS)
        k.i('dve', 'reciprocal', reads=[ss], writes=[ss], out=ss[0:n, :], in_=ss[0:n, :])
        k.i('dve', 'scalar_tensor_tensor', reads=[x, ss, lnb], writes=[u], out=u[0:n, :], in0=x[0:n, :], scalar=ss[0:n, 0:1], in1=lnb[0:n, :],
            op0=ALU.mult, op1=ALU.mult)
        transpose16(u, n, uT, uT_ap_fn)

    def transpose16(u, n, uT, uT_ap_fn, nchunks=16):
        for half in range((nchunks + 7) // 8):
            pt = pT[half % 2]
            m = min(8, nchunks - half * 8)
            for cc in range(m):
                dc = half * 8 + cc
                k.i('pe', 'transpose', reads=[u, identb], writes=[pt] if cc == 0 else [], pwrites=[] if cc == 0 else [pt],
                    out=pt[:, cc, 0:n], in_=u[0:n, dc * 128:(dc + 1) * 128], identity=identb[0:n, 0:n])
            dst = uT_ap_fn(half)
            if half % 2 == 0:
                k.i('act', 'copy', reads=[pt], pwrites=[uT], out=dst, in_=pt[:, 0:m, 0:n])
            else:
                k.i('dve', 'tensor_copy', reads=[pt], pwrites=[uT], out=dst, in_=pt[:, 0:m, 0:n])

    def linear_tm(uT, uT_ap_fn, n, W, W_ap_fn, ncols, nk=16, ps=None):
        if ps is None:
            ps = p.pM[p.mi % len(p.pM)]
            p.mi += 1
        for dc in range(nk):
            k.i('pe', 'matmul', reads=[uT, W], writes=[ps] if dc == 0 else [], pwrites=[] if dc == 0 else [ps],
                out=ps[0:n, 0:ncols], lhsT=uT_ap_fn(dc), rhs=W_ap_fn(dc), start=(dc == 0), stop=(dc == nk - 1))
        return ps

    def load_w(dst, src_name, rows, c0, c1, step=512, q='pool'):
        k.begin_fill(dst)
        for c in range(c0, c1, step):
            ce = min(c + step, c1)
            k.dma(q, dst, dst[:, :, c - c0:ce - c0], inp[src_name],
                  inp[src_name].ap()[:, c:ce].rearrange("(c p) n -> p c n", p=128), partial=True)

    p.front = front; p.transpose16 = transpose16; p.linear_tm = linear_tm; p.load_w = load_w
    p.inp = inp; p.nc = nc; p.H = H; p.Fb = Fb; p.pT = pT; p.load_ln = load_ln; p.bcast_load = bcast_load
    p.out = dict(o_y=o_y, o_wkp=o_wkp, o_wvp=o_wvp, o_shp=o_shp, o_wkvp=o_wkvp, o_mk=o_mk, o_mv=o_mv, o_swk=o_swk, o_swv=o_swv,
                 o_wkvs=o_wkvs, o_shs=o_shs)
    p.H2t = [ResView(Res('H2t%d' % i, H2.h, 'dram'), H2.h) for i in range(9)]
    p.dr = dict(ATT_O=ATT_O, RWIN=RWIN, RWG=RWG, QS=QS, H2=H2, RWS=RWS, SRd=SRd, SV=SV, YNd=YNd, QXd=QXd, OXd=OXd)

    p.MEMKT = k.sb('MEMKT', [128, 4, 256], BF16)
    p.MEMV = k.sb('MEMV', [128, 2, 512], BF16)
    k.begin_fill(p.MEMKT, p.MEMV)
    mark0 = nc.sbuf_base

    def phase_end():
        k.barrier()
        nc.sbuf_base = mark0

    if 'A' in stages:
        alloc_front()
        phase_A(k, p)
        phase_end()
    if 'MEM' in stages:
        alloc_front()
        phase_MEM(k, p)
        phase_end()
    if 'R' in stages:
        alloc_front()
        ln1b = load_ln('ln1')
        WH = k.sb('WH', [128, 16, RWH], BF16)
        load_w(WH, 'w_rwh', D, 0, RWH, step=384)
        rwkv_consts(k, p, inp)
        def gather_st(st_):
            k.custom('pool', lambda e, st_=st_: e.collective_compute("AllGather", ALU.bypass, replica_groups=[[0, 1, 2, 3], [4, 5, 6, 7]],
                                                                   ins=[RWIN[st_].h.ap().opt()], outs=[RWG[st_].h.ap().opt()]),
                     1, RWG[st_], reads=[RWIN[st_]], writes=[RWG[st_]])
        rwkv_phase(k, p, inp, T_SEQ, inp['x_seq'], lambda t: inp['x_seq'].ap()[t * 128:(t + 1) * 128, :], WH, ln1b, front,
                   RWIN, o_wkvp, o_shp, after_st=gather_st)
        phase_end()
    if 'SR' in stages:
        phase_SR(k, p)
        phase_end()
    if 'SA' in stages:
        phase_SA(k, p)
        phase_end()
    if 'OX' in stages:
        alloc_front()
        phase_OX(k, p)
        phase_end()
    if 'MT' in stages:
        h2in = I('h2_in', [1152, D])
        for t_ in range(9):
            k.dma('sp', p.H2t[t_], H2.ap()[t_ * 128:(t_ + 1) * 128, :], h2in, h2in.ap()[t_ * 128:(t_ + 1) * 128, :])
    if 'SX' in stages:
        phase_SX(k, p)
        phase_end()
    if 'M' in stages:
        phase_M(k, p, mark0)
        phase_end()
    if 'DBG' in stages:
        o_dbg = k.outp('o_dbg', [1152, 1024], BF16)
        k.dma('sp', o_dbg, o_dbg.ap(), ATT_O, ATT_O.ap())
        o_dbg2 = k.outp('o_dbg2', [1024, T_SEQ], BF16)
        for i_ in range(8):
            k.dma('sp', o_dbg2, o_dbg2.ap()[:, i_ * 512:(i_ + 1) * 512], RWG[i_], RWG[i_].ap(), partial=True)
        o_dbg4 = k.outp('o_dbg4', [16, 1024], BF16)
        k.dma('sp', o_dbg4, o_dbg4.ap(), RWS, RWS.ap())
        o_dbg3 = k.outp('o_dbg3', [1152, D])
        k.dma('sp', o_dbg3, o_dbg3.ap(), H2, H2.ap())
    k.finish()
    print('instr counts', {e: len(k.prog[e]) for e in k.prog}, 'dma sems', k.n_dsem, flush=True)
    return nc


def head_norm(k, p, src, src2d, n, nh, hd, nwb, dst, dst2d, scr, cs=None):
    sq, st, xn, r1, r2, r3 = scr
    w = nh * hd
    k.i('act', 'activation', reads=[src], writes=[sq], out=sq[0:n, 0:w], in_=src2d, func=AF.Square)
    k.i('dve', 'tensor_reduce', reads=[sq], writes=[st], out=st[0:n, 0:nh], in_=sq[0:n, 0:w].rearrange("p (a b) -> p a b", a=nh), axis=AX.X, op=ALU.add)
    k.i('act', 'activation', reads=[st], writes=[st], out=st[0:n, 0:nh], in_=st[0:n, 0:nh], func=AF.Sqrt, scale=1.0 / hd, bias=EPS)
    k.i('dve', 'reciprocal', reads=[st], writes=[st], out=st[0:n, 0:nh], in_=st[0:n, 0:nh])
    k.i('dve', 'tensor_tensor', reads=[src, st], writes=[xn], out=xn[0:n, 0:w].rearrange("p (a b) -> p a b", a=nh),
        in0=src2d.rearrange("p (a b) -> p a b", a=nh), in1=st[0:n, 0:nh].unsqueeze(2).to_broadcast([n, nh, hd]), op=ALU.mult)
    d3 = dst2d.rearrange("p (a b) -> p a b", a=nh)
    k.i('dve', 'tensor_tensor', reads=[xn, nwb], writes=[dst], out=d3, in0=xn[0:n, 0:w].rearrange("p (a b) -> p a b", a=nh),
        in1=nwb[0:n, 0:hd].unsqueeze(1).to_broadcast([n, nh, hd]), op=ALU.mult)
    if cs is not None:
        x1 = d3[:, :, 0:8]; x2 = d3[:, :, 8:16]
        cosb = cs[0:n, 0:8].unsqueeze(1).to_broadcast([n, nh, 8])
        sinb = cs[0:n, 8:16].unsqueeze(1).to_broadcast([n, nh, 8])
        a1, a2, a3 = r1[0:n, 0:nh, :], r2[0:n, 0:nh, :], r3[0:n, 0:nh, :]
        k.i('dve', 'tensor_tensor', reads=[dst, cs], writes=[r1], out=a1, in0=x1, in1=cosb, op=ALU.mult)
        k.i('dve', 'tensor_tensor', reads=[dst, cs], writes=[r2], out=a2, in0=x2, in1=sinb, op=ALU.mult)
        k.i('dve', 'tensor_tensor', reads=[dst, cs], writes=[r3], out=a3, in0=x1, in1=sinb, op=ALU.mult)
        k.i('dve', 'tensor_tensor', reads=[r1, r2], writes=[r1], out=a1, in0=a1, in1=a2, op=ALU.subtract)
        k.i('dve', 'tensor_tensor', reads=[dst, cs], writes=[r2], out=a2, in0=x2, in1=cosb, op=ALU.mult)
        k.i('dve', 'tensor_tensor', reads=[r2, r3], writes=[dst], out=x2, in0=a2, in1=a3, op=ALU.add)
        k.i('dve', 'tensor_copy', reads=[r1], writes=[dst], out=x1, in_=a1)


def phase_A(k, p):
    inp, sb = p.inp, k.sb
    H, Fb = p.H, p.Fb
    identb = p.identb
    ln1b = p.load_ln('ln1')
    qnb = p.bcast_load('qn', 64); knb = p.bcast_load('kn', 64); sinkb = p.bcast_load('sinks', 16)
    maskb = sb('maskb', [128, 2, 256])
    k.dma('sp', maskb, maskb[:], inp['masks'], inp['masks'].ap().rearrange("m p n -> p m n"))
    WA = sb('WA', [128, 16, ATT_COLS], BF16)
    p.load_w(WA, 'w_att', D, 0, ATT_COLS)
    WS = sb('WSr', [128, 16, 480], BF16)
    scr = (sb('sq', [128, 1024]), sb('st', [128, 16]), sb('xn', [128, 1024]), sb('r1', [128, 16, 8]), sb('r2', [128, 16, 8]), sb('r3', [128, 16, 8]))
    uTs = [sb('uT%d' % i, [128, 16, 128], BF16) for i in range(2)]
    cst = [sb('cst%d' % i, [128, 16]) for i in range(2)]
    QF = sb('QF', [128, 1024]); QB = sb('QB', [128, 1024], BF16)
    KF = [sb('KF%d' % i, [128, 512]) for i in range(2)]
    kdup = sb('kdup', [128, 4, 2, 64], BF16)
    KT2 = [sb('KT2_%d' % i, [128, 4, 128], BF16) for i in range(2)]
    VB = [sb('VB%d' % i, [128, 256], BF16) for i in range(2)]
    qT = sb('qT', [128, 8, 128], BF16)
    SM = sb('SM', [128, 4, 256]); E = sb('E', [128, 4, 256], BF16); ET = sb('ET', [128, 8, 128], BF16)
    mx = sb('mx', [128, 4]); negm = sb('negm', [128, 4]); rs = sb('rs', [128, 4]); es = sb('es', [128, 4])
    ATTO = [sb('ATTO%d' % i, [128, 1024], BF16) for i in range(2)]
    srs = [sb('srs%d' % i, [16, 480]) for i in range(2)]
    HT = H[2]
    for t in range(10):
        n = 16 if t == 9 else 128
        r0 = t * 128
        slot = t % 2
        uT = uTs[t % 2]
        k.begin_fill(uT)
        cs = cst[t % 2]
        k.dma('sp', cs, cs[0:n, :], inp['cs_all'], inp['cs_all'].ap()[r0:r0 + n, :])
        p.front(inp['x_tok'], inp['x_tok'].ap()[r0:r0 + n, :], n, ln1b, uT, lambda half, uT=uT, n=n: uT[:, half * 8:(half + 1) * 8, 0:n])
        uf = lambda dc, uT=uT, n=n: uT[:, dc, 0:n]
        kf = KF[slot]
        if t >= 1:
            for hq in range(2):
                ps = p.linear_tm(uT, uf, n, WA, lambda dc, hq=hq: WA[:, dc, hq * 512:(hq + 1) * 512], 512)
                head_norm(k, p, ps, ps[0:n, 0:512], n, 8, 64, qnb, QF, QF[0:n, hq * 512:(hq + 1) * 512], scr, cs=cs)
            k.i('act', 'activation', reads=[QF], writes=[QB], out=QB[0:n, :], in_=QF[0:n, :], func=AF.Identity, scale=0.125)
        ps = p.linear_tm(uT, uf, n, WA, lambda dc: WA[:, dc, 1024:1536], 512)
        head_norm(k, p, ps, ps[0:n, 0:256], n, 4, 64, knb, kf, kf[0:n, 0:256], scr, cs=cs)
        k.i('dve', 'tensor_copy', reads=[ps], writes=[], pwrites=[kf], out=kf[0:n, 256:512], in_=ps[0:n, 256:512])
        if t == 8:
            k.dma('sp', p.out['o_wkp'], p.out['o_wkp'].ap(), kf, kf[:, 0:256])
            k.dma('sp', p.out['o_wvp'], p.out['o_wvp'].ap(), kf, kf[:, 256:512])
        if t == 9:
            k.dma('sp', p.out['o_swk'], p.out['o_swk'].ap()[:, 127, :], kf, kf[0:16, 0:256], partial=True)
            k.dma('sp', p.out['o_swv'], p.out['o_swv'].ap()[:, 127, :], kf, kf[0:16, 256:512], partial=True)
            QSd = p.dr['QS']
            k.dma('sp', QSd, QSd.ap()[:, 0:1024], QF, QF[0:16, :])
            k.dma('sp', QSd, QSd.ap()[:, 1024:1536], kf, kf[0:16, :], partial=True)
            for c in range(7):
                k.dma('pool', WS, WS[:], inp['w_rw'], inp['w_rw'].ap()[:, c * 480:(c + 1) * 480].rearrange("(c p) n -> p c n", p=128))
                ps = p.linear_tm(uT, uf, 16, WS, lambda dc: WS[:, dc, :], 480)
                sr = srs[c % 2]
                k.i('act' if c % 2 == 0 else 'dve', 'copy' if c % 2 == 0 else 'tensor_copy', reads=[ps], writes=[sr], out=sr[0:16, :], in_=ps[0:16, 0:480])
                k.dma('sp', p.out['o_shs'], p.out['o_shs'].ap()[:, c * 480:(c + 1) * 480], sr, sr[:], partial=(c > 0))
                k.dma('sp', p.dr['SRd'], p.dr['SRd'].ap()[:, c * 480:(c + 1) * 480], sr, sr[:], partial=True)
            continue
        k.i('dve', 'tensor_copy', reads=[kf], writes=[kdup], out=kdup[:],
            in_=kf[:, 0:256].rearrange("p (a b) -> p a b", a=4).unsqueeze(2).to_broadcast([128, 4, 2, 64]))
        k.i('act', 'copy', reads=[kf], writes=[VB[slot]], out=VB[slot][:], in_=kf[:, 256:512])
        for kh in range(4):
            k.i('pe', 'transpose', reads=[kdup, identb], writes=[HT] if kh == 0 else [], pwrites=[] if kh == 0 else [HT],
                out=HT[:, kh, :], in_=kdup[:, kh, :, :].rearrange("p a b -> p (a b)"), identity=identb[:])
        k.i('act', 'copy', reads=[HT], writes=[KT2[slot]], out=KT2[slot][:], in_=HT[:, 0:4, :])
        if t == 0:
            continue
        for m in range(8):
            k.i('pe', 'transpose', reads=[QB, identb], writes=[HT] if m == 0 else [], pwrites=[] if m == 0 else [HT],
                out=HT[:, m, :], in_=QB[:, m * 128:(m + 1) * 128], identity=identb[:])
        k.i('dve', 'tensor_copy', reads=[HT], writes=[qT], out=qT[:], in_=HT[:, :, :])
        mi_ = 1 if t == 1 else 0
        ao = ATTO[t % 2]
        k.begin_fill(ao)
        for kh in range(4):
            for hp in range(2):
                bank = Fb[2 + hp]
                first = True
                for g2 in range(2):
                    g = g2 * 2 + hp
                    h = 4 * kh + g
                    m = h // 2
                    for half, sl in ((0, 1 - slot), (1, slot)):
                        k.i('pe', 'matmul', reads=[qT, KT2[sl]], writes=[bank] if first else [], pwrites=[] if first else [bank],
                            out=bank[:, g2 * 256 + half * 128:g2 * 256 + (half + 1) * 128],
                            lhsT=qT[hp * 64:(hp + 1) * 64, m, :], rhs=KT2[sl][hp * 64:(hp + 1) * 64, kh, :], start=True, stop=True)
                        first = False
                k.i('dve', 'tensor_tensor', reads=[bank, maskb], writes=[SM] if hp == 0 else [], pwrites=[] if hp == 0 else [SM],
                    out=SM[:, hp:4:2, :], in0=bank[:, :].rearrange("p (a b) -> p a b", a=2),
                    in1=maskb[:, mi_, :].unsqueeze(1).to_broadcast([128, 2, 256]), op=ALU.add)
            k.i('dve', 'tensor_reduce', reads=[SM], writes=[mx], out=mx[:], in_=SM[:], axis=AX.X, op=ALU.max)
            k.i('dve', 'tensor_tensor', reads=[mx, sinkb], writes=[mx], out=mx[:], in0=mx[:], in1=sinkb[:, kh * 4:(kh + 1) * 4], op=ALU.max)
            k.i('dve', 'tensor_scalar', reads=[mx], writes=[negm], out=negm[:], in0=mx[:], scalar1=-1.0, scalar2=None, op0=ALU.mult)
            for g in range(4):
                k.i('act', 'activation', reads=[SM, negm], writes=[E, rs] if g == 0 else [], pwrites=[] if g == 0 else [E, rs],
                    out=E[:, g, :], in_=SM[:, g, :], func=AF.Exp, bias=negm[:, g:g + 1], accum_out=rs[:, g:g + 1])
            k.i('dve', 'tensor_tensor', reads=[sinkb, negm], writes=[es], out=es[:], in0=sinkb[:, kh * 4:(kh + 1) * 4], in1=negm[:], op=ALU.add)
            k.i('act', 'activation', reads=[es], writes=[es], out=es[:], in_=es[:], func=AF.Exp)
            k.i('dve', 'tensor_tensor', reads=[rs, es], writes=[rs], out=rs[:], in0=rs[:], in1=es[:], op=ALU.add)
            k.i('dve', 'reciprocal', reads=[rs], writes=[rs], out=rs[:], in_=rs[:])
            for g in range(4):
                for half in range(2):
                    i8 = g * 2 + half
                    k.i('pe', 'transpose', reads=[E, identb], writes=[HT] if i8 == 0 else [], pwrites=[] if i8 == 0 else [HT],
                        out=HT[:, i8, :], in_=E[:, g, half * 128:(half + 1) * 128], identity=identb[:])
            k.i('dve', 'tensor_copy', reads=[HT], writes=[ET], out=ET[:], in_=HT[:, :, :])
            ob = Fb[4]
            first = True
            for g in range(4):
                for half, sl in ((0, 1 - slot), (1, slot)):
                    k.i('pe', 'matmul', reads=[ET, VB[sl]], writes=[ob] if first else [], pwrites=[] if first else [ob],
                        out=ob[:, g * 64:(g + 1) * 64], lhsT=ET[:, g * 2 + half, :], rhs=VB[sl][:, kh * 64:(kh + 1) * 64],
                        start=(half == 0), stop=(half == 1))
                    first = False
            k.i('dve', 'tensor_tensor', reads=[ob, rs], pwrites=[ao], out=ao[:, kh * 256:(kh + 1) * 256].rearrange("p (a b) -> p a b", a=4),
                in0=ob[:, 0:256].rearrange("p (a b) -> p a b", a=4), in1=rs[:].unsqueeze(2).to_broadcast([128, 4, 64]), op=ALU.mult)
        AO = p.dr['ATT_O']
        k.dma('sp', AO, AO.ap()[(t - 1) * 128:t * 128, :], ao, ao[:], partial=True)


def phase_MEM(k, p):
    inp, sb = p.inp, k.sb
    memnb = p.load_ln('memn')
    xknb = p.bcast_load('xkn', 128)
    WB = sb('WB', [128, 16, 1024], BF16)
    p.load_w(WB, 'w_xkv', D, 0, 1024)
    scr = (sb('sq', [128, 1024]), sb('st', [128, 16]), sb('xn', [128, 1024]), sb('r1', [128, 16, 8]), sb('r2', [128, 16, 8]), sb('r3', [128, 16, 8]))
    uTs = [sb('uTm%d' % i, [128, 16, 128], BF16) for i in range(2)]
    mkv = [sb('mkv%d' % i, [128, 1024]) for i in range(2)]
    for t in range(2):
        uT = uTs[t]
        k.begin_fill(uT)
        p.front(inp['mem'], inp['mem'].ap()[t * 128:(t + 1) * 128, :], 128, memnb, uT, lambda half, uT=uT: uT[:, half * 8:(half + 1) * 8, :])
        uf = lambda dc, uT=uT: uT[:, dc, :]
        psk = p.linear_tm(uT, uf, 128, WB, lambda dc: WB[:, dc, 0:512], 512)
        psv = p.linear_tm(uT, uf, 128, WB, lambda dc: WB[:, dc, 512:1024], 512)
        m = mkv[t]
        head_norm(k, p, psk, psk[:, 0:512], 128, 4, 128, xknb, m, m[:, 0:512], scr)
        k.i('act', 'copy', reads=[psv], pwrites=[m], out=m[:, 512:1024], in_=psv[:, 0:512])
        k.i('act', 'copy', reads=[m], pwrites=[p.MEMV], out=p.MEMV[:, t, :], in_=m[:, 512:1024])
        kb = sb('kb%d' % t, [128, 512], BF16)
        k.i('dve', 'tensor_copy', reads=[m], writes=[kb], out=kb[:], in_=m[:, 0:512])
        HT = p.H[2]
        for hd in range(4):
            k.i('pe', 'transpose', reads=[kb, p.identb], writes=[HT] if hd == 0 else [], pwrites=[] if hd == 0 else [HT],
                out=HT[:, hd, :], in_=kb[:, hd * 128:(hd + 1) * 128], identity=p.identb[:])
        k.i('act', 'copy', reads=[HT], pwrites=[p.MEMKT], out=p.MEMKT[:, :, t * 128:(t + 1) * 128], in_=HT[:, 0:4, :])
        k.dma('sp', p.out['o_mk'], p.out['o_mk'].ap()[t * 128:(t + 1) * 128, :], m, m[:, 0:512], partial=(t > 0))
        k.dma('sp', p.out['o_mv'], p.out['o_mv'].ap()[t * 128:(t + 1) * 128, :], m, m[:, 512:1024], partial=(t > 0))


def phase_OX(k, p):
    inp, sb = p.inp, k.sb
    H, Fb, identb = p.H, p.Fb, p.identb
    ln2b = p.load_ln('ln2')
    xqnb = p.bcast_load('xqn', 128)
    WO = sb('WO', [128, 16, D], BF16)
    p.load_w(WO, 'w_out', D, 0, D)
    WQ = sb('WQ', [128, 16, 512], BF16)
    p.load_w(WQ, 'w_xq', D, 0, 512)
    WX = sb('WX', [128, 4, D], BF16)
    p.load_w(WX, 'w_xo', 512, 0, D)
    scr = (sb('sq', [128, 1024]), sb('st', [128, 16]), sb('xn', [128, 1024]), sb('r1', [128, 16, 8]), sb('r2', [128, 16, 8]), sb('r3', [128, 16, 8]))
    att_sb = [sb('att_sb%d' % i, [128, 1024], BF16) for i in range(2)]
    aT = [sb('aT%d' % i, [128, 8, 128], BF16) for i in range(2)]
    rwT = [sb('rwT%d' % i, [128, 8, 128], BF16) for i in range(2)]
    xres = [sb('xres0', [128, D])] * 2
    h1 = [sb('h1_0', [128, D])] * 2
    ub2 = sb('ub2', [128, D], BF16)
    ss2 = sb('ss2', [128, 1])
    uT2 = sb('uT2', [128, 16, 128], BF16)
    qx = sb('qx', [128, 512]); qxb = sb('qxb', [128, 512], BF16); qxT = sb('qxT', [128, 4, 128], BF16)
    Ex = sb('Ex', [128, 4, 256], BF16); ETx = sb('ETx', [128, 8, 128], BF16)
    mx = sb('mxx', [128, 4]); negm = sb('negmx', [128, 4]); rs = sb('rsx', [128, 4])
    oxb = sb('oxb', [128, 512], BF16); oxT = sb('oxT', [128, 4, 128], BF16)
    rws_sb = sb('rws_sb', [128, 1024], BF16)
    cand = [sb('cand%d' % q, [128, 8, 128], BF16) for q in range(4)]
    ohb = p.bcast_load('ohj', 4)
    HT = H[2]
    AO, RWG, H2, RWS = p.dr['ATT_O'], p.dr['RWG'], p.dr['H2'], p.dr['RWS']
    for t in range(9):
        n = 16 if t == 8 else 128
        b2 = t % 2
        xr_ = xres[b2]; a_sb = att_sb[b2]; aT_ = aT[b2]; rT = rwT[b2]; hh = h1[b2]
        xrow = 128 + t * 128
        k.dma('sp', xr_, xr_[0:n, :], inp['x_tok'], inp['x_tok'].ap()[xrow:xrow + n, :])
        k.dma('sp', a_sb, a_sb[0:n, :], AO, AO.ap()[t * 128:t * 128 + n, :])
        k.begin_fill(aT_)
        p.transpose16(a_sb, n, aT_, lambda half, aT_=aT_, n=n: aT_[:, 0:8, 0:n], nchunks=8)
        if t < 8:
            for q in range(4):
                rg = RWG[2 * q + t // 4]
                k.dma('sp', cand[q], cand[q][:], rg, rg.ap()[:, (t % 4) * 128:(t % 4 + 1) * 128].rearrange("(c p) t -> p c t", p=128))
            k.i('dve', 'tensor_scalar', reads=[cand[0], ohb], writes=[rT], out=rT[:], in0=cand[0][:], scalar1=ohb[:, 0:1], scalar2=None, op0=ALU.mult)
            for q in range(1, 4):
                k.i('dve', 'scalar_tensor_tensor', reads=[cand[q], ohb, rT], writes=[rT], out=rT[:], in0=cand[q][:], scalar=ohb[:, q:q + 1], in1=rT[:],
                    op0=ALU.mult, op1=ALU.add)
        else:
            k.dma('sp', rws_sb, rws_sb[0:16, :], RWS, RWS.ap())
            k.begin_fill(rT)
            p.transpose16(rws_sb, 16, rT, lambda half, rT=rT: rT[:, 0:8, 0:16], nchunks=8)
        for nchk in range(4):
            ps = Fb[nchk]
            for c in range(16):
                src = aT_ if c < 8 else rT
                k.i('pe', 'matmul', reads=[src, WO], writes=[ps] if c == 0 else [], pwrites=[] if c == 0 else [ps],
                    out=ps[0:n, :], lhsT=src[:, c % 8, 0:n], rhs=WO[:, c, nchk * 512:(nchk + 1) * 512], start=(c == 0), stop=(c == 15))
            k.i('dve', 'tensor_tensor', reads=[ps, xr_], writes=[hh] if nchk == 0 else [], pwrites=[] if nchk == 0 else [hh],
                out=hh[0:n, nchk * 512:(nchk + 1) * 512], in0=ps[0:n, :], in1=xr_[0:n, nchk * 512:(nchk + 1) * 512], op=ALU.add)
        k.i('act', 'activation', reads=[hh], writes=[ub2, ss2], out=ub2[0:n, :], in_=hh[0:n, :], func=AF.Square, accum_out=ss2[0:n, :])
        k.i('act', 'activation', reads=[ss2], writes=[ss2], out=ss2[0:n, :], in_=ss2[0:n, :], func=AF.Sqrt, scale=1.0 / D, bias=EPS)
        k.i('dve', 'reciprocal', reads=[ss2], writes=[ss2], out=ss2[0:n, :], in_=ss2[0:n, :])
        k.i('dve', 'scalar_tensor_tensor', reads=[hh, ss2, ln2b], writes=[ub2], out=ub2[0:n, :], in0=hh[0:n, :], scalar=ss2[0:n, 0:1],
            in1=ln2b[0:n, :], op0=ALU.mult, op1=ALU.mult)
        k.begin_fill(uT2)
        p.transpose16(ub2, n, uT2, lambda half, n=n: uT2[:, half * 8:(half + 1) * 8, 0:n])
        ps = p.linear_tm(uT2, lambda dc, n=n: uT2[:, dc, 0:n], n, WQ, lambda dc: WQ[:, dc, :], 512, ps=Fb[4])
        head_norm(k, p, ps, ps[0:n, 0:512], n, 4, 128, xqnb, qx, qx[0:n, :], scr)
        if t < 8:
            k.i('act', 'activation', reads=[qx], writes=[qxb], out=qxb[0:n, :], in_=qx[0:n, :], func=AF.Identity, scale=1.0 / math.sqrt(128.0))
            k.begin_fill(qxT)
            p.transpose16(qxb, n, qxT, lambda half, n=n: qxT[:, 0:4, 0:n], nchunks=4)
            for hp in range(2):
                bank = Fb[2 + hp]
                for h2_ in range(2):
                    hd = hp * 2 + h2_
                    k.i('pe', 'matmul', reads=[qxT, p.MEMKT], writes=[bank] if h2_ == 0 else [], pwrites=[] if h2_ == 0 else [bank],
                        out=bank[0:n, h2_ * 256:(h2_ + 1) * 256], lhsT=qxT[:, hd, 0:n], rhs=p.MEMKT[:, hd, :], start=True, stop=True)
                k.i('dve', 'tensor_reduce', reads=[bank], writes=[mx] if hp == 0 else [], pwrites=[] if hp == 0 else [mx],
                    out=mx[0:n, hp * 2:hp * 2 + 2], in_=bank[0:n, :].rearrange("p (a b) -> p a b", a=2), axis=AX.X, op=ALU.max)
            k.i('dve', 'tensor_scalar', reads=[mx], writes=[negm], out=negm[0:n, :], in0=mx[0:n, :], scalar1=-1.0, scalar2=None, op0=ALU.mult)
            for hd in range(4):
                bank = Fb[2 + hd // 2]
                k.i('act', 'activation', reads=[bank, negm], writes=[Ex, rs] if hd == 0 else [], pwrites=[] if hd == 0 else [Ex, rs],
                    out=Ex[0:n, hd, :], in_=bank[0:n, (hd % 2) * 256:(hd % 2 + 1) * 256], func=AF.Exp, bias=negm[0:n, hd:hd + 1],
                    accum_out=rs[0:n, hd:hd + 1])
            k.i('dve', 'reciprocal', reads=[rs], writes=[rs], out=rs[0:n, :], in_=rs[0:n, :])
            for hd in range(4):
                for half in range(2):
                    i8 = hd * 2 + half
                    k.i('pe', 'transpose', reads=[Ex, identb], writes=[HT] if i8 == 0 else [], pwrites=[] if i8 == 0 else [HT],
                        out=HT[:, i8, 0:n], in_=Ex[0:n, hd, half * 128:(half + 1) * 128], identity=identb[0:n, 0:n])
            k.i('dve', 'tensor_copy', reads=[HT], writes=[ETx], out=ETx[:, :, 0:n], in_=HT[:, :, 0:n])
            ob = Fb[4]
            first = True
            for hd in range(4):
                for half in range(2):
                    k.i('pe', 'matmul', reads=[ETx, p.MEMV], writes=[ob] if first else [], pwrites=[] if first else [ob],
                        out=ob[0:n, hd * 128:(hd + 1) * 128], lhsT=ETx[:, hd * 2 + half, 0:n], rhs=p.MEMV[:, half, hd * 128:(hd + 1) * 128],
                        start=(half == 0), stop=(half == 1))
                    first = False
            k.i('dve', 'tensor_tensor', reads=[ob, rs], writes=[oxb], out=oxb[0:n, :].rearrange("p (a b) -> p a b", a=4),
                in0=ob[0:n, :].rearrange("p (a b) -> p a b", a=4), in1=rs[0:n, :].unsqueeze(2).to_broadcast([n, 4, 128]), op=ALU.mult)
        else:
            k.dma('sp', p.dr['QXd'], p.dr['QXd'].ap(), qx, qx[0:16, :])
            k.dma('sp', p.H2t[8], H2.ap()[1024:1040, :], hh, hh[0:16, :])
            continue
        k.begin_fill(oxT)
        p.transpose16(oxb, n, oxT, lambda half, n=n: oxT[:, 0:4, 0:n], nchunks=4)
        for nchk in range(4):
            ps = Fb[nchk]
            for c in range(4):
                k.i('pe', 'matmul', reads=[oxT, WX], writes=[ps] if c == 0 else [], pwrites=[] if c == 0 else [ps],
                    out=ps[0:n, :], lhsT=oxT[:, c, 0:n], rhs=WX[:, c, nchk * 512:(nchk + 1) * 512], start=(c == 0), stop=(c == 3))
            k.i('dve', 'tensor_tensor', reads=[ps, hh], writes=[hh], out=hh[0:n, nchk * 512:(nchk + 1) * 512], in0=ps[0:n, :],
                in1=hh[0:n, nchk * 512:(nchk + 1) * 512], op=ALU.add)
        k.dma('sp', p.H2t[t], H2.ap()[t * 128:t * 128 + n, :], hh, hh[0:n, :])


def sample_xattn(k, p, qx, oxb):
    k.i('pool', 'memset', writes=[oxb], ap=oxb[0:16, :], constant=0.0)


def phase_M(k, p, mark0):
    inp, sb, nc = p.inp, k.sb, p.nc
    H, Fb, identb, identf = p.H, p.Fb, p.identb, p.identf
    H2 = p.dr['H2']
    CAP = 64
    U16 = sb('U16', [128, 9, D], BF16)
    GATE = sb('GATEm', [128, 9, 64]); ASG = sb('ASG', [128, 9, 64], BF16); RANK = sb('RANK', [128, 9, 64])
    iota = p.bcast_load('iota64', 64)
    onesb = sb('onesb', [128, 128], BF16)
    trib = sb('trib', [128, 128], BF16)
    k.i('pool', 'memset', writes=[onesb], ap=onesb[:], constant=1.0)
    k.i('pool', 'memset', writes=[U16], ap=U16[:], constant=0.0)
    k.i('pool', 'memset', writes=[GATE], ap=GATE[:], constant=0.0)
    k.i('pool', 'memset', writes=[ASG], ap=ASG[:], constant=0.0)
    trif = sb('trif', [128, 128])
    k.dma('sp', trif, trif[:], inp['rmask'], inp['rmask'].ap()[0])
    k.i('dve', 'tensor_copy', reads=[trif], writes=[trib], out=trib[:], in_=trif[:])
    WG = [sb('WG%d' % i, [128, 16, 512], BF16) for i in range(2)]
    WU = [sb('WU%d' % i, [128, 16, 512], BF16) for i in range(2)]
    WD = [sb('WD%d' % i, [128, 4, D], BF16) for i in range(2)]

    def load_expert(e):
        b = e % 2
        k.dma('pool', WG[b], WG[b][:], inp['e_g'], inp['e_g'].ap()[e].rearrange("(p c) n -> p c n", c=16))
        k.dma('pool', WU[b], WU[b][:], inp['e_u'], inp['e_u'].ap()[e].rearrange("(p c) n -> p c n", c=16))
        k.dma('pool', WD[b], WD[b][:], inp['e_d'], inp['e_d'].ap()[e].rearrange("(p c) n -> p c n", c=4))
    load_expert(0)
    load_expert(1)
    mark1 = nc.sbuf_base
    p.alloc_front()
    fb = p.fb
    ln3b = p.load_ln('ln3')
    WR = sb('WR', [128, 16, 72])
    k.dma('sp', WR, WR[:], inp['w_rt'], inp['w_rt'].ap().rearrange("(c p) n -> p c n", p=128))
    brt = p.bcast_load('b_rt', 72)
    u32 = sb('u32', [128, D]); uT32 = sb('uT32', [128, 16, 128])
    LG = sb('LG', [128, 72])
    sm = {n: sb('m_' + n, [128, 8]) for n in ['ohg', 'e8', 'oh1', 'oh2', 'e8b', 'g8', 't8']}
    s1 = {n: sb('m1_' + n, [128, 1]) for n in ['gmax', 'ngmax', 'se', 'm1', 'm2', 'w1', 'w2']}
    sel3 = sb('sel3', [128, 8, 8])
    k.begin_fill(U16, GATE, ASG)
    for t in range(9):
        n = 16 if t == 8 else 128
        x = fb.xt[t % 2]; ss = fb.ssb[t % 4]; ubf = fb.ub[t % 2]
        k.dma('sp', x, x[0:n, :], p.H2t[t], H2.ap()[t * 128:t * 128 + n, :])
        k.i('act', 'activation', reads=[x], writes=[ubf, ss], out=ubf[0:n, :], in_=x[0:n, :], func=AF.Square, accum_out=ss[0:n, :])
        k.i('act', 'activation', reads=[ss], writes=[ss], out=ss[0:n, :], in_=ss[0:n, :], func=AF.Sqrt, scale=1.0 / D, bias=EPS)
        k.i('dve', 'reciprocal', reads=[ss], writes=[ss], out=ss[0:n, :], in_=ss[0:n, :])
        k.i('dve', 'scalar_tensor_tensor', reads=[x, ss, ln3b], writes=[u32], out=u32[0:n, :], in0=x[0:n, :], scalar=ss[0:n, 0:1], in1=ln3b[0:n, :],
            op0=ALU.mult, op1=ALU.mult)
        k.i('act', 'copy', reads=[u32], pwrites=[U16], out=U16[0:n, t, :], in_=u32[0:n, :])
        k.begin_fill(uT32)
        for q4 in range(4):
            bank = Fb[q4 % 2]
            for c4 in range(4):
                dc = q4 * 4 + c4
                k.i('pe', 'transpose', reads=[u32, identf], writes=[bank] if c4 == 0 else [], pwrites=[] if c4 == 0 else [bank],
                    out=bank[:, c4 * 128:c4 * 128 + n], in_=u32[0:n, dc * 128:(dc + 1) * 128], identity=identf[0:n, 0:n])
            k.i('act' if q4 % 2 == 0 else 'dve', 'copy' if q4 % 2 == 0 else 'tensor_copy', reads=[bank], pwrites=[uT32],
                out=uT32[:, q4 * 4:(q4 + 1) * 4, 0:n], in_=bank[:, :].rearrange("p (a b) -> p a b", a=4)[:, :, 0:n])
        ps = Fb[4]
        for dc in range(16):
            k.i('pe', 'matmul', reads=[uT32, WR], writes=[ps] if dc == 0 else [], pwrites=[] if dc == 0 else [ps],
                out=ps[0:n, 0:72], lhsT=uT32[:, dc, 0:n], rhs=WR[:, dc, :], start=(dc == 0), stop=(dc == 15))
        k.i('dve', 'tensor_tensor', reads=[ps, brt], writes=[LG], out=LG[0:n, :], in0=ps[0:n, 0:72], in1=brt[0:n, :], op=ALU.add)
        a_ = lambda r: r[0:n, :]
        gl = LG[0:n, 0:8]
        k.i('dve', 'tensor_reduce', reads=[LG], writes=[s1['gmax']], out=a_(s1['gmax']), in_=gl, axis=AX.X, op=ALU.max)
        k.i('dve', 'tensor_scalar', reads=[LG, s1['gmax']], writes=[sm['ohg']], out=a_(sm['ohg']), in0=gl, scalar1=s1['gmax'][0:n, 0:1], scalar2=None, op0=ALU.is_equal)
        k.i('dve', 'tensor_scalar', reads=[s1['gmax']], writes=[s1['ngmax']], out=a_(s1['ngmax']), in0=a_(s1['gmax']), scalar1=-1.0, scalar2=None, op0=ALU.mult)
        k.i('act', 'activation', reads=[LG, s1['ngmax']], writes=[sm['t8'], s1['se']], out=a_(sm['t8']), in_=gl, func=AF.Exp, bias=s1['ngmax'][0:n, 0:1],
            accum_out=a_(s1['se']))
        k.i('dve', 'reciprocal', reads=[s1['se']], writes=[s1['se']], out=a_(s1['se']), in_=a_(s1['se']))
        k.i('dve', 'tensor_tensor', reads=[LG, sm['ohg']], writes=[sel3], out=sel3[0:n, :, :], in0=LG[0:n, 8:72].rearrange("p (g e) -> p g e", g=8),
            in1=sm['ohg'][0:n, :].unsqueeze(2).to_broadcast([n, 8, 8]), op=ALU.mult)
        k.i('dve', 'tensor_reduce', reads=[sel3], writes=[sm['e8']], out=a_(sm['e8']), in_=sel3[0:n, :, :].rearrange("p g e -> p e g"), axis=AX.X, op=ALU.add)
        k.i('dve', 'tensor_reduce', reads=[sm['e8']], writes=[s1['m1']], out=a_(s1['m1']), in_=a_(sm['e8']), axis=AX.X, op=ALU.max)
        k.i('dve', 'tensor_scalar', reads=[sm['e8'], s1['m1']], writes=[sm['oh1']], out=a_(sm['oh1']), in0=a_(sm['e8']), scalar1=s1['m1'][0:n, 0:1], scalar2=None, op0=ALU.is_equal)
        k.i('dve', 'scalar_tensor_tensor', reads=[sm['oh1'], sm['e8']], writes=[sm['e8b']], out=a_(sm['e8b']), in0=a_(sm['oh1']), scalar=-1e30, in1=a_(sm['e8']),
            op0=ALU.mult, op1=ALU.add)
        k.i('dve', 'tensor_reduce', reads=[sm['e8b']], writes=[s1['m2']], out=a_(s1['m2']), in_=a_(sm['e8b']), axis=AX.X, op=ALU.max)
        k.i('dve', 'tensor_scalar', reads=[sm['e8b'], s1['m2']], writes=[sm['oh2']], out=a_(sm['oh2']), in0=a_(sm['e8b']), scalar1=s1['m2'][0:n, 0:1], scalar2=None, op0=ALU.is_equal)
        k.i('dve', 'tensor_tensor', reads=[s1['m2'], s1['m1']], writes=[s1['w1']], out=a_(s1['w1']), in0=a_(s1['m2']), in1=a_(s1['m1']), op=ALU.subtract)
        k.i('act', 'activation', reads=[s1['w1']], writes=[s1['w1']], out=a_(s1['w1']), in_=a_(s1['w1']), func=AF.Exp)
        k.i('dve', 'tensor_scalar', reads=[s1['w1']], writes=[s1['w1']], out=a_(s1['w1']), in0=a_(s1['w1']), scalar1=1.0, scalar2=None, op0=ALU.add)
        k.i('dve', 'reciprocal', reads=[s1['w1']], writes=[s1['w1']], out=a_(s1['w1']), in_=a_(s1['w1']))
        k.i('dve', 'tensor_scalar', reads=[s1['w1']], writes=[s1['w2']], out=a_(s1['w2']), in0=a_(s1['w1']), scalar1=-1.0, scalar2=1.0, op0=ALU.mult, op1=ALU.add)
        k.i('dve', 'tensor_tensor', reads=[s1['w1'], s1['se']], writes=[s1['w1']], out=a_(s1['w1']), in0=a_(s1['w1']), in1=a_(s1['se']), op=ALU.mult)
        k.i('dve', 'tensor_tensor', reads=[s1['w2'], s1['se']], writes=[s1['w2']], out=a_(s1['w2']), in0=a_(s1['w2']), in1=a_(s1['se']), op=ALU.mult)
        k.i('dve', 'tensor_scalar', reads=[sm['oh1'], s1['w1']], writes=[sm['g8']], out=a_(sm['g8']), in0=a_(sm['oh1']), scalar1=s1['w1'][0:n, 0:1], scalar2=None, op0=ALU.mult)
        k.i('dve', 'scalar_tensor_tensor', reads=[sm['oh2'], s1['w2'], sm['g8']], writes=[sm['g8']], out=a_(sm['g8']), in0=a_(sm['oh2']), scalar=s1['w2'][0:n, 0:1],
            in1=a_(sm['g8']), op0=ALU.mult, op1=ALU.add)
        k.i('dve', 'tensor_tensor', reads=[sm['oh1'], sm['oh2']], writes=[sm['t8']], out=a_(sm['t8']), in0=a_(sm['oh1']), in1=a_(sm['oh2']), op=ALU.add)
        k.i('dve', 'tensor_tensor', reads=[sm['ohg'], sm['g8']], pwrites=[GATE], out=GATE[0:n, t, :].rearrange("p (g e) -> p g e", g=8),
            in0=sm['ohg'][0:n, :].unsqueeze(2).to_broadcast([n, 8, 8]), in1=sm['g8'][0:n, :].unsqueeze(1).to_broadcast([n, 8, 8]), op=ALU.mult)
        k.i('dve', 'tensor_tensor', reads=[sm['ohg'], sm['t8']], pwrites=[ASG], out=ASG[0:n, t, :].rearrange("p (g e) -> p g e", g=8),
            in0=sm['ohg'][0:n, :].unsqueeze(2).to_broadcast([n, 8, 8]), in1=sm['t8'][0:n, :].unsqueeze(1).to_broadcast([n, 8, 8]), op=ALU.mult)
    k.begin_fill(RANK)
    for t in range(9):
        ps = Fb[t % 2]
        k.i('pe', 'matmul', reads=[trib, ASG], writes=[ps], out=ps[:, 0:64], lhsT=trib[:], rhs=ASG[:, t, :], start=True, stop=(t == 0))
        for t2 in range(t):
            k.i('pe', 'matmul', reads=[onesb, ASG], pwrites=[ps], out=ps[:, 0:64], lhsT=onesb[:], rhs=ASG[:, t2, :], start=False, stop=(t2 == t - 1))
        k.i('dve', 'scalar_tensor_tensor', reads=[ps, ASG], pwrites=[RANK], out=RANK[:, t, :], in0=ps[:, 0:64], scalar=1.0, in1=ASG[:, t, :], op0=ALU.add, op1=ALU.mult)
    k.i('dve', 'tensor_scalar', reads=[RANK], writes=[RANK], out=RANK[:], in0=RANK[:], scalar1=-1.0, scalar2=None, op0=ALU.add)
    k.barrier()
    nc.sbuf_base = mark1
    SEL = sb('SEL', [128, 9, 128], BF16); SELG = sb('SELG', [128, 9, 128], BF16)
    SELGT = [sb('SELGT%d' % i, [128, 9, 128], BF16) for i in range(4)]
    UT = sb('UTp', [128, 16, 128], BF16)
    HB = sb('HB', [128, 512], BF16); HTs = sb('HTs', [128, 4, 128], BF16)
    sg = sb('sg', [128, 512])
    YG = sb('YG', [128, 4, D], BF16)
    OUTt = sb('OUTt', [128, D])
    iota3 = iota[:, :].unsqueeze(1).to_broadcast([128, 9, 64])

    for grp in range(8):
        k.begin_fill(YG)
        for pr in range(4):
            e0 = grp * 8 + pr * 2
            for e2 in range(2):
                e = e0 + e2
                k.i('dve', 'tensor_tensor', reads=[iota, RANK], writes=[SEL] if e2 == 0 else [], pwrites=[] if e2 == 0 else [SEL],
                    out=SEL[:, :, e2 * 64:(e2 + 1) * 64], in0=iota3, in1=RANK[:, :, e:e + 1].to_broadcast([128, 9, 64]), op=ALU.is_equal)
                k.i('dve', 'tensor_tensor', reads=[SEL, GATE], writes=[SELG] if e2 == 0 else [], pwrites=[] if e2 == 0 else [SELG],
                    out=SELG[:, :, e2 * 64:(e2 + 1) * 64], in0=SEL[:, :, e2 * 64:(e2 + 1) * 64], in1=GATE[:, :, e:e + 1].to_broadcast([128, 9, 64]), op=ALU.mult)
            sgt = SELGT[pr]
            k.begin_fill(sgt)
            for t in range(9):
                hb_ = H[t // 8]
                k.i('pe', 'transpose', reads=[SELG, identb], writes=[hb_] if t % 8 == 0 else [], pwrites=[] if t % 8 == 0 else [hb_],
                    out=hb_[:, t % 8, :], in_=SELG[:, t, :], identity=identb[:])
            k.i('act', 'copy', reads=[H[0]], pwrites=[sgt], out=sgt[:, 0:8, :], in_=H[0][:, :, :])
            k.i('dve', 'tensor_copy', reads=[H[1]], pwrites=[sgt], out=sgt[:, 8:9, :], in_=H[1][:, 0:1, :])
            k.begin_fill(UT)
            for q4 in range(4):
                bank = Fb[q4]
                for c4 in range(4):
                    dc = q4 * 4 + c4
                    for t in range(9):
                        k.i('pe', 'matmul', reads=[U16, SEL], writes=[bank] if (c4 == 0 and t == 0) else [], pwrites=[] if (c4 == 0 and t == 0) else [bank],
                            out=bank[:, c4 * 128:(c4 + 1) * 128], lhsT=U16[:, t, :].rearrange("q (p c) -> q c p", c=16)[:, dc, :], rhs=SEL[:, t, :], start=(t == 0), stop=(t == 8))
                k.i('act' if q4 % 2 == 0 else 'dve', 'copy' if q4 % 2 == 0 else 'tensor_copy', reads=[bank], pwrites=[UT],
                    out=UT[:, q4 * 4:(q4 + 1) * 4, :], in_=bank[:, :].rearrange("p (a b) -> p a b", a=4))
            for e2 in range(2):
                e = e0 + e2
                b = e % 2
                if 1 <= e and e + 1 < 64:
                    load_expert(e + 1)
                lo, hi = e2 * 64, (e2 + 1) * 64
                tp = (0, lo)
                for (W_, bank) in ((WG[b], Fb[0]), (WU[b], Fb[1])):
                    for dc in range(16):
                        k.i('pe', 'matmul', reads=[UT, W_], writes=[bank] if dc == 0 else [], pwrites=[] if dc == 0 else [bank],
                            out=bank[lo:hi, :], lhsT=UT[:, dc, lo:hi], rhs=W_[:, dc, :], start=(dc == 0), stop=(dc == 15), tile_position=tp)
                k.i('act', 'activation', reads=[Fb[0]], writes=[sg], out=sg[lo:hi, :], in_=Fb[0][lo:hi, :], func=AF.Silu)
                k.i('dve', 'tensor_tensor', reads=[sg, Fb[1]], writes=[HB], out=HB[lo:hi, :], in0=sg[lo:hi, :], in1=Fb[1][lo:hi, :], op=ALU.mult)
                hb_ = H[2]
                for fc in range(4):
                    k.i('pe', 'transpose', reads=[HB, identb], writes=[hb_] if fc == 0 else [], pwrites=[] if fc == 0 else [hb_],
                        out=hb_[:, fc, 0:64], in_=HB[lo:hi, :].rearrange("q (p c) -> q c p", c=4)[:, fc, :], identity=identb[lo:hi, lo:hi])
                k.i('act', 'copy', reads=[hb_], writes=[HTs], out=HTs[:, :, 0:64], in_=hb_[:, 0:4, 0:64])
                for half in range(2):
                    for nc2 in range(2):
                        bank = Fb[2 + nc2]
                        ncol = half * 2 + nc2
                        for fc in range(4):
                            k.i('pe', 'matmul', reads=[HTs, WD[b]], writes=[bank] if fc == 0 else [], pwrites=[] if fc == 0 else [bank],
                                out=bank[lo:hi, :], lhsT=HTs[:, fc, 0:64], rhs=WD[b][:, fc, ncol * 512:(ncol + 1) * 512], start=(fc == 0), stop=(fc == 3),
                                tile_position=tp)
                        k.i('act' if nc2 == 0 else 'dve', 'copy' if nc2 == 0 else 'tensor_copy', reads=[bank], pwrites=[YG],
                            out=YG[lo:hi, pr, ncol * 512:(ncol + 1) * 512], in_=bank[lo:hi, :])
        for t in range(9):
            n = 16 if t == 8 else 128
            k.begin_fill(OUTt)
            for nchk in range(4):
                bank = Fb[nchk]
                for pr in range(4):
                    k.i('pe', 'matmul', reads=[SELGT[pr], YG], writes=[bank] if pr == 0 else [], pwrites=[] if pr == 0 else [bank],
                        out=bank[:, :], lhsT=SELGT[pr][:, t, :], rhs=YG[:, pr, nchk * 512:(nchk + 1) * 512], start=(pr == 0), stop=(pr == 3))
                k.i('act' if nchk % 2 == 0 else 'dve', 'copy' if nchk % 2 == 0 else 'tensor_copy', reads=[bank],
                    pwrites=[OUTt], out=OUTt[:, nchk * 512:(nchk + 1) * 512], in_=bank[:, :])
            k.dma('pool', p.H2t[t], H2.ap()[t * 128:t * 128 + n, :], OUTt, OUTt[0:n, :], accum_op=ALU.add)
    for t in range(9):
        n = 16 if t == 8 else 128
        k.dma('sp', p.out['o_y'], p.out['o_y'].ap()[t * 128:t * 128 + n, :], p.H2t[t], H2.ap()[t * 128:t * 128 + n, :], partial=True)


def phase_SR(k, p):
    inp, sb = p.inp, k.sb
    Fb, H, identb, identf = p.Fb, p.H, p.identb, p.identf
    N = 16
    SRd, SV, YNd, RWS = p.dr['SRd'], p.dr['SV'], p.dr['YNd'], p.dr['RWS']
    sr = sb('s_sr', [N, RW_COLS]); pv = sb('s_prev', [N, RW_COLS]); mub = sb('s_mu', [N, RW_COLS])
    k.dma('sp', sr, sr[:], SRd, SRd.ap())
    k.dma('sp', pv, pv[:], inp['sh_s'], inp['sh_s'].ap())
    k.dma('sp', mub, mub[:], inp['mu_f'], inp['mu_f'].ap().partition_broadcast(N))
    k.i('dve', 'tensor_tensor', reads=[pv, sr], writes=[pv], out=pv[:], in0=pv[:], in1=sr[:], op=ALU.subtract)
    k.i('dve', 'tensor_tensor', reads=[pv, mub], writes=[pv], out=pv[:], in0=pv[:], in1=mub[:], op=ALU.mult)
    k.i('dve', 'tensor_tensor', reads=[pv, sr], writes=[pv], out=pv[:], in0=pv[:], in1=sr[:], op=ALU.add)
    xm = pv
    xr, xk, xv = xm[:, 0:1024], xm[:, 1024:2048], xm[:, 2048:3072]
    pvb = sb('s_pvb', [N, 7, 1024])
    k.dma('sp', pvb, pvb[:], inp['pvf'], inp['pvf'].ap().partition_broadcast(N))
    lf = sb('s_lf', [128, 4, 1024]); lwb = sb('s_lwb', [128, 4, 1024], BF16)
    k.i('pool', 'memset', writes=[lf], ap=lf[:], constant=0.0)
    k.dma('sp', lf, lf[0:64, 0, :], inp['w2_f'], inp['w2_f'].ap())
    k.dma('sp', lf, lf[0:64, 1, :], inp['a2_f'], inp['a2_f'].ap(), partial=True)
    k.dma('sp', lf, lf[:, 2, :], inp['g2_f'], inp['g2_f'].ap()[0:128, :], partial=True)
    k.dma('sp', lf, lf[0:32, 3, :], inp['g2_f'], inp['g2_f'].ap()[128:160, :], partial=True)
    k.i('dve', 'tensor_copy', reads=[lf], writes=[lwb], out=lwb[:], in_=lf[:])
    li = sb('s_li', [N, 4, 128], BF16)
    k.i('pool', 'memset', writes=[li], ap=li[:], constant=0.0)
    k.i('act', 'activation', reads=[xm], writes=[li], out=li[:, 0, 0:64], in_=xm[:, 3072:3136], func=AF.Tanh)
    k.i('act', 'copy', reads=[xm], pwrites=[li], out=li[:, 1, 0:64], in_=xm[:, 3136:3200])
    k.i('act', 'activation', reads=[xm], pwrites=[li], out=li[:, 2, :], in_=xm[:, 3200:3328], func=AF.Sigmoid)
    k.i('act', 'activation', reads=[xm], pwrites=[li], out=li[:, 3, 0:32], in_=xm[:, 3328:3360], func=AF.Sigmoid)
    liT = sb('s_liT', [128, 4, N], BF16)
    HT = H[2]
    for i in range(4):
        k.i('pe', 'transpose', reads=[li, identb], writes=[HT] if i == 0 else [], pwrites=[] if i == 0 else [HT],
            out=HT[:, i, 0:N], in_=li[:, i, :], identity=identb[0:N, 0:N])
    k.i('act', 'copy', reads=[HT], writes=[liT], out=liT[:], in_=HT[:, 0:4, 0:N])
    lw = sb('s_lw', [N, 1024]); a = sb('s_a', [N, 1024]); gt = sb('s_g', [N, 1024])
    for hh in range(2):
        cs_ = slice(hh * 512, (hh + 1) * 512)
        ps = Fb[0]
        k.i('pe', 'matmul', reads=[liT, lwb], writes=[ps], out=ps[0:N, :], lhsT=liT[0:64, 0, :], rhs=lwb[0:64, 0, cs_], start=True, stop=True)
        k.i('dve', 'tensor_tensor', reads=[ps, pvb], writes=[lw] if hh == 0 else [], pwrites=[] if hh == 0 else [lw], out=lw[:, cs_], in0=ps[0:N, :], in1=pvb[:, 0, cs_], op=ALU.add)
        ps = Fb[1]
        k.i('pe', 'matmul', reads=[liT, lwb], writes=[ps], out=ps[0:N, :], lhsT=liT[0:64, 1, :], rhs=lwb[0:64, 1, cs_], start=True, stop=True)
        k.i('dve', 'tensor_tensor', reads=[ps, pvb], writes=[a] if hh == 0 else [], pwrites=[] if hh == 0 else [a], out=a[:, cs_], in0=ps[0:N, :], in1=pvb[:, 1, cs_], op=ALU.add)
        ps = Fb[2]
        k.i('pe', 'matmul', reads=[liT, lwb], writes=[ps], out=ps[0:N, :], lhsT=liT[:, 2, :], rhs=lwb[:, 2, cs_], start=True, stop=False)
        k.i('pe', 'matmul', reads=[liT, lwb], pwrites=[ps], out=ps[0:N, :], lhsT=liT[0:32, 3, :], rhs=lwb[0:32, 3, cs_], start=False, stop=True)
        k.i('dve', 'tensor_copy', reads=[ps], writes=[gt] if hh == 0 else [], pwrites=[] if hh == 0 else [gt], out=gt[:, cs_], in_=ps[0:N, :])
    k.i('act', 'activation', reads=[lw], writes=[lw], out=lw[:], in_=lw[:], func=AF.Sigmoid)
    k.i('act', 'activation', reads=[lw], writes=[lw], out=lw[:], in_=lw[:], func=AF.Exp, scale=-C_DEC)
    k.i('act', 'activation', reads=[a], writes=[a], out=a[:], in_=a[:], func=AF.Sigmoid)
    VT = sb('s_VT', [N, 6, 1024])
    t1 = sb('s_t1', [N, 1024]); st = sb('s_st', [N, 16])
    k.begin_fill(VT)
    k.i('act', 'copy', reads=[xm], pwrites=[VT], out=VT[:, 0, :], in_=xr)
    k.i('act', 'copy', reads=[lw], pwrites=[VT], out=VT[:, 1, :], in_=lw[:])
    k.i('act', 'copy', reads=[xm], pwrites=[VT], out=VT[:, 3, :], in_=xv)
    k.i('dve', 'tensor_tensor', reads=[xm, pvb], writes=[t1], out=t1[:], in0=xk, in1=pvb[:, 2, :], op=ALU.mult)
    sq = sb('s_sq', [N, 1024])
    k.i('dve', 'tensor_tensor', reads=[t1], writes=[sq], out=sq[:], in0=t1[:], in1=t1[:], op=ALU.mult)
    k.i('dve', 'tensor_reduce', reads=[sq], writes=[st], out=st[:], in_=sq[:].rearrange("p (h n) -> p h n", h=16), axis=AX.X, op=ALU.add)
    k.i('act', 'activation', reads=[st], writes=[st], out=st[:], in_=st[:], func=AF.Sqrt)
    k.i('dve', 'tensor_scalar', reads=[st], writes=[st], out=st[:], in0=st[:], scalar1=1e-12, scalar2=None, op0=ALU.max)
    k.i('dve', 'reciprocal', reads=[st], writes=[st], out=st[:], in_=st[:])
    k.i('dve', 'tensor_tensor', reads=[t1, st], pwrites=[VT], out=VT[:, 4, :].rearrange("p (h n) -> p h n", h=16),
        in0=t1[:].rearrange("p (h n) -> p h n", h=16), in1=st[:].unsqueeze(2).to_broadcast([N, 16, 64]), op=ALU.mult)
    k.i('dve', 'tensor_scalar', reads=[a], writes=[sq], out=sq[:], in0=a[:], scalar1=-1.0, scalar2=None, op0=ALU.add)
    k.i('dve', 'tensor_tensor', reads=[sq, pvb], writes=[sq], out=sq[:], in0=sq[:], in1=pvb[:, 3, :], op=ALU.mult)
    k.i('dve', 'tensor_scalar', reads=[sq], writes=[sq], out=sq[:], in0=sq[:], scalar1=1.0, scalar2=None, op0=ALU.add)
    k.i('dve', 'tensor_tensor', reads=[sq, xm], pwrites=[VT], out=VT[:, 2, :], in0=sq[:], in1=xk, op=ALU.mult)
    k.i('dve', 'tensor_tensor', reads=[VT, a], pwrites=[VT], out=VT[:, 5, :], in0=VT[:, 4, :], in1=a[:], op=ALU.mult)
    bonus = sb('s_bonus', [N, 1024])
    k.i('dve', 'tensor_tensor', reads=[xm, VT], writes=[t1], out=t1[:], in0=xr, in1=VT[:, 2, :], op=ALU.mult)
    k.i('dve', 'tensor_tensor', reads=[t1, pvb], writes=[t1], out=t1[:], in0=t1[:], in1=pvb[:, 4, :], op=ALU.mult)
    k.i('dve', 'tensor_reduce', reads=[t1], writes=[st], out=st[:], in_=t1[:].rearrange("p (h n) -> p h n", h=16), axis=AX.X, op=ALU.add)
    k.i('dve', 'tensor_tensor', reads=[xm, st], writes=[bonus], out=bonus[:].rearrange("p (h n) -> p h n", h=16),
        in0=xv.rearrange("p (h n) -> p h n", h=16), in1=st[:].unsqueeze(2).to_broadcast([N, 16, 64]), op=ALU.mult)
    for i in range(6):
        k.dma('sp', SV, SV.ap()[i], VT, VT[:, i, :], partial=True)
    VEC = sb('s_VEC', [128, 2, 6, 64])
    k.begin_fill(VEC)
    for r in range(2):
        for i in range(6):
            k.dma('sp', VEC, VEC[:, r, i, :], SV, SV.ap()[i].rearrange("b (h n) -> (b h) n", n=64)[r * 128:(r + 1) * 128, :], partial=True)
    lnwb = sb('s_lnwb', [128, 2, 2, 64])
    k.dma('sp', lnwb, lnwb[:], inp['lnwb_s'], inp['lnwb_s'].ap().rearrange("w (r p) n -> p w r n", p=128))
    YN = sb('s_YN', [128, 2, 64])
    k.begin_fill(YN)
    wkv_in = inp['wkv_s'].ap().rearrange("b h v k -> (b h) (v k)")
    wkv_out = p.out['o_wkvs'].ap().rearrange("b h v k -> (b h) (v k)")
    S = sb('s_S', [128, 64, 64]); T1 = sb('s_T', [128, 64, 64])
    for r in range(2):
        sa = sb('s_sa%d' % r, [128, 64]); y = sb('s_y%d' % r, [128, 64]); ms = sb('s_ms%d' % r, [128, 4])
        k.dma('sp', S, S[:].rearrange("p v k -> p (v k)"), inp['wkv_s'], wkv_in[r * 128:(r + 1) * 128, :])
        vec = lambda i, r=r: VEC[:, r, i, :]
        bk = lambda i, r=r: VEC[:, r, i, :].unsqueeze(1).to_broadcast([128, 64, 64])
        bv_ = lambda ap: ap.unsqueeze(2).to_broadcast([128, 64, 64])
        eng2 = 'pool'
        k.i(eng2, 'tensor_tensor', reads=[S, VEC], writes=[T1], out=T1[:], in0=S[:], in1=bk(4), op=ALU.mult)
        k.i('dve', 'tensor_reduce', reads=[T1], writes=[sa], out=sa[:], in_=T1[:], axis=AX.X, op=ALU.add)
        k.i('dve', 'tensor_scalar', reads=[sa], writes=[sa], out=sa[:], in0=sa[:], scalar1=-1.0, scalar2=None, op0=ALU.mult)
        k.i('dve', 'tensor_tensor', reads=[S, VEC], writes=[S], out=S[:], in0=S[:], in1=bk(1), op=ALU.mult)
        k.i(eng2, 'tensor_tensor', reads=[sa, VEC], writes=[T1], out=T1[:], in0=bv_(sa[:]), in1=bk(5), op=ALU.mult)
        k.i('dve', 'tensor_tensor', reads=[S, T1], writes=[S], out=S[:], in0=S[:], in1=T1[:], op=ALU.add)
        k.i(eng2, 'tensor_tensor', reads=[VEC], writes=[T1], out=T1[:], in0=bv_(vec(3)), in1=bk(2), op=ALU.mult)
        k.i('dve', 'tensor_tensor', reads=[S, T1], writes=[S], out=S[:], in0=S[:], in1=T1[:], op=ALU.add)
        k.dma('sp', p.out['o_wkvs'], wkv_out[r * 128:(r + 1) * 128, :], S, S[:].rearrange("p v k -> p (v k)"), partial=True)
        k.i(eng2, 'tensor_tensor', reads=[S, VEC], writes=[T1], out=T1[:], in0=S[:], in1=bk(0), op=ALU.mult)
        k.i('dve', 'tensor_reduce', reads=[T1], writes=[y], out=y[:], in_=T1[:], axis=AX.X, op=ALU.add)
        k.i('dve', 'tensor_reduce', reads=[y], writes=[ms], out=ms[:, 0:1], in_=y[:], axis=AX.X, op=ALU.add)
        k.i('dve', 'tensor_scalar', reads=[ms], writes=[ms], out=ms[:, 0:1], in0=ms[:, 0:1], scalar1=-1.0 / 64, scalar2=None, op0=ALU.mult)
        k.i('dve', 'tensor_scalar', reads=[y, ms], writes=[y], out=y[:], in0=y[:], scalar1=ms[:, 0:1], scalar2=None, op0=ALU.add)
        k.i('dve', 'tensor_tensor', reads=[y], writes=[sa], out=sa[:], in0=y[:], in1=y[:], op=ALU.mult)
        k.i('dve', 'tensor_reduce', reads=[sa], writes=[ms], out=ms[:, 1:2], in_=sa[:], axis=AX.X, op=ALU.add)
        k.i('act', 'activation', reads=[ms], writes=[ms], out=ms[:, 1:2], in_=ms[:, 1:2], func=AF.Sqrt, scale=1.0 / 64, bias=GN_EPS)
        k.i('dve', 'reciprocal', reads=[ms], writes=[ms], out=ms[:, 1:2], in_=ms[:, 1:2])
        k.i('dve', 'scalar_tensor_tensor', reads=[y, ms, lnwb], writes=[y], out=y[:], in0=y[:], scalar=ms[:, 1:2], in1=lnwb[:, 0, r, :], op0=ALU.mult, op1=ALU.mult)
        k.i('dve', 'tensor_tensor', reads=[y, lnwb], pwrites=[YN], out=YN[:, r, :], in0=y[:], in1=lnwb[:, 1, r, :], op=ALU.add)
        k.dma('sp', YNd, YNd.ap()[r * 128:(r + 1) * 128, :], YN, YN[:, r, :], partial=True)
    ynt = sb('s_ynt', [N, 1024]); rwb = sb('s_rwb', [N, 1024], BF16)
    k.dma('sp', ynt, ynt[:], YNd, YNd.ap().rearrange("(b h) n -> b (h n)", h=16))
    k.i('dve', 'tensor_tensor', reads=[ynt, bonus], writes=[ynt], out=ynt[:], in0=ynt[:], in1=bonus[:], op=ALU.add)
    k.i('dve', 'tensor_tensor', reads=[ynt, gt], writes=[rwb], out=rwb[:], in0=ynt[:], in1=gt[:], op=ALU.mult)
    k.dma('sp', RWS, RWS.ap(), rwb, rwb[:])


def phase_SA(k, p):
    inp, sb = p.inp, k.sb
    QS, AO = p.dr['QS'], p.dr['ATT_O']
    q = sb('a_q', [64, 4, 64]); kn_ = sb('a_kn', [64, 64]); vn = sb('a_vn', [64, 64])
    KC = sb('a_KC', [64, 128, 64]); VC = sb('a_VC', [64, 128, 64]); TT = sb('a_T', [64, 128, 64])
    sinks = sb('a_sinks', [64, 4])
    k.dma('sp', sinks, sinks[:], inp['sinks_s'], inp['sinks_s'].ap())
    k.begin_fill(q, kn_, vn, KC, VC)
    for kh in range(4):
        ps_ = slice(kh * 16, (kh + 1) * 16)
        k.dma('sp', q, q[ps_, :, :].rearrange("p g d -> p (g d)"), QS, QS.ap()[:, kh * 256:(kh + 1) * 256], partial=True)
        k.dma('sp', kn_, kn_[ps_, :], QS, QS.ap()[:, 1024 + kh * 64:1024 + (kh + 1) * 64], partial=True)
        k.dma('sp', vn, vn[ps_, :], QS, QS.ap()[:, 1280 + kh * 64:1280 + (kh + 1) * 64], partial=True)
        k.dma('sp', KC, KC[ps_, :, :], inp['cwk'], inp['cwk'].ap()[:, :, kh * 64:(kh + 1) * 64], partial=True)
        k.dma('sp', VC, VC[ps_, :, :], inp['cwv'], inp['cwv'].ap()[:, :, kh * 64:(kh + 1) * 64], partial=True)
    sc = sb('a_sc', [64, 4, 129]); E = sb('a_E', [64, 4, 129])
    mx = sb('a_mx', [64, 4]); negm = sb('a_negm', [64, 4]); rs = sb('a_rs', [64, 4]); es = sb('a_es', [64, 4])
    t4 = sb('a_t4', [64, 4, 64])
    k.begin_fill(sc)
    for g in range(4):
        k.i('pool', 'tensor_tensor', reads=[KC, q], writes=[TT], out=TT[:], in0=KC[:], in1=q[:, g, :].unsqueeze(1).to_broadcast([64, 128, 64]), op=ALU.mult)
        k.i('dve', 'tensor_reduce', reads=[TT], pwrites=[sc], out=sc[:, g, 0:128], in_=TT[:], axis=AX.X, op=ALU.add)
    k.i('dve', 'tensor_tensor', reads=[q, kn_], writes=[t4], out=t4[:], in0=q[:], in1=kn_[:].unsqueeze(1).to_broadcast([64, 4, 64]), op=ALU.mult)
    k.i('dve', 'tensor_reduce', reads=[t4], pwrites=[sc], out=sc[:, :, 128], in_=t4[:], axis=AX.X, op=ALU.add)
    k.i('dve', 'tensor_reduce', reads=[sc], writes=[mx], out=mx[:], in_=sc[:], axis=AX.X, op=ALU.max)
    k.i('dve', 'tensor_scalar', reads=[mx], writes=[mx], out=mx[:], in0=mx[:], scalar1=0.125, scalar2=None, op0=ALU.mult)
    k.i('dve', 'tensor_tensor', reads=[mx, sinks], writes=[mx], out=mx[:], in0=mx[:], in1=sinks[:], op=ALU.max)
    k.i('dve', 'tensor_scalar', reads=[mx], writes=[negm], out=negm[:], in0=mx[:], scalar1=-1.0, scalar2=None, op0=ALU.mult)
    for g in range(4):
        k.i('act', 'activation', reads=[sc, negm], writes=[E, rs] if g == 0 else [], pwrites=[] if g == 0 else [E, rs],
            out=E[:, g, :], in_=sc[:, g, :], func=AF.Exp, scale=0.125, bias=negm[:, g:g + 1], accum_out=rs[:, g:g + 1])
    k.i('dve', 'tensor_tensor', reads=[sinks, negm], writes=[es], out=es[:], in0=sinks[:], in1=negm[:], op=ALU.add)
    k.i('act', 'activation', reads=[es], writes=[es], out=es[:], in_=es[:], func=AF.Exp)
    k.i('dve', 'tensor_tensor', reads=[rs, es], writes=[rs], out=rs[:], in0=rs[:], in1=es[:], op=ALU.add)
    k.i('dve', 'reciprocal', reads=[rs], writes=[rs], out=rs[:], in_=rs[:])
    o = sb('a_o', [64, 4, 64]); ob = sb('a_ob', [64, 4, 64], BF16)
    k.begin_fill(o)
    for g in range(4):
        k.i('pool', 'tensor_tensor', reads=[VC, E], writes=[TT], out=TT[:], in0=VC[:], in1=E[:, g, 0:128].unsqueeze(2).to_broadcast([64, 128, 64]), op=ALU.mult)
        k.i('dve', 'tensor_reduce', reads=[TT], pwrites=[o], out=o[:, g, :], in_=TT[:].rearrange("p s d -> p d s"), axis=AX.X, op=ALU.add)
        k.i('dve', 'scalar_tensor_tensor', reads=[vn, E, o], pwrites=[o], out=o[:, g, :], in0=vn[:], scalar=E[:, g, 128:129], in1=o[:, g, :], op0=ALU.mult, op1=ALU.add)
    k.i('dve', 'tensor_tensor', reads=[o, rs], writes=[ob], out=ob[:], in0=o[:], in1=rs[:].unsqueeze(2).to_broadcast([64, 4, 64]), op=ALU.mult)
    for kh in range(4):
        k.dma('sp', AO, AO.ap()[1024:1040, kh * 256:(kh + 1) * 256], ob, ob[kh * 16:(kh + 1) * 16, :, :].rearrange("p g d -> p (g d)"), partial=True)


def phase_SX(k, p):
    inp, sb = p.inp, k.sb
    H, Fb, identb = p.H, p.Fb, p.identb
    QXd, OXd, H2 = p.dr['QXd'], p.dr['OXd'], p.dr['H2']
    SC = 1.0 / math.sqrt(128.0)
    q = sb('x_q', [64, 128])
    k.begin_fill(q)
    for h in range(4):
        k.dma('sp', q, q[h * 16:(h + 1) * 16, :], QXd, QXd.ap()[:, h * 128:(h + 1) * 128], partial=True)
    KCH = [sb('x_K%d' % i, [64, 32, 128]) for i in range(2)]
    VCH = [sb('x_V%d' % i, [64, 32, 128]) for i in range(2)]
    TT = sb('x_T', [64, 32, 128])
    sc = sb('x_sc', [64, 256]); E = sb('x_E', [64, 256])
    mx = sb('x_mx', [64, 1]); rs = sb('x_rs', [64, 1])
    o = sb('x_o', [64, 128]); part = sb('x_part', [64, 128])
    k.begin_fill(sc)
    for c8 in range(8):
        kc = KCH[c8 % 2]
        k.begin_fill(kc)
        for h in range(4):
            k.dma('sp', kc, kc[h * 16:(h + 1) * 16, :, :], inp['cmk_s'], inp['cmk_s'].ap()[:, c8 * 32:(c8 + 1) * 32, h * 128:(h + 1) * 128], partial=True)
        k.i('pool', 'tensor_tensor', reads=[kc, q], writes=[TT], out=TT[:], in0=kc[:], in1=q[:].unsqueeze(1).to_broadcast([64, 32, 128]), op=ALU.mult)
        k.i('dve', 'tensor_reduce', reads=[TT], pwrites=[sc], out=sc[:, c8 * 32:(c8 + 1) * 32], in_=TT[:], axis=AX.X, op=ALU.add)
    k.i('dve', 'tensor_reduce', reads=[sc], writes=[mx], out=mx[:], in_=sc[:], axis=AX.X, op=ALU.max)
    k.i('dve', 'tensor_scalar', reads=[mx], writes=[mx], out=mx[:], in0=mx[:], scalar1=-SC, scalar2=None, op0=ALU.mult)
    k.i('act', 'activation', reads=[sc, mx], writes=[E, rs], out=E[:], in_=sc[:], func=AF.Exp, scale=SC, bias=mx[:, 0:1], accum_out=rs[:, 0:1])
    k.i('dve', 'reciprocal', reads=[rs], writes=[rs], out=rs[:], in_=rs[:])
    for c8 in range(8):
        vc = VCH[c8 % 2]
        k.begin_fill(vc)
        for h in range(4):
            k.dma('sp', vc, vc[h * 16:(h + 1) * 16, :, :], inp['cmv_s'], inp['cmv_s'].ap()[:, c8 * 32:(c8 + 1) * 32, h * 128:(h + 1) * 128], partial=True)
        k.i('pool', 'tensor_tensor', reads=[vc, E], writes=[TT], out=TT[:], in0=vc[:], in1=E[:, c8 * 32:(c8 + 1) * 32].unsqueeze(2).to_broadcast([64, 32, 128]), op=ALU.mult)
        if c8 == 0:
            k.i('dve', 'tensor_reduce', reads=[TT], writes=[o], out=o[:], in_=TT[:].rearrange("p m d -> p d m"), axis=AX.X, op=ALU.add)
        else:
            k.i('dve', 'tensor_reduce', reads=[TT], writes=[part], out=part[:], in_=TT[:].rearrange("p m d -> p d m"), axis=AX.X, op=ALU.add)
            k.i('dve', 'tensor_tensor', reads=[o, part], writes=[o], out=o[:], in0=o[:], in1=part[:], op=ALU.add)
    k.i('dve', 'tensor_scalar', reads=[o, rs], writes=[o], out=o[:], in0=o[:], scalar1=rs[:, 0:1], scalar2=None, op0=ALU.mult)
    for h in range(4):
        k.dma('sp', OXd, OXd.ap()[:, h * 128:(h + 1) * 128], o, o[h * 16:(h + 1) * 16, :], partial=True)
    WX = sb('x_WX', [128, 4, D], BF16)
    p.load_w(WX, 'w_xo', 512, 0, D)
    oxs = sb('x_oxs', [16, 512]); oxb = sb('x_oxb', [16, 512], BF16); oxT = sb('x_oxT', [128, 4, 16], BF16)
    hh = sb('x_hh', [16, D])
    k.dma('sp', oxs, oxs[:], OXd, OXd.ap())
    k.dma('sp', hh, hh[:], p.H2t[8], H2.ap()[1024:1040, :])
    k.i('act', 'copy', reads=[oxs], writes=[oxb], out=oxb[:], in_=oxs[:])
    k.begin_fill(oxT)
    p.transpose16(oxb, 16, oxT, lambda half: oxT[:, 0:4, 0:16], nchunks=4)
    for nchk in range(4):
        ps = Fb[nchk]
        for c in range(4):
            k.i('pe', 'matmul', reads=[oxT, WX], writes=[ps] if c == 0 else [], pwrites=[] if c == 0 else [ps],
                out=ps[0:16, :], lhsT=oxT[:, c, 0:16], rhs=WX[:, c, nchk * 512:(nchk + 1) * 512], start=(c == 0), stop=(c == 3))
        k.i('dve', 'tensor_tensor', reads=[ps, hh], writes=[hh], out=hh[:, nchk * 512:(nchk + 1) * 512], in0=ps[0:16, :],
            in1=hh[:, nchk * 512:(nchk + 1) * 512], op=ALU.add)
    k.dma('sp', p.H2t[8], H2.ap()[1024:1040, :], hh, hh[:])


def rope_table(pos):
    inv = (np.float32(500000.0) ** (-np.arange(8, dtype=np.float32) * np.float32(2.0) / np.float32(16.0))).astype(np.float32)
    ang = pos.astype(np.float32)[:, None] * inv[None, :]
    return np.concatenate([np.cos(ang), np.sin(ang)], axis=1).astype(np.float32)


def rmasks():
    m = np.zeros((12, 128, 128), np.float32)
    i = np.arange(128)
    tu_s = (i[:, None] < i[None, :]); tl_s = (i[:, None] > i[None, :]); tu_i = (i[:, None] <= i[None, :])
    blk = (i[:, None] // 64 == i[None, :] // 64)
    for n_, mm_ in enumerate([tu_s, tl_s, tu_s, tl_s, tu_s, tu_s, tu_i, tu_i, tu_i, tu_i, blk, blk]):
        m[n_] = mm_
    return m


def swa_masks(j):
    qi = np.arange(128)[:, None]; si = np.arange(256)[None, :]
    rel = 128 + qi - si
    band = (rel >= 0) & (rel <= 128)
    mn = np.where(band, 0.0, NEG).astype(np.float32)
    mf = np.where(band & (si >= 128), 0.0, NEG).astype(np.float32) if j == 0 else mn
    return np.stack([mn, mf])


def own_cols(j):
    cols = [np.arange(part * 1024 + 256 * j, part * 1024 + 256 * j + 256) for part in range(3)]
    cols.append(np.arange(3072, 3360))
    return np.concatenate(cols)


_INPUT_NAMES = []


def nc_input_names(nc):
    return list(_INPUT_NAMES)


import os as _os
STAGES = ('A', 'MEM', 'R', 'SR', 'SA', 'OX', 'SX', 'M')


def kernel(**inp):
    f = lambda a: np.ascontiguousarray(a, dtype=np.float32)
    x_prompt = inp['x_prompt']; x_sample = inp['x_sample']
    w_in = inp['w_in'][0]
    nc = build(STAGES)
    w_rt = f(np.concatenate([inp['router_group_w'][0], inp['router_expert_w'][0]], axis=1))
    b_rt = f(np.concatenate([inp['router_group_b'][0], inp['router_expert_b'][0]]))
    e_g = f(inp['exp_w_gate'][0]); e_u = f(inp['exp_w_up'][0]); e_d = f(inp['exp_w_down'][0])
    pvf = np.stack([inp[n][0] for n in ['rw_w0', 'rw_a0', 'rw_k_k', 'rw_k_a', 'rw_r_k', 'rw_ln_w', 'rw_ln_b']]).astype(np.float32)
    lnwb_s = np.stack([np.tile(inp['rw_ln_w'][0].reshape(16, 64), (16, 1)), np.tile(inp['rw_ln_b'][0].reshape(16, 64), (16, 1))]).astype(np.float32)
    sinks_s = np.repeat(inp['attn_sinks'][0].reshape(4, 4), 16, axis=0).astype(np.float32)
    in_maps = []
    for c in range(N_CORES):
        b, j = c // 4, c % 4
        cols = own_cols(j)
        wh = np.zeros((D, RWH), np.float32); wh[:, :cols.size] = w_in[:, ATT_COLS + cols]
        mu = np.zeros(RWH, np.float32); mu[:cols.size] = inp['rw_mu'][0][cols]
        hs = slice(256 * j, 256 * j + 256)
        pv = np.stack([inp[n][0][hs] for n in ['rw_w0', 'rw_a0', 'rw_k_k', 'rw_k_a', 'rw_r_k', 'rw_ln_w', 'rw_ln_b']]).astype(np.float32)
        x_tok = np.zeros((1168, D), np.float32)
        if j > 0:
            x_tok[0:128] = x_prompt[b, 1024 * j - 128:1024 * j]
        x_tok[128:1152] = x_prompt[b, 1024 * j:1024 * j + 1024]
        x_tok[1152:1168] = x_sample[16 * c:16 * c + 16, 0]
        pos = np.concatenate([np.arange(1024 * j - 128, 1024 * j + 1024), np.full(16, 16384)])
        m = {
            'x_tok': x_tok, 'x_seq': f(x_prompt[b]), 'mem': f(inp['mem_prompt'][b]),
            'ln1': f(inp['ln1_w'][0]), 'ln2': f(inp['ln2_w'][0]), 'ln3': f(inp['ln3_w'][0]), 'memn': f(inp['mem_norm_w'][0]),
            'qn': f(inp['q_norm_w'][0]), 'kn': f(inp['k_norm_w'][0]), 'xqn': f(inp['xq_norm_w'][0]), 'xkn': f(inp['xk_norm_w'][0]),
            'w_att': f(w_in[:, :ATT_COLS]), 'w_rw': f(w_in[:, ATT_COLS:]), 'w_rwh': wh, 'w_xkv': f(inp['xkv_w'][0]),
            'cs_all': rope_table(pos), 'ident': np.eye(128, dtype=np.float32), 'masks': swa_masks(j), 'sinks': f(inp['attn_sinks'][0]),
            'cwk': f(inp['cache_win_k'][0, 16 * c:16 * c + 16].reshape(16, 128, 256)),
            'cwv': f(inp['cache_win_v'][0, 16 * c:16 * c + 16].reshape(16, 128, 256)),
            'mu_h': mu, 'pv_h': pv, 'w2_h': f(inp['rw_w2'][0][:, hs]), 'a2_h': f(inp['rw_a2'][0][:, hs]), 'g2_h': f(inp['rw_g2'][0][:, hs]),
            'rmask': rmasks(),
            'cmk_s': f(inp['cache_mem_k'][0, 16 * c:16 * c + 16].reshape(16, 256, 512)), 'cmv_s': f(inp['cache_mem_v'][0, 16 * c:16 * c + 16].reshape(16, 256, 512)),
            'sh_s': f(inp['state_shift'][0, 16 * c:16 * c + 16]), 'wkv_s': f(inp['state_wkv'][0, 16 * c:16 * c + 16]), 'mu_f': f(inp['rw_mu'][0]),
            'pvf': pvf, 'w2_f': f(inp['rw_w2'][0]), 'a2_f': f(inp['rw_a2'][0]), 'g2_f': f(inp['rw_g2'][0]), 'lnwb_s': lnwb_s, 'sinks_s': sinks_s,
            'ohj': np.eye(4, dtype=np.float32)[j], 'w_rt': w_rt, 'b_rt': b_rt, 'e_g': e_g, 'e_u': e_u, 'e_d': e_d, 'iota64': np.arange(64, dtype=np.float32), 'w_out': f(inp['w_out'][0]), 'w_xq': f(inp['xq_w'][0]), 'w_xo': f(inp['xo_w'][0]),
        }
        in_maps.append(m)
    names = set(nc_input_names(nc))
    in_maps = [{kk: vv for kk, vv in m.items() if kk in names} for m in in_maps]
    res = run_bass_kernel_spmd(nc, in_maps, core_ids=list(range(N_CORES)))
    R = res.results
    global _DBG
    _DBG = R
    y_prompt = np.stack([np.concatenate([R[4 * b + j]['o_y'][0:1024] for j in range(4)]) for b in range(2)])
    y_sample = np.concatenate([R[c]['o_y'][1024:1040] for c in range(8)])[:, None, :]
    wkp = np.stack([R[4 * b + 3]['o_wkp'].reshape(128, 4, 64) for b in range(2)])[None]
    wvp = np.stack([R[4 * b + 3]['o_wvp'].reshape(128, 4, 64) for b in range(2)])[None]
    wkv_p = np.stack([np.concatenate([R[4 * b + j]['o_wkvp'] for j in range(4)]) for b in range(2)])[None]
    shp = np.zeros((1, 2, RW_COLS), np.float32)
    for b in range(2):
        for j in range(4):
            o = R[4 * b + j]['o_shp']
            for part in range(3):
                shp[0, b, part * 1024 + 256 * j: part * 1024 + 256 * j + 256] = o[part * 256:(part + 1) * 256]
            shp[0, b, 3072:3360] = o[768:768 + 288]
    mk = np.stack([R[4 * b]['o_mk'].reshape(256, 4, 128) for b in range(2)])[None]
    mv = np.stack([R[4 * b]['o_mv'].reshape(256, 4, 128) for b in range(2)])[None]
    swk = np.concatenate([R[c]['o_swk'].reshape(16, 128, 4, 64) for c in range(8)])[None]
    swv = np.concatenate([R[c]['o_swv'].reshape(16, 128, 4, 64) for c in range(8)])[None]
    wkv_s = np.concatenate([R[c]['o_wkvs'] for c in range(8)])[None]
    shs = np.concatenate([R[c]['o_shs'] for c in range(8)])[None]
    return (y_prompt, y_sample, wkp, wvp, wkv_p, shp, mk, mv, swk, swv, wkv_s, shs)
```
